# Optimizing a Trainium2 kernel written in Bass

```python
import jax, jax.numpy as jnp
from jax import lax
import numpy as np

D_MODEL = 1024
BATCH = 2
SEQ = 16384
DEPTH = 2

GRID_W = 64
CTX_LEN = 256
N_MOD = 6
EPS = 1e-6
ROPE_THETA = 10000.0
BLOCK = 128
CONV_DIM = 512
MLA_HEADS = 8
MLA_Q_LORA = 256
MLA_KV_LORA = 128
MLA_NOPE = 64
MLA_ROPE = 32
MLA_V = 64
SWA_HEADS = 16
SWA_KV_HEADS = 4
SWA_HEAD_DIM = 64
WINDOW = 128
N_EXPERTS = 16
EC_CAPACITY = 2
D_FF_EXPERT = 1024
AB_SPLITS = [CONV_DIM, 2 * CONV_DIM, 3 * CONV_DIM, 3 * CONV_DIM + MLA_Q_LORA, 3 * CONV_DIM + MLA_Q_LORA + MLA_KV_LORA]
AB_IN = 3 * CONV_DIM + MLA_Q_LORA + MLA_KV_LORA + MLA_ROPE
AB_OUT = CONV_DIM + MLA_HEADS * MLA_V
SWA_QKV = (SWA_HEADS + 2 * SWA_KV_HEADS) * SWA_HEAD_DIM
N_EVEN = (DEPTH + 1) // 2
N_ODD = DEPTH // 2

kernel_name = "hybrid_dit_conv_mla_swa_ecmoe"


def rms_norm(x, g):
    xf = x.astype(jnp.float32)
    y = xf * lax.rsqrt(jnp.mean(xf * xf, axis=-1, keepdims=True) + EPS)
    return (y * g.astype(jnp.float32)).astype(x.dtype)


def modulate(z, shift, scale):
    return z * (1 + scale) + shift


def axial_rope_tables(row, col, dim):
    half = dim // 2
    freqs = ROPE_THETA ** (-jnp.arange(0, half, 2, dtype=jnp.float32) / half)
    ang = jnp.concatenate([row[:, None] * freqs, col[:, None] * freqs], axis=-1)
    return jnp.cos(ang), jnp.sin(ang)


def apply_axial_rope(x, cos, sin):
    b, n, h, d = x.shape
    q = d // 4
    xr = x.astype(jnp.float32).reshape(b, n, h, 2, 2, q)
    x1, x2 = xr[..., 0, :], xr[..., 1, :]
    c = cos.reshape(n, 1, 2, q)
    s = sin.reshape(n, 1, 2, q)
    out = jnp.stack([x1 * c - x2 * s, x1 * s + x2 * c], axis=-2)
    return out.reshape(b, n, h, d).astype(x.dtype)


def softmax_attend(q, k, v, scale):
    s = jnp.einsum('bqhd,bkhd->bhqk', q, k).astype(jnp.float32) * scale
    p = jax.nn.softmax(s, axis=-1).astype(v.dtype)
    return jnp.einsum('bhqk,bkhd->bqhd', p, v)


def blocked_dense_attend(q, k, v, scale):
    b, n, h, dk = q.shape
    nb = n // BLOCK
    qb = jnp.moveaxis(q.reshape(b, nb, BLOCK, h, dk), 1, 0)
    ob = lax.map(lambda qi: softmax_attend(qi, k, v, scale), qb)
    return jnp.moveaxis(ob, 0, 1).reshape(b, n, h * v.shape[-1])


def short_conv_mixer(gate_b, gate_c, u, conv_w, conv_b):
    z = gate_c * u
    zp = jnp.pad(z, ((0, 0), (1, 1), (0, 0)))
    y = zp[:, :-2] * conv_w[0] + zp[:, 1:-1] * conv_w[1] + zp[:, 2:] * conv_w[2] + conv_b
    return gate_b * y


def mla_qkv(q_lat, kv_lat, k_rope, q_norm_g, w_uq, kv_norm_g, w_ukv, rope):
    b, n, _ = q_lat.shape
    q = (rms_norm(q_lat, q_norm_g) @ w_uq).reshape(b, n, MLA_HEADS, MLA_NOPE + MLA_ROPE)
    kv = (rms_norm(kv_lat, kv_norm_g) @ w_ukv).reshape(b, n, MLA_HEADS, MLA_NOPE + MLA_V)
    q_nope, q_rot = q[..., :MLA_NOPE], q[..., MLA_NOPE:]
    k_nope, v = kv[..., :MLA_NOPE], kv[..., MLA_NOPE:]
    k_rot = k_rope[:, :, None, :]
    if rope is not None:
        q_rot = apply_axial_rope(q_rot, *rope)
        k_rot = apply_axial_rope(k_rot, *rope)
    q = jnp.concatenate([q_nope, q_rot], axis=-1)
    k = jnp.concatenate([k_nope, jnp.broadcast_to(k_rot, (b, n, MLA_HEADS, MLA_ROPE))], axis=-1)
    return q, k, v


def mixer_conv_mla(h, hc, w_in, conv_w, conv_b, q_norm_g, w_uq, kv_norm_g, w_ukv, w_out, rope, need_ctx):
    b, n, _ = h.shape
    scale = (MLA_NOPE + MLA_ROPE) ** -0.5
    gb, gc, u, ql, kvl, kr = jnp.split(h @ w_in, AB_SPLITS, axis=-1)
    gbc, gcc, uc, qlc, kvlc, krc = jnp.split(hc @ w_in, AB_SPLITS, axis=-1)
    q, k, v = mla_qkv(ql, kvl, kr, q_norm_g, w_uq, kv_norm_g, w_ukv, rope)
    qc, kc, vc = mla_qkv(qlc, kvlc, krc, q_norm_g, w_uq, kv_norm_g, w_ukv, None)
    att = blocked_dense_attend(q, jnp.concatenate([kc, k], axis=1), jnp.concatenate([vc, v], axis=1), scale)
    y = jnp.concatenate([short_conv_mixer(gb, gc, u, conv_w, conv_b), att], axis=-1) @ w_out
    if not need_ctx:
        return y, None
    att_c = softmax_attend(qc, kc, vc, scale).reshape(b, hc.shape[1], MLA_HEADS * MLA_V)
    yc = jnp.concatenate([short_conv_mixer(gbc, gcc, uc, conv_w, conv_b), att_c], axis=-1) @ w_out
    return y, yc


def mixer_window_gqa(h, hc, w_qkv, sink, w_out, rope, need_ctx):
    b, n, _ = h.shape
    hd, kvh = SWA_HEAD_DIM, SWA_KV_HEADS
    grp = SWA_HEADS // kvh
    scale = hd ** -0.5

    def qkv(z):
        L = z.shape[1]
        p = z @ w_qkv
        q = p[..., :SWA_HEADS * hd].reshape(b, L, SWA_HEADS, hd)
        k = p[..., SWA_HEADS * hd:(SWA_HEADS + kvh) * hd].reshape(b, L, kvh, hd)
        v = p[..., (SWA_HEADS + kvh) * hd:].reshape(b, L, kvh, hd)
        return q, k, v

    q, k, v = qkv(h)
    qc, kc, vc = qkv(hc)
    L_ctx = hc.shape[1]
    q = apply_axial_rope(q, *rope).reshape(b, n, kvh, grp, hd)
    k = apply_axial_rope(k, *rope)
    sink_l = sink.astype(jnp.float32).reshape(kvh, grp)
    kp = jnp.pad(k, ((0, 0), (BLOCK, BLOCK), (0, 0), (0, 0)))
    vp = jnp.pad(v, ((0, 0), (BLOCK, BLOCK), (0, 0), (0, 0)))
    r = jnp.arange(BLOCK, dtype=jnp.int32)
    s = jnp.arange(3 * BLOCK, dtype=jnp.int32)

    def block(args):
        qi, i = args
        start = i * BLOCK
        kb = lax.dynamic_slice_in_dim(kp, start, 3 * BLOCK, axis=1)
        vb = lax.dynamic_slice_in_dim(vp, start, 3 * BLOCK, axis=1)
        qpos = start + r
        kpos = start - BLOCK + s
        valid = (jnp.abs(qpos[:, None] - kpos[None, :]) <= WINDOW) & (kpos[None, :] >= 0) & (kpos[None, :] < n)
        s_loc = jnp.einsum('bqkgd,bskd->bkgqs', qi, kb).astype(jnp.float32) * scale
        s_loc = jnp.where(valid, s_loc, -1e30)
        s_ctx = jnp.einsum('bqkgd,bskd->bkgqs', qi, kc).astype(jnp.float32) * scale
        sink_col = jnp.broadcast_to(sink_l[None, :, :, None, None], (b, kvh, grp, BLOCK, 1))
        p = jax.nn.softmax(jnp.concatenate([s_ctx, s_loc, sink_col], axis=-1), axis=-1).astype(v.dtype)
        return (jnp.einsum('bkgqs,bskd->bqkgd', p[..., :L_ctx], vc)
                + jnp.einsum('bkgqs,bskd->bqkgd', p[..., L_ctx:L_ctx + 3 * BLOCK], vb))

    nb = n // BLOCK
    qb = jnp.moveaxis(q.reshape(b, nb, BLOCK, kvh, grp, hd), 1, 0)
    ob = lax.map(block, (qb, jnp.arange(nb, dtype=jnp.int32)))
    y = jnp.moveaxis(ob, 0, 1).reshape(b, n, SWA_HEADS * hd) @ w_out
    if not need_ctx:
        return y, None
    qc = qc.reshape(b, L_ctx, kvh, grp, hd)
    sc = jnp.einsum('bqkgd,bskd->bkgqs', qc, kc).astype(jnp.float32) * scale
    sink_c = jnp.broadcast_to(sink_l[None, :, :, None, None], (b, kvh, grp, L_ctx, 1))
    pc = jax.nn.softmax(jnp.concatenate([sc, sink_c], axis=-1), axis=-1)[..., :L_ctx].astype(vc.dtype)
    yc = jnp.einsum('bkgqs,bskd->bqkgd', pc, vc).reshape(b, L_ctx, SWA_HEADS * hd) @ w_out
    return y, yc


def ec_moe(h, router_w, w_gate, w_up, w_down):
    b, n, _ = h.shape
    cap = max(1, (EC_CAPACITY * n) // N_EXPERTS)
    aff = jax.nn.softmax(jnp.einsum('bnd,de->bne', h, router_w).astype(jnp.float32), axis=-1)
    g, idx = lax.top_k(jnp.swapaxes(aff, 1, 2), cap)
    bidx = jnp.arange(b)[:, None, None]
    xg = h[bidx, idx]
    hid = jax.nn.silu(jnp.einsum('becd,edf->becf', xg, w_gate)) * jnp.einsum('becd,edf->becf', xg, w_up)
    ye = jnp.einsum('becf,efd->becd', hid, w_down) * g[..., None].astype(h.dtype)
    return jnp.zeros_like(h).at[bidx, idx].add(ye)


def setup_inputs(seed: int = 0) -> dict:
    key = jax.random.key(seed)
    ks = jax.random.split(key, 32)
    nrm = jax.random.normal
    D, F, E = D_MODEL, D_FF_EXPERT, N_EXPERTS
    f32 = jnp.float32
    return {
        "x": nrm(ks[0], (BATCH, SEQ, D), f32),
        "c": nrm(ks[1], (BATCH, D), f32),
        "ctx": nrm(ks[2], (BATCH, CTX_LEN, D), f32),
        "c_ctx": nrm(ks[3], (D,), f32),
        "mod_w": nrm(ks[4], (DEPTH, D, N_MOD * D), f32) * (0.5 * D ** -0.5),
        "mod_b": nrm(ks[5], (DEPTH, N_MOD * D), f32) * 0.01,
        "norm1_g": 1.0 + 0.01 * nrm(ks[6], (DEPTH, D), f32),
        "norm2_g": 1.0 + 0.01 * nrm(ks[7], (DEPTH, D), f32),
        "ab_w_in": nrm(ks[8], (N_EVEN, D, AB_IN), f32) * D ** -0.5,
        "conv_w": nrm(ks[9], (N_EVEN, 3, CONV_DIM), f32) * 3 ** -0.5,
        "conv_b": nrm(ks[10], (N_EVEN, CONV_DIM), f32) * 0.01,
        "mla_q_norm_g": 1.0 + 0.01 * nrm(ks[11], (N_EVEN, MLA_Q_LORA), f32),
        "mla_w_uq": nrm(ks[12], (N_EVEN, MLA_Q_LORA, MLA_HEADS * (MLA_NOPE + MLA_ROPE)), f32) * MLA_Q_LORA ** -0.5,
        "mla_kv_norm_g": 1.0 + 0.01 * nrm(ks[13], (N_EVEN, MLA_KV_LORA), f32),
        "mla_w_ukv": nrm(ks[14], (N_EVEN, MLA_KV_LORA, MLA_HEADS * (MLA_NOPE + MLA_V)), f32) * MLA_KV_LORA ** -0.5,
        "ab_w_out": nrm(ks[15], (N_EVEN, AB_OUT, D), f32) * AB_OUT ** -0.5,
        "swa_w_qkv": nrm(ks[16], (N_ODD, D, SWA_QKV), f32) * D ** -0.5,
        "swa_sink": nrm(ks[17], (N_ODD, SWA_HEADS), f32) * 0.5,
        "swa_w_out": nrm(ks[18], (N_ODD, SWA_HEADS * SWA_HEAD_DIM, D), f32) * (SWA_HEADS * SWA_HEAD_DIM) ** -0.5,
        "router_w": nrm(ks[19], (DEPTH, D, E), f32) * D ** -0.5,
        "exp_w_gate": nrm(ks[20], (DEPTH, E, D, F), f32) * D ** -0.5,
        "exp_w_up": nrm(ks[21], (DEPTH, E, D, F), f32) * D ** -0.5,
        "exp_w_down": nrm(ks[22], (DEPTH, E, F, D), f32) * F ** -0.5,
        "final_g": 1.0 + 0.01 * nrm(ks[23], (D,), f32),
    }


def reference(x, c, ctx, c_ctx, mod_w, mod_b, norm1_g, norm2_g, ab_w_in, conv_w, conv_b,
              mla_q_norm_g, mla_w_uq, mla_kv_norm_g, mla_w_ukv, ab_w_out, swa_w_qkv, swa_sink,
              swa_w_out, router_w, exp_w_gate, exp_w_up, exp_w_down, final_g):
    n = x.shape[1]
    n_rows = n // GRID_W
    row = jnp.repeat(jnp.arange(n_rows, dtype=jnp.float32), GRID_W)
    col = jnp.tile(jnp.arange(GRID_W, dtype=jnp.float32), n_rows)
    rope_mla = axial_rope_tables(row, col, MLA_ROPE)
    rope_swa = axial_rope_tables(row, col, SWA_HEAD_DIM)

    xs, xc = x, ctx
    for layer in range(DEPTH):
        need_ctx = layer < DEPTH - 1
        mod = jax.nn.silu(c) @ mod_w[layer] + mod_b[layer]
        sh1, sc1, g1, sh2, sc2, g2 = jnp.split(mod[:, None, :], N_MOD, axis=-1)
        modc = jax.nn.silu(c_ctx) @ mod_w[layer] + mod_b[layer]
        csh1, csc1, cg1, csh2, csc2, cg2 = jnp.split(modc, N_MOD, axis=-1)

        h = modulate(rms_norm(xs, norm1_g[layer]), sh1, sc1)
        hc = modulate(rms_norm(xc, norm1_g[layer]), csh1, csc1)
        if layer % 2 == 0:
            e = layer // 2
            y, yc = mixer_conv_mla(h, hc, ab_w_in[e], conv_w[e], conv_b[e], mla_q_norm_g[e], mla_w_uq[e],
                                   mla_kv_norm_g[e], mla_w_ukv[e], ab_w_out[e], rope_mla, need_ctx)
        else:
            o = layer // 2
            y, yc = mixer_window_gqa(h, hc, swa_w_qkv[o], swa_sink[o], swa_w_out[o], rope_swa, need_ctx)
        xs = xs + g1 * y

        h2 = modulate(rms_norm(xs, norm2_g[layer]), sh2, sc2)
        xs = xs + g2 * ec_moe(h2, router_w[layer], exp_w_gate[layer], exp_w_up[layer], exp_w_down[layer])
        if need_ctx:
            xc = xc + cg1 * yc
            hc2 = modulate(rms_norm(xc, norm2_g[layer]), csh2, csc2)
            xc = xc + cg2 * ec_moe(hc2, router_w[layer], exp_w_gate[layer], exp_w_up[layer], exp_w_down[layer])
    return rms_norm(xs, final_g)
```

```python
import numpy as np
import ml_dtypes
from contextlib import ExitStack
import concourse.bass as bass
import concourse.mybir as mybir
from concourse.bass_utils import run_bass_kernel_spmd

F32 = mybir.dt.float32
BF16 = mybir.dt.bfloat16
I32 = mybir.dt.int32
ALU = mybir.AluOpType
AF = mybir.ActivationFunctionType
AX = mybir.AxisListType

D = 1024
NL = 4096
NCX = 256
NT = NL + NCX
EPS = 1e-6
NE = 16
ENGS = ["pe", "act", "dve", "pool", "sp"]
NDMA_SEM = 56


class Buf:
    __slots__ = ("name", "last_w", "readers", "t", "relaxed")

    def __init__(self, name, t=None):
        self.name = name
        self.last_w = None
        self.readers = []
        self.t = t
        self.relaxed = False

    def __getitem__(self, k):
        return self.t[k]


class Op:
    __slots__ = ("eng", "fn", "deps", "dma", "idx", "need_inc", "sem", "val", "waits", "cc")


class Prog:
    def __init__(self, nc, ctx):
        self.nc = nc
        self.ctx = ctx
        self.ops = []
        self.rings = {}
        self.phase = "p"
        self.pstack = None

    def phase_begin(self, name):
        self.phase = name
        self.pstack = ExitStack()
        self.rings = {}

    def phase_end(self):
        self.ops.append(None)
        self.pstack.close()
        self.pstack = None

    def sb(self, name, shape, dt):
        t = self.pstack.enter_context(self.nc.sbuf_tensor("%s_%s" % (self.phase, name), shape, dt))
        return Buf(name, t)

    def ps(self, name, shape, dt):
        t = self.pstack.enter_context(self.nc.psum_tensor("%s_%s" % (self.phase, name), shape, dt))
        return Buf(name, t)

    def cc_allgather(self, src, dst, groups, src_ap=None, dst_ap=None):
        src_ap = src.t if src_ap is None else src_ap
        dst_ap = dst.t if dst_ap is None else dst_ap
        o = self.op("pool", lambda e: e.collective_compute("AllGather", ALU.bypass, replica_groups=groups,
                                                           ins=[src_ap], outs=[dst_ap]), reads=[src], writes=[dst])
        o.cc = True
        return o

    def ring(self, name, shape, dt, n, psum=False):
        bufs = [(self.ps if psum else self.sb)("%s%d" % (name, i), shape, dt) for i in range(n)]
        self.rings[name] = [bufs, 0]

    def nxt(self, name):
        r = self.rings[name]
        b = r[0][r[1] % len(r[0])]
        r[1] += 1
        return b

    def dram(self, name, shape, dt, kind=None):
        if kind is None:
            t = self.nc.dram_tensor(name, list(shape), dt)
        else:
            t = self.nc.dram_tensor(name, list(shape), dt, kind=kind)
        return Buf(name, t.ap())

    def op(self, eng, fn, reads=(), writes=(), dma=False, extra=()):
        o = Op()
        o.eng = eng
        o.fn = fn
        o.dma = dma
        o.idx = len(self.ops)
        deps = set()
        for b in reads:
            if b.last_w is not None:
                deps.add(b.last_w)
        for b in writes:
            if b.relaxed:
                continue
            if b.last_w is not None:
                deps.add(b.last_w)
            for r in b.readers:
                deps.add(r)
        for x in extra:
            deps.add(x.idx)
        deps.discard(o.idx)
        o.deps = deps
        for b in reads:
            b.readers.append(o.idx)
        for b in writes:
            b.last_w = o.idx
            b.readers = []
        o.need_inc = False
        o.cc = False
        self.ops.append(o)
        return o

    def finalize(self):
        nc = self.nc
        ctx = self.ctx
        ops = self.ops

        def pe_pe(p, o):
            return p.eng == "pe" and o.eng == "pe" and not p.dma and not o.dma

        last = {}
        for o in ops:
            if o is None:
                for e_, lo in last.items():
                    lo.need_inc = True
                continue
            last[o.eng] = o
            for d in o.deps:
                if not pe_pe(ops[d], o):
                    ops[d].need_inc = True
            if o.dma or o.cc:
                o.need_inc = True
        eng_sem = {e: ctx.enter_context(nc.semaphore("s_" + e)) for e in ENGS}
        dma_sems = [ctx.enter_context(nc.semaphore("d%d" % i)) for i in range(NDMA_SEM)]
        cc_sem = ctx.enter_context(nc.semaphore("cc_sem"))
        cnt = {e: 0 for e in ENGS}
        dcnt = [0] * NDMA_SEM
        ccnt = 0
        dma_rr = 0
        seen = {e: {} for e in ENGS}
        pending = {e: None for e in ENGS}
        for o in ops:
            if o is None:
                snap = [(("e", e), eng_sem[e], cnt[e]) for e in ENGS if cnt[e] > 0]
                snap += [(("d", k), dma_sems[k], dcnt[k]) for k in range(NDMA_SEM) if dcnt[k] > 0]
                if ccnt > 0:
                    snap.append((("cc",), cc_sem, ccnt))
                for e in ENGS:
                    pending[e] = snap
                continue
            waits = []
            s = seen[o.eng]
            if pending[o.eng] is not None:
                for key, sem, val in pending[o.eng]:
                    if s.get(key, 0) < val:
                        s[key] = val
                        waits.append((sem, val))
                pending[o.eng] = None
            for d in sorted(o.deps):
                p = ops[d]
                if pe_pe(p, o):
                    continue
                key, sem = p.sem
                if s.get(key, 0) < p.val:
                    s[key] = p.val
                    waits.append((sem, p.val))
            if o.cc:
                ccnt += 1
                o.sem = (("cc",), cc_sem)
                o.val = ccnt
            elif o.dma:
                k = dma_rr
                dma_rr = (dma_rr + 1) % NDMA_SEM
                key = ("d", k)
                if dcnt[k] > 0 and s.get(key, 0) < dcnt[k]:
                    s[key] = dcnt[k]
                    waits.append((dma_sems[k], dcnt[k]))
                dcnt[k] += 16
                o.sem = (key, dma_sems[k])
                o.val = dcnt[k]
            elif o.need_inc:
                cnt[o.eng] += 1
                o.sem = (("e", o.eng), eng_sem[o.eng])
                o.val = cnt[o.eng]
            o.waits = waits
        final_waits = [(dma_sems[k], dcnt[k]) for k in range(NDMA_SEM) if dcnt[k] > 0]
        final_waits += [(eng_sem[e], cnt[e]) for e in ENGS if cnt[e] > 0]
        if ccnt > 0:
            final_waits.append((cc_sem, ccnt))
        per = {e: [o for o in ops if o is not None and o.eng == e] for e in ENGS}
        engmap = {"pe": "tensor", "act": "scalar", "dve": "vector", "pool": "gpsimd", "sp": "sync"}
        with nc.Block() as block:
            for e in ENGS:
                def body(eng, lst=per[e], final=(e == "sp")):
                    for o in lst:
                        for (sem, val) in o.waits:
                            eng.wait_ge(sem, val)
                        ins = o.fn(eng)
                        if o.cc:
                            ins.then_inc(o.sem[1])
                        elif o.need_inc:
                            ins.then_inc(o.sem[1], 16 if o.dma else 1)
                    if final:
                        for (sem, val) in final_waits:
                            eng.wait_ge(sem, val)
                getattr(block, engmap[e])(body)
        return len(per["pe"]) + len(per["act"]) + len(per["dve"]) + len(per["pool"]) + len(per["sp"])

    def mm(self, ps, out, lhsT, rhs, start, stop, rd):
        self.op("pe", lambda e: e.matmul(out, lhsT=lhsT, rhs=rhs, start=start, stop=stop), reads=rd, writes=[ps])

    def tr(self, ps, out, in_, ident, rd):
        self.op("pe", lambda e: e.transpose(out, in_, ident), reads=rd, writes=[ps])

    def act(self, wr, out, rd, in_, func, scale=1.0, bias=None, accum=None):
        kw = {}
        if bias is not None:
            kw["bias"] = bias
        if accum is not None:
            kw["accum_out"] = accum
        self.op("act", lambda e: e.activation(out=out, in_=in_, func=func, scale=scale, **kw), reads=rd, writes=wr)

    def ts(self, eng, wr, out, rd, in0, s1, s2, op0, op1=None, accum=None):
        kw = {}
        if op1 is not None:
            kw["op1"] = op1
        if accum is not None:
            kw["accum_out"] = accum
        self.op(eng, lambda e: e.tensor_scalar(out, in0, s1, s2, op0, **kw), reads=rd, writes=wr)

    def tt(self, eng, wr, out, rd, in0, in1, op):
        self.op(eng, lambda e: e.tensor_tensor(out, in0, in1, op), reads=rd, writes=wr)

    def stt(self, eng, wr, out, rd, in0, scalar, in1, op0, op1):
        self.op(eng, lambda e: e.scalar_tensor_tensor(out, in0, scalar, in1, op0, op1), reads=rd, writes=wr)

    def cp(self, eng, wr, out, rd, in_):
        if eng == "act":
            self.op("act", lambda e: e.copy(out, in_), reads=rd, writes=wr)
        else:
            self.op(eng, lambda e: e.tensor_copy(out, in_), reads=rd, writes=wr)

    def memset(self, eng, wr, out, val):
        self.op(eng, lambda e: e.memset(out, val), writes=wr)

    def dma(self, q, wr, out, rd, in_):
        self.op(q, lambda e: e.dma_start(out=out, in_=in_), reads=rd, writes=wr, dma=True)

    def red(self, eng, wr, out, rd, in_, op, axis=AX.X):
        self.op(eng, lambda e: e.tensor_reduce(out, in_, axis, op), reads=rd, writes=wr)

    def make_ident(self):
        idf = self.sb("idf", [128, 128], F32)
        idb = self.sb("idb", [128, 128], BF16)
        self.memset("pool", [idf], idf[:], 1.0)
        self.op("pool", lambda e: e.affine_select(idf[:], idf[:], [[-1, 128]], ALU.is_equal, 0.0, base=0,
                                                 channel_multiplier=1), reads=[idf], writes=[idf])
        self.cp("dve", [idb], idb[:], [idf], idf[:])
        self.idf, self.idb = idf, idb
        eb = self.sb("epsb", [128, 1], F32)
        self.memset("dve", [eb], eb[:], EPS)
        self.epsb = eb

    def rstd(self, out_buf, out, ssq_buf, ssq, n, scale):
        tmp = self.nxt("rstd_tmp")
        self.act([tmp], tmp[:, 0:n], [ssq_buf, self.epsb], ssq, AF.Ln, scale=scale, bias=self.epsb[:, 0:1])
        self.act([out_buf], out, [tmp], tmp[:, 0:n], AF.Exp, scale=-0.5)


def new_prog(ctx):
    nc = bass.Bass("TRN2", target_bir_lowering=False)
    P = Prog(nc, ctx)
    return nc, P


def load_bf16(P, name, src_ap, shape):
    b = P.sb(name, shape, BF16)
    P.dma("pool", [b], b[:], [], src_ap)
    return b


def tile_T(P, xrows, row0, A, Bm):
    xt = P.nxt("xt")
    P.dma("pool", [xt], xt[:], [xrows], xrows[row0:row0 + 128, :])
    junk = P.nxt("junk")
    ssq = P.nxt("ssq")
    P.act([junk, ssq], junk[:], [xt], xt[:], AF.Square, accum=ssq[:, 0:1])
    rs = P.nxt("rs")
    P.rstd(rs, rs[:, 0:1], ssq, ssq[:, 0:1], 1, 1.0 / D)
    h32 = P.nxt("h32")
    P.stt("dve", [h32], h32[:], [xt, rs, A], xt[:], rs[:, 0:1], A[:], ALU.mult, ALU.mult)
    hb = P.nxt("hb")
    P.tt("dve", [hb], hb[:], [h32, Bm], h32[:], Bm[:], ALU.add)
    tp = P.nxt("tp")
    for k in range(8):
        P.tr(tp, tp[:, k, :], hb[:, k * 128:(k + 1) * 128], P.idb[:], [hb, P.idb])
    return tp


def compute_mod(P, ccols_ap, modw_ap, modb_ap, col0, ncols, names):
    nseg = ncols // 1024
    cc = P.sb("cc", [128, 16], F32)
    P.dma("sp", [cc], cc[:], [], ccols_ap)
    sc = P.sb("sc", [128, 16], F32)
    P.act([sc], sc[:], [cc], cc[:], AF.Silu)
    ones2 = P.sb("ones2", [1, 2], F32)
    P.memset("dve", [ones2], ones2[:], 1.0)
    mb = P.sb("mb", [1, ncols], F32)
    P.dma("sp", [mb], mb[:], [], modb_ap[0:1, col0:col0 + ncols])
    modrows = P.sb("modrows", [2, ncols], F32)
    MWC = 128
    P.ring("mw", [128, 8, MWC], F32, 2)
    for j in range(ncols // MWC):
        mw = P.nxt("mw")
        P.dma("sp", [mw], mw[:], [], modw_ap[:, col0 + j * MWC: col0 + (j + 1) * MWC].rearrange("(k p) n -> p k n", p=128))
        ps = P.nxt("ps")
        for k in range(8):
            P.mm(ps, ps[0:2, 0:MWC], sc[:, 2 * k:2 * k + 2], mw[:, k, :], k == 0, False, [sc, mw])
        P.mm(ps, ps[0:2, 0:MWC], ones2[:], mb[:, j * MWC:(j + 1) * MWC], False, True, [ones2, mb])
        P.cp("dve", [modrows], modrows[:, j * MWC:(j + 1) * MWC], [ps], ps[0:2, 0:MWC])
    sel = P.sb("sel", [2, 2, 128], F32)
    P.memset("dve", [sel], sel[:], 0.0)
    P.memset("dve", [sel], sel[0:1, 0, :], 1.0)
    P.ts("dve", [sel], sel[:, 1, :], [sel], sel[:, 0, :], -1.0, 1.0, ALU.mult, ALU.add)
    out = {}
    for si, nm in enumerate(names):
        reps = []
        for w in range(2):
            rep = P.sb("mod_%s_%d" % (nm, w), [128, 1024], F32)
            for hh in range(2):
                ps = P.nxt("ps")
                P.mm(ps, ps[:, :], sel[:, w, :], modrows[:, si * 1024 + hh * 512: si * 1024 + (hh + 1) * 512], True, True,
                     [sel, modrows])
                P.cp("act", [rep], rep[:, hh * 512:(hh + 1) * 512], [ps], ps[:, :])
            reps.append(rep)
        out[nm] = reps
    return out


def phase_A(P, T):
    P.phase_begin("A")
    xrows, xhalo, hmask, ccols, modw, modb, n1g = T["xrows"], T["xhalo"], T["hmask"], T["ccols"], T["modw0"], T["modb0"], T["n1g0"]
    w_in, convp, qg, w_uq, w_uqs, w_ukv, ropeq, ropek = (T[k] for k in ("w_in", "convp", "qg", "w_uq", "w_uqs", "w_ukv", "ropeq", "ropek"))
    QT, KTn, KTr, Vt, convT = T["QT"], T["KTn"], T["KTr"], T["Vt"], T["convT"]

    P.make_ident()
    P.ring("ps", [128, 512], F32, 6, psum=True)
    P.ring("tp", [128, 8, 128], BF16, 2, psum=True)
    P.ring("xt", [128, D], F32, 2)
    P.ring("junk", [128, D], BF16, 1)
    P.ring("ssq", [128, 1], F32, 2)
    P.ring("rs", [128, 1], F32, 2)
    P.ring("rstd_tmp", [128, 512], F32, 1)
    P.ring("h32", [128, D], F32, 1)
    P.ring("hb", [128, D], BF16, 2)

    mods = compute_mod(P, ccols.t, modw.t, modb.t, 0, 2048, ["sh1", "sc1"])
    g1 = P.sb("g1", [128, D], F32)
    P.dma("sp", [g1], g1[:], [], n1g.t)
    AB = []
    for w in range(2):
        A = mods["sc1"][w]
        P.stt("dve", [A], A[:], [A, g1], A[:], 1.0, g1[:], ALU.add, ALU.mult)
        AB.append((A, mods["sh1"][w]))

    w_in_b = load_bf16(P, "w_in_b", w_in.t.rearrange("(k p) n -> p k n", p=128), [128, 8, 1984])
    w_uq_b = load_bf16(P, "w_uq_b", w_uq.t.rearrange("(k p) n -> p k n", p=128), [128, 2, 768])
    w_uqs_b = load_bf16(P, "w_uqs_b", w_uqs.t.rearrange("(k p) n -> p k n", p=128), [128, 2, 768])
    w_ukv_b = load_bf16(P, "w_ukv_b", w_ukv.t, [128, 1024])
    cvp = P.sb("cvp", [128, 16], F32)
    P.dma("sp", [cvp], cvp[:], [], convp.t)
    qgs = P.sb("qgs", [128, 3], F32)
    P.dma("sp", [qgs], qgs[:], [], qg.t)
    hm = P.sb("hm", [128, 2], F32)
    P.dma("sp", [hm], hm[:], [], hmask.t)
    P.ring("rq", [96, 2, 512], F32, 1)
    P.ring("rk", [32, 2, 512], F32, 1)
    onesb = P.sb("onesb", [128, 128], BF16)
    P.memset("dve", [onesb], onesb[:], 1.0)

    P.ring("hseg", [128, 8, 1022], BF16, 2)
    halo_tmp = P.sb("halo_tmp", [128, 8, 2], BF16)
    tph = tile_T(P, xhalo, 0, AB[0][0], AB[0][1])
    P.cp("act", [halo_tmp], halo_tmp[:], [tph], tph[:, :, 0:2])

    def fill_seg(hseg, G0, G1):
        t_lo = max(0, (G0 - 1) // 128)
        t_hi = min(NL // 128 - 1, (G1 - 2) // 128)
        for t in range(t_lo, t_hi + 1):
            a_ = max(G0, 1 + 128 * t)
            b_ = min(G1, 129 + 128 * t)
            if b_ <= a_:
                continue
            tp = tile_T(P, xrows, t * 128, AB[0][0], AB[0][1])
            P.cp("act", [hseg], hseg[:, :, a_ - G0:b_ - G0], [tp], tp[:, :, a_ - 1 - 128 * t:b_ - 1 - 128 * t])
        if G0 == 0:
            P.cp("dve", [hseg], hseg[:, :, 0:1], [halo_tmp], halo_tmp[:, :, 0:1])
        if G1 == NL + 2:
            P.cp("dve", [hseg], hseg[:, :, NL + 1 - G0:NL + 2 - G0], [halo_tmp], halo_tmp[:, :, 1:2])

    P.ring("gcs", [128, 512], F32, 2)
    P.ring("zw", [128, 512], F32, 2)
    P.ring("ca", [128, 512], F32, 2)
    P.ring("co", [128, 512], BF16, 3)
    P.ring("lat", [128, 512], F32, 3)
    P.ring("sq", [128, 512], BF16, 3)
    P.ring("rstdL", [128, 512], F32, 1)
    P.ring("qn", [128, 2, 512], BF16, 2)
    P.ring("kvn", [128, 512], BF16, 2)
    P.ring("t1", [96, 512], F32, 1)
    P.ring("t2", [96, 512], F32, 1)
    P.ring("qo", [96, 512], BF16, 3)
    P.ring("ko", [128, 512], BF16, 3)
    P.ring("vo", [128, 512], BF16, 3)

    def proj(hT, c0, n, col_lo, col_n):
        ps = P.nxt("ps")
        for k in range(8):
            P.mm(ps, ps[0:col_n, 0:n], w_in_b[:, k, col_lo:col_lo + col_n], hT[:, k, c0:c0 + n], k == 0, k == 7,
                 [w_in_b, hT])
        return ps

    def latent_norm(pss, nchunk, n, gcol0, out_ring):
        lats, sqs = [], []
        for c in range(nchunk):
            lt = P.nxt("lat")
            P.cp("act", [lt], lt[:, 0:n], [pss[c]], pss[c][:, 0:n])
            sq = P.nxt("sq")
            P.act([sq], sq[:, 0:n], [pss[c]], pss[c][:, 0:n], AF.Square)
            lats.append(lt)
            sqs.append(sq)
        ps = P.nxt("ps")
        for c in range(nchunk):
            P.mm(ps, ps[:, 0:n], onesb[:], sqs[c][:, 0:n], c == 0, c == nchunk - 1, [onesb, sqs[c]])
        rl = P.nxt("rstdL")
        P.rstd(rl, rl[:, 0:n], ps, ps[:, 0:n], n, 1.0 / (128 * nchunk))
        ob = P.nxt(out_ring)
        for c in range(nchunk):
            o_ap = ob[:, c, 0:n] if nchunk > 1 else ob[:, 0:n]
            P.stt("dve", [ob], o_ap, [lats[c], qgs, rl], lats[c][:, 0:n], qgs[:, gcol0 + c:gcol0 + c + 1], rl[:, 0:n],
                  ALU.mult, ALU.mult)
        return ob

    def block(hT, c0, n, tok0, is_ctx, first, last):
        no = n - 2
        if not is_ctx:
            rq = P.nxt("rq")
            P.dma("pool", [rq], rq[:, :, 0:no], [], ropeq[:, :, tok0:tok0 + no])
            rk = P.nxt("rk")
            P.dma("pool", [rk], rk[:, :, 0:no], [], ropek[:, :, tok0:tok0 + no])
        for c in range(4):
            ps_gc = proj(hT, c0, n, 512 + c * 128, 128)
            gcs = P.nxt("gcs")
            P.cp("act", [gcs], gcs[:, 0:n], [ps_gc], ps_gc[:, 0:n])
            ps_u = proj(hT, c0, n, 1024 + c * 128, 128)
            zw = P.nxt("zw")
            P.tt("dve", [zw], zw[:, 0:n], [ps_u, gcs], ps_u[:, 0:n], gcs[:, 0:n], ALU.mult)
            if is_ctx:
                P.memset("dve", [zw], zw[:, 0:1], 0.0)
                P.memset("dve", [zw], zw[:, n - 1:n], 0.0)
            else:
                if first:
                    P.ts("dve", [zw], zw[:, 0:1], [zw, hm], zw[:, 0:1], hm[:, 0:1], None, ALU.mult)
                if last:
                    P.ts("dve", [zw], zw[:, n - 1:n], [zw, hm], zw[:, n - 1:n], hm[:, 1:2], None, ALU.mult)
            ca = P.nxt("ca")
            P.ts("dve", [ca], ca[:, 0:no], [zw, cvp], zw[:, 1:n - 1], cvp[:, c * 4 + 1:c * 4 + 2], cvp[:, c * 4 + 3:c * 4 + 4],
                 ALU.mult, ALU.add)
            P.stt("dve", [ca], ca[:, 0:no], [zw, cvp, ca], zw[:, 0:n - 2], cvp[:, c * 4:c * 4 + 1], ca[:, 0:no], ALU.mult, ALU.add)
            P.stt("dve", [ca], ca[:, 0:no], [zw, cvp, ca], zw[:, 2:n], cvp[:, c * 4 + 2:c * 4 + 3], ca[:, 0:no], ALU.mult, ALU.add)
            ps_gb = proj(hT, c0, n, c * 128, 128)
            co = P.nxt("co")
            P.tt("dve", [co], co[:, 0:no], [ps_gb, ca], ps_gb[:, 1:n - 1], ca[:, 0:no], ALU.mult)
            P.dma("sp", [convT], convT[c * 128:(c + 1) * 128, tok0:tok0 + no], [co], co[:, 0:no])
        pq = [proj(hT, c0, n, 1536 + c * 128, 128) for c in range(2)]
        qn = latent_norm(pq, 2, n, 0, "qn")
        for h in range(8):
            psq = P.nxt("ps")
            for k in range(2):
                P.mm(psq, psq[0:96, 0:n], w_uq_b[:, k, h * 96:(h + 1) * 96], qn[:, k, 0:n], k == 0, k == 1, [w_uq_b, qn])
            qo = P.nxt("qo")
            if is_ctx:
                P.cp("act", [qo], qo[:, 0:no], [psq], psq[0:96, 1:n - 1])
            else:
                pss = P.nxt("ps")
                for k in range(2):
                    P.mm(pss, pss[0:96, 0:n], w_uqs_b[:, k, h * 96:(h + 1) * 96], qn[:, k, 0:n], k == 0, k == 1, [w_uqs_b, qn])
                t1 = P.nxt("t1")
                P.tt("dve", [t1], t1[:, 0:no], [psq, rq], psq[0:96, 1:n - 1], rq[:, 0, 0:no], ALU.mult)
                t2 = P.nxt("t2")
                P.tt("dve", [t2], t2[:, 0:no], [pss, rq], pss[0:96, 1:n - 1], rq[:, 1, 0:no], ALU.mult)
                P.tt("dve", [qo], qo[:, 0:no], [t1, t2], t1[:, 0:no], t2[:, 0:no], ALU.add)
            P.dma("sp", [QT], QT[h, :, tok0:tok0 + no], [qo], qo[:, 0:no])
        pk = [proj(hT, c0, n, 1792, 128)]
        kvn = latent_norm(pk, 1, n, 2, "kvn")
        for c in range(4):
            psk = P.nxt("ps")
            P.mm(psk, psk[:, 0:n], w_ukv_b[:, c * 128:(c + 1) * 128], kvn[:, 0:n], True, True, [w_ukv_b, kvn])
            ko = P.nxt("ko")
            P.cp("act", [ko], ko[:, 0:no], [psk], psk[:, 1:n - 1])
            P.dma("sp", [KTn], KTn[c * 128:(c + 1) * 128, tok0:tok0 + no], [ko], ko[:, 0:no])
        t0 = 0
        while t0 < no:
            m = min(128, no - t0)
            psv = P.nxt("ps")
            P.mm(psv, psv[0:m, :], kvn[:, 1 + t0:1 + t0 + m], w_ukv_b[:, 512:1024], True, True, [kvn, w_ukv_b])
            vo = P.nxt("vo")
            P.cp("act", [vo], vo[0:m, :], [psv], psv[0:m, :])
            P.dma("sp", [Vt], Vt[tok0 + t0:tok0 + t0 + m, :], [vo], vo[0:m, :])
            t0 += m
        psr = proj(hT, c0, n, 1920, 32)
        ko = P.nxt("ko")
        if is_ctx:
            P.cp("act", [ko], ko[0:32, 0:no], [psr], psr[0:32, 1:n - 1])
        else:
            psrs = proj(hT, c0, n, 1952, 32)
            t1 = P.nxt("t1")
            P.tt("dve", [t1], t1[0:32, 0:no], [psr, rk], psr[0:32, 1:n - 1], rk[:, 0, 0:no], ALU.mult)
            t2 = P.nxt("t2")
            P.tt("dve", [t2], t2[0:32, 0:no], [psrs, rk], psrs[0:32, 1:n - 1], rk[:, 1, 0:no], ALU.mult)
            P.tt("dve", [ko], ko[0:32, 0:no], [t1, t2], t1[0:32, 0:no], t2[0:32, 0:no], ALU.add)
        P.dma("sp", [KTr], KTr[:, tok0:tok0 + no], [ko], ko[0:32, 0:no])

    nb = (NL + 509) // 510
    for sg in range((nb + 1) // 2):
        G0 = 1020 * sg
        G1 = min(G0 + 1022, NL + 2)
        hseg = P.nxt("hseg")
        fill_seg(hseg, G0, G1)
        for j in (2 * sg, 2 * sg + 1):
            if j >= nb:
                continue
            c0 = 510 * j
            n = min(512, NL + 2 - c0)
            block(hseg, c0 - G0, n, c0, False, j == 0, j == nb - 1)
    hseg = P.nxt("hseg")
    for t in range(2):
        tp = tile_T(P, xrows, NL + t * 128, AB[1][0], AB[1][1])
        P.cp("act", [hseg], hseg[:, :, 1 + t * 128:129 + t * 128], [tp], tp[:])
    P.cp("dve", [hseg], hseg[:, :, 0:1], [hseg], hseg[:, :, 1:2])
    P.cp("dve", [hseg], hseg[:, :, 257:258], [hseg], hseg[:, :, 1:2])
    block(hseg, 0, 258, NL, True, True, True)
    P.phase_end()


def _swap_perm(dim):
    q = dim // 4
    d = np.arange(dim)
    return np.where((d % (2 * q)) < q, d + q, d - q)


def _rope_tables(pos, dim):
    half = dim // 2
    q = dim // 4
    freqs = (10000.0 ** (-np.arange(0, half, 2, dtype=np.float32) / np.float32(half))).astype(np.float32)
    row = (pos // 64).astype(np.float32)
    col = (pos % 64).astype(np.float32)
    ang = np.concatenate([row[:, None] * freqs, col[:, None] * freqs], axis=-1).astype(np.float32)
    cos, sin = np.cos(ang).astype(np.float32), np.sin(ang).astype(np.float32)
    d = np.arange(dim)
    j = (d // (2 * q)) * q + d % q
    sign = np.where((d % (2 * q)) < q, -1.0, 1.0).astype(np.float32)
    C = cos[:, j].T.copy()
    S = (sin[:, j] * sign[None, :]).T.copy()
    return C, S


def _rep(v):
    return np.ascontiguousarray(np.broadcast_to(np.asarray(v, np.float32)[None, :], (128, v.shape[0])))


def _cols(v, k):
    return np.ascontiguousarray(np.asarray(v, np.float32).reshape(k, 128).T)


def prep_A(inp):
    x, c, cx, c_ctx = inp["x"], inp["c"], inp["ctx"], inp["c_ctx"]
    w_in = inp["ab_w_in"][0]
    p32 = _swap_perm(32)
    w_in_ext = np.ascontiguousarray(np.concatenate([w_in, w_in[:, 1920 + p32]], axis=1))
    w_uq = inp["mla_w_uq"][0]
    w_uqs = w_uq.copy()
    for h in range(8):
        w_uqs[:, h * 96 + 64:h * 96 + 96] = w_uq[:, h * 96 + 64 + p32]
    wk = inp["mla_w_ukv"][0].reshape(128, 8, 128)
    w_ukv = np.ascontiguousarray(np.concatenate([wk[:, :, :64].reshape(128, 512), wk[:, :, 64:].reshape(128, 512)], axis=1))
    convp = np.zeros((128, 16), np.float32)
    for ch in range(4):
        for i in range(3):
            convp[:, ch * 4 + i] = inp["conv_w"][0][i, ch * 128:(ch + 1) * 128]
        convp[:, ch * 4 + 3] = inp["conv_b"][0][ch * 128:(ch + 1) * 128]
    qg = np.concatenate([_cols(inp["mla_q_norm_g"][0], 2), _cols(inp["mla_kv_norm_g"][0], 1)], axis=1)
    maps = []
    for core in range(8):
        b, q = core // 4, core % 4
        T0 = q * NL
        xr = np.ascontiguousarray(np.concatenate([x[b, T0:T0 + NL], cx[b]], axis=0))
        xh = np.zeros((128, D), np.float32)
        if q > 0:
            xh[0] = x[b, T0 - 1]
        if q < 3:
            xh[1] = x[b, T0 + NL]
        hm = np.zeros((128, 2), np.float32)
        hm[:, 0] = 1.0 if q > 0 else 0.0
        hm[:, 1] = 1.0 if q < 3 else 0.0
        cc = np.zeros((128, 16), np.float32)
        cc[:, 0::2] = _cols(c[b], 8)
        cc[:, 1::2] = _cols(c_ctx, 8)
        C, S = _rope_tables(np.arange(T0, T0 + NL), 32)
        rq = np.zeros((96, 2, NL), np.float32)
        rq[:64, 0] = 1.0
        rq[64:, 0] = C
        rq[64:, 1] = S
        rk = np.stack([C, S], axis=1)
        maps.append({
            "xrows": xr, "xhalo": xh, "hmask": hm, "ccols": cc,
            "modw": np.ascontiguousarray(inp["mod_w"][0]), "modb": np.ascontiguousarray(inp["mod_b"][0][None, :]),
            "n1g": _rep(inp["norm1_g"][0]), "w_in": w_in_ext, "convp": convp, "qg": np.ascontiguousarray(qg),
            "w_uq": np.ascontiguousarray(w_uq), "w_uqs": np.ascontiguousarray(w_uqs), "w_ukv": w_ukv,
            "ropeq": rq, "ropek": np.ascontiguousarray(rk),
        })
    return maps


NK0 = NCX + 4 * NL
NKT0 = NK0 // 128


def phase_B(P, T):
    P.phase_begin("B")
    QT, KTn, KTr, Vt, KTr_all, Vt_all, attT = (T[k] for k in ("QT", "KTn", "KTr", "Vt", "KTr_all", "Vt_all", "attT"))
    scale = 96.0 ** -0.5

    P.ring("sps", [128, 2, 512], F32, 2, psum=True)
    P.ring("ops", [128, 512], F32, 2, psum=True)
    P.ring("bps", [128, 512], F32, 1, psum=True)
    P.ring("kt", [96, NK0], BF16, 2)
    P.ring("vp", [128, NKT0, 65], BF16, 2)
    P.ring("qt", [96, NT], BF16, 2)
    P.ring("pT", [128, 2, 512], BF16, 4)
    P.ring("rl", [1, 512], F32, 2)
    P.ring("bsb", [65, 512], F32, 2)
    P.ring("ao", [65, 512], BF16, 2)
    ones = P.sb("ones", [1, 128], F32)
    P.memset("dve", [ones], ones[:], 1.0)

    pending = [None]

    def qblock(h, kt_b, vp_b, qt_b, q0, nq, ntile):
        o_ps = P.nxt("ops")
        ng = ntile // 2

        def emit_pv(g, pT):
            for i in range(2):
                t = 2 * g + i
                P.mm(o_ps, o_ps[0:65, 0:nq], vp_b[:, t, :], pT[:, i, 0:nq], t == 0, t == ntile - 1, [vp_b, pT])

        prev = None
        for g in range(ng):
            s_ps = P.nxt("sps")
            for i in range(2):
                t = 2 * g + i
                P.mm(s_ps, s_ps[:, i, 0:nq], kt_b[:, t * 128:(t + 1) * 128], qt_b[:, q0:q0 + nq], True, True, [kt_b, qt_b])
            if prev is not None:
                emit_pv(*prev)
            pT = P.nxt("pT")
            P.act([pT], pT[:, :, 0:nq], [s_ps], s_ps[:, :, 0:nq], AF.Exp, scale=scale)
            prev = (g, pT)
            if g == min(3, ng - 1) and pending[0] is not None:
                pending[0]()
                pending[0] = None
        emit_pv(*prev)

        def fin():
            rl = P.nxt("rl")
            P.op("dve", lambda e: e.reciprocal(rl[:, 0:nq], o_ps[0:1, 0:nq]), reads=[o_ps], writes=[rl])
            b_ps = P.nxt("bps")
            P.mm(b_ps, b_ps[0:65, 0:nq], ones[:, 0:65], rl[:, 0:nq], True, True, [ones, rl])
            bsb = P.nxt("bsb")
            P.cp("act", [bsb], bsb[:, 0:nq], [b_ps], b_ps[0:65, 0:nq])
            ao = P.nxt("ao")
            P.tt("dve", [ao], ao[:, 0:nq], [o_ps, bsb], o_ps[0:65, 0:nq], bsb[:, 0:nq], ALU.mult)
            P.dma("pool", [attT], attT[h * 64:(h + 1) * 64, q0:q0 + nq], [ao], ao[1:65, 0:nq])
        pending[0] = fin

    for b_ in P.rings["vp"][0]:
        P.memset("dve", [b_], b_[:, :, 0:1], 1.0)
    for h in range(8):
        kt_b = P.nxt("kt")
        P.dma("sp", [kt_b], kt_b[0:64, 0:NCX], [KTn], KTn[h * 64:(h + 1) * 64, NL:NT])
        P.dma("sp", [kt_b], kt_b[64:96, 0:NCX], [KTr], KTr[:, NL:NT])
        for r in range(4):
            P.dma("sp", [kt_b], kt_b[0:64, NCX + r * NL:NCX + (r + 1) * NL], [T["KTn_all%d" % h]], T["KTn_all%d" % h][r * 64:(r + 1) * 64, 0:NL])
            P.dma("sp", [kt_b], kt_b[64:96, NCX + r * NL:NCX + (r + 1) * NL], [KTr_all], KTr_all[r * 32:(r + 1) * 32, 0:NL])
        vp_b = P.nxt("vp")
        P.dma("sp", [vp_b], vp_b[:, 0:2, 1:65], [Vt], Vt[NL:NT, h * 64:(h + 1) * 64].rearrange("(t p) d -> p t d", p=128))
        for r in range(4):
            for k in range(4):
                P.dma("sp", [vp_b], vp_b[:, 2 + 32 * r + 8 * k:2 + 32 * r + 8 * k + 8, 1:65], [Vt_all],
                      Vt_all[k, r * 1024:(r + 1) * 1024, h * 64:(h + 1) * 64].rearrange("(t p) d -> p t d", p=128))
        qt_b = P.nxt("qt")
        P.dma("sp", [qt_b], qt_b[:], [QT], QT[h])
        for qb in range(NL // 512):
            qblock(h, kt_b, vp_b, qt_b, qb * 512, 512, NKT0)
        qblock(h, kt_b, vp_b, qt_b, NL, NCX, NCX // 128)
    pending[0]()
    P.phase_end()


def phase_post(P, T, layer, ntok, cats):
    P.phase_begin("post%d" % layer)
    xrows, ccols, modw, modb, n2g, w_out, rw = T["xres%d" % layer], T["ccols"], T["modw%d" % layer], T["modb%d" % layer], T["n2g%d" % layer], T["w_out%d" % layer], T["rw%d" % layer]
    xs1, h2o, affo, affl = T["xacc%d" % layer], T["h2_%d" % layer], T["aff_%d" % layer], T["affl_%d" % layer]

    P.make_ident()
    P.ring("ps", [128, 512], F32, 2, psum=True)
    P.ring("yps", [128, 2, 512], F32, 2, psum=True)
    P.ring("tp32", [128, 8, 128], F32, 1, psum=True)
    P.ring("rstd_tmp", [128, 512], F32, 1)
    mods = compute_mod(P, ccols.t, modw.t, modb.t, 2048, 3072, ["g1", "sh2", "sc2"])
    g2n = P.sb("g2n", [128, D], F32)
    P.dma("sp", [g2n], g2n[:], [], n2g.t)
    for w in range(2):
        A = mods["sc2"][w]
        P.stt("dve", [A], A[:], [A, g2n], A[:], 1.0, g2n[:], ALU.add, ALU.mult)
    w_out_b = load_bf16(P, "w_out_b", w_out.t.rearrange("(k p) n -> p k n", p=128), [128, 8, D])
    rws = P.sb("rws", [128, 8, NE], F32)
    P.dma("sp", [rws], rws[:], [], rw.t.rearrange("(k p) n -> p k n", p=128))

    P.ring("cat", [128, 8, 128], BF16, 2)
    P.ring("xt", [128, D], F32, 2)
    P.ring("xsr", [128, D], F32, 2)
    P.ring("junk", [128, D], BF16, 1)
    P.ring("ssq", [128, 1], F32, 2)
    P.ring("rs", [128, 1], F32, 2)
    P.ring("h32", [128, D], F32, 3)
    P.ring("hb", [128, D], BF16, 2)
    P.ring("hT32", [128, 8, 128], F32, 1)
    P.ring("sm", [128, 4], F32, 3)
    P.ring("ex", [128, NE], F32, 2)
    P.ring("af", [128, NE], F32, 2)

    def p1(t, out):
        w = 0 if t < NL // 128 else 1
        r0 = t * 128
        cat = P.nxt("cat")
        c0_ = 0
        for (cb, nch) in cats:
            P.dma("sp", [cat], cat[:, c0_:c0_ + nch, :], [cb], cb[:, r0:r0 + 128].rearrange("(c p) t -> p c t", p=128))
            c0_ += nch
        xt = P.nxt("xt")
        P.dma("sp", [xt], xt[:], [xrows], xrows[r0:r0 + 128, :])
        y = P.nxt("yps")
        for hh in range(2):
            for k in range(8):
                P.mm(y, y[:, hh, :], cat[:, k, :], w_out_b[:, k, hh * 512:(hh + 1) * 512], k == 0, k == 7, [cat, w_out_b])
            yield
        xs = P.nxt("xsr")
        g1r = mods["g1"][w]
        P.tt("dve", [xs], xs[:], [y, g1r], y[:].rearrange("p a b -> p (a b)"), g1r[:], ALU.mult)
        yield
        P.tt("dve", [xs], xs[:], [xs, xt], xs[:], xt[:], ALU.add)
        P.dma("pool", [xs1], xs1[r0:r0 + 128, :], [xs], xs[:])
        yield
        junk = P.nxt("junk")
        ssq = P.nxt("ssq")
        P.act([junk, ssq], junk[:], [xs], xs[:], AF.Square, accum=ssq[:, 0:1])
        yield
        rs = P.nxt("rs")
        P.rstd(rs, rs[:, 0:1], ssq, ssq[:, 0:1], 1, 1.0 / D)
        yield
        h32 = P.nxt("h32")
        P.stt("dve", [h32], h32[:], [xs, rs, mods["sc2"][w]], xs[:], rs[:, 0:1], mods["sc2"][w][:], ALU.mult, ALU.mult)
        yield
        P.tt("dve", [h32], h32[:], [h32, mods["sh2"][w]], h32[:], mods["sh2"][w][:], ALU.add)
        yield
        hb = P.nxt("hb")
        P.cp("act", [hb], hb[:], [h32], h32[:])
        P.dma("pool", [h2o], h2o[r0:r0 + 128, :], [hb], hb[:])
        out["h32"] = h32
        yield

    def p2(t, h32):
        r0 = t * 128
        tp = P.nxt("tp32")
        for k in range(8):
            P.tr(tp, tp[:, k, :], h32[:, k * 128:(k + 1) * 128], P.idf[:], [h32, P.idf])
        yield
        hT = P.nxt("hT32")
        P.cp("act", [hT], hT[:], [tp], tp[:])
        yield
        lg = P.nxt("ps")
        for k in range(8):
            P.mm(lg, lg[:, 0:NE], hT[:, k, :], rws[:, k, :], k == 0, k == 7, [hT, rws])
        yield
        sm = P.nxt("sm")
        P.red("dve", [sm], sm[:, 0:1], [lg], lg[:, 0:NE], ALU.max)
        P.ts("dve", [sm], sm[:, 1:2], [sm], sm[:, 0:1], -1.0, None, ALU.mult)
        yield
        ex = P.nxt("ex")
        P.act([ex, sm], ex[:], [lg, sm], lg[:, 0:NE], AF.Exp, bias=sm[:, 1:2], accum=sm[:, 2:3])
        yield
        P.op("dve", lambda e, sm=sm: e.reciprocal(sm[:, 3:4], sm[:, 2:3]), reads=[sm], writes=[sm])
        af = P.nxt("af")
        P.ts("dve", [af], af[:], [ex, sm], ex[:], sm[:, 3:4], None, ALU.mult)
        P.dma("pool", [affo], affo[r0:r0 + 128, :], [af], af[:])
        if t < NL // 128 and affl is not affo:
            P.dma("pool", [affl], affl[r0:r0 + 128, :], [af], af[:])
        yield

    ntile = ntok // 128
    o0 = {}
    for _ in p1(0, o0):
        pass
    hprev = o0["h32"]
    for t in range(ntile):
        on = {}
        ga = p1(t + 1, on) if t + 1 < ntile else iter(())
        gb = p2(t, hprev)
        da = db = False
        while not (da and db):
            if not da:
                da = next(ga, "end") == "end"
            if not db:
                db = next(gb, "end") == "end"
        hprev = on.get("h32")
    P.phase_end()


def _ccols(inp, b):
    cc = np.zeros((128, 16), np.float32)
    cc[:, 0::2] = _cols(inp["c"][b], 8)
    cc[:, 1::2] = _cols(inp["c_ctx"], 8)
    return cc


SMAX = 640
CAP_L = 2048
CAP_C = 32
NBIS = 30


def bisect_gen(P, affs, J, e0, e1, kcap, ones_f, name, psbuf, thr):
    ne = e1 - e0
    lo = P.sb(name + "_lo", [128, ne], F32)
    mid = P.sb(name + "_mid", [128, ne], F32)
    cnt = P.sb(name + "_cnt", [128, ne], F32)
    stp = P.sb(name + "_stp", [128, ne], F32)
    cmp = P.sb(name + "_cmp", [128, J, ne], BF16)
    P.memset("dve", [lo], lo[:], 0.0)
    for it in range(NBIS):
        c = 2.0 ** -(it + 1)
        P.ts("dve", [mid], mid[:], [lo], lo[:], c, None, ALU.add)
        P.tt("dve", [cmp], cmp[:], [affs, mid], affs[:, :, e0:e1], mid[:].unsqueeze(1).to_broadcast([128, J, ne]), ALU.is_ge)
        P.red("dve", [cnt], cnt[:], [cmp], cmp[:].rearrange("p j e -> p e j"), ALU.add)
        P.mm(psbuf, psbuf[:, 0:ne], ones_f[:], cnt[:], True, True, [ones_f, cnt])
        P.ts("dve", [stp], stp[:], [psbuf], psbuf[:, 0:ne], float(kcap) - 0.5, c, ALU.is_ge, ALU.mult)
        P.tt("dve", [lo], lo[:], [lo, stp], lo[:], stp[:], ALU.add)
        yield
    P.cp("dve", [thr], thr[:, e0:e1], [lo], lo[:])
    yield


def phase_moe(P, T, layer):
    has_ctx = (layer == 0)
    ntok = NT if has_ctx else NL
    T_ = ntok // 128
    TL = NL // 128
    NS = SMAX // 128
    NSLOT = SMAX + (CAP_C if has_ctx else 0)
    P.phase_begin("moe%d" % layer)
    h2, aff, aff_all, ccols, modw, modb = T["h2_%d" % layer], T["aff_%d" % layer], T["affall_%d" % layer], T["ccols"], T["modw%d" % layer], T["modb%d" % layer]
    wg, wu, wd = T["wg%d" % layer], T["wu%d" % layer], T["wd%d" % layer]
    xacc = T["xacc%d" % layer]
    if not has_ctx:
        fing, outp = T["fing"], T["out"]
    TRASH0 = ntok
    T = T_

    P.make_ident()
    P.ring("ps", [128, 512], F32, 1, psum=True)
    P.ring("tp", [128, 8, 128], BF16, 1, psum=True)
    P.ring("accb", [128, 512], F32, 2, psum=True)
    P.ring("hb", [128, 512], F32, 4, psum=True)
    P.ring("rstd_tmp", [128, 512], F32, 1)

    P.ring("xt", [128, D], F32, 1)
    zt = P.nxt("xt")
    P.memset("dve", [zt], zt[:], 0.0)
    zt_op = P.op("sp", lambda e: e.dma_start(out=xacc[ntok:ntok + 128, :], in_=zt[:]), reads=[zt], writes=[xacc], dma=True)

    mods = compute_mod(P, ccols.t, modw.t, modb.t, 5120, 1024, ["g2"])
    g2rep = mods["g2"]

    ones_f = P.sb("ones_f", [128, 128], F32)
    P.memset("dve", [ones_f], ones_f[:], 1.0)
    ones_b = P.sb("ones_b", [128, 128], BF16)
    P.memset("dve", [ones_b], ones_b[:], 1.0)
    ltf = P.sb("ltf", [128, 128], F32)
    P.memset("pool", [ltf], ltf[:], 1.0)
    P.op("pool", lambda e: e.affine_select(ltf[:], ltf[:], [[1, 128]], ALU.is_gt, 0.0, base=0, channel_multiplier=-1),
         reads=[ltf], writes=[ltf])
    ltb = P.sb("ltb", [128, 128], BF16)
    P.cp("dve", [ltb], ltb[:], [ltf], ltf[:])
    io_i = P.sb("io_i", [128, SMAX], I32)
    P.op("pool", lambda e: e.iota(io_i[:], [[1, SMAX]], base=0, channel_multiplier=0), writes=[io_i])
    io_f = P.sb("io_f", [128, SMAX], F32)
    P.cp("dve", [io_f], io_f[:], [io_i], io_i[:])
    ip_i = P.sb("ip_i", [128, 1], I32)
    P.op("pool", lambda e: e.iota(ip_i[:], [[0, 1]], base=0, channel_multiplier=1), writes=[ip_i])
    ip_f = P.sb("ip_f", [128, 2], F32)
    P.cp("dve", [ip_f], ip_f[:, 0:1], [ip_i], ip_i[:])
    P.ts("dve", [ip_f], ip_f[:, 1:2], [ip_f], ip_f[:, 0:1], float(TRASH0), None, ALU.add)

    P.ring("wgb", [128, 8, D], BF16, 2)
    P.ring("wub", [128, 8, D], BF16, 2)
    P.ring("wdb", [128, 8, D], BF16, 2)

    def load_one(nm, src, e):
        b = P.nxt(nm)
        for k in range(8):
            P.dma("pool", [b], b[:, k, :], [], src[e, k * 128:(k + 1) * 128, :])
        return b

    w_next = (load_one("wgb", wg, 0), load_one("wub", wu, 0), load_one("wdb", wd, 0))

    af = P.sb("af", [128, T, NE], F32)
    P.dma("sp", [af], af[:], [aff], aff.t.rearrange("(t p) e -> p t e", p=128))
    thr_l = P.sb("thr_l", [128, NE], F32)
    thr_c = P.sb("thr_c", [128, NE], F32)
    main_stack = P.pstack
    sub = ExitStack()
    P.pstack = sub
    affs = P.sb("affs", [128, 128, NE], F32)
    P.dma("sp", [affs], affs[:], [aff_all], aff_all.t.rearrange("(p j) e -> p j e", p=128))
    pbanks = P.rings["accb"][0] + P.rings["hb"][0]
    gens = [bisect_gen(P, affs, 128, 0, 8, CAP_L, ones_f, "bla", pbanks[0], thr_l),
            bisect_gen(P, affs, 128, 8, 16, CAP_L, ones_f, "blb", pbanks[1], thr_l)]
    if has_ctx:
        afc = P.sb("afc", [128, 2, NE], F32)
        P.cp("dve", [afc], afc[:], [af], af[:, TL:T, :])
        gens.append(bisect_gen(P, afc, 2, 0, 16, CAP_C, ones_f, "bc", pbanks[2], thr_c))
    for _ in range(NBIS + 1):
        for g_ in gens:
            next(g_)
    P.pstack = main_stack
    P.ops.append(None)
    sub.close()

    mask = P.sb("mask", [128, T, NE], F32)
    P.tt("dve", [mask], mask[:, 0:TL, :], [af, thr_l], af[:, 0:TL, :], thr_l[:].unsqueeze(1).to_broadcast([128, TL, NE]), ALU.is_ge)
    if has_ctx:
        P.tt("dve", [mask], mask[:, TL:T, :], [af, thr_c], af[:, TL:T, :], thr_c[:].unsqueeze(1).to_broadcast([128, 2, NE]), ALU.is_ge)
    maskb = P.sb("maskb", [128, T, NE], BF16)
    P.cp("dve", [maskb], maskb[:], [mask], mask[:])
    pos = P.sb("pos", [128, T, NE], F32)
    tot = P.sb("tot", [128, T, NE], F32)
    mflat = maskb[:].rearrange("p t e -> p (t e)")
    pflat = pos[:].rearrange("p t e -> p (t e)")
    tflat = tot[:].rearrange("p t e -> p (t e)")
    n_all = T * NE
    c = 0
    while c < n_all:
        w_ = min(512, n_all - c)
        ps = P.nxt("ps")
        P.mm(ps, ps[:, 0:w_], ltb[:], mflat[:, c:c + w_], True, True, [ltb, maskb])
        P.cp("dve", [pos], pflat[:, c:c + w_], [ps], ps[:, 0:w_])
        ps = P.nxt("ps")
        P.mm(ps, ps[:, 0:w_], ones_b[:], mflat[:, c:c + w_], True, True, [ones_b, maskb])
        P.cp("dve", [tot], tflat[:, c:c + w_], [ps], ps[:, 0:w_])
        c += w_
    base = P.sb("base", [128, T, NE], F32)
    P.memset("dve", [base], base[:], 0.0)
    for t in range(1, TL):
        P.tt("dve", [base], base[:, t, :], [base, tot], base[:, t - 1, :], tot[:, t - 1, :], ALU.add)
    if has_ctx:
        P.cp("dve", [base], base[:, TL + 1, :], [tot], tot[:, TL, :])
    P.tt("dve", [pos], pos[:], [pos, base], pos[:], base[:], ALU.add)

    vals = P.sb("vals", [128, T, NE, 6], BF16)
    for t in range(T):
        P.memset("dve", [vals], vals[:, t, :, 0], float(t))
    P.ts("dve", [vals], vals[:, :, :, 1], [af, ip_f], af[:], 0.0, ip_f[:, 0:1], ALU.mult, ALU.add)
    P.memset("dve", [vals], vals[:, :, :, 5], 1.0)
    r1 = tot
    P.cp("dve", [vals], vals[:, :, :, 2], [af], af[:])
    P.tt("dve", [r1], r1[:], [af, vals], af[:], vals[:, :, :, 2], ALU.subtract)
    P.cp("dve", [vals], vals[:, :, :, 3], [r1], r1[:])
    P.tt("dve", [r1], r1[:], [r1, vals], r1[:], vals[:, :, :, 3], ALU.subtract)
    P.cp("dve", [vals], vals[:, :, :, 4], [r1], r1[:])

    P.ring("oh", [128, SMAX], BF16, 3)
    P.ring("siT", [6, NSLOT], F32, 1)
    P.ring("si", [128, 8, 6], F32, 2)
    P.ring("sx", [128, 8, 4], F32, 2)
    P.ring("ixg", [128, 8], I32, 2)
    P.ring("ixs", [128, 8], I32, 2)
    P.ring("xg", [128, D], BF16, 6 if has_ctx else 5)
    P.ring("xgT", [128, 8, NSLOT], BF16, 1)
    P.ring("sil", [128, 512], F32, 2)
    P.ring("hidT", [128, 8, NSLOT], BF16, 1)
    P.ring("ye", [128, D], F32, 2)

    NST = NS + (1 if has_ctx else 0)
    n2 = NSLOT - 512

    def stage1a(e, st):
        acc0 = P.nxt("accb")
        acc1 = P.nxt("accb")
        ohq = st["ohq"] = []

        def flush():
            while ohq:
                t, oh = ohq.pop(0)
                P.mm(acc0, acc0[0:6, :], vals[:, t, e, :], oh[:, 0:512], t == 0, t == TL - 1, [vals, oh])
                P.mm(acc1, acc1[0:6, 0:SMAX - 512], vals[:, t, e, :], oh[:, 512:SMAX], t == 0, t == TL - 1, [vals, oh])
        st["flush"] = flush
        for t in range(TL):
            oh = P.nxt("oh")
            P.ts("dve", [oh], oh[:], [io_f, pos, mask], io_f[:], pos[:, t, e:e + 1], mask[:, t, e:e + 1], ALU.is_equal, ALU.mult)
            ohq.append((t, oh))
            yield "oh"
        flush()
        siT = P.nxt("siT")
        P.cp("act", [siT], siT[:, 0:512], [acc0], acc0[0:6, :])
        P.cp("act", [siT], siT[:, 512:SMAX], [acc1], acc1[0:6, 0:SMAX - 512])
        if has_ctx:
            accc = P.nxt("ps")
            for t in range(TL, T):
                oh = P.nxt("oh")
                P.ts("dve", [oh], oh[:, 0:CAP_C], [io_f, pos, mask], io_f[:, 0:CAP_C], pos[:, t, e:e + 1], mask[:, t, e:e + 1],
                     ALU.is_equal, ALU.mult)
                P.mm(accc, accc[0:6, 0:CAP_C], vals[:, t, e, :], oh[:, 0:CAP_C], t == TL, t == T - 1, [vals, oh])
            P.cp("act", [siT], siT[:, SMAX:SMAX + CAP_C], [accc], accc[0:6, 0:CAP_C])
        sps = P.nxt("ps")
        for s_ in range(NST):
            ns = 128 if s_ < NS else CAP_C
            P.mm(sps, sps[0:ns, s_ * 6:(s_ + 1) * 6], siT[:, s_ * 128:s_ * 128 + ns], P.idf[0:6, 0:6], True, True, [siT, P.idf])
        si = P.nxt("si")
        P.memset("dve", [si], si[:], 0.0)
        P.cp("dve", [si], si[:, 0:NS, :], [sps], sps[:, 0:NS * 6].rearrange("p (s k) -> p s k", k=6))
        if has_ctx:
            P.cp("dve", [si], si[0:CAP_C, NS, :], [sps], sps[0:CAP_C, NS * 6:NS * 6 + 6])
        sx = P.nxt("sx")
        P.stt("dve", [sx], sx[:, :, 0], [si], si[:, :, 0], 128.0, si[:, :, 1], ALU.mult, ALU.add)
        P.tt("dve", [sx], sx[:, :, 1], [si], si[:, :, 2], si[:, :, 3], ALU.add)
        P.tt("dve", [sx], sx[:, :, 1], [sx, si], sx[:, :, 1], si[:, :, 4], ALU.add)
        P.ts("dve", [sx], sx[:, :, 2], [si], si[:, :, 5], -1.0, 1.0, ALU.mult, ALU.add)
        P.stt("dve", [sx], sx[:, :, 3], [sx, ip_f], sx[:, :, 2], ip_f[:, 1:2], sx[:, :, 0], ALU.mult, ALU.add)
        ixg = P.nxt("ixg")
        P.cp("dve", [ixg], ixg[:], [sx], sx[:, :, 0])
        ixs = P.nxt("ixs")
        P.cp("dve", [ixs], ixs[:], [sx], sx[:, :, 3])
        st["sx"], st["ixs"], st["ixg"], st["xgs"] = sx, ixs, ixg, [None] * NST
        yield

    def issue_gather(st, s_):
        xg = P.nxt("xg")
        ixg = st["ixg"]
        P.op("pool", lambda en: en.indirect_dma_start(
            out=xg[:], out_offset=None, in_=h2.t, in_offset=bass.IndirectOffsetOnAxis(ap=ixg[:, s_:s_ + 1], axis=0)),
            reads=[ixg, h2], writes=[xg], dma=True)
        st["xgs"][s_] = xg

    def stage1b(st):
        xgT = P.nxt("xgT")
        for s_ in range(NST):
            ns = 128 if s_ < NS else CAP_C
            xg = st["xgs"][s_]
            tp = P.nxt("tp")
            for k in range(8):
                P.tr(tp, tp[:, k, :], xg[:, k * 128:(k + 1) * 128], P.idb[:], [xg, P.idb])
            P.cp("act", [xgT], xgT[:, :, s_ * 128:s_ * 128 + ns], [tp], tp[:, :, 0:ns])
        st["xgT"] = xgT

    def stage2(e, st, wgb, wub, wdb, gen, st_next, prev_sc):
        xgT, sx, ixs = st["xgT"], st["sx"], st["ixs"]
        hidT = P.nxt("hidT")
        for fc in range(8):
            for (c0, cn) in ((0, 512), (512, n2)):
                if gen is not None and not st_next.get("ohdone"):
                    for _ in range(3):
                        if next(gen, "end") != "oh":
                            st_next["ohdone"] = True
                            break
                gp = P.nxt("hb")
                up = P.nxt("hb")
                for (dst, wb) in ((gp, wgb), (up, wub)):
                    for k in range(8):
                        P.mm(dst, dst[:, 0:cn], wb[:, k, fc * 128:(fc + 1) * 128], xgT[:, k, c0:c0 + cn], k == 0, k == 7, [wb, xgT])
                if gen is not None and "flush" in st_next:
                    st_next["flush"]()
                sil = P.nxt("sil")
                P.act([sil], sil[:, 0:cn], [gp], gp[:, 0:cn], AF.Silu)
                P.tt("dve", [hidT], hidT[:, fc, c0:c0 + cn], [sil, up], sil[:, 0:cn], up[:, 0:cn], ALU.mult)
        if gen is not None:
            for _ in gen:
                pass
        my_sc = []
        for s_ in range(NST):
            ns = 128 if s_ < NS else CAP_C
            ye = P.nxt("ye")
            g2r = g2rep[0] if s_ < NS else g2rep[1]
            for hh in range(2):
                yp = P.nxt("hb")
                for fc in range(8):
                    P.mm(yp, yp[0:ns, :], hidT[:, fc, s_ * 128:s_ * 128 + ns], wdb[:, fc, hh * 512:(hh + 1) * 512], fc == 0, fc == 7,
                         [hidT, wdb])
                P.stt("dve", [ye], ye[0:ns, hh * 512:(hh + 1) * 512], [yp, sx, g2r], yp[0:ns, :], sx[0:ns, s_, 1:2],
                      g2r[0:ns, hh * 512:(hh + 1) * 512], ALU.mult, ALU.mult)
            o_ = P.op("pool", lambda en, ye=ye, ixs=ixs, s_=s_: en.indirect_dma_start(
                out=xacc.t, out_offset=bass.IndirectOffsetOnAxis(ap=ixs[:, s_:s_ + 1], axis=0), in_=ye[:, :], in_offset=None,
                compute_op=ALU.add), reads=[ixs, ye], writes=[], dma=True, extra=prev_sc)
            my_sc.append(o_)
            if st_next is not None:
                issue_gather(st_next, s_)
        return my_sc

    st = {}
    for _ in stage1a(0, st):
        st["flush"]()
    for s_ in range(NST):
        issue_gather(st, s_)
    stage1b(st)
    prev_sc = [zt_op]
    for e in range(NE):
        wgb, wub, wdb = w_next
        st_next = None
        gen = None
        if e + 1 < NE:
            w_next = (load_one("wgb", wg, e + 1), load_one("wub", wu, e + 1), load_one("wdb", wd, e + 1))
            st_next = {}
            gen = stage1a(e + 1, st_next)
        prev_sc = stage2(e, st, wgb, wub, wdb, gen, st_next, prev_sc)
        if e + 1 < NE:
            stage1b(st_next)
        st = st_next

    if not has_ctx:
        fg = P.sb("fg", [128, D], F32)
        P.dma("sp", [fg], fg[:], [], fing.t)
        P.ring("junk", [128, D], BF16, 1)
        P.ring("ssq", [128, 1], F32, 2)
        P.ring("rs", [128, 1], F32, 2)
        for t in range(T):
            xt = P.nxt("xt")
            P.op("sp", lambda e, xt=xt, t=t: e.dma_start(out=xt[:], in_=xacc[t * 128:(t + 1) * 128, :]), reads=[xacc], writes=[xt],
                 dma=True, extra=prev_sc)
            junk = P.nxt("junk")
            ssq = P.nxt("ssq")
            P.act([junk, ssq], junk[:], [xt], xt[:], AF.Square, accum=ssq[:, 0:1])
            rs = P.nxt("rs")
            P.rstd(rs, rs[:, 0:1], ssq, ssq[:, 0:1], 1, 1.0 / D)
            P.stt("dve", [xt], xt[:], [xt, rs, fg], xt[:], rs[:, 0:1], fg[:], ALU.mult, ALU.mult)
            P.dma("sp", [outp], outp[t * 128:(t + 1) * 128, :], [xt], xt[:])
    P.phase_end()


def phase_qkv1(P, T):
    P.phase_begin("qkv1")
    xrows, ccols, modw, modb, n1g, wqkv, wqks, rope = T["xacc0"], T["ccols"], T["modw1"], T["modb1"], T["n1g1"], T["wqkv"], T["wqks"], T["rope1"]
    Q1T, K1T, V1, Kb, Vb = T["Q1T"], T["K1T"], T["V1"], T["Kb"], T["Vb"]

    P.make_ident()
    P.ring("ps", [128, 512], F32, 6, psum=True)
    P.ring("tp", [128, 8, 128], BF16, 2, psum=True)
    P.ring("xt", [128, D], F32, 2)
    P.ring("junk", [128, D], BF16, 1)
    P.ring("ssq", [128, 1], F32, 2)
    P.ring("rs", [128, 1], F32, 2)
    P.ring("rstd_tmp", [128, 512], F32, 1)
    P.ring("h32", [128, D], F32, 1)
    P.ring("hb", [128, D], BF16, 2)
    mods = compute_mod(P, ccols.t, modw.t, modb.t, 0, 2048, ["sh1", "sc1"])
    g1 = P.sb("g1", [128, D], F32)
    P.dma("sp", [g1], g1[:], [], n1g.t)
    AB = []
    for w in range(2):
        A = mods["sc1"][w]
        P.stt("dve", [A], A[:], [A, g1], A[:], 1.0, g1[:], ALU.add, ALU.mult)
        AB.append((A, mods["sh1"][w]))
    wq_b = load_bf16(P, "wq_b", wqkv.t.rearrange("(k p) n -> p k n", p=128), [128, 8, 1536])
    ws_b = load_bf16(P, "ws_b", wqks.t.rearrange("(k p) n -> p k n", p=128), [128, 8, 1280])
    P.ring("hT", [128, 8, 512], BF16, 2)
    P.ring("rp", [128, 2, 512], F32, 2)
    P.ring("t1", [128, 512], F32, 2)
    P.ring("t2", [128, 512], F32, 2)
    P.ring("qo", [128, 512], BF16, 3)
    P.ring("vo", [128, 256], BF16, 3)

    for blk in range(NT // 512 + 1):
        tok0 = blk * 512
        n = min(512, NT - tok0)
        if n <= 0:
            break
        is_ctx = tok0 >= NL
        hT = P.nxt("hT")
        for t in range(n // 128):
            tp = tile_T(P, xrows, tok0 + t * 128, AB[1 if is_ctx else 0][0], AB[1 if is_ctx else 0][1])
            P.cp("act", [hT], hT[:, :, t * 128:(t + 1) * 128], [tp], tp[:])
        if not is_ctx:
            rp = P.nxt("rp")
            P.dma("pool", [rp], rp[:], [], rope[:, :, tok0:tok0 + 512])
        for c in (range(10) if not is_ctx else (8, 9)):
            ps = P.nxt("ps")
            for k in range(8):
                P.mm(ps, ps[:, 0:n], wq_b[:, k, c * 128:(c + 1) * 128], hT[:, k, 0:n], k == 0, k == 7, [wq_b, hT])
            qo = P.nxt("qo")
            if is_ctx:
                P.cp("act", [qo], qo[:, 0:n], [ps], ps[:, 0:n])
            else:
                pss = P.nxt("ps")
                for k in range(8):
                    P.mm(pss, pss[:, 0:n], ws_b[:, k, c * 128:(c + 1) * 128], hT[:, k, 0:n], k == 0, k == 7, [ws_b, hT])
                t1 = P.nxt("t1")
                P.tt("dve", [t1], t1[:, 0:n], [ps, rp], ps[:, 0:n], rp[:, 0, 0:n], ALU.mult)
                t2 = P.nxt("t2")
                P.tt("dve", [t2], t2[:, 0:n], [pss, rp], pss[:, 0:n], rp[:, 1, 0:n], ALU.mult)
                P.tt("dve", [qo], qo[:, 0:n], [t1, t2], t1[:, 0:n], t2[:, 0:n], ALU.add)
            if c < 8:
                P.dma("sp", [Q1T], Q1T[c * 128:(c + 1) * 128, tok0:tok0 + n], [qo], qo[:, 0:n])
            else:
                P.dma("sp", [K1T], K1T[(c - 8) * 128:(c - 7) * 128, tok0:tok0 + n], [qo], qo[:, 0:n])
                if tok0 == 0:
                    P.dma("sp", [Kb], Kb[(c - 8) * 128:(c - 7) * 128, 0:128], [qo], qo[:, 0:128])
                if tok0 == NL - 512:
                    P.dma("sp", [Kb], Kb[(c - 8) * 128:(c - 7) * 128, 128:256], [qo], qo[:, 384:512])
        for t in range(n // 128):
            psv = P.nxt("ps")
            for k in range(8):
                P.mm(psv, psv[:, 0:256], hT[:, k, t * 128:(t + 1) * 128], wq_b[:, k, 1280:1536], k == 0, k == 7, [hT, wq_b])
            vo = P.nxt("vo")
            P.cp("act", [vo], vo[:], [psv], psv[:, 0:256])
            P.dma("sp", [V1], V1[tok0 + t * 128:tok0 + (t + 1) * 128, :], [vo], vo[:])
            if tok0 + t * 128 == 0:
                P.dma("sp", [Vb], Vb[0:128, :], [vo], vo[:])
            if tok0 + t * 128 == NL - 128:
                P.dma("sp", [Vb], Vb[128:256, :], [vo], vo[:])
    P.phase_end()


NB1 = NL // 128
NKL = NL + 256


def phase_attn1(P, T):
    P.phase_begin("attn1")
    Q1T, K1T, V1, Kb_all, Vb_all, masks, sinkr, selh, catT1 = (T[k] for k in ("Q1T", "K1T", "V1", "Kb_all", "Vb_all", "masks", "sinkr", "selh", "catT1"))
    scale = 64.0 ** -0.5

    P.ring("s1b", [128, 512], F32, 5, psum=True)
    P.ring("ops", [128, 512], F32, 2, psum=True)
    P.ring("bps", [128, 512], F32, 1, psum=True)
    P.ring("p1", [128, 512], BF16, 10)
    P.ring("rl", [1, 512], F32, 3)
    P.ring("lsum", [1, 512], F32, 4)
    P.ring("rhl", [1, 2, 512], BF16, 3)
    onesb16 = P.sb("onesb16", [1, 128], BF16)
    P.memset("dve", [onesb16], onesb16[:], 1.0)

    P.ring("bsb", [65, 512], F32, 2)
    P.ring("ao", [65, 512], BF16, 3)
    P.ring("qg", [64, NB1 * 512], BF16, 2)
    P.ring("kl", [64, NKL], BF16, 2)
    P.ring("kc", [64, NCX], BF16, 2)
    P.ring("vl", [128, NKL // 128, 65], BF16, 2)
    P.ring("vc", [128, 2, 65], BF16, 2)
    ones = P.sb("ones", [1, 128], F32)
    P.memset("dve", [ones], ones[:], 1.0)
    mk = P.sb("mk", [128, 4, 512], BF16)
    P.dma("sp", [mk], mk[:], [], masks.t)
    sk = P.sb("sk", [1, 2048], F32)
    P.dma("sp", [sk], sk[:], [], sinkr.t)
    P.act([sk], sk[:], [sk], sk[:], AF.Exp)
    skb = P.sb("skb", [1, 2, 2048], BF16)
    skr = P.sb("skr", [1, 2048], F32)
    P.cp("dve", [skb], skb[:, 0, :], [sk], sk[:])
    P.tt("dve", [skr], skr[:], [sk, skb], sk[:], skb[:, 0, :], ALU.subtract)
    P.cp("dve", [skb], skb[:, 1, :], [skr], skr[:])
    e0 = P.sb("e0", [1, 65], BF16)
    P.memset("dve", [e0], e0[:], 0.0)
    P.memset("dve", [e0], e0[:, 0:1], 1.0)

    sel = P.sb("sel", [128, 8], F32)
    P.dma("sp", [sel], sel[:], [], selh.t)
    P.ring("kcand", [64, 4, 256], BF16, 2)
    P.ring("vcand", [128, 4, 2, 64], BF16, 2)
    for b_ in P.rings["vl"][0]:
        P.memset("dve", [b_], b_[:, :, 0:1], 1.0)
    for b_ in P.rings["vc"][0]:
        P.memset("dve", [b_], b_[:, :, 0:1], 1.0)
    for g in range(4):
        qg = P.nxt("qg")
        for j in range(4):
            P.dma("sp", [qg], qg[:].rearrange("d (i j r) -> d i j r", j=4, r=128)[:, :, j, :], [Q1T],
                  Q1T[(g * 4 + j) * 64:(g * 4 + j + 1) * 64, :].rearrange("d (i r) -> d i r", r=128))
        kl = P.nxt("kl")
        P.dma("sp", [kl], kl[:, 128:128 + NL], [K1T], K1T[g * 64:(g + 1) * 64, 0:NL])
        kcand = P.nxt("kcand")
        P.dma("sp", [kcand], kcand[:], [Kb_all], Kb_all.t.rearrange("(r c) k -> c r k", r=4)[g * 64:(g + 1) * 64])
        for (dst0, src0, so) in ((0, 128, 0), (128 + NL, 0, 4)):
            P.ts("dve", [kl], kl[:, dst0:dst0 + 128], [kcand, sel], kcand[:, 0, src0:src0 + 128], sel[0:64, so:so + 1], None, ALU.mult)
            for r in range(1, 4):
                P.stt("dve", [kl], kl[:, dst0:dst0 + 128], [kcand, sel, kl], kcand[:, r, src0:src0 + 128], sel[0:64, so + r:so + r + 1],
                      kl[:, dst0:dst0 + 128], ALU.mult, ALU.add)
        kc = P.nxt("kc")
        P.dma("sp", [kc], kc[:], [K1T], K1T[g * 64:(g + 1) * 64, NL:NT])
        vl = P.nxt("vl")
        P.dma("sp", [vl], vl[:, 1:1 + NB1, 1:65], [V1], V1[0:NL, g * 64:(g + 1) * 64].rearrange("(t p) d -> p t d", p=128))
        vcand = P.nxt("vcand")
        P.dma("sp", [vcand], vcand[:], [Vb_all], Vb_all.t.rearrange("(r f p) c -> p r f c", r=4, f=2)[:, :, :, g * 64:(g + 1) * 64])
        for (dt_, f_, so) in ((0, 1, 0), (NB1 + 1, 0, 4)):
            P.ts("dve", [vl], vl[:, dt_, 1:65], [vcand, sel], vcand[:, 0, f_, :], sel[:, so:so + 1], None, ALU.mult)
            for r in range(1, 4):
                P.stt("dve", [vl], vl[:, dt_, 1:65], [vcand, sel, vl], vcand[:, r, f_, :], sel[:, so + r:so + r + 1], vl[:, dt_, 1:65],
                      ALU.mult, ALU.add)
        vc = P.nxt("vc")
        P.dma("sp", [vc], vc[:, :, 1:65], [V1], V1[NL:NT, g * 64:(g + 1) * 64].rearrange("(t p) d -> p t d", p=128))
        def emit_s(i):
            q_ap = qg[:, i * 512:(i + 1) * 512]
            tiles = []
            for (kb, c0) in ((kc, 0), (kc, 128), (kl, i * 128), (kl, (i + 1) * 128), (kl, (i + 2) * 128)):
                sp_ = P.nxt("s1b")
                P.mm(sp_, sp_[:, :], kb[:, c0:c0 + 128], q_ap, True, True, [kb, qg])
                tiles.append(sp_)
            return tiles

        def emit_exp(i, tiles):
            ps_ = []
            for j, sp_ in enumerate(tiles):
                pt = P.nxt("p1")
                P.act([pt], pt[:], [sp_], sp_[:], AF.Exp, scale=scale)
                if j == 2:
                    P.tt("dve", [pt], pt[:], [pt, mk], pt[:], mk[:, 2, :] if i == 0 else mk[:, 0, :], ALU.mult)
                if j == 4:
                    P.tt("dve", [pt], pt[:], [pt, mk], pt[:], mk[:, 3, :] if i == NB1 - 1 else mk[:, 1, :], ALU.mult)
                ps_.append(pt)
            return ps_

        def emit_pv(i, ps_, g=g, vc=vc, vl=vl):
            o_ps = P.nxt("ops")
            vs = (vc[:, 0, :], vc[:, 1, :], vl[:, i, :], vl[:, i + 1, :], vl[:, i + 2, :])
            vb = (vc, vc, vl, vl, vl)
            for j in range(5):
                P.mm(o_ps, o_ps[0:65, :], vs[j], ps_[j][:], j == 0, j == 4, [vb[j], ps_[j]])
            lsum = P.nxt("lsum")
            P.tt("dve", [lsum], lsum[:], [o_ps, sk], o_ps[0:1, :], sk[:, g * 512:(g + 1) * 512], ALU.add)
            return (i, o_ps, lsum)

        def emit_rl(i, o_ps, lsum):
            lnl = P.nxt("lsum")
            P.act([lnl], lnl[:], [lsum], lsum[:], AF.Ln)
            rl = P.nxt("rl")
            P.act([rl], rl[:], [lnl], lnl[:], AF.Exp, scale=-1.0)
            rh = P.nxt("rhl")
            P.cp("dve", [rh], rh[:, 0, :], [rl], rl[:])
            P.tt("dve", [rl], rl[:], [rl, rh], rl[:], rh[:, 0, :], ALU.subtract)
            P.cp("dve", [rh], rh[:, 1, :], [rl], rl[:])
            return (i, o_ps, rh)

        def emit_fin(i, o_ps, rl, g=g):
            b_ps = P.nxt("bps")
            P.mm(b_ps, b_ps[0:65, :], onesb16[:, 0:65], rl[:, 0, :], True, False, [onesb16, rl])
            P.mm(b_ps, b_ps[0:65, :], onesb16[:, 0:65], rl[:, 1, :], False, True, [onesb16, rl])
            bsb = P.nxt("bsb")
            P.cp("act", [bsb], bsb[:], [b_ps], b_ps[0:65, :])
            ao = P.nxt("ao")
            P.tt("dve", [ao], ao[:], [o_ps, bsb], o_ps[0:65, :], bsb[:], ALU.mult)
            for j in range(4):
                P.dma("sp", [catT1], catT1[(g * 4 + j) * 64:(g * 4 + j + 1) * 64, i * 128:(i + 1) * 128], [ao], ao[1:65, j * 128:(j + 1) * 128])

        prev = None
        pfin = None
        for i in range(NB1 + 1):
            tiles = emit_s(i) if i < NB1 else None
            o_ = emit_pv(*prev) if prev is not None else None
            pexp = emit_exp(i, tiles) if i < NB1 else None
            if o_ is not None:
                nfin = emit_rl(*o_)
                if pfin is not None:
                    emit_fin(*pfin)
                pfin = nfin
            prev = (i, pexp) if i < NB1 else None
        emit_fin(*pfin)
    P.phase_end()


GRP = [[0, 1, 2, 3], [4, 5, 6, 7]]


def build_fused():
    ctx = ExitStack()
    nc, P = new_prog(ctx)
    T = {}

    def di(n, s, dt=F32):
        T[n] = P.dram(n, s, dt, "ExternalInput")

    def dn(n, s, dt=F32):
        T[n] = P.dram(n, s, dt)
        T[n].relaxed = not n.startswith("xacc")

    di("xrows", [NT, D]); di("xhalo", [128, D]); di("hmask", [128, 2]); di("ccols", [128, 16])
    for l in (0, 1):
        di("modw%d" % l, [D, 6144]); di("modb%d" % l, [1, 6144]); di("n1g%d" % l, [128, D]); di("n2g%d" % l, [128, D])
        di("w_out%d" % l, [D, D]); di("rw%d" % l, [D, NE])
        di("wg%d" % l, [NE, D, D]); di("wu%d" % l, [NE, D, D]); di("wd%d" % l, [NE, D, D])
    di("fing", [128, D])
    di("w_in", [D, 1984]); di("convp", [128, 16]); di("qg", [128, 3]); di("w_uq", [256, 768]); di("w_uqs", [256, 768])
    di("w_ukv", [128, 1024]); di("ropeq", [96, 2, NL]); di("ropek", [32, 2, NL])
    di("wqkv", [D, 1536]); di("wqks", [D, 1280]); di("rope1", [128, 2, NL])
    di("masks", [128, 4, 512], BF16); di("sinkr", [1, 2048]); di("selh", [128, 8])
    T["out"] = P.dram("out", [NL, D], F32, "ExternalOutput")
    dn("QT", [8, 96, NT], BF16); dn("KTn", [512, NT], BF16); dn("KTr", [32, NT], BF16); dn("Vt", [NT, 512], BF16)
    dn("convT", [512, NT], BF16)
    for h_ in range(8):
        dn("KTn_all%d" % h_, [4 * 64, NT], BF16)
    dn("KTr_all", [4 * 32, NT], BF16); dn("Vt_all", [4, 4 * 1024, 512], BF16)
    dn("attT", [512, NT], BF16)
    dn("xacc0", [NT + 128, D]); dn("h2_0", [NT, D], BF16); dn("aff_0", [NT, NE]); dn("affl_0", [NL, NE]); dn("affall_0", [4 * NL, NE])
    dn("Q1T", [1024, NL], BF16); dn("K1T", [256, NT], BF16); dn("V1", [NT, 256], BF16)
    dn("Kb", [256, 256], BF16); dn("Vb", [256, 256], BF16); dn("Kb_all", [1024, 256], BF16); dn("Vb_all", [1024, 256], BF16)
    dn("catT1", [1024, NL], BF16)
    dn("xacc1", [NL + 128, D]); dn("h2_1", [NL, D], BF16); dn("aff_1", [NL, NE]); dn("affall_1", [4 * NL, NE])
    T["affl_1"] = T["aff_1"]
    T["xres0"] = T["xrows"]
    T["xres1"] = T["xacc0"]

    phase_A(P, T)
    P.cc_allgather(T["KTr"], T["KTr_all"], GRP)
    for k in range(4):
        P.cc_allgather(T["Vt"], T["Vt_all"], GRP, T["Vt"].t[k * 1024:(k + 1) * 1024, :], T["Vt_all"].t[k])
    for h in range(8):
        P.cc_allgather(T["KTn"], T["KTn_all%d" % h], GRP, T["KTn"].t[h * 64:(h + 1) * 64, :], T["KTn_all%d" % h].t)
    phase_B(P, T)
    phase_post(P, T, 0, NT, [(T["convT"], 4), (T["attT"], 4)])
    P.cc_allgather(T["affl_0"], T["affall_0"], GRP)
    phase_moe(P, T, 0)
    phase_qkv1(P, T)
    P.cc_allgather(T["Kb"], T["Kb_all"], GRP)
    P.cc_allgather(T["Vb"], T["Vb_all"], GRP)
    phase_attn1(P, T)
    phase_post(P, T, 1, NL, [(T["catT1"], 8)])
    P.cc_allgather(T["aff_1"], T["affall_1"], GRP)
    phase_moe(P, T, 1)
    n = P.finalize()
    return nc, ctx, n


def prep_all(inp):
    bf = ml_dtypes.bfloat16
    maps = prep_A(inp)
    w = inp["swa_w_qkv"][0]
    p64 = _swap_perm(64)
    ws = np.empty((D, 1280), np.float32)
    for h in range(20):
        ws[:, h * 64:(h + 1) * 64] = w[:, h * 64 + p64]
    r = np.arange(128)
    tri_prev = (r[:, None] >= r[None, :]).astype(np.float32)
    tri_next = (r[:, None] <= r[None, :]).astype(np.float32)
    sinkr = np.ascontiguousarray(np.repeat(inp["swa_sink"][0], 128)[None, :].astype(np.float32))
    shared = {}
    for l in (0, 1):
        shared["modw%d" % l] = np.ascontiguousarray(inp["mod_w"][l])
        shared["modb%d" % l] = np.ascontiguousarray(inp["mod_b"][l][None, :])
        shared["n1g%d" % l] = _rep(inp["norm1_g"][l])
        shared["n2g%d" % l] = _rep(inp["norm2_g"][l])
        shared["rw%d" % l] = np.ascontiguousarray(inp["router_w"][l])
        shared["wg%d" % l] = np.ascontiguousarray(inp["exp_w_gate"][l])
        shared["wu%d" % l] = np.ascontiguousarray(inp["exp_w_up"][l])
        shared["wd%d" % l] = np.ascontiguousarray(inp["exp_w_down"][l])
    shared["w_out0"] = np.ascontiguousarray(inp["ab_w_out"][0])
    shared["w_out1"] = np.ascontiguousarray(inp["swa_w_out"][0])
    shared["fing"] = _rep(inp["final_g"])
    shared["wqkv"] = np.ascontiguousarray(w)
    shared["wqks"] = ws
    shared["sinkr"] = sinkr
    out = []
    for core in range(8):
        b, q = core // 4, core % 4
        m = dict(maps[core])
        m["modw0"] = m.pop("modw"); m["modb0"] = m.pop("modb"); m["n1g0"] = m.pop("n1g")
        m.update(shared)
        C, S = _rope_tables(np.arange(q * NL, (q + 1) * NL), 64)
        rp = np.empty((128, 2, NL), np.float32)
        rp[:64, 0] = C; rp[64:, 0] = C; rp[:64, 1] = S; rp[64:, 1] = S
        m["rope1"] = rp
        mk = np.zeros((128, 4, 512), np.float32)
        mk[:, 0] = np.tile(tri_prev, (1, 4))
        mk[:, 1] = np.tile(tri_next, (1, 4))
        mk[:, 2] = mk[:, 0] if q > 0 else 0.0
        mk[:, 3] = mk[:, 1] if q < 3 else 0.0
        m["masks"] = mk.astype(bf)
        sel = np.zeros((128, 8), np.float32)
        if q > 0:
            sel[:, q - 1] = 1.0
        if q < 3:
            sel[:, 4 + q + 1] = 1.0
        m["selh"] = sel
        out.append(m)
    return out


def kernel(**inputs):
    inp = {k: np.asarray(v) for k, v in inputs.items()}
    maps = prep_all(inp)
    nc, ctx, n = build_fused()
    res = run_bass_kernel_spmd(nc, maps, core_ids=list(range(8)))
    ctx.close()
    out = np.empty((2, 4 * NL, D), np.float32)
    for c in range(8):
        out[c // 4, (c % 4) * NL:(c % 4 + 1) * NL] = np.asarray(res.results[c]["out"])
    return out
```

```python
import numpy as np
import ml_dtypes
from contextlib import ExitStack
import concourse.bass as bass
import concourse.mybir as mybir
from concourse.bass_utils import run_bass_kernel_spmd

F32 = mybir.dt.float32
BF16 = mybir.dt.bfloat16
I32 = mybir.dt.int32
ALU = mybir.AluOpType
AF = mybir.ActivationFunctionType
AX = mybir.AxisListType

D = 1024
NL = 4096
NCX = 256
NT = NL + NCX
EPS = 1e-6
NE = 16
ENGS = ["pe", "act", "dve", "pool", "sp"]
NDMA_SEM = 56


class Buf:
    __slots__ = ("name", "last_w", "readers", "t", "relaxed")

    def __init__(self, name, t=None):
        self.name = name
        self.last_w = None
        self.readers = []
        self.t = t
        self.relaxed = False

    def __getitem__(self, k):
        return self.t[k]


class Op:
    __slots__ = ("eng", "fn", "deps", "dma", "idx", "need_inc", "sem", "val", "waits", "cc")


class Prog:
    def __init__(self, nc, ctx):
        self.nc = nc
        self.ctx = ctx
        self.ops = []
        self.rings = {}
        self.phase = "p"
        self.pstack = None

    def phase_begin(self, name):
        self.phase = name
        self.pstack = ExitStack()
        self.rings = {}

    def phase_end(self):
        self.ops.append(None)
        self.pstack.close()
        self.pstack = None

    def sb(self, name, shape, dt):
        t = self.pstack.enter_context(self.nc.sbuf_tensor("%s_%s" % (self.phase, name), shape, dt))
        return Buf(name, t)

    def ps(self, name, shape, dt):
        t = self.pstack.enter_context(self.nc.psum_tensor("%s_%s" % (self.phase, name), shape, dt))
        return Buf(name, t)

    def cc_allgather(self, src, dst, groups, src_ap=None, dst_ap=None):
        src_ap = src.t if src_ap is None else src_ap
        dst_ap = dst.t if dst_ap is None else dst_ap
        o = self.op("pool", lambda e: e.collective_compute("AllGather", ALU.bypass, replica_groups=groups,
                                                           ins=[src_ap], outs=[dst_ap]), reads=[src], writes=[dst])
        o.cc = True
        return o

    def ring(self, name, shape, dt, n, psum=False):
        bufs = [(self.ps if psum else self.sb)("%s%d" % (name, i), shape, dt) for i in range(n)]
        self.rings[name] = [bufs, 0]

    def nxt(self, name):
        r = self.rings[name]
        b = r[0][r[1] % len(r[0])]
        r[1] += 1
        return b

    def dram(self, name, shape, dt, kind=None):
        if kind is None:
            t = self.nc.dram_tensor(name, list(shape), dt)
        else:
            t = self.nc.dram_tensor(name, list(shape), dt, kind=kind)
        return Buf(name, t.ap())

    def op(self, eng, fn, reads=(), writes=(), dma=False, extra=()):
        o = Op()
        o.eng = eng
        o.fn = fn
        o.dma = dma
        o.idx = len(self.ops)
        deps = set()
        for b in reads:
            if b.last_w is not None:
                deps.add(b.last_w)
        for b in writes:
            if b.relaxed:
                continue
            if b.last_w is not None:
                deps.add(b.last_w)
            for r in b.readers:
                deps.add(r)
        for x in extra:
            deps.add(x.idx)
        deps.discard(o.idx)
        o.deps = deps
        for b in reads:
            b.readers.append(o.idx)
        for b in writes:
            b.last_w = o.idx
            b.readers = []
        o.need_inc = False
        o.cc = False
        self.ops.append(o)
        return o

    def finalize(self):
        nc = self.nc
        ctx = self.ctx
        ops = self.ops

        def pe_pe(p, o):
            return p.eng == "pe" and o.eng == "pe" and not p.dma and not o.dma

        last = {}
        for o in ops:
            if o is None:
                for e_, lo in last.items():
                    lo.need_inc = True
                continue
            last[o.eng] = o
            for d in o.deps:
                if not pe_pe(ops[d], o):
                    ops[d].need_inc = True
            if o.dma or o.cc:
                o.need_inc = True
        eng_sem = {e: ctx.enter_context(nc.semaphore("s_" + e)) for e in ENGS}
        dma_sems = [ctx.enter_context(nc.semaphore("d%d" % i)) for i in range(NDMA_SEM)]
        cc_sem = ctx.enter_context(nc.semaphore("cc_sem"))
        cnt = {e: 0 for e in ENGS}
        dcnt = [0] * NDMA_SEM
        ccnt = 0
        dma_rr = 0
        seen = {e: {} for e in ENGS}
        pending = {e: None for e in ENGS}
        for o in ops:
            if o is None:
                snap = [(("e", e), eng_sem[e], cnt[e]) for e in ENGS if cnt[e] > 0]
                snap += [(("d", k), dma_sems[k], dcnt[k]) for k in range(NDMA_SEM) if dcnt[k] > 0]
                if ccnt > 0:
                    snap.append((("cc",), cc_sem, ccnt))
                for e in ENGS:
                    pending[e] = snap
                continue
            waits = []
            s = seen[o.eng]
            if pending[o.eng] is not None:
                for key, sem, val in pending[o.eng]:
                    if s.get(key, 0) < val:
                        s[key] = val
                        waits.append((sem, val))
                pending[o.eng] = None
            for d in sorted(o.deps):
                p = ops[d]
                if pe_pe(p, o):
                    continue
                key, sem = p.sem
                if s.get(key, 0) < p.val:
                    s[key] = p.val
                    waits.append((sem, p.val))
            if o.cc:
                ccnt += 1
                o.sem = (("cc",), cc_sem)
                o.val = ccnt
            elif o.dma:
                k = dma_rr
                dma_rr = (dma_rr + 1) % NDMA_SEM
                key = ("d", k)
                if dcnt[k] > 0 and s.get(key, 0) < dcnt[k]:
                    s[key] = dcnt[k]
                    waits.append((dma_sems[k], dcnt[k]))
                dcnt[k] += 16
                o.sem = (key, dma_sems[k])
                o.val = dcnt[k]
            elif o.need_inc:
                cnt[o.eng] += 1
                o.sem = (("e", o.eng), eng_sem[o.eng])
                o.val = cnt[o.eng]
            o.waits = waits
        final_waits = [(dma_sems[k], dcnt[k]) for k in range(NDMA_SEM) if dcnt[k] > 0]
        final_waits += [(eng_sem[e], cnt[e]) for e in ENGS if cnt[e] > 0]
        if ccnt > 0:
            final_waits.append((cc_sem, ccnt))
        per = {e: [o for o in ops if o is not None and o.eng == e] for e in ENGS}
        engmap = {"pe": "tensor", "act": "scalar", "dve": "vector", "pool": "gpsimd", "sp": "sync"}
        with nc.Block() as block:
            for e in ENGS:
                def body(eng, lst=per[e], final=(e == "sp")):
                    for o in lst:
                        for (sem, val) in o.waits:
                            eng.wait_ge(sem, val)
                        ins = o.fn(eng)
                        if o.cc:
                            ins.then_inc(o.sem[1])
                        elif o.need_inc:
                            ins.then_inc(o.sem[1], 16 if o.dma else 1)
                    if final:
                        for (sem, val) in final_waits:
                            eng.wait_ge(sem, val)
                getattr(block, engmap[e])(body)
        return len(per["pe"]) + len(per["act"]) + len(per["dve"]) + len(per["pool"]) + len(per["sp"])

    def mm(self, ps, out, lhsT, rhs, start, stop, rd):
        self.op("pe", lambda e: e.matmul(out, lhsT=lhsT, rhs=rhs, start=start, stop=stop), reads=rd, writes=[ps])

    def tr(self, ps, out, in_, ident, rd):
        self.op("pe", lambda e: e.transpose(out, in_, ident), reads=rd, writes=[ps])

    def act(self, wr, out, rd, in_, func, scale=1.0, bias=None, accum=None):
        kw = {}
        if bias is not None:
            kw["bias"] = bias
        if accum is not None:
            kw["accum_out"] = accum
        self.op("act", lambda e: e.activation(out=out, in_=in_, func=func, scale=scale, **kw), reads=rd, writes=wr)

    def ts(self, eng, wr, out, rd, in0, s1, s2, op0, op1=None, accum=None):
        kw = {}
        if op1 is not None:
            kw["op1"] = op1
        if accum is not None:
            kw["accum_out"] = accum
        self.op(eng, lambda e: e.tensor_scalar(out, in0, s1, s2, op0, **kw), reads=rd, writes=wr)

    def tt(self, eng, wr, out, rd, in0, in1, op):
        self.op(eng, lambda e: e.tensor_tensor(out, in0, in1, op), reads=rd, writes=wr)

    def stt(self, eng, wr, out, rd, in0, scalar, in1, op0, op1):
        self.op(eng, lambda e: e.scalar_tensor_tensor(out, in0, scalar, in1, op0, op1), reads=rd, writes=wr)

    def cp(self, eng, wr, out, rd, in_):
        if eng == "act":
            self.op("act", lambda e: e.copy(out, in_), reads=rd, writes=wr)
        else:
            self.op(eng, lambda e: e.tensor_copy(out, in_), reads=rd, writes=wr)

    def memset(self, eng, wr, out, val):
        self.op(eng, lambda e: e.memset(out, val), writes=wr)

    def dma(self, q, wr, out, rd, in_):
        self.op(q, lambda e: e.dma_start(out=out, in_=in_), reads=rd, writes=wr, dma=True)

    def red(self, eng, wr, out, rd, in_, op, axis=AX.X):
        self.op(eng, lambda e: e.tensor_reduce(out, in_, axis, op), reads=rd, writes=wr)

    def make_ident(self):
        idf = self.sb("idf", [128, 128], F32)
        idb = self.sb("idb", [128, 128], BF16)
        self.memset("pool", [idf], idf[:], 1.0)
        self.op("pool", lambda e: e.affine_select(idf[:], idf[:], [[-1, 128]], ALU.is_equal, 0.0, base=0,
                                                 channel_multiplier=1), reads=[idf], writes=[idf])
        self.cp("dve", [idb], idb[:], [idf], idf[:])
        self.idf, self.idb = idf, idb
        eb = self.sb("epsb", [128, 1], F32)
        self.memset("dve", [eb], eb[:], EPS)
        self.epsb = eb

    def rstd(self, out_buf, out, ssq_buf, ssq, n, scale):
        tmp = self.nxt("rstd_tmp")
        self.act([tmp], tmp[:, 0:n], [ssq_buf, self.epsb], ssq, AF.Ln, scale=scale, bias=self.epsb[:, 0:1])
        self.act([out_buf], out, [tmp], tmp[:, 0:n], AF.Exp, scale=-0.5)


def new_prog(ctx):
    nc = bass.Bass("TRN2", target_bir_lowering=False)
    P = Prog(nc, ctx)
    return nc, P


def load_bf16(P, name, src_ap, shape):
    b = P.sb(name, shape, BF16)
    P.dma("pool", [b], b[:], [], src_ap)
    return b


def tile_T(P, xrows, row0, A, Bm):
    xt = P.nxt("xt")
    P.dma("pool", [xt], xt[:], [xrows], xrows[row0:row0 + 128, :])
    junk = P.nxt("junk")
    ssq = P.nxt("ssq")
    P.act([junk, ssq], junk[:], [xt], xt[:], AF.Square, accum=ssq[:, 0:1])
    rs = P.nxt("rs")
    P.rstd(rs, rs[:, 0:1], ssq, ssq[:, 0:1], 1, 1.0 / D)
    h32 = P.nxt("h32")
    P.stt("dve", [h32], h32[:], [xt, rs, A], xt[:], rs[:, 0:1], A[:], ALU.mult, ALU.mult)
    hb = P.nxt("hb")
    P.tt("dve", [hb], hb[:], [h32, Bm], h32[:], Bm[:], ALU.add)
    tp = P.nxt("tp")
    for k in range(8):
        P.tr(tp, tp[:, k, :], hb[:, k * 128:(k + 1) * 128], P.idb[:], [hb, P.idb])
    return tp


def compute_mod(P, ccols_ap, modw_ap, modb_ap, col0, ncols, names):
    nseg = ncols // 1024
    cc = P.sb("cc", [128, 16], F32)
    P.dma("sp", [cc], cc[:], [], ccols_ap)
    sc = P.sb("sc", [128, 16], F32)
    P.act([sc], sc[:], [cc], cc[:], AF.Silu)
    ones2 = P.sb("ones2", [1, 2], F32)
    P.memset("dve", [ones2], ones2[:], 1.0)
    mb = P.sb("mb", [1, ncols], F32)
    P.dma("sp", [mb], mb[:], [], modb_ap[0:1, col0:col0 + ncols])
    modrows = P.sb("modrows", [2, ncols], F32)
    MWC = 128
    P.ring("mw", [128, 8, MWC], F32, 2)
    for j in range(ncols // MWC):
        mw = P.nxt("mw")
        P.dma("sp", [mw], mw[:], [], modw_ap[:, col0 + j * MWC: col0 + (j + 1) * MWC].rearrange("(k p) n -> p k n", p=128))
        ps = P.nxt("ps")
        for k in range(8):
            P.mm(ps, ps[0:2, 0:MWC], sc[:, 2 * k:2 * k + 2], mw[:, k, :], k == 0, False, [sc, mw])
        P.mm(ps, ps[0:2, 0:MWC], ones2[:], mb[:, j * MWC:(j + 1) * MWC], False, True, [ones2, mb])
        P.cp("dve", [modrows], modrows[:, j * MWC:(j + 1) * MWC], [ps], ps[0:2, 0:MWC])
    sel = P.sb("sel", [2, 2, 128], F32)
    P.memset("dve", [sel], sel[:], 0.0)
    P.memset("dve", [sel], sel[0:1, 0, :], 1.0)
    P.ts("dve", [sel], sel[:, 1, :], [sel], sel[:, 0, :], -1.0, 1.0, ALU.mult, ALU.add)
    out = {}
    for si, nm in enumerate(names):
        reps = []
        for w in range(2):
            rep = P.sb("mod_%s_%d" % (nm, w), [128, 1024], F32)
            for hh in range(2):
                ps = P.nxt("ps")
                P.mm(ps, ps[:, :], sel[:, w, :], modrows[:, si * 1024 + hh * 512: si * 1024 + (hh + 1) * 512], True, True,
                     [sel, modrows])
                P.cp("act", [rep], rep[:, hh * 512:(hh + 1) * 512], [ps], ps[:, :])
            reps.append(rep)
        out[nm] = reps
    return out


def phase_A(P, T):
    P.phase_begin("A")
    xrows, xhalo, hmask, ccols, modw, modb, n1g = T["xrows"], T["xhalo"], T["hmask"], T["ccols"], T["modw0"], T["modb0"], T["n1g0"]
    w_in, convp, qg, w_uq, w_uqs, w_ukv, ropeq, ropek = (T[k] for k in ("w_in", "convp", "qg", "w_uq", "w_uqs", "w_ukv", "ropeq", "ropek"))
    QT, KTn, KTr, Vt, convT = T["QT"], T["KTn"], T["KTr"], T["Vt"], T["convT"]

    P.make_ident()
    P.ring("ps", [128, 512], F32, 6, psum=True)
    P.ring("tp", [128, 8, 128], BF16, 2, psum=True)
    P.ring("xt", [128, D], F32, 2)
    P.ring("junk", [128, D], BF16, 1)
    P.ring("ssq", [128, 1], F32, 2)
    P.ring("rs", [128, 1], F32, 2)
    P.ring("rstd_tmp", [128, 512], F32, 1)
    P.ring("h32", [128, D], F32, 1)
    P.ring("hb", [128, D], BF16, 2)

    mods = compute_mod(P, ccols.t, modw.t, modb.t, 0, 2048, ["sh1", "sc1"])
    g1 = P.sb("g1", [128, D], F32)
    P.dma("sp", [g1], g1[:], [], n1g.t)
    AB = []
    for w in range(2):
        A = mods["sc1"][w]
        P.stt("dve", [A], A[:], [A, g1], A[:], 1.0, g1[:], ALU.add, ALU.mult)
        AB.append((A, mods["sh1"][w]))

    w_in_b = load_bf16(P, "w_in_b", w_in.t.rearrange("(k p) n -> p k n", p=128), [128, 8, 1984])
    w_uq_b = load_bf16(P, "w_uq_b", w_uq.t.rearrange("(k p) n -> p k n", p=128), [128, 2, 768])
    w_uqs_b = load_bf16(P, "w_uqs_b", w_uqs.t.rearrange("(k p) n -> p k n", p=128), [128, 2, 768])
    w_ukv_b = load_bf16(P, "w_ukv_b", w_ukv.t, [128, 1024])
    cvp = P.sb("cvp", [128, 16], F32)
    P.dma("sp", [cvp], cvp[:], [], convp.t)
    qgs = P.sb("qgs", [128, 3], F32)
    P.dma("sp", [qgs], qgs[:], [], qg.t)
    hm = P.sb("hm", [128, 2], F32)
    P.dma("sp", [hm], hm[:], [], hmask.t)
    P.ring("rq", [96, 2, 512], F32, 1)
    P.ring("rk", [32, 2, 512], F32, 1)
    onesb = P.sb("onesb", [128, 128], BF16)
    P.memset("dve", [onesb], onesb[:], 1.0)

    P.ring("hseg", [128, 8, 1022], BF16, 2)
    halo_tmp = P.sb("halo_tmp", [128, 8, 2], BF16)
    tph = tile_T(P, xhalo, 0, AB[0][0], AB[0][1])
    P.cp("act", [halo_tmp], halo_tmp[:], [tph], tph[:, :, 0:2])

    def fill_seg(hseg, G0, G1):
        t_lo = max(0, (G0 - 1) // 128)
        t_hi = min(NL // 128 - 1, (G1 - 2) // 128)
        for t in range(t_lo, t_hi + 1):
            a_ = max(G0, 1 + 128 * t)
            b_ = min(G1, 129 + 128 * t)
            if b_ <= a_:
                continue
            tp = tile_T(P, xrows, t * 128, AB[0][0], AB[0][1])
            P.cp("act", [hseg], hseg[:, :, a_ - G0:b_ - G0], [tp], tp[:, :, a_ - 1 - 128 * t:b_ - 1 - 128 * t])
        if G0 == 0:
            P.cp("dve", [hseg], hseg[:, :, 0:1], [halo_tmp], halo_tmp[:, :, 0:1])
        if G1 == NL + 2:
            P.cp("dve", [hseg], hseg[:, :, NL + 1 - G0:NL + 2 - G0], [halo_tmp], halo_tmp[:, :, 1:2])

    P.ring("gcs", [128, 512], F32, 2)
    P.ring("zw", [128, 512], F32, 2)
    P.ring("ca", [128, 512], F32, 2)
    P.ring("co", [128, 512], BF16, 3)
    P.ring("lat", [128, 512], F32, 3)
    P.ring("sq", [128, 512], BF16, 3)
    P.ring("rstdL", [128, 512], F32, 1)
    P.ring("qn", [128, 2, 512], BF16, 2)
    P.ring("kvn", [128, 512], BF16, 2)
    P.ring("t1", [96, 512], F32, 1)
    P.ring("t2", [96, 512], F32, 1)
    P.ring("qo", [96, 512], BF16, 3)
    P.ring("ko", [128, 512], BF16, 3)
    P.ring("vo", [128, 512], BF16, 3)

    def proj(hT, c0, n, col_lo, col_n):
        ps = P.nxt("ps")
        for k in range(8):
            P.mm(ps, ps[0:col_n, 0:n], w_in_b[:, k, col_lo:col_lo + col_n], hT[:, k, c0:c0 + n], k == 0, k == 7,
                 [w_in_b, hT])
        return ps

    def latent_norm(pss, nchunk, n, gcol0, out_ring):
        lats, sqs = [], []
        for c in range(nchunk):
            lt = P.nxt("lat")
            P.cp("act", [lt], lt[:, 0:n], [pss[c]], pss[c][:, 0:n])
            sq = P.nxt("sq")
            P.act([sq], sq[:, 0:n], [pss[c]], pss[c][:, 0:n], AF.Square)
            lats.append(lt)
            sqs.append(sq)
        ps = P.nxt("ps")
        for c in range(nchunk):
            P.mm(ps, ps[:, 0:n], onesb[:], sqs[c][:, 0:n], c == 0, c == nchunk - 1, [onesb, sqs[c]])
        rl = P.nxt("rstdL")
        P.rstd(rl, rl[:, 0:n], ps, ps[:, 0:n], n, 1.0 / (128 * nchunk))
        ob = P.nxt(out_ring)
        for c in range(nchunk):
            o_ap = ob[:, c, 0:n] if nchunk > 1 else ob[:, 0:n]
            P.stt("dve", [ob], o_ap, [lats[c], qgs, rl], lats[c][:, 0:n], qgs[:, gcol0 + c:gcol0 + c + 1], rl[:, 0:n],
                  ALU.mult, ALU.mult)
        return ob

    def block(hT, c0, n, tok0, is_ctx, first, last):
        no = n - 2
        if not is_ctx:
            rq = P.nxt("rq")
            P.dma("pool", [rq], rq[:, :, 0:no], [], ropeq[:, :, tok0:tok0 + no])
            rk = P.nxt("rk")
            P.dma("pool", [rk], rk[:, :, 0:no], [], ropek[:, :, tok0:tok0 + no])
        for c in range(4):
            ps_gc = proj(hT, c0, n, 512 + c * 128, 128)
            gcs = P.nxt("gcs")
            P.cp("act", [gcs], gcs[:, 0:n], [ps_gc], ps_gc[:, 0:n])
            ps_u = proj(hT, c0, n, 1024 + c * 128, 128)
            zw = P.nxt("zw")
            P.tt("dve", [zw], zw[:, 0:n], [ps_u, gcs], ps_u[:, 0:n], gcs[:, 0:n], ALU.mult)
            if is_ctx:
                P.memset("dve", [zw], zw[:, 0:1], 0.0)
                P.memset("dve", [zw], zw[:, n - 1:n], 0.0)
            else:
                if first:
                    P.ts("dve", [zw], zw[:, 0:1], [zw, hm], zw[:, 0:1], hm[:, 0:1], None, ALU.mult)
                if last:
                    P.ts("dve", [zw], zw[:, n - 1:n], [zw, hm], zw[:, n - 1:n], hm[:, 1:2], None, ALU.mult)
            ca = P.nxt("ca")
            P.ts("dve", [ca], ca[:, 0:no], [zw, cvp], zw[:, 1:n - 1], cvp[:, c * 4 + 1:c * 4 + 2], cvp[:, c * 4 + 3:c * 4 + 4],
                 ALU.mult, ALU.add)
            P.stt("dve", [ca], ca[:, 0:no], [zw, cvp, ca], zw[:, 0:n - 2], cvp[:, c * 4:c * 4 + 1], ca[:, 0:no], ALU.mult, ALU.add)
            P.stt("dve", [ca], ca[:, 0:no], [zw, cvp, ca], zw[:, 2:n], cvp[:, c * 4 + 2:c * 4 + 3], ca[:, 0:no], ALU.mult, ALU.add)
            ps_gb = proj(hT, c0, n, c * 128, 128)
            co = P.nxt("co")
            P.tt("dve", [co], co[:, 0:no], [ps_gb, ca], ps_gb[:, 1:n - 1], ca[:, 0:no], ALU.mult)
            P.dma("sp", [convT], convT[c * 128:(c + 1) * 128, tok0:tok0 + no], [co], co[:, 0:no])
        pq = [proj(hT, c0, n, 1536 + c * 128, 128) for c in range(2)]
        qn = latent_norm(pq, 2, n, 0, "qn")
        for h in range(8):
            psq = P.nxt("ps")
            for k in range(2):
                P.mm(psq, psq[0:96, 0:n], w_uq_b[:, k, h * 96:(h + 1) * 96], qn[:, k, 0:n], k == 0, k == 1, [w_uq_b, qn])
            qo = P.nxt("qo")
            if is_ctx:
                P.cp("act", [qo], qo[:, 0:no], [psq], psq[0:96, 1:n - 1])
            else:
                pss = P.nxt("ps")
                for k in range(2):
                    P.mm(pss, pss[0:96, 0:n], w_uqs_b[:, k, h * 96:(h + 1) * 96], qn[:, k, 0:n], k == 0, k == 1, [w_uqs_b, qn])
                t1 = P.nxt("t1")
                P.tt("dve", [t1], t1[:, 0:no], [psq, rq], psq[0:96, 1:n - 1], rq[:, 0, 0:no], ALU.mult)
                t2 = P.nxt("t2")
                P.tt("dve", [t2], t2[:, 0:no], [pss, rq], pss[0:96, 1:n - 1], rq[:, 1, 0:no], ALU.mult)
                P.tt("dve", [qo], qo[:, 0:no], [t1, t2], t1[:, 0:no], t2[:, 0:no], ALU.add)
            P.dma("sp", [QT], QT[h, :, tok0:tok0 + no], [qo], qo[:, 0:no])
        pk = [proj(hT, c0, n, 1792, 128)]
        kvn = latent_norm(pk, 1, n, 2, "kvn")
        for c in range(4):
            psk = P.nxt("ps")
            P.mm(psk, psk[:, 0:n], w_ukv_b[:, c * 128:(c + 1) * 128], kvn[:, 0:n], True, True, [w_ukv_b, kvn])
            ko = P.nxt("ko")
            P.cp("act", [ko], ko[:, 0:no], [psk], psk[:, 1:n - 1])
            P.dma("sp", [KTn], KTn[c * 128:(c + 1) * 128, tok0:tok0 + no], [ko], ko[:, 0:no])
        t0 = 0
        while t0 < no:
            m = min(128, no - t0)
            psv = P.nxt("ps")
            P.mm(psv, psv[0:m, :], kvn[:, 1 + t0:1 + t0 + m], w_ukv_b[:, 512:1024], True, True, [kvn, w_ukv_b])
            vo = P.nxt("vo")
            P.cp("act", [vo], vo[0:m, :], [psv], psv[0:m, :])
            P.dma("sp", [Vt], Vt[tok0 + t0:tok0 + t0 + m, :], [vo], vo[0:m, :])
            t0 += m
        psr = proj(hT, c0, n, 1920, 32)
        ko = P.nxt("ko")
        if is_ctx:
            P.cp("act", [ko], ko[0:32, 0:no], [psr], psr[0:32, 1:n - 1])
        else:
            psrs = proj(hT, c0, n, 1952, 32)
            t1 = P.nxt("t1")
            P.tt("dve", [t1], t1[0:32, 0:no], [psr, rk], psr[0:32, 1:n - 1], rk[:, 0, 0:no], ALU.mult)
            t2 = P.nxt("t2")
            P.tt("dve", [t2], t2[0:32, 0:no], [psrs, rk], psrs[0:32, 1:n - 1], rk[:, 1, 0:no], ALU.mult)
            P.tt("dve", [ko], ko[0:32, 0:no], [t1, t2], t1[0:32, 0:no], t2[0:32, 0:no], ALU.add)
        P.dma("sp", [KTr], KTr[:, tok0:tok0 + no], [ko], ko[0:32, 0:no])

    nb = (NL + 509) // 510
    for sg in range((nb + 1) // 2):
        G0 = 1020 * sg
        G1 = min(G0 + 1022, NL + 2)
        hseg = P.nxt("hseg")
        fill_seg(hseg, G0, G1)
        for j in (2 * sg, 2 * sg + 1):
            if j >= nb:
                continue
            c0 = 510 * j
            n = min(512, NL + 2 - c0)
            block(hseg, c0 - G0, n, c0, False, j == 0, j == nb - 1)
    hseg = P.nxt("hseg")
    for t in range(2):
        tp = tile_T(P, xrows, NL + t * 128, AB[1][0], AB[1][1])
        P.cp("act", [hseg], hseg[:, :, 1 + t * 128:129 + t * 128], [tp], tp[:])
    P.cp("dve", [hseg], hseg[:, :, 0:1], [hseg], hseg[:, :, 1:2])
    P.cp("dve", [hseg], hseg[:, :, 257:258], [hseg], hseg[:, :, 1:2])
    block(hseg, 0, 258, NL, True, True, True)
    P.phase_end()


def _swap_perm(dim):
    q = dim // 4
    d = np.arange(dim)
    return np.where((d % (2 * q)) < q, d + q, d - q)


def _rope_tables(pos, dim):
    half = dim // 2
    q = dim // 4
    freqs = (10000.0 ** (-np.arange(0, half, 2, dtype=np.float32) / np.float32(half))).astype(np.float32)
    row = (pos // 64).astype(np.float32)
    col = (pos % 64).astype(np.float32)
    ang = np.concatenate([row[:, None] * freqs, col[:, None] * freqs], axis=-1).astype(np.float32)
    cos, sin = np.cos(ang).astype(np.float32), np.sin(ang).astype(np.float32)
    d = np.arange(dim)
    j = (d // (2 * q)) * q + d % q
    sign = np.where((d % (2 * q)) < q, -1.0, 1.0).astype(np.float32)
    C = cos[:, j].T.copy()
    S = (sin[:, j] * sign[None, :]).T.copy()
    return C, S


def _rep(v):
    return np.ascontiguousarray(np.broadcast_to(np.asarray(v, np.float32)[None, :], (128, v.shape[0])))


def _cols(v, k):
    return np.ascontiguousarray(np.asarray(v, np.float32).reshape(k, 128).T)


def prep_A(inp):
    x, c, cx, c_ctx = inp["x"], inp["c"], inp["ctx"], inp["c_ctx"]
    w_in = inp["ab_w_in"][0]
    p32 = _swap_perm(32)
    w_in_ext = np.ascontiguousarray(np.concatenate([w_in, w_in[:, 1920 + p32]], axis=1))
    w_uq = inp["mla_w_uq"][0]
    w_uqs = w_uq.copy()
    for h in range(8):
        w_uqs[:, h * 96 + 64:h * 96 + 96] = w_uq[:, h * 96 + 64 + p32]
    wk = inp["mla_w_ukv"][0].reshape(128, 8, 128)
    w_ukv = np.ascontiguousarray(np.concatenate([wk[:, :, :64].reshape(128, 512), wk[:, :, 64:].reshape(128, 512)], axis=1))
    convp = np.zeros((128, 16), np.float32)
    for ch in range(4):
        for i in range(3):
            convp[:, ch * 4 + i] = inp["conv_w"][0][i, ch * 128:(ch + 1) * 128]
        convp[:, ch * 4 + 3] = inp["conv_b"][0][ch * 128:(ch + 1) * 128]
    qg = np.concatenate([_cols(inp["mla_q_norm_g"][0], 2), _cols(inp["mla_kv_norm_g"][0], 1)], axis=1)
    maps = []
    for core in range(8):
        b, q = core // 4, core % 4
        T0 = q * NL
        xr = np.ascontiguousarray(np.concatenate([x[b, T0:T0 + NL], cx[b]], axis=0))
        xh = np.zeros((128, D), np.float32)
        if q > 0:
            xh[0] = x[b, T0 - 1]
        if q < 3:
            xh[1] = x[b, T0 + NL]
        hm = np.zeros((128, 2), np.float32)
        hm[:, 0] = 1.0 if q > 0 else 0.0
        hm[:, 1] = 1.0 if q < 3 else 0.0
        cc = np.zeros((128, 16), np.float32)
        cc[:, 0::2] = _cols(c[b], 8)
        cc[:, 1::2] = _cols(c_ctx, 8)
        C, S = _rope_tables(np.arange(T0, T0 + NL), 32)
        rq = np.zeros((96, 2, NL), np.float32)
        rq[:64, 0] = 1.0
        rq[64:, 0] = C
        rq[64:, 1] = S
        rk = np.stack([C, S], axis=1)
        maps.append({
            "xrows": xr, "xhalo": xh, "hmask": hm, "ccols": cc,
            "modw": np.ascontiguousarray(inp["mod_w"][0]), "modb": np.ascontiguousarray(inp["mod_b"][0][None, :]),
            "n1g": _rep(inp["norm1_g"][0]), "w_in": w_in_ext, "convp": convp, "qg": np.ascontiguousarray(qg),
            "w_uq": np.ascontiguousarray(w_uq), "w_uqs": np.ascontiguousarray(w_uqs), "w_ukv": w_ukv,
            "ropeq": rq, "ropek": np.ascontiguousarray(rk),
        })
    return maps


NK0 = NCX + 4 * NL
NKT0 = NK0 // 128


def phase_B(P, T):
    P.phase_begin("B")
    QT, KTn, KTr, Vt, KTr_all, Vt_all, attT = (T[k] for k in ("QT", "KTn", "KTr", "Vt", "KTr_all", "Vt_all", "attT"))
    scale = 96.0 ** -0.5

    P.ring("sps", [128, 2, 512], F32, 2, psum=True)
    P.ring("ops", [128, 512], F32, 2, psum=True)
    P.ring("bps", [128, 512], F32, 1, psum=True)
    P.ring("kt", [96, NK0], BF16, 2)
    P.ring("vp", [128, NKT0, 65], BF16, 2)
    P.ring("qt", [96, NT], BF16, 2)
    P.ring("pT", [128, 2, 512], BF16, 4)
    P.ring("rl", [1, 512], F32, 2)
    P.ring("bsb", [65, 512], F32, 2)
    P.ring("ao", [65, 512], BF16, 2)
    ones = P.sb("ones", [1, 128], F32)
    P.memset("dve", [ones], ones[:], 1.0)

    pending = [None]

    def qblock(h, kt_b, vp_b, qt_b, q0, nq, ntile):
        o_ps = P.nxt("ops")
        ng = ntile // 2

        def emit_pv(g, pT):
            for i in range(2):
                t = 2 * g + i
                P.mm(o_ps, o_ps[0:65, 0:nq], vp_b[:, t, :], pT[:, i, 0:nq], t == 0, t == ntile - 1, [vp_b, pT])

        prev = None
        for g in range(ng):
            s_ps = P.nxt("sps")
            for i in range(2):
                t = 2 * g + i
                P.mm(s_ps, s_ps[:, i, 0:nq], kt_b[:, t * 128:(t + 1) * 128], qt_b[:, q0:q0 + nq], True, True, [kt_b, qt_b])
            if prev is not None:
                emit_pv(*prev)
            pT = P.nxt("pT")
            P.act([pT], pT[:, :, 0:nq], [s_ps], s_ps[:, :, 0:nq], AF.Exp, scale=scale)
            prev = (g, pT)
            if g == min(3, ng - 1) and pending[0] is not None:
                pending[0]()
                pending[0] = None
        emit_pv(*prev)

        def fin():
            rl = P.nxt("rl")
            P.op("dve", lambda e: e.reciprocal(rl[:, 0:nq], o_ps[0:1, 0:nq]), reads=[o_ps], writes=[rl])
            b_ps = P.nxt("bps")
            P.mm(b_ps, b_ps[0:65, 0:nq], ones[:, 0:65], rl[:, 0:nq], True, True, [ones, rl])
            bsb = P.nxt("bsb")
            P.cp("act", [bsb], bsb[:, 0:nq], [b_ps], b_ps[0:65, 0:nq])
            ao = P.nxt("ao")
            P.tt("dve", [ao], ao[:, 0:nq], [o_ps, bsb], o_ps[0:65, 0:nq], bsb[:, 0:nq], ALU.mult)
            P.dma("pool", [attT], attT[h * 64:(h + 1) * 64, q0:q0 + nq], [ao], ao[1:65, 0:nq])
        pending[0] = fin

    for b_ in P.rings["vp"][0]:
        P.memset("dve", [b_], b_[:, :, 0:1], 1.0)
    for h in range(8):
        kt_b = P.nxt("kt")
        P.dma("sp", [kt_b], kt_b[0:64, 0:NCX], [KTn], KTn[h * 64:(h + 1) * 64, NL:NT])
        P.dma("sp", [kt_b], kt_b[64:96, 0:NCX], [KTr], KTr[:, NL:NT])
        for r in range(4):
            P.dma("sp", [kt_b], kt_b[0:64, NCX + r * NL:NCX + (r + 1) * NL], [T["KTn_all%d" % h]], T["KTn_all%d" % h][r * 64:(r + 1) * 64, 0:NL])
            P.dma("sp", [kt_b], kt_b[64:96, NCX + r * NL:NCX + (r + 1) * NL], [KTr_all], KTr_all[r * 32:(r + 1) * 32, 0:NL])
        vp_b = P.nxt("vp")
        P.dma("sp", [vp_b], vp_b[:, 0:2, 1:65], [Vt], Vt[NL:NT, h * 64:(h + 1) * 64].rearrange("(t p) d -> p t d", p=128))
        for r in range(4):
            for k in range(4):
                P.dma("sp", [vp_b], vp_b[:, 2 + 32 * r + 8 * k:2 + 32 * r + 8 * k + 8, 1:65], [Vt_all],
                      Vt_all[k, r * 1024:(r + 1) * 1024, h * 64:(h + 1) * 64].rearrange("(t p) d -> p t d", p=128))
        qt_b = P.nxt("qt")
        P.dma("sp", [qt_b], qt_b[:], [QT], QT[h])
        for qb in range(NL // 512):
            qblock(h, kt_b, vp_b, qt_b, qb * 512, 512, NKT0)
        qblock(h, kt_b, vp_b, qt_b, NL, NCX, NCX // 128)
    pending[0]()
    P.phase_end()


def phase_post(P, T, layer, ntok, cats):
    P.phase_begin("post%d" % layer)
    xrows, ccols, modw, modb, n2g, w_out, rw = T["xres%d" % layer], T["ccols"], T["modw%d" % layer], T["modb%d" % layer], T["n2g%d" % layer], T["w_out%d" % layer], T["rw%d" % layer]
    xs1, h2o, affo, affl = T["xacc%d" % layer], T["h2_%d" % layer], T["aff_%d" % layer], T["affl_%d" % layer]

    P.make_ident()
    P.ring("ps", [128, 512], F32, 2, psum=True)
    P.ring("yps", [128, 2, 512], F32, 2, psum=True)
    P.ring("tp32", [128, 8, 128], F32, 1, psum=True)
    P.ring("rstd_tmp", [128, 512], F32, 1)
    mods = compute_mod(P, ccols.t, modw.t, modb.t, 2048, 3072, ["g1", "sh2", "sc2"])
    g2n = P.sb("g2n", [128, D], F32)
    P.dma("sp", [g2n], g2n[:], [], n2g.t)
    for w in range(2):
        A = mods["sc2"][w]
        P.stt("dve", [A], A[:], [A, g2n], A[:], 1.0, g2n[:], ALU.add, ALU.mult)
    w_out_b = load_bf16(P, "w_out_b", w_out.t.rearrange("(k p) n -> p k n", p=128), [128, 8, D])
    rws = P.sb("rws", [128, 8, NE], F32)
    P.dma("sp", [rws], rws[:], [], rw.t.rearrange("(k p) n -> p k n", p=128))

    P.ring("cat", [128, 8, 128], BF16, 2)
    P.ring("xt", [128, D], F32, 2)
    P.ring("xsr", [128, D], F32, 2)
    P.ring("junk", [128, D], BF16, 1)
    P.ring("ssq", [128, 1], F32, 2)
    P.ring("rs", [128, 1], F32, 2)
    P.ring("h32", [128, D], F32, 3)
    P.ring("hb", [128, D], BF16, 2)
    P.ring("hT32", [128, 8, 128], F32, 1)
    P.ring("sm", [128, 4], F32, 3)
    P.ring("ex", [128, NE], F32, 2)
    P.ring("af", [128, NE], F32, 2)

    def p1(t, out):
        w = 0 if t < NL // 128 else 1
        r0 = t * 128
        cat = P.nxt("cat")
        c0_ = 0
        for (cb, nch) in cats:
            P.dma("sp", [cat], cat[:, c0_:c0_ + nch, :], [cb], cb[:, r0:r0 + 128].rearrange("(c p) t -> p c t", p=128))
            c0_ += nch
        xt = P.nxt("xt")
        P.dma("sp", [xt], xt[:], [xrows], xrows[r0:r0 + 128, :])
        y = P.nxt("yps")
        for hh in range(2):
            for k in range(8):
                P.mm(y, y[:, hh, :], cat[:, k, :], w_out_b[:, k, hh * 512:(hh + 1) * 512], k == 0, k == 7, [cat, w_out_b])
            yield
        xs = P.nxt("xsr")
        g1r = mods["g1"][w]
        P.tt("dve", [xs], xs[:], [y, g1r], y[:].rearrange("p a b -> p (a b)"), g1r[:], ALU.mult)
        yield
        P.tt("dve", [xs], xs[:], [xs, xt], xs[:], xt[:], ALU.add)
        P.dma("pool", [xs1], xs1[r0:r0 + 128, :], [xs], xs[:])
        yield
        junk = P.nxt("junk")
        ssq = P.nxt("ssq")
        P.act([junk, ssq], junk[:], [xs], xs[:], AF.Square, accum=ssq[:, 0:1])
        yield
        rs = P.nxt("rs")
        P.rstd(rs, rs[:, 0:1], ssq, ssq[:, 0:1], 1, 1.0 / D)
        yield
        h32 = P.nxt("h32")
        P.stt("dve", [h32], h32[:], [xs, rs, mods["sc2"][w]], xs[:], rs[:, 0:1], mods["sc2"][w][:], ALU.mult, ALU.mult)
        yield
        P.tt("dve", [h32], h32[:], [h32, mods["sh2"][w]], h32[:], mods["sh2"][w][:], ALU.add)
        yield
        hb = P.nxt("hb")
        P.cp("act", [hb], hb[:], [h32], h32[:])
        P.dma("pool", [h2o], h2o[r0:r0 + 128, :], [hb], hb[:])
        out["h32"] = h32
        yield

    def p2(t, h32):
        r0 = t * 128
        tp = P.nxt("tp32")
        for k in range(8):
            P.tr(tp, tp[:, k, :], h32[:, k * 128:(k + 1) * 128], P.idf[:], [h32, P.idf])
        yield
        hT = P.nxt("hT32")
        P.cp("act", [hT], hT[:], [tp], tp[:])
        yield
        lg = P.nxt("ps")
        for k in range(8):
            P.mm(lg, lg[:, 0:NE], hT[:, k, :], rws[:, k, :], k == 0, k == 7, [hT, rws])
        yield
        sm = P.nxt("sm")
        P.red("dve", [sm], sm[:, 0:1], [lg], lg[:, 0:NE], ALU.max)
        P.ts("dve", [sm], sm[:, 1:2], [sm], sm[:, 0:1], -1.0, None, ALU.mult)
        yield
        ex = P.nxt("ex")
        P.act([ex, sm], ex[:], [lg, sm], lg[:, 0:NE], AF.Exp, bias=sm[:, 1:2], accum=sm[:, 2:3])
        yield
        P.op("dve", lambda e, sm=sm: e.reciprocal(sm[:, 3:4], sm[:, 2:3]), reads=[sm], writes=[sm])
        af = P.nxt("af")
        P.ts("dve", [af], af[:], [ex, sm], ex[:], sm[:, 3:4], None, ALU.mult)
        P.dma("pool", [affo], affo[r0:r0 + 128, :], [af], af[:])
        if t < NL // 128 and affl is not affo:
            P.dma("pool", [affl], affl[r0:r0 + 128, :], [af], af[:])
        yield

    ntile = ntok // 128
    o0 = {}
    for _ in p1(0, o0):
        pass
    hprev = o0["h32"]
    for t in range(ntile):
        on = {}
        ga = p1(t + 1, on) if t + 1 < ntile else iter(())
        gb = p2(t, hprev)
        da = db = False
        while not (da and db):
            if not da:
                da = next(ga, "end") == "end"
            if not db:
                db = next(gb, "end") == "end"
        hprev = on.get("h32")
    P.phase_end()


def _ccols(inp, b):
    cc = np.zeros((128, 16), np.float32)
    cc[:, 0::2] = _cols(inp["c"][b], 8)
    cc[:, 1::2] = _cols(inp["c_ctx"], 8)
    return cc


SMAX = 640
CAP_L = 2048
CAP_C = 32
NBIS = 30


def bisect_gen(P, affs, J, e0, e1, kcap, ones_f, name, psbuf, thr):
    ne = e1 - e0
    lo = P.sb(name + "_lo", [128, ne], F32)
    mid = P.sb(name + "_mid", [128, ne], F32)
    cnt = P.sb(name + "_cnt", [128, ne], F32)
    stp = P.sb(name + "_stp", [128, ne], F32)
    cmp = P.sb(name + "_cmp", [128, J, ne], BF16)
    P.memset("dve", [lo], lo[:], 0.0)
    for it in range(NBIS):
        c = 2.0 ** -(it + 1)
        P.ts("dve", [mid], mid[:], [lo], lo[:], c, None, ALU.add)
        P.tt("dve", [cmp], cmp[:], [affs, mid], affs[:, :, e0:e1], mid[:].unsqueeze(1).to_broadcast([128, J, ne]), ALU.is_ge)
        P.red("dve", [cnt], cnt[:], [cmp], cmp[:].rearrange("p j e -> p e j"), ALU.add)
        P.mm(psbuf, psbuf[:, 0:ne], ones_f[:], cnt[:], True, True, [ones_f, cnt])
        P.ts("dve", [stp], stp[:], [psbuf], psbuf[:, 0:ne], float(kcap) - 0.5, c, ALU.is_ge, ALU.mult)
        P.tt("dve", [lo], lo[:], [lo, stp], lo[:], stp[:], ALU.add)
        yield
    P.cp("dve", [thr], thr[:, e0:e1], [lo], lo[:])
    yield


def phase_moe(P, T, layer):
    has_ctx = (layer == 0)
    ntok = NT if has_ctx else NL
    T_ = ntok // 128
    TL = NL // 128
    NS = SMAX // 128
    NSLOT = SMAX + (CAP_C if has_ctx else 0)
    P.phase_begin("moe%d" % layer)
    h2, aff, aff_all, ccols, modw, modb = T["h2_%d" % layer], T["aff_%d" % layer], T["affall_%d" % layer], T["ccols"], T["modw%d" % layer], T["modb%d" % layer]
    wg, wu, wd = T["wg%d" % layer], T["wu%d" % layer], T["wd%d" % layer]
    xacc = T["xacc%d" % layer]
    if not has_ctx:
        fing, outp = T["fing"], T["out"]
    TRASH0 = ntok
    T = T_

    P.make_ident()
    P.ring("ps", [128, 512], F32, 1, psum=True)
    P.ring("tp", [128, 8, 128], BF16, 1, psum=True)
    P.ring("accb", [128, 512], F32, 2, psum=True)
    P.ring("hb", [128, 512], F32, 4, psum=True)
    P.ring("rstd_tmp", [128, 512], F32, 1)

    P.ring("xt", [128, D], F32, 1)
    zt = P.nxt("xt")
    P.memset("dve", [zt], zt[:], 0.0)
    zt_op = P.op("sp", lambda e: e.dma_start(out=xacc[ntok:ntok + 128, :], in_=zt[:]), reads=[zt], writes=[xacc], dma=True)

    mods = compute_mod(P, ccols.t, modw.t, modb.t, 5120, 1024, ["g2"])
    g2rep = mods["g2"]

    ones_f = P.sb("ones_f", [128, 128], F32)
    P.memset("dve", [ones_f], ones_f[:], 1.0)
    ones_b = P.sb("ones_b", [128, 128], BF16)
    P.memset("dve", [ones_b], ones_b[:], 1.0)
    ltf = P.sb("ltf", [128, 128], F32)
    P.memset("pool", [ltf], ltf[:], 1.0)
    P.op("pool", lambda e: e.affine_select(ltf[:], ltf[:], [[1, 128]], ALU.is_gt, 0.0, base=0, channel_multiplier=-1),
         reads=[ltf], writes=[ltf])
    ltb = P.sb("ltb", [128, 128], BF16)
    P.cp("dve", [ltb], ltb[:], [ltf], ltf[:])
    io_i = P.sb("io_i", [128, SMAX], I32)
    P.op("pool", lambda e: e.iota(io_i[:], [[1, SMAX]], base=0, channel_multiplier=0), writes=[io_i])
    io_f = P.sb("io_f", [128, SMAX], F32)
    P.cp("dve", [io_f], io_f[:], [io_i], io_i[:])
    ip_i = P.sb("ip_i", [128, 1], I32)
    P.op("pool", lambda e: e.iota(ip_i[:], [[0, 1]], base=0, channel_multiplier=1), writes=[ip_i])
    ip_f = P.sb("ip_f", [128, 2], F32)
    P.cp("dve", [ip_f], ip_f[:, 0:1], [ip_i], ip_i[:])
    P.ts("dve", [ip_f], ip_f[:, 1:2], [ip_f], ip_f[:, 0:1], float(TRASH0), None, ALU.add)

    P.ring("wgb", [128, 8, D], BF16, 2)
    P.ring("wub", [128, 8, D], BF16, 2)
    P.ring("wdb", [128, 8, D], BF16, 2)

    def load_one(nm, src, e):
        b = P.nxt(nm)
        for k in range(8):
            P.dma("pool", [b], b[:, k, :], [], src[e, k * 128:(k + 1) * 128, :])
        return b

    w_next = (load_one("wgb", wg, 0), load_one("wub", wu, 0), load_one("wdb", wd, 0))

    af = P.sb("af", [128, T, NE], F32)
    P.dma("sp", [af], af[:], [aff], aff.t.rearrange("(t p) e -> p t e", p=128))
    thr_l = P.sb("thr_l", [128, NE], F32)
    thr_c = P.sb("thr_c", [128, NE], F32)
    main_stack = P.pstack
    sub = ExitStack()
    P.pstack = sub
    affs = P.sb("affs", [128, 128, NE], F32)
    P.dma("sp", [affs], affs[:], [aff_all], aff_all.t.rearrange("(p j) e -> p j e", p=128))
    pbanks = P.rings["accb"][0] + P.rings["hb"][0]
    gens = [bisect_gen(P, affs, 128, 0, 8, CAP_L, ones_f, "bla", pbanks[0], thr_l),
            bisect_gen(P, affs, 128, 8, 16, CAP_L, ones_f, "blb", pbanks[1], thr_l)]
    if has_ctx:
        afc = P.sb("afc", [128, 2, NE], F32)
        P.cp("dve", [afc], afc[:], [af], af[:, TL:T, :])
        gens.append(bisect_gen(P, afc, 2, 0, 16, CAP_C, ones_f, "bc", pbanks[2], thr_c))
    for _ in range(NBIS + 1):
        for g_ in gens:
            next(g_)
    P.pstack = main_stack
    P.ops.append(None)
    sub.close()

    mask = P.sb("mask", [128, T, NE], F32)
    P.tt("dve", [mask], mask[:, 0:TL, :], [af, thr_l], af[:, 0:TL, :], thr_l[:].unsqueeze(1).to_broadcast([128, TL, NE]), ALU.is_ge)
    if has_ctx:
        P.tt("dve", [mask], mask[:, TL:T, :], [af, thr_c], af[:, TL:T, :], thr_c[:].unsqueeze(1).to_broadcast([128, 2, NE]), ALU.is_ge)
    maskb = P.sb("maskb", [128, T, NE], BF16)
    P.cp("dve", [maskb], maskb[:], [mask], mask[:])
    pos = P.sb("pos", [128, T, NE], F32)
    tot = P.sb("tot", [128, T, NE], F32)
    mflat = maskb[:].rearrange("p t e -> p (t e)")
    pflat = pos[:].rearrange("p t e -> p (t e)")
    tflat = tot[:].rearrange("p t e -> p (t e)")
    n_all = T * NE
    c = 0
    while c < n_all:
        w_ = min(512, n_all - c)
        ps = P.nxt("ps")
        P.mm(ps, ps[:, 0:w_], ltb[:], mflat[:, c:c + w_], True, True, [ltb, maskb])
        P.cp("dve", [pos], pflat[:, c:c + w_], [ps], ps[:, 0:w_])
        ps = P.nxt("ps")
        P.mm(ps, ps[:, 0:w_], ones_b[:], mflat[:, c:c + w_], True, True, [ones_b, maskb])
        P.cp("dve", [tot], tflat[:, c:c + w_], [ps], ps[:, 0:w_])
        c += w_
    base = P.sb("base", [128, T, NE], F32)
    P.memset("dve", [base], base[:], 0.0)
    for t in range(1, TL):
        P.tt("dve", [base], base[:, t, :], [base, tot], base[:, t - 1, :], tot[:, t - 1, :], ALU.add)
    if has_ctx:
        P.cp("dve", [base], base[:, TL + 1, :], [tot], tot[:, TL, :])
    P.tt("dve", [pos], pos[:], [pos, base], pos[:], base[:], ALU.add)

    vals = P.sb("vals", [128, T, NE, 6], BF16)
    for t in range(T):
        P.memset("dve", [vals], vals[:, t, :, 0], float(t))
    P.ts("dve", [vals], vals[:, :, :, 1], [af, ip_f], af[:], 0.0, ip_f[:, 0:1], ALU.mult, ALU.add)
    P.memset("dve", [vals], vals[:, :, :, 5], 1.0)
    r1 = tot
    P.cp("dve", [vals], vals[:, :, :, 2], [af], af[:])
    P.tt("dve", [r1], r1[:], [af, vals], af[:], vals[:, :, :, 2], ALU.subtract)
    P.cp("dve", [vals], vals[:, :, :, 3], [r1], r1[:])
    P.tt("dve", [r1], r1[:], [r1, vals], r1[:], vals[:, :, :, 3], ALU.subtract)
    P.cp("dve", [vals], vals[:, :, :, 4], [r1], r1[:])

    P.ring("oh", [128, SMAX], BF16, 3)
    P.ring("siT", [6, NSLOT], F32, 1)
    P.ring("si", [128, 8, 6], F32, 2)
    P.ring("sx", [128, 8, 4], F32, 2)
    P.ring("ixg", [128, 8], I32, 2)
    P.ring("ixs", [128, 8], I32, 2)
    P.ring("xg", [128, D], BF16, 6 if has_ctx else 5)
    P.ring("xgT", [128, 8, NSLOT], BF16, 1)
    P.ring("sil", [128, 512], F32, 2)
    P.ring("hidT", [128, 8, NSLOT], BF16, 1)
    P.ring("ye", [128, D], F32, 2)

    NST = NS + (1 if has_ctx else 0)
    n2 = NSLOT - 512

    def stage1a(e, st):
        acc0 = P.nxt("accb")
        acc1 = P.nxt("accb")
        ohq = st["ohq"] = []

        def flush():
            while ohq:
                t, oh = ohq.pop(0)
                P.mm(acc0, acc0[0:6, :], vals[:, t, e, :], oh[:, 0:512], t == 0, t == TL - 1, [vals, oh])
                P.mm(acc1, acc1[0:6, 0:SMAX - 512], vals[:, t, e, :], oh[:, 512:SMAX], t == 0, t == TL - 1, [vals, oh])
        st["flush"] = flush
        for t in range(TL):
            oh = P.nxt("oh")
            P.ts("dve", [oh], oh[:], [io_f, pos, mask], io_f[:], pos[:, t, e:e + 1], mask[:, t, e:e + 1], ALU.is_equal, ALU.mult)
            ohq.append((t, oh))
            yield "oh"
        flush()
        siT = P.nxt("siT")
        P.cp("act", [siT], siT[:, 0:512], [acc0], acc0[0:6, :])
        P.cp("act", [siT], siT[:, 512:SMAX], [acc1], acc1[0:6, 0:SMAX - 512])
        if has_ctx:
            accc = P.nxt("ps")
            for t in range(TL, T):
                oh = P.nxt("oh")
                P.ts("dve", [oh], oh[:, 0:CAP_C], [io_f, pos, mask], io_f[:, 0:CAP_C], pos[:, t, e:e + 1], mask[:, t, e:e + 1],
                     ALU.is_equal, ALU.mult)
                P.mm(accc, accc[0:6, 0:CAP_C], vals[:, t, e, :], oh[:, 0:CAP_C], t == TL, t == T - 1, [vals, oh])
            P.cp("act", [siT], siT[:, SMAX:SMAX + CAP_C], [accc], accc[0:6, 0:CAP_C])
        sps = P.nxt("ps")
        for s_ in range(NST):
            ns = 128 if s_ < NS else CAP_C
            P.mm(sps, sps[0:ns, s_ * 6:(s_ + 1) * 6], siT[:, s_ * 128:s_ * 128 + ns], P.idf[0:6, 0:6], True, True, [siT, P.idf])
        si = P.nxt("si")
        P.memset("dve", [si], si[:], 0.0)
        P.cp("dve", [si], si[:, 0:NS, :], [sps], sps[:, 0:NS * 6].rearrange("p (s k) -> p s k", k=6))
        if has_ctx:
            P.cp("dve", [si], si[0:CAP_C, NS, :], [sps], sps[0:CAP_C, NS * 6:NS * 6 + 6])
        sx = P.nxt("sx")
        P.stt("dve", [sx], sx[:, :, 0], [si], si[:, :, 0], 128.0, si[:, :, 1], ALU.mult, ALU.add)
        P.tt("dve", [sx], sx[:, :, 1], [si], si[:, :, 2], si[:, :, 3], ALU.add)
        P.tt("dve", [sx], sx[:, :, 1], [sx, si], sx[:, :, 1], si[:, :, 4], ALU.add)
        P.ts("dve", [sx], sx[:, :, 2], [si], si[:, :, 5], -1.0, 1.0, ALU.mult, ALU.add)
        P.stt("dve", [sx], sx[:, :, 3], [sx, ip_f], sx[:, :, 2], ip_f[:, 1:2], sx[:, :, 0], ALU.mult, ALU.add)
        ixg = P.nxt("ixg")
        P.cp("dve", [ixg], ixg[:], [sx], sx[:, :, 0])
        ixs = P.nxt("ixs")
        P.cp("dve", [ixs], ixs[:], [sx], sx[:, :, 3])
        st["sx"], st["ixs"], st["ixg"], st["xgs"] = sx, ixs, ixg, [None] * NST
        yield

    def issue_gather(st, s_):
        xg = P.nxt("xg")
        ixg = st["ixg"]
        P.op("pool", lambda en: en.indirect_dma_start(
            out=xg[:], out_offset=None, in_=h2.t, in_offset=bass.IndirectOffsetOnAxis(ap=ixg[:, s_:s_ + 1], axis=0)),
            reads=[ixg, h2], writes=[xg], dma=True)
        st["xgs"][s_] = xg

    def stage1b(st):
        xgT = P.nxt("xgT")
        for s_ in range(NST):
            ns = 128 if s_ < NS else CAP_C
            xg = st["xgs"][s_]
            tp = P.nxt("tp")
            for k in range(8):
                P.tr(tp, tp[:, k, :], xg[:, k * 128:(k + 1) * 128], P.idb[:], [xg, P.idb])
            P.cp("act", [xgT], xgT[:, :, s_ * 128:s_ * 128 + ns], [tp], tp[:, :, 0:ns])
        st["xgT"] = xgT

    def stage2(e, st, wgb, wub, wdb, gen, st_next, prev_sc):
        xgT, sx, ixs = st["xgT"], st["sx"], st["ixs"]
        hidT = P.nxt("hidT")
        for fc in range(8):
            for (c0, cn) in ((0, 512), (512, n2)):
                if gen is not None and not st_next.get("ohdone"):
                    for _ in range(3):
                        if next(gen, "end") != "oh":
                            st_next["ohdone"] = True
                            break
                gp = P.nxt("hb")
                up = P.nxt("hb")
                for (dst, wb) in ((gp, wgb), (up, wub)):
                    for k in range(8):
                        P.mm(dst, dst[:, 0:cn], wb[:, k, fc * 128:(fc + 1) * 128], xgT[:, k, c0:c0 + cn], k == 0, k == 7, [wb, xgT])
                if gen is not None and "flush" in st_next:
                    st_next["flush"]()
                sil = P.nxt("sil")
                P.act([sil], sil[:, 0:cn], [gp], gp[:, 0:cn], AF.Silu)
                P.tt("dve", [hidT], hidT[:, fc, c0:c0 + cn], [sil, up], sil[:, 0:cn], up[:, 0:cn], ALU.mult)
        if gen is not None:
            for _ in gen:
                pass
        my_sc = []
        for s_ in range(NST):
            ns = 128 if s_ < NS else CAP_C
            ye = P.nxt("ye")
            g2r = g2rep[0] if s_ < NS else g2rep[1]
            for hh in range(2):
                yp = P.nxt("hb")
                for fc in range(8):
                    P.mm(yp, yp[0:ns, :], hidT[:, fc, s_ * 128:s_ * 128 + ns], wdb[:, fc, hh * 512:(hh + 1) * 512], fc == 0, fc == 7,
                         [hidT, wdb])
                P.stt("dve", [ye], ye[0:ns, hh * 512:(hh + 1) * 512], [yp, sx, g2r], yp[0:ns, :], sx[0:ns, s_, 1:2],
                      g2r[0:ns, hh * 512:(hh + 1) * 512], ALU.mult, ALU.mult)
            o_ = P.op("pool", lambda en, ye=ye, ixs=ixs, s_=s_: en.indirect_dma_start(
                out=xacc.t, out_offset=bass.IndirectOffsetOnAxis(ap=ixs[:, s_:s_ + 1], axis=0), in_=ye[:, :], in_offset=None,
                compute_op=ALU.add), reads=[ixs, ye], writes=[], dma=True, extra=prev_sc)
            my_sc.append(o_)
            if st_next is not None:
                issue_gather(st_next, s_)
        return my_sc

    st = {}
    for _ in stage1a(0, st):
        st["flush"]()
    for s_ in range(NST):
        issue_gather(st, s_)
    stage1b(st)
    prev_sc = [zt_op]
    for e in range(NE):
        wgb, wub, wdb = w_next
        st_next = None
        gen = None
        if e + 1 < NE:
            wg_n, wu_n = load_one("wgb", wg, e + 1), load_one("wub", wu, e + 1)
            st_next = {}
            gen = stage1a(e + 1, st_next)
        prev_sc = stage2(e, st, wgb, wub, wdb, gen, st_next, prev_sc)
        if e + 1 < NE:
            w_next = (wg_n, wu_n, load_one("wdb", wd, e + 1))
            stage1b(st_next)
        st = st_next

    if not has_ctx:
        fg = P.sb("fg", [128, D], F32)
        P.dma("sp", [fg], fg[:], [], fing.t)
        P.ring("junk", [128, D], BF16, 1)
        P.ring("ssq", [128, 1], F32, 2)
        P.ring("rs", [128, 1], F32, 2)
        for t in range(T):
            xt = P.nxt("xt")
            P.op("sp", lambda e, xt=xt, t=t: e.dma_start(out=xt[:], in_=xacc[t * 128:(t + 1) * 128, :]), reads=[xacc], writes=[xt],
                 dma=True, extra=prev_sc)
            junk = P.nxt("junk")
            ssq = P.nxt("ssq")
            P.act([junk, ssq], junk[:], [xt], xt[:], AF.Square, accum=ssq[:, 0:1])
            rs = P.nxt("rs")
            P.rstd(rs, rs[:, 0:1], ssq, ssq[:, 0:1], 1, 1.0 / D)
            P.stt("dve", [xt], xt[:], [xt, rs, fg], xt[:], rs[:, 0:1], fg[:], ALU.mult, ALU.mult)
            P.dma("sp", [outp], outp[t * 128:(t + 1) * 128, :], [xt], xt[:])
    P.phase_end()


def phase_qkv1(P, T):
    P.phase_begin("qkv1")
    xrows, ccols, modw, modb, n1g, wqkv, wqks, rope = T["xacc0"], T["ccols"], T["modw1"], T["modb1"], T["n1g1"], T["wqkv"], T["wqks"], T["rope1"]
    Q1T, K1T, V1, Kb, Vb = T["Q1T"], T["K1T"], T["V1"], T["Kb"], T["Vb"]

    P.make_ident()
    P.ring("ps", [128, 512], F32, 6, psum=True)
    P.ring("tp", [128, 8, 128], BF16, 2, psum=True)
    P.ring("xt", [128, D], F32, 2)
    P.ring("junk", [128, D], BF16, 1)
    P.ring("ssq", [128, 1], F32, 2)
    P.ring("rs", [128, 1], F32, 2)
    P.ring("rstd_tmp", [128, 512], F32, 1)
    P.ring("h32", [128, D], F32, 1)
    P.ring("hb", [128, D], BF16, 2)
    mods = compute_mod(P, ccols.t, modw.t, modb.t, 0, 2048, ["sh1", "sc1"])
    g1 = P.sb("g1", [128, D], F32)
    P.dma("sp", [g1], g1[:], [], n1g.t)
    AB = []
    for w in range(2):
        A = mods["sc1"][w]
        P.stt("dve", [A], A[:], [A, g1], A[:], 1.0, g1[:], ALU.add, ALU.mult)
        AB.append((A, mods["sh1"][w]))
    wq_b = load_bf16(P, "wq_b", wqkv.t.rearrange("(k p) n -> p k n", p=128), [128, 8, 1536])
    ws_b = load_bf16(P, "ws_b", wqks.t.rearrange("(k p) n -> p k n", p=128), [128, 8, 1280])
    P.ring("hT", [128, 8, 512], BF16, 2)
    P.ring("rp", [128, 2, 512], F32, 2)
    P.ring("t1", [128, 512], F32, 2)
    P.ring("t2", [128, 512], F32, 2)
    P.ring("qo", [128, 512], BF16, 3)
    P.ring("vo", [128, 256], BF16, 3)

    for blk in range(NT // 512 + 1):
        tok0 = blk * 512
        n = min(512, NT - tok0)
        if n <= 0:
            break
        is_ctx = tok0 >= NL
        hT = P.nxt("hT")
        for t in range(n // 128):
            tp = tile_T(P, xrows, tok0 + t * 128, AB[1 if is_ctx else 0][0], AB[1 if is_ctx else 0][1])
            P.cp("act", [hT], hT[:, :, t * 128:(t + 1) * 128], [tp], tp[:])
        if not is_ctx:
            rp = P.nxt("rp")
            P.dma("pool", [rp], rp[:], [], rope[:, :, tok0:tok0 + 512])
        for c in (range(10) if not is_ctx else (8, 9)):
            ps = P.nxt("ps")
            for k in range(8):
                P.mm(ps, ps[:, 0:n], wq_b[:, k, c * 128:(c + 1) * 128], hT[:, k, 0:n], k == 0, k == 7, [wq_b, hT])
            qo = P.nxt("qo")
            if is_ctx:
                P.cp("act", [qo], qo[:, 0:n], [ps], ps[:, 0:n])
            else:
                pss = P.nxt("ps")
                for k in range(8):
                    P.mm(pss, pss[:, 0:n], ws_b[:, k, c * 128:(c + 1) * 128], hT[:, k, 0:n], k == 0, k == 7, [ws_b, hT])
                t1 = P.nxt("t1")
                P.tt("dve", [t1], t1[:, 0:n], [ps, rp], ps[:, 0:n], rp[:, 0, 0:n], ALU.mult)
                t2 = P.nxt("t2")
                P.tt("dve", [t2], t2[:, 0:n], [pss, rp], pss[:, 0:n], rp[:, 1, 0:n], ALU.mult)
                P.tt("dve", [qo], qo[:, 0:n], [t1, t2], t1[:, 0:n], t2[:, 0:n], ALU.add)
            if c < 8:
                P.dma("sp", [Q1T], Q1T[c * 128:(c + 1) * 128, tok0:tok0 + n], [qo], qo[:, 0:n])
            else:
                P.dma("sp", [K1T], K1T[(c - 8) * 128:(c - 7) * 128, tok0:tok0 + n], [qo], qo[:, 0:n])
                if tok0 == 0:
                    P.dma("sp", [Kb], Kb[(c - 8) * 128:(c - 7) * 128, 0:128], [qo], qo[:, 0:128])
                if tok0 == NL - 512:
                    P.dma("sp", [Kb], Kb[(c - 8) * 128:(c - 7) * 128, 128:256], [qo], qo[:, 384:512])
        for t in range(n // 128):
            psv = P.nxt("ps")
            for k in range(8):
                P.mm(psv, psv[:, 0:256], hT[:, k, t * 128:(t + 1) * 128], wq_b[:, k, 1280:1536], k == 0, k == 7, [hT, wq_b])
            vo = P.nxt("vo")
            P.cp("act", [vo], vo[:], [psv], psv[:, 0:256])
            P.dma("sp", [V1], V1[tok0 + t * 128:tok0 + (t + 1) * 128, :], [vo], vo[:])
            if tok0 + t * 128 == 0:
                P.dma("sp", [Vb], Vb[0:128, :], [vo], vo[:])
            if tok0 + t * 128 == NL - 128:
                P.dma("sp", [Vb], Vb[128:256, :], [vo], vo[:])
    P.phase_end()


NB1 = NL // 128
NKL = NL + 256


def phase_attn1(P, T):
    P.phase_begin("attn1")
    Q1T, K1T, V1, Kb_all, Vb_all, masks, sinkr, selh, catT1 = (T[k] for k in ("Q1T", "K1T", "V1", "Kb_all", "Vb_all", "masks", "sinkr", "selh", "catT1"))
    scale = 64.0 ** -0.5

    P.ring("s1b", [128, 512], F32, 5, psum=True)
    P.ring("ops", [128, 512], F32, 2, psum=True)
    P.ring("bps", [128, 512], F32, 1, psum=True)
    P.ring("p1", [128, 512], BF16, 10)
    P.ring("rl", [1, 512], F32, 3)
    P.ring("lsum", [1, 512], F32, 4)
    P.ring("rhl", [1, 2, 512], BF16, 3)
    onesb16 = P.sb("onesb16", [1, 128], BF16)
    P.memset("dve", [onesb16], onesb16[:], 1.0)

    P.ring("bsb", [65, 512], F32, 2)
    P.ring("ao", [65, 512], BF16, 3)
    P.ring("qg", [64, NB1 * 512], BF16, 2)
    P.ring("kl", [64, NKL], BF16, 2)
    P.ring("kc", [64, NCX], BF16, 2)
    P.ring("vl", [128, NKL // 128, 65], BF16, 2)
    P.ring("vc", [128, 2, 65], BF16, 2)
    ones = P.sb("ones", [1, 128], F32)
    P.memset("dve", [ones], ones[:], 1.0)
    mk = P.sb("mk", [128, 4, 512], BF16)
    P.dma("sp", [mk], mk[:], [], masks.t)
    sk = P.sb("sk", [1, 2048], F32)
    P.dma("sp", [sk], sk[:], [], sinkr.t)
    P.act([sk], sk[:], [sk], sk[:], AF.Exp)
    skb = P.sb("skb", [1, 2, 2048], BF16)
    skr = P.sb("skr", [1, 2048], F32)
    P.cp("dve", [skb], skb[:, 0, :], [sk], sk[:])
    P.tt("dve", [skr], skr[:], [sk, skb], sk[:], skb[:, 0, :], ALU.subtract)
    P.cp("dve", [skb], skb[:, 1, :], [skr], skr[:])
    e0 = P.sb("e0", [1, 65], BF16)
    P.memset("dve", [e0], e0[:], 0.0)
    P.memset("dve", [e0], e0[:, 0:1], 1.0)

    sel = P.sb("sel", [128, 8], F32)
    P.dma("sp", [sel], sel[:], [], selh.t)
    P.ring("kcand", [64, 4, 256], BF16, 2)
    P.ring("vcand", [128, 4, 2, 64], BF16, 2)
    for b_ in P.rings["vl"][0]:
        P.memset("dve", [b_], b_[:, :, 0:1], 1.0)
    for b_ in P.rings["vc"][0]:
        P.memset("dve", [b_], b_[:, :, 0:1], 1.0)
    for g in range(4):
        qg = P.nxt("qg")
        for j in range(4):
            P.dma("sp", [qg], qg[:].rearrange("d (i j r) -> d i j r", j=4, r=128)[:, :, j, :], [Q1T],
                  Q1T[(g * 4 + j) * 64:(g * 4 + j + 1) * 64, :].rearrange("d (i r) -> d i r", r=128))
        kl = P.nxt("kl")
        P.dma("sp", [kl], kl[:, 128:128 + NL], [K1T], K1T[g * 64:(g + 1) * 64, 0:NL])
        kcand = P.nxt("kcand")
        P.dma("sp", [kcand], kcand[:], [Kb_all], Kb_all.t.rearrange("(r c) k -> c r k", r=4)[g * 64:(g + 1) * 64])
        for (dst0, src0, so) in ((0, 128, 0), (128 + NL, 0, 4)):
            P.ts("dve", [kl], kl[:, dst0:dst0 + 128], [kcand, sel], kcand[:, 0, src0:src0 + 128], sel[0:64, so:so + 1], None, ALU.mult)
            for r in range(1, 4):
                P.stt("dve", [kl], kl[:, dst0:dst0 + 128], [kcand, sel, kl], kcand[:, r, src0:src0 + 128], sel[0:64, so + r:so + r + 1],
                      kl[:, dst0:dst0 + 128], ALU.mult, ALU.add)
        kc = P.nxt("kc")
        P.dma("sp", [kc], kc[:], [K1T], K1T[g * 64:(g + 1) * 64, NL:NT])
        vl = P.nxt("vl")
        P.dma("sp", [vl], vl[:, 1:1 + NB1, 1:65], [V1], V1[0:NL, g * 64:(g + 1) * 64].rearrange("(t p) d -> p t d", p=128))
        vcand = P.nxt("vcand")
        P.dma("sp", [vcand], vcand[:], [Vb_all], Vb_all.t.rearrange("(r f p) c -> p r f c", r=4, f=2)[:, :, :, g * 64:(g + 1) * 64])
        for (dt_, f_, so) in ((0, 1, 0), (NB1 + 1, 0, 4)):
            P.ts("dve", [vl], vl[:, dt_, 1:65], [vcand, sel], vcand[:, 0, f_, :], sel[:, so:so + 1], None, ALU.mult)
            for r in range(1, 4):
                P.stt("dve", [vl], vl[:, dt_, 1:65], [vcand, sel, vl], vcand[:, r, f_, :], sel[:, so + r:so + r + 1], vl[:, dt_, 1:65],
                      ALU.mult, ALU.add)
        vc = P.nxt("vc")
        P.dma("sp", [vc], vc[:, :, 1:65], [V1], V1[NL:NT, g * 64:(g + 1) * 64].rearrange("(t p) d -> p t d", p=128))
        def emit_s(i):
            q_ap = qg[:, i * 512:(i + 1) * 512]
            tiles = []
            for (kb, c0) in ((kc, 0), (kc, 128), (kl, i * 128), (kl, (i + 1) * 128), (kl, (i + 2) * 128)):
                sp_ = P.nxt("s1b")
                P.mm(sp_, sp_[:, :], kb[:, c0:c0 + 128], q_ap, True, True, [kb, qg])
                tiles.append(sp_)
            return tiles

        def emit_exp(i, tiles):
            ps_ = []
            for j, sp_ in enumerate(tiles):
                pt = P.nxt("p1")
                P.act([pt], pt[:], [sp_], sp_[:], AF.Exp, scale=scale)
                if j == 2:
                    P.tt("dve", [pt], pt[:], [pt, mk], pt[:], mk[:, 2, :] if i == 0 else mk[:, 0, :], ALU.mult)
                if j == 4:
                    P.tt("dve", [pt], pt[:], [pt, mk], pt[:], mk[:, 3, :] if i == NB1 - 1 else mk[:, 1, :], ALU.mult)
                ps_.append(pt)
            return ps_

        def emit_pv(i, ps_, g=g, vc=vc, vl=vl):
            o_ps = P.nxt("ops")
            vs = (vc[:, 0, :], vc[:, 1, :], vl[:, i, :], vl[:, i + 1, :], vl[:, i + 2, :])
            vb = (vc, vc, vl, vl, vl)
            for j in range(5):
                P.mm(o_ps, o_ps[0:65, :], vs[j], ps_[j][:], j == 0, j == 4, [vb[j], ps_[j]])
            lsum = P.nxt("lsum")
            P.tt("dve", [lsum], lsum[:], [o_ps, sk], o_ps[0:1, :], sk[:, g * 512:(g + 1) * 512], ALU.add)
            return (i, o_ps, lsum)

        def emit_rl(i, o_ps, lsum):
            lnl = P.nxt("lsum")
            P.act([lnl], lnl[:], [lsum], lsum[:], AF.Ln)
            rl = P.nxt("rl")
            P.act([rl], rl[:], [lnl], lnl[:], AF.Exp, scale=-1.0)
            rh = P.nxt("rhl")
            P.cp("dve", [rh], rh[:, 0, :], [rl], rl[:])
            P.tt("dve", [rl], rl[:], [rl, rh], rl[:], rh[:, 0, :], ALU.subtract)
            P.cp("dve", [rh], rh[:, 1, :], [rl], rl[:])
            return (i, o_ps, rh)

        def emit_fin(i, o_ps, rl, g=g):
            b_ps = P.nxt("bps")
            P.mm(b_ps, b_ps[0:65, :], onesb16[:, 0:65], rl[:, 0, :], True, False, [onesb16, rl])
            P.mm(b_ps, b_ps[0:65, :], onesb16[:, 0:65], rl[:, 1, :], False, True, [onesb16, rl])
            bsb = P.nxt("bsb")
            P.cp("act", [bsb], bsb[:], [b_ps], b_ps[0:65, :])
            ao = P.nxt("ao")
            P.tt("dve", [ao], ao[:], [o_ps, bsb], o_ps[0:65, :], bsb[:], ALU.mult)
            for j in range(4):
                P.dma("sp", [catT1], catT1[(g * 4 + j) * 64:(g * 4 + j + 1) * 64, i * 128:(i + 1) * 128], [ao], ao[1:65, j * 128:(j + 1) * 128])

        prev = None
        pfin = None
        for i in range(NB1 + 1):
            tiles = emit_s(i) if i < NB1 else None
            o_ = emit_pv(*prev) if prev is not None else None
            pexp = emit_exp(i, tiles) if i < NB1 else None
            if o_ is not None:
                nfin = emit_rl(*o_)
                if pfin is not None:
                    emit_fin(*pfin)
                pfin = nfin
            prev = (i, pexp) if i < NB1 else None
        emit_fin(*pfin)
    P.phase_end()


GRP = [[0, 1, 2, 3], [4, 5, 6, 7]]


def build_fused():
    ctx = ExitStack()
    nc, P = new_prog(ctx)
    T = {}

    def di(n, s, dt=F32):
        T[n] = P.dram(n, s, dt, "ExternalInput")

    def dn(n, s, dt=F32):
        T[n] = P.dram(n, s, dt)
        T[n].relaxed = not n.startswith("xacc")

    di("xrows", [NT, D]); di("xhalo", [128, D]); di("hmask", [128, 2]); di("ccols", [128, 16])
    for l in (0, 1):
        di("modw%d" % l, [D, 6144]); di("modb%d" % l, [1, 6144]); di("n1g%d" % l, [128, D]); di("n2g%d" % l, [128, D])
        di("w_out%d" % l, [D, D]); di("rw%d" % l, [D, NE])
        di("wg%d" % l, [NE, D, D]); di("wu%d" % l, [NE, D, D]); di("wd%d" % l, [NE, D, D])
    di("fing", [128, D])
    di("w_in", [D, 1984]); di("convp", [128, 16]); di("qg", [128, 3]); di("w_uq", [256, 768]); di("w_uqs", [256, 768])
    di("w_ukv", [128, 1024]); di("ropeq", [96, 2, NL]); di("ropek", [32, 2, NL])
    di("wqkv", [D, 1536]); di("wqks", [D, 1280]); di("rope1", [128, 2, NL])
    di("masks", [128, 4, 512], BF16); di("sinkr", [1, 2048]); di("selh", [128, 8])
    T["out"] = P.dram("out", [NL, D], F32, "ExternalOutput")
    dn("QT", [8, 96, NT], BF16); dn("KTn", [512, NT], BF16); dn("KTr", [32, NT], BF16); dn("Vt", [NT, 512], BF16)
    dn("convT", [512, NT], BF16)
    for h_ in range(8):
        dn("KTn_all%d" % h_, [4 * 64, NT], BF16)
    dn("KTr_all", [4 * 32, NT], BF16); dn("Vt_all", [4, 4 * 1024, 512], BF16)
    dn("attT", [512, NT], BF16)
    dn("xacc0", [NT + 128, D]); dn("h2_0", [NT, D], BF16); dn("aff_0", [NT, NE]); dn("affl_0", [NL, NE]); dn("affall_0", [4 * NL, NE])
    dn("Q1T", [1024, NL], BF16); dn("K1T", [256, NT], BF16); dn("V1", [NT, 256], BF16)
    dn("Kb", [256, 256], BF16); dn("Vb", [256, 256], BF16); dn("Kb_all", [1024, 256], BF16); dn("Vb_all", [1024, 256], BF16)
    dn("catT1", [1024, NL], BF16)
    dn("xacc1", [NL + 128, D]); dn("h2_1", [NL, D], BF16); dn("aff_1", [NL, NE]); dn("affall_1", [4 * NL, NE])
    T["affl_1"] = T["aff_1"]
    T["xres0"] = T["xrows"]
    T["xres1"] = T["xacc0"]

    phase_A(P, T)
    P.cc_allgather(T["KTr"], T["KTr_all"], GRP)
    for k in range(4):
        P.cc_allgather(T["Vt"], T["Vt_all"], GRP, T["Vt"].t[k * 1024:(k + 1) * 1024, :], T["Vt_all"].t[k])
    for h in range(8):
        P.cc_allgather(T["KTn"], T["KTn_all%d" % h], GRP, T["KTn"].t[h * 64:(h + 1) * 64, :], T["KTn_all%d" % h].t)
    phase_B(P, T)
    phase_post(P, T, 0, NT, [(T["convT"], 4), (T["attT"], 4)])
    P.cc_allgather(T["affl_0"], T["affall_0"], GRP)
    phase_moe(P, T, 0)
    phase_qkv1(P, T)
    P.cc_allgather(T["Kb"], T["Kb_all"], GRP)
    P.cc_allgather(T["Vb"], T["Vb_all"], GRP)
    phase_attn1(P, T)
    phase_post(P, T, 1, NL, [(T["catT1"], 8)])
    P.cc_allgather(T["aff_1"], T["affall_1"], GRP)
    phase_moe(P, T, 1)
    n = P.finalize()
    return nc, ctx, n


def prep_all(inp):
    bf = ml_dtypes.bfloat16
    maps = prep_A(inp)
    w = inp["swa_w_qkv"][0]
    p64 = _swap_perm(64)
    ws = np.empty((D, 1280), np.float32)
    for h in range(20):
        ws[:, h * 64:(h + 1) * 64] = w[:, h * 64 + p64]
    r = np.arange(128)
    tri_prev = (r[:, None] >= r[None, :]).astype(np.float32)
    tri_next = (r[:, None] <= r[None, :]).astype(np.float32)
    sinkr = np.ascontiguousarray(np.repeat(inp["swa_sink"][0], 128)[None, :].astype(np.float32))
    shared = {}
    for l in (0, 1):
        shared["modw%d" % l] = np.ascontiguousarray(inp["mod_w"][l])
        shared["modb%d" % l] = np.ascontiguousarray(inp["mod_b"][l][None, :])
        shared["n1g%d" % l] = _rep(inp["norm1_g"][l])
        shared["n2g%d" % l] = _rep(inp["norm2_g"][l])
        shared["rw%d" % l] = np.ascontiguousarray(inp["router_w"][l])
        shared["wg%d" % l] = np.ascontiguousarray(inp["exp_w_gate"][l])
        shared["wu%d" % l] = np.ascontiguousarray(inp["exp_w_up"][l])
        shared["wd%d" % l] = np.ascontiguousarray(inp["exp_w_down"][l])
    shared["w_out0"] = np.ascontiguousarray(inp["ab_w_out"][0])
    shared["w_out1"] = np.ascontiguousarray(inp["swa_w_out"][0])
    shared["fing"] = _rep(inp["final_g"])
    shared["wqkv"] = np.ascontiguousarray(w)
    shared["wqks"] = ws
    shared["sinkr"] = sinkr
    out = []
    for core in range(8):
        b, q = core // 4, core % 4
        m = dict(maps[core])
        m["modw0"] = m.pop("modw"); m["modb0"] = m.pop("modb"); m["n1g0"] = m.pop("n1g")
        m.update(shared)
        C, S = _rope_tables(np.arange(q * NL, (q + 1) * NL), 64)
        rp = np.empty((128, 2, NL), np.float32)
        rp[:64, 0] = C; rp[64:, 0] = C; rp[:64, 1] = S; rp[64:, 1] = S
        m["rope1"] = rp
        mk = np.zeros((128, 4, 512), np.float32)
        mk[:, 0] = np.tile(tri_prev, (1, 4))
        mk[:, 1] = np.tile(tri_next, (1, 4))
        mk[:, 2] = mk[:, 0] if q > 0 else 0.0
        mk[:, 3] = mk[:, 1] if q < 3 else 0.0
        m["masks"] = mk.astype(bf)
        sel = np.zeros((128, 8), np.float32)
        if q > 0:
            sel[:, q - 1] = 1.0
        if q < 3:
            sel[:, 4 + q + 1] = 1.0
        m["selh"] = sel
        out.append(m)
    return out


def kernel(**inputs):
    inp = {k: np.asarray(v) for k, v in inputs.items()}
    maps = prep_all(inp)
    nc, ctx, n = build_fused()
    res = run_bass_kernel_spmd(nc, maps, core_ids=list(range(8)))
    ctx.close()
    out = np.empty((2, 4 * NL, D), np.float32)
    for c in range(8):
        out[c // 4, (c % 4) * NL:(c % 4 + 1) * NL] = np.asarray(res.results[c]["out"])
    return out
```

```python
import numpy as np
import ml_dtypes
from contextlib import ExitStack
import concourse.bass as bass
import concourse.mybir as mybir
from concourse.bass_utils import run_bass_kernel_spmd

F32 = mybir.dt.float32
BF16 = mybir.dt.bfloat16
I32 = mybir.dt.int32
ALU = mybir.AluOpType
AF = mybir.ActivationFunctionType
AX = mybir.AxisListType

D = 1024
NL = 4096
NCX = 256
NT = NL + NCX
EPS = 1e-6
NE = 16
ENGS = ["pe", "act", "dve", "pool", "sp"]
NDMA_SEM = 56


class Buf:
    __slots__ = ("name", "last_w", "readers", "t", "relaxed")

    def __init__(self, name, t=None):
        self.name = name
        self.last_w = None
        self.readers = []
        self.t = t
        self.relaxed = False

    def __getitem__(self, k):
        return self.t[k]


class Op:
    __slots__ = ("eng", "fn", "deps", "dma", "idx", "need_inc", "sem", "val", "waits", "cc")


class Prog:
    def __init__(self, nc, ctx):
        self.nc = nc
        self.ctx = ctx
        self.ops = []
        self.rings = {}
        self.phase = "p"
        self.pstack = None

    def phase_begin(self, name):
        self.phase = name
        self.pstack = ExitStack()
        self.rings = {}

    def phase_end(self):
        self.ops.append(None)
        self.pstack.close()
        self.pstack = None

    def sb(self, name, shape, dt):
        t = self.pstack.enter_context(self.nc.sbuf_tensor("%s_%s" % (self.phase, name), shape, dt))
        return Buf(name, t)

    def ps(self, name, shape, dt):
        t = self.pstack.enter_context(self.nc.psum_tensor("%s_%s" % (self.phase, name), shape, dt))
        return Buf(name, t)

    def cc_allgather(self, src, dst, groups, src_ap=None, dst_ap=None):
        src_ap = src.t if src_ap is None else src_ap
        dst_ap = dst.t if dst_ap is None else dst_ap
        o = self.op("pool", lambda e: e.collective_compute("AllGather", ALU.bypass, replica_groups=groups,
                                                           ins=[src_ap], outs=[dst_ap]), reads=[src], writes=[dst])
        o.cc = True
        return o

    def ring(self, name, shape, dt, n, psum=False):
        bufs = [(self.ps if psum else self.sb)("%s%d" % (name, i), shape, dt) for i in range(n)]
        self.rings[name] = [bufs, 0]

    def nxt(self, name):
        r = self.rings[name]
        b = r[0][r[1] % len(r[0])]
        r[1] += 1
        return b

    def dram(self, name, shape, dt, kind=None):
        if kind is None:
            t = self.nc.dram_tensor(name, list(shape), dt)
        else:
            t = self.nc.dram_tensor(name, list(shape), dt, kind=kind)
        return Buf(name, t.ap())

    def op(self, eng, fn, reads=(), writes=(), dma=False, extra=()):
        o = Op()
        o.eng = eng
        o.fn = fn
        o.dma = dma
        o.idx = len(self.ops)
        deps = set()
        for b in reads:
            if b.last_w is not None:
                deps.add(b.last_w)
        for b in writes:
            if b.relaxed:
                continue
            if b.last_w is not None:
                deps.add(b.last_w)
            for r in b.readers:
                deps.add(r)
        for x in extra:
            deps.add(x.idx)
        deps.discard(o.idx)
        o.deps = deps
        for b in reads:
            b.readers.append(o.idx)
        for b in writes:
            b.last_w = o.idx
            b.readers = []
        o.need_inc = False
        o.cc = False
        self.ops.append(o)
        return o

    def finalize(self):
        nc = self.nc
        ctx = self.ctx
        ops = self.ops

        def pe_pe(p, o):
            return p.eng == "pe" and o.eng == "pe" and not p.dma and not o.dma

        last = {}
        for o in ops:
            if o is None:
                for e_, lo in last.items():
                    lo.need_inc = True
                continue
            last[o.eng] = o
            for d in o.deps:
                if not pe_pe(ops[d], o):
                    ops[d].need_inc = True
            if o.dma or o.cc:
                o.need_inc = True
        eng_sem = {e: ctx.enter_context(nc.semaphore("s_" + e)) for e in ENGS}
        dma_sems = [ctx.enter_context(nc.semaphore("d%d" % i)) for i in range(NDMA_SEM)]
        cc_sem = ctx.enter_context(nc.semaphore("cc_sem"))
        cnt = {e: 0 for e in ENGS}
        dcnt = [0] * NDMA_SEM
        ccnt = 0
        dma_rr = 0
        seen = {e: {} for e in ENGS}
        pending = {e: None for e in ENGS}
        for o in ops:
            if o is None:
                snap = [(("e", e), eng_sem[e], cnt[e]) for e in ENGS if cnt[e] > 0]
                snap += [(("d", k), dma_sems[k], dcnt[k]) for k in range(NDMA_SEM) if dcnt[k] > 0]
                if ccnt > 0:
                    snap.append((("cc",), cc_sem, ccnt))
                for e in ENGS:
                    pending[e] = snap
                continue
            waits = []
            s = seen[o.eng]
            if pending[o.eng] is not None:
                for key, sem, val in pending[o.eng]:
                    if s.get(key, 0) < val:
                        s[key] = val
                        waits.append((sem, val))
                pending[o.eng] = None
            for d in sorted(o.deps):
                p = ops[d]
                if pe_pe(p, o):
                    continue
                key, sem = p.sem
                if s.get(key, 0) < p.val:
                    s[key] = p.val
                    waits.append((sem, p.val))
            if o.cc:
                ccnt += 1
                o.sem = (("cc",), cc_sem)
                o.val = ccnt
            elif o.dma:
                k = dma_rr
                dma_rr = (dma_rr + 1) % NDMA_SEM
                key = ("d", k)
                if dcnt[k] > 0 and s.get(key, 0) < dcnt[k]:
                    s[key] = dcnt[k]
                    waits.append((dma_sems[k], dcnt[k]))
                dcnt[k] += 16
                o.sem = (key, dma_sems[k])
                o.val = dcnt[k]
            elif o.need_inc:
                cnt[o.eng] += 1
                o.sem = (("e", o.eng), eng_sem[o.eng])
                o.val = cnt[o.eng]
            o.waits = waits
        final_waits = [(dma_sems[k], dcnt[k]) for k in range(NDMA_SEM) if dcnt[k] > 0]
        final_waits += [(eng_sem[e], cnt[e]) for e in ENGS if cnt[e] > 0]
        if ccnt > 0:
            final_waits.append((cc_sem, ccnt))
        per = {e: [o for o in ops if o is not None and o.eng == e] for e in ENGS}
        engmap = {"pe": "tensor", "act": "scalar", "dve": "vector", "pool": "gpsimd", "sp": "sync"}
        with nc.Block() as block:
            for e in ENGS:
                def body(eng, lst=per[e], final=(e == "sp")):
                    for o in lst:
                        for (sem, val) in o.waits:
                            eng.wait_ge(sem, val)
                        ins = o.fn(eng)
                        if o.cc:
                            ins.then_inc(o.sem[1])
                        elif o.need_inc:
                            ins.then_inc(o.sem[1], 16 if o.dma else 1)
                    if final:
                        for (sem, val) in final_waits:
                            eng.wait_ge(sem, val)
                getattr(block, engmap[e])(body)
        return len(per["pe"]) + len(per["act"]) + len(per["dve"]) + len(per["pool"]) + len(per["sp"])

    def mm(self, ps, out, lhsT, rhs, start, stop, rd):
        self.op("pe", lambda e: e.matmul(out, lhsT=lhsT, rhs=rhs, start=start, stop=stop), reads=rd, writes=[ps])

    def tr(self, ps, out, in_, ident, rd):
        self.op("pe", lambda e: e.transpose(out, in_, ident), reads=rd, writes=[ps])

    def act(self, wr, out, rd, in_, func, scale=1.0, bias=None, accum=None):
        kw = {}
        if bias is not None:
            kw["bias"] = bias
        if accum is not None:
            kw["accum_out"] = accum
        self.op("act", lambda e: e.activation(out=out, in_=in_, func=func, scale=scale, **kw), reads=rd, writes=wr)

    def ts(self, eng, wr, out, rd, in0, s1, s2, op0, op1=None, accum=None):
        kw = {}
        if op1 is not None:
            kw["op1"] = op1
        if accum is not None:
            kw["accum_out"] = accum
        self.op(eng, lambda e: e.tensor_scalar(out, in0, s1, s2, op0, **kw), reads=rd, writes=wr)

    def tt(self, eng, wr, out, rd, in0, in1, op):
        self.op(eng, lambda e: e.tensor_tensor(out, in0, in1, op), reads=rd, writes=wr)

    def stt(self, eng, wr, out, rd, in0, scalar, in1, op0, op1):
        self.op(eng, lambda e: e.scalar_tensor_tensor(out, in0, scalar, in1, op0, op1), reads=rd, writes=wr)

    def cp(self, eng, wr, out, rd, in_):
        if eng == "act":
            self.op("act", lambda e: e.copy(out, in_), reads=rd, writes=wr)
        else:
            self.op(eng, lambda e: e.tensor_copy(out, in_), reads=rd, writes=wr)

    def memset(self, eng, wr, out, val):
        self.op(eng, lambda e: e.memset(out, val), writes=wr)

    def dma(self, q, wr, out, rd, in_):
        self.op(q, lambda e: e.dma_start(out=out, in_=in_), reads=rd, writes=wr, dma=True)

    def red(self, eng, wr, out, rd, in_, op, axis=AX.X):
        self.op(eng, lambda e: e.tensor_reduce(out, in_, axis, op), reads=rd, writes=wr)

    def make_ident(self):
        idf = self.sb("idf", [128, 128], F32)
        idb = self.sb("idb", [128, 128], BF16)
        self.memset("pool", [idf], idf[:], 1.0)
        self.op("pool", lambda e: e.affine_select(idf[:], idf[:], [[-1, 128]], ALU.is_equal, 0.0, base=0,
                                                 channel_multiplier=1), reads=[idf], writes=[idf])
        self.cp("dve", [idb], idb[:], [idf], idf[:])
        self.idf, self.idb = idf, idb
        eb = self.sb("epsb", [128, 1], F32)
        self.memset("dve", [eb], eb[:], EPS)
        self.epsb = eb

    def rstd(self, out_buf, out, ssq_buf, ssq, n, scale):
        tmp = self.nxt("rstd_tmp")
        self.act([tmp], tmp[:, 0:n], [ssq_buf, self.epsb], ssq, AF.Ln, scale=scale, bias=self.epsb[:, 0:1])
        self.act([out_buf], out, [tmp], tmp[:, 0:n], AF.Exp, scale=-0.5)


def new_prog(ctx):
    nc = bass.Bass("TRN2", target_bir_lowering=False)
    P = Prog(nc, ctx)
    return nc, P


def load_bf16(P, name, src_ap, shape):
    b = P.sb(name, shape, BF16)
    P.dma("pool", [b], b[:], [], src_ap)
    return b


def tile_T(P, xrows, row0, A, Bm):
    xt = P.nxt("xt")
    P.dma("pool", [xt], xt[:], [xrows], xrows[row0:row0 + 128, :])
    junk = P.nxt("junk")
    ssq = P.nxt("ssq")
    P.act([junk, ssq], junk[:], [xt], xt[:], AF.Square, accum=ssq[:, 0:1])
    rs = P.nxt("rs")
    P.rstd(rs, rs[:, 0:1], ssq, ssq[:, 0:1], 1, 1.0 / D)
    h32 = P.nxt("h32")
    P.stt("dve", [h32], h32[:], [xt, rs, A], xt[:], rs[:, 0:1], A[:], ALU.mult, ALU.mult)
    hb = P.nxt("hb")
    P.tt("dve", [hb], hb[:], [h32, Bm], h32[:], Bm[:], ALU.add)
    tp = P.nxt("tp")
    for k in range(8):
        P.tr(tp, tp[:, k, :], hb[:, k * 128:(k + 1) * 128], P.idb[:], [hb, P.idb])
    return tp


def compute_mod(P, ccols_ap, modw_ap, modb_ap, col0, ncols, names):
    nseg = ncols // 1024
    cc = P.sb("cc", [128, 16], F32)
    P.dma("sp", [cc], cc[:], [], ccols_ap)
    sc = P.sb("sc", [128, 16], F32)
    P.act([sc], sc[:], [cc], cc[:], AF.Silu)
    ones2 = P.sb("ones2", [1, 2], F32)
    P.memset("dve", [ones2], ones2[:], 1.0)
    mb = P.sb("mb", [1, ncols], F32)
    P.dma("sp", [mb], mb[:], [], modb_ap[0:1, col0:col0 + ncols])
    modrows = P.sb("modrows", [2, ncols], F32)
    MWC = 128
    P.ring("mw", [128, 8, MWC], F32, 2)
    for j in range(ncols // MWC):
        mw = P.nxt("mw")
        P.dma("sp", [mw], mw[:], [], modw_ap[:, col0 + j * MWC: col0 + (j + 1) * MWC].rearrange("(k p) n -> p k n", p=128))
        ps = P.nxt("ps")
        for k in range(8):
            P.mm(ps, ps[0:2, 0:MWC], sc[:, 2 * k:2 * k + 2], mw[:, k, :], k == 0, False, [sc, mw])
        P.mm(ps, ps[0:2, 0:MWC], ones2[:], mb[:, j * MWC:(j + 1) * MWC], False, True, [ones2, mb])
        P.cp("dve", [modrows], modrows[:, j * MWC:(j + 1) * MWC], [ps], ps[0:2, 0:MWC])
    sel = P.sb("sel", [2, 2, 128], F32)
    P.memset("dve", [sel], sel[:], 0.0)
    P.memset("dve", [sel], sel[0:1, 0, :], 1.0)
    P.ts("dve", [sel], sel[:, 1, :], [sel], sel[:, 0, :], -1.0, 1.0, ALU.mult, ALU.add)
    out = {}
    for si, nm in enumerate(names):
        reps = []
        for w in range(2):
            rep = P.sb("mod_%s_%d" % (nm, w), [128, 1024], F32)
            for hh in range(2):
                ps = P.nxt("ps")
                P.mm(ps, ps[:, :], sel[:, w, :], modrows[:, si * 1024 + hh * 512: si * 1024 + (hh + 1) * 512], True, True,
                     [sel, modrows])
                P.cp("act", [rep], rep[:, hh * 512:(hh + 1) * 512], [ps], ps[:, :])
            reps.append(rep)
        out[nm] = reps
    return out


def phase_A(P, T):
    P.phase_begin("A")
    xrows, xhalo, hmask, ccols, modw, modb, n1g = T["xrows"], T["xhalo"], T["hmask"], T["ccols"], T["modw0"], T["modb0"], T["n1g0"]
    w_in, convp, qg, w_uq, w_uqs, w_ukv, ropeq, ropek = (T[k] for k in ("w_in", "convp", "qg", "w_uq", "w_uqs", "w_ukv", "ropeq", "ropek"))
    QT, KTn, KTr, Vt, convT = T["QT"], T["KTn"], T["KTr"], T["Vt"], T["convT"]

    P.make_ident()
    P.ring("ps", [128, 512], F32, 6, psum=True)
    P.ring("tp", [128, 8, 128], BF16, 2, psum=True)
    P.ring("xt", [128, D], F32, 2)
    P.ring("junk", [128, D], BF16, 1)
    P.ring("ssq", [128, 1], F32, 2)
    P.ring("rs", [128, 1], F32, 2)
    P.ring("rstd_tmp", [128, 512], F32, 1)
    P.ring("h32", [128, D], F32, 1)
    P.ring("hb", [128, D], BF16, 2)

    mods = compute_mod(P, ccols.t, modw.t, modb.t, 0, 2048, ["sh1", "sc1"])
    g1 = P.sb("g1", [128, D], F32)
    P.dma("sp", [g1], g1[:], [], n1g.t)
    AB = []
    for w in range(2):
        A = mods["sc1"][w]
        P.stt("dve", [A], A[:], [A, g1], A[:], 1.0, g1[:], ALU.add, ALU.mult)
        AB.append((A, mods["sh1"][w]))

    w_in_b = load_bf16(P, "w_in_b", w_in.t.rearrange("(k p) n -> p k n", p=128), [128, 8, 1984])
    w_uq_b = load_bf16(P, "w_uq_b", w_uq.t.rearrange("(k p) n -> p k n", p=128), [128, 2, 768])
    w_uqs_b = load_bf16(P, "w_uqs_b", w_uqs.t.rearrange("(k p) n -> p k n", p=128), [128, 2, 768])
    w_ukv_b = load_bf16(P, "w_ukv_b", w_ukv.t, [128, 1024])
    cvp = P.sb("cvp", [128, 16], F32)
    P.dma("sp", [cvp], cvp[:], [], convp.t)
    qgs = P.sb("qgs", [128, 3], F32)
    P.dma("sp", [qgs], qgs[:], [], qg.t)
    hm = P.sb("hm", [128, 2], F32)
    P.dma("sp", [hm], hm[:], [], hmask.t)
    P.ring("rq", [96, 2, 512], F32, 1)
    P.ring("rk", [32, 2, 512], F32, 1)
    onesb = P.sb("onesb", [128, 128], BF16)
    P.memset("dve", [onesb], onesb[:], 1.0)

    P.ring("hseg", [128, 8, 1022], BF16, 2)
    halo_tmp = P.sb("halo_tmp", [128, 8, 2], BF16)
    tph = tile_T(P, xhalo, 0, AB[0][0], AB[0][1])
    P.cp("act", [halo_tmp], halo_tmp[:], [tph], tph[:, :, 0:2])

    def fill_seg(hseg, G0, G1):
        t_lo = max(0, (G0 - 1) // 128)
        t_hi = min(NL // 128 - 1, (G1 - 2) // 128)
        for t in range(t_lo, t_hi + 1):
            a_ = max(G0, 1 + 128 * t)
            b_ = min(G1, 129 + 128 * t)
            if b_ <= a_:
                continue
            tp = tile_T(P, xrows, t * 128, AB[0][0], AB[0][1])
            P.cp("act", [hseg], hseg[:, :, a_ - G0:b_ - G0], [tp], tp[:, :, a_ - 1 - 128 * t:b_ - 1 - 128 * t])
        if G0 == 0:
            P.cp("dve", [hseg], hseg[:, :, 0:1], [halo_tmp], halo_tmp[:, :, 0:1])
        if G1 == NL + 2:
            P.cp("dve", [hseg], hseg[:, :, NL + 1 - G0:NL + 2 - G0], [halo_tmp], halo_tmp[:, :, 1:2])

    P.ring("gcs", [128, 512], F32, 2)
    P.ring("zw", [128, 512], F32, 2)
    P.ring("ca", [128, 512], F32, 2)
    P.ring("co", [128, 512], BF16, 3)
    P.ring("lat", [128, 512], F32, 3)
    P.ring("sq", [128, 512], BF16, 3)
    P.ring("rstdL", [128, 512], F32, 1)
    P.ring("qn", [128, 2, 512], BF16, 2)
    P.ring("kvn", [128, 512], BF16, 2)
    P.ring("t1", [96, 512], F32, 1)
    P.ring("t2", [96, 512], F32, 1)
    P.ring("qo", [96, 512], BF16, 3)
    P.ring("ko", [128, 512], BF16, 3)
    P.ring("vo", [128, 512], BF16, 3)

    def proj(hT, c0, n, col_lo, col_n):
        ps = P.nxt("ps")
        for k in range(8):
            P.mm(ps, ps[0:col_n, 0:n], w_in_b[:, k, col_lo:col_lo + col_n], hT[:, k, c0:c0 + n], k == 0, k == 7,
                 [w_in_b, hT])
        return ps

    def latent_norm(pss, nchunk, n, gcol0, out_ring):
        lats, sqs = [], []
        for c in range(nchunk):
            lt = P.nxt("lat")
            P.cp("act", [lt], lt[:, 0:n], [pss[c]], pss[c][:, 0:n])
            sq = P.nxt("sq")
            P.act([sq], sq[:, 0:n], [pss[c]], pss[c][:, 0:n], AF.Square)
            lats.append(lt)
            sqs.append(sq)
        ps = P.nxt("ps")
        for c in range(nchunk):
            P.mm(ps, ps[:, 0:n], onesb[:], sqs[c][:, 0:n], c == 0, c == nchunk - 1, [onesb, sqs[c]])
        rl = P.nxt("rstdL")
        P.rstd(rl, rl[:, 0:n], ps, ps[:, 0:n], n, 1.0 / (128 * nchunk))
        ob = P.nxt(out_ring)
        for c in range(nchunk):
            o_ap = ob[:, c, 0:n] if nchunk > 1 else ob[:, 0:n]
            P.stt("dve", [ob], o_ap, [lats[c], qgs, rl], lats[c][:, 0:n], qgs[:, gcol0 + c:gcol0 + c + 1], rl[:, 0:n],
                  ALU.mult, ALU.mult)
        return ob

    def block(hT, c0, n, tok0, is_ctx, first, last):
        no = n - 2
        if not is_ctx:
            rq = P.nxt("rq")
            P.dma("pool", [rq], rq[:, :, 0:no], [], ropeq[:, :, tok0:tok0 + no])
            rk = P.nxt("rk")
            P.dma("pool", [rk], rk[:, :, 0:no], [], ropek[:, :, tok0:tok0 + no])
        for c in range(4):
            ps_gc = proj(hT, c0, n, 512 + c * 128, 128)
            gcs = P.nxt("gcs")
            P.cp("act", [gcs], gcs[:, 0:n], [ps_gc], ps_gc[:, 0:n])
            ps_u = proj(hT, c0, n, 1024 + c * 128, 128)
            zw = P.nxt("zw")
            P.tt("dve", [zw], zw[:, 0:n], [ps_u, gcs], ps_u[:, 0:n], gcs[:, 0:n], ALU.mult)
            if is_ctx:
                P.memset("dve", [zw], zw[:, 0:1], 0.0)
                P.memset("dve", [zw], zw[:, n - 1:n], 0.0)
            else:
                if first:
                    P.ts("dve", [zw], zw[:, 0:1], [zw, hm], zw[:, 0:1], hm[:, 0:1], None, ALU.mult)
                if last:
                    P.ts("dve", [zw], zw[:, n - 1:n], [zw, hm], zw[:, n - 1:n], hm[:, 1:2], None, ALU.mult)
            ca = P.nxt("ca")
            P.ts("dve", [ca], ca[:, 0:no], [zw, cvp], zw[:, 1:n - 1], cvp[:, c * 4 + 1:c * 4 + 2], cvp[:, c * 4 + 3:c * 4 + 4],
                 ALU.mult, ALU.add)
            P.stt("dve", [ca], ca[:, 0:no], [zw, cvp, ca], zw[:, 0:n - 2], cvp[:, c * 4:c * 4 + 1], ca[:, 0:no], ALU.mult, ALU.add)
            P.stt("dve", [ca], ca[:, 0:no], [zw, cvp, ca], zw[:, 2:n], cvp[:, c * 4 + 2:c * 4 + 3], ca[:, 0:no], ALU.mult, ALU.add)
            ps_gb = proj(hT, c0, n, c * 128, 128)
            co = P.nxt("co")
            P.tt("dve", [co], co[:, 0:no], [ps_gb, ca], ps_gb[:, 1:n - 1], ca[:, 0:no], ALU.mult)
            P.dma("sp", [convT], convT[c * 128:(c + 1) * 128, tok0:tok0 + no], [co], co[:, 0:no])
        pq = [proj(hT, c0, n, 1536 + c * 128, 128) for c in range(2)]
        qn = latent_norm(pq, 2, n, 0, "qn")
        for h in range(8):
            psq = P.nxt("ps")
            for k in range(2):
                P.mm(psq, psq[0:96, 0:n], w_uq_b[:, k, h * 96:(h + 1) * 96], qn[:, k, 0:n], k == 0, k == 1, [w_uq_b, qn])
            qo = P.nxt("qo")
            if is_ctx:
                P.cp("act", [qo], qo[:, 0:no], [psq], psq[0:96, 1:n - 1])
            else:
                pss = P.nxt("ps")
                for k in range(2):
                    P.mm(pss, pss[0:96, 0:n], w_uqs_b[:, k, h * 96:(h + 1) * 96], qn[:, k, 0:n], k == 0, k == 1, [w_uqs_b, qn])
                t1 = P.nxt("t1")
                P.tt("dve", [t1], t1[:, 0:no], [psq, rq], psq[0:96, 1:n - 1], rq[:, 0, 0:no], ALU.mult)
                t2 = P.nxt("t2")
                P.tt("dve", [t2], t2[:, 0:no], [pss, rq], pss[0:96, 1:n - 1], rq[:, 1, 0:no], ALU.mult)
                P.tt("dve", [qo], qo[:, 0:no], [t1, t2], t1[:, 0:no], t2[:, 0:no], ALU.add)
            P.dma("sp", [QT], QT[h, :, tok0:tok0 + no], [qo], qo[:, 0:no])
        pk = [proj(hT, c0, n, 1792, 128)]
        kvn = latent_norm(pk, 1, n, 2, "kvn")
        for c in range(4):
            psk = P.nxt("ps")
            P.mm(psk, psk[:, 0:n], w_ukv_b[:, c * 128:(c + 1) * 128], kvn[:, 0:n], True, True, [w_ukv_b, kvn])
            ko = P.nxt("ko")
            P.cp("act", [ko], ko[:, 0:no], [psk], psk[:, 1:n - 1])
            P.dma("sp", [KTn], KTn[c * 128:(c + 1) * 128, tok0:tok0 + no], [ko], ko[:, 0:no])
        t0 = 0
        while t0 < no:
            m = min(128, no - t0)
            psv = P.nxt("ps")
            P.mm(psv, psv[0:m, :], kvn[:, 1 + t0:1 + t0 + m], w_ukv_b[:, 512:1024], True, True, [kvn, w_ukv_b])
            vo = P.nxt("vo")
            P.cp("act", [vo], vo[0:m, :], [psv], psv[0:m, :])
            P.dma("sp", [Vt], Vt[tok0 + t0:tok0 + t0 + m, :], [vo], vo[0:m, :])
            t0 += m
        psr = proj(hT, c0, n, 1920, 32)
        ko = P.nxt("ko")
        if is_ctx:
            P.cp("act", [ko], ko[0:32, 0:no], [psr], psr[0:32, 1:n - 1])
        else:
            psrs = proj(hT, c0, n, 1952, 32)
            t1 = P.nxt("t1")
            P.tt("dve", [t1], t1[0:32, 0:no], [psr, rk], psr[0:32, 1:n - 1], rk[:, 0, 0:no], ALU.mult)
            t2 = P.nxt("t2")
            P.tt("dve", [t2], t2[0:32, 0:no], [psrs, rk], psrs[0:32, 1:n - 1], rk[:, 1, 0:no], ALU.mult)
            P.tt("dve", [ko], ko[0:32, 0:no], [t1, t2], t1[0:32, 0:no], t2[0:32, 0:no], ALU.add)
        P.dma("sp", [KTr], KTr[:, tok0:tok0 + no], [ko], ko[0:32, 0:no])

    nb = (NL + 509) // 510
    for sg in range((nb + 1) // 2):
        G0 = 1020 * sg
        G1 = min(G0 + 1022, NL + 2)
        hseg = P.nxt("hseg")
        fill_seg(hseg, G0, G1)
        for j in (2 * sg, 2 * sg + 1):
            if j >= nb:
                continue
            c0 = 510 * j
            n = min(512, NL + 2 - c0)
            block(hseg, c0 - G0, n, c0, False, j == 0, j == nb - 1)
    hseg = P.nxt("hseg")
    for t in range(2):
        tp = tile_T(P, xrows, NL + t * 128, AB[1][0], AB[1][1])
        P.cp("act", [hseg], hseg[:, :, 1 + t * 128:129 + t * 128], [tp], tp[:])
    P.cp("dve", [hseg], hseg[:, :, 0:1], [hseg], hseg[:, :, 1:2])
    P.cp("dve", [hseg], hseg[:, :, 257:258], [hseg], hseg[:, :, 1:2])
    block(hseg, 0, 258, NL, True, True, True)
    P.phase_end()


def _swap_perm(dim):
    q = dim // 4
    d = np.arange(dim)
    return np.where((d % (2 * q)) < q, d + q, d - q)


def _rope_tables(pos, dim):
    half = dim // 2
    q = dim // 4
    freqs = (10000.0 ** (-np.arange(0, half, 2, dtype=np.float32) / np.float32(half))).astype(np.float32)
    row = (pos // 64).astype(np.float32)
    col = (pos % 64).astype(np.float32)
    ang = np.concatenate([row[:, None] * freqs, col[:, None] * freqs], axis=-1).astype(np.float32)
    cos, sin = np.cos(ang).astype(np.float32), np.sin(ang).astype(np.float32)
    d = np.arange(dim)
    j = (d // (2 * q)) * q + d % q
    sign = np.where((d % (2 * q)) < q, -1.0, 1.0).astype(np.float32)
    C = cos[:, j].T.copy()
    S = (sin[:, j] * sign[None, :]).T.copy()
    return C, S


def _rep(v):
    return np.ascontiguousarray(np.broadcast_to(np.asarray(v, np.float32)[None, :], (128, v.shape[0])))


def _cols(v, k):
    return np.ascontiguousarray(np.asarray(v, np.float32).reshape(k, 128).T)


def prep_A(inp):
    x, c, cx, c_ctx = inp["x"], inp["c"], inp["ctx"], inp["c_ctx"]
    w_in = inp["ab_w_in"][0]
    p32 = _swap_perm(32)
    w_in_ext = np.ascontiguousarray(np.concatenate([w_in, w_in[:, 1920 + p32]], axis=1))
    w_uq = inp["mla_w_uq"][0]
    w_uqs = w_uq.copy()
    for h in range(8):
        w_uqs[:, h * 96 + 64:h * 96 + 96] = w_uq[:, h * 96 + 64 + p32]
    wk = inp["mla_w_ukv"][0].reshape(128, 8, 128)
    w_ukv = np.ascontiguousarray(np.concatenate([wk[:, :, :64].reshape(128, 512), wk[:, :, 64:].reshape(128, 512)], axis=1))
    convp = np.zeros((128, 16), np.float32)
    for ch in range(4):
        for i in range(3):
            convp[:, ch * 4 + i] = inp["conv_w"][0][i, ch * 128:(ch + 1) * 128]
        convp[:, ch * 4 + 3] = inp["conv_b"][0][ch * 128:(ch + 1) * 128]
    qg = np.concatenate([_cols(inp["mla_q_norm_g"][0], 2), _cols(inp["mla_kv_norm_g"][0], 1)], axis=1)
    maps = []
    for core in range(8):
        b, q = core // 4, core % 4
        T0 = q * NL
        xr = np.ascontiguousarray(np.concatenate([x[b, T0:T0 + NL], cx[b]], axis=0))
        xh = np.zeros((128, D), np.float32)
        if q > 0:
            xh[0] = x[b, T0 - 1]
        if q < 3:
            xh[1] = x[b, T0 + NL]
        hm = np.zeros((128, 2), np.float32)
        hm[:, 0] = 1.0 if q > 0 else 0.0
        hm[:, 1] = 1.0 if q < 3 else 0.0
        cc = np.zeros((128, 16), np.float32)
        cc[:, 0::2] = _cols(c[b], 8)
        cc[:, 1::2] = _cols(c_ctx, 8)
        C, S = _rope_tables(np.arange(T0, T0 + NL), 32)
        rq = np.zeros((96, 2, NL), np.float32)
        rq[:64, 0] = 1.0
        rq[64:, 0] = C
        rq[64:, 1] = S
        rk = np.stack([C, S], axis=1)
        maps.append({
            "xrows": xr, "xhalo": xh, "hmask": hm, "ccols": cc,
            "modw": np.ascontiguousarray(inp["mod_w"][0]), "modb": np.ascontiguousarray(inp["mod_b"][0][None, :]),
            "n1g": _rep(inp["norm1_g"][0]), "w_in": w_in_ext, "convp": convp, "qg": np.ascontiguousarray(qg),
            "w_uq": np.ascontiguousarray(w_uq), "w_uqs": np.ascontiguousarray(w_uqs), "w_ukv": w_ukv,
            "ropeq": rq, "ropek": np.ascontiguousarray(rk),
        })
    return maps


NK0 = NCX + 4 * NL
NKT0 = NK0 // 128


def phase_B(P, T):
    P.phase_begin("B")
    QT, KTn, KTr, Vt, KTr_all, Vt_all, attT = (T[k] for k in ("QT", "KTn", "KTr", "Vt", "KTr_all", "Vt_all", "attT"))
    scale = 96.0 ** -0.5

    P.ring("sps", [128, 2, 512], F32, 2, psum=True)
    P.ring("ops", [128, 512], F32, 2, psum=True)
    P.ring("bps", [128, 512], F32, 1, psum=True)
    P.ring("kt", [96, NK0], BF16, 2)
    P.ring("vp", [128, NKT0, 65], BF16, 2)
    P.ring("qt", [96, NT], BF16, 2)
    P.ring("pT", [128, 2, 512], BF16, 4)
    P.ring("rl", [1, 512], F32, 2)
    P.ring("bsb", [65, 512], F32, 2)
    P.ring("ao", [65, 512], BF16, 2)
    ones = P.sb("ones", [1, 128], F32)
    P.memset("dve", [ones], ones[:], 1.0)

    pending = [None]

    def qblock(h, kt_b, vp_b, qt_b, q0, nq, ntile):
        o_ps = P.nxt("ops")
        ng = ntile // 2

        def emit_pv(g, pT):
            for i in range(2):
                t = 2 * g + i
                P.mm(o_ps, o_ps[0:65, 0:nq], vp_b[:, t, :], pT[:, i, 0:nq], t == 0, t == ntile - 1, [vp_b, pT])

        prev = None
        for g in range(ng):
            s_ps = P.nxt("sps")
            for i in range(2):
                t = 2 * g + i
                P.mm(s_ps, s_ps[:, i, 0:nq], kt_b[:, t * 128:(t + 1) * 128], qt_b[:, q0:q0 + nq], True, True, [kt_b, qt_b])
            if prev is not None:
                emit_pv(*prev)
            pT = P.nxt("pT")
            P.act([pT], pT[:, :, 0:nq], [s_ps], s_ps[:, :, 0:nq], AF.Exp, scale=scale)
            prev = (g, pT)
            if g == min(3, ng - 1) and pending[0] is not None:
                pending[0]()
                pending[0] = None
        emit_pv(*prev)

        def fin():
            rl = P.nxt("rl")
            P.op("dve", lambda e: e.reciprocal(rl[:, 0:nq], o_ps[0:1, 0:nq]), reads=[o_ps], writes=[rl])
            b_ps = P.nxt("bps")
            P.mm(b_ps, b_ps[0:65, 0:nq], ones[:, 0:65], rl[:, 0:nq], True, True, [ones, rl])
            bsb = P.nxt("bsb")
            P.cp("act", [bsb], bsb[:, 0:nq], [b_ps], b_ps[0:65, 0:nq])
            ao = P.nxt("ao")
            P.tt("dve", [ao], ao[:, 0:nq], [o_ps, bsb], o_ps[0:65, 0:nq], bsb[:, 0:nq], ALU.mult)
            P.dma("pool", [attT], attT[h * 64:(h + 1) * 64, q0:q0 + nq], [ao], ao[1:65, 0:nq])
        pending[0] = fin

    for b_ in P.rings["vp"][0]:
        P.memset("dve", [b_], b_[:, :, 0:1], 1.0)
    for h in range(8):
        kt_b = P.nxt("kt")
        P.dma("sp", [kt_b], kt_b[0:64, 0:NCX], [KTn], KTn[h * 64:(h + 1) * 64, NL:NT])
        P.dma("sp", [kt_b], kt_b[64:96, 0:NCX], [KTr], KTr[:, NL:NT])
        for r in range(4):
            P.dma("sp", [kt_b], kt_b[0:64, NCX + r * NL:NCX + (r + 1) * NL], [T["KTn_all%d" % h]], T["KTn_all%d" % h][r * 64:(r + 1) * 64, 0:NL])
            P.dma("sp", [kt_b], kt_b[64:96, NCX + r * NL:NCX + (r + 1) * NL], [KTr_all], KTr_all[r * 32:(r + 1) * 32, 0:NL])
        vp_b = P.nxt("vp")
        P.dma("sp", [vp_b], vp_b[:, 0:2, 1:65], [Vt], Vt[NL:NT, h * 64:(h + 1) * 64].rearrange("(t p) d -> p t d", p=128))
        for r in range(4):
            for k in range(4):
                P.dma("sp", [vp_b], vp_b[:, 2 + 32 * r + 8 * k:2 + 32 * r + 8 * k + 8, 1:65], [Vt_all],
                      Vt_all[k, r * 1024:(r + 1) * 1024, h * 64:(h + 1) * 64].rearrange("(t p) d -> p t d", p=128))
        qt_b = P.nxt("qt")
        P.dma("sp", [qt_b], qt_b[:], [QT], QT[h])
        for qb in range(NL // 512):
            qblock(h, kt_b, vp_b, qt_b, qb * 512, 512, NKT0)
        qblock(h, kt_b, vp_b, qt_b, NL, NCX, NCX // 128)
    pending[0]()
    P.phase_end()


def phase_post(P, T, layer, ntok, cats):
    P.phase_begin("post%d" % layer)
    xrows, ccols, modw, modb, n2g, w_out, rw = T["xres%d" % layer], T["ccols"], T["modw%d" % layer], T["modb%d" % layer], T["n2g%d" % layer], T["w_out%d" % layer], T["rw%d" % layer]
    xs1, h2o, affo, affl = T["xacc%d" % layer], T["h2_%d" % layer], T["aff_%d" % layer], T["affl_%d" % layer]

    P.make_ident()
    P.ring("ps", [128, 512], F32, 2, psum=True)
    P.ring("yps", [128, 2, 512], F32, 2, psum=True)
    P.ring("tp32", [128, 8, 128], F32, 1, psum=True)
    P.ring("rstd_tmp", [128, 512], F32, 1)
    mods = compute_mod(P, ccols.t, modw.t, modb.t, 2048, 3072, ["g1", "sh2", "sc2"])
    g2n = P.sb("g2n", [128, D], F32)
    P.dma("sp", [g2n], g2n[:], [], n2g.t)
    for w in range(2):
        A = mods["sc2"][w]
        P.stt("dve", [A], A[:], [A, g2n], A[:], 1.0, g2n[:], ALU.add, ALU.mult)
    w_out_b = load_bf16(P, "w_out_b", w_out.t.rearrange("(k p) n -> p k n", p=128), [128, 8, D])
    rws = P.sb("rws", [128, 8, NE], F32)
    P.dma("sp", [rws], rws[:], [], rw.t.rearrange("(k p) n -> p k n", p=128))

    P.ring("cat", [128, 8, 128], BF16, 2)
    P.ring("xt", [128, D], F32, 2)
    P.ring("xsr", [128, D], F32, 2)
    P.ring("junk", [128, D], BF16, 1)
    P.ring("ssq", [128, 1], F32, 2)
    P.ring("rs", [128, 1], F32, 2)
    P.ring("h32", [128, D], F32, 3)
    P.ring("hb", [128, D], BF16, 2)
    P.ring("hT32", [128, 8, 128], F32, 1)
    P.ring("sm", [128, 4], F32, 3)
    P.ring("ex", [128, NE], F32, 2)
    P.ring("af", [128, NE], F32, 2)

    def p1(t, out):
        w = 0 if t < NL // 128 else 1
        r0 = t * 128
        cat = P.nxt("cat")
        c0_ = 0
        for (cb, nch) in cats:
            P.dma("sp", [cat], cat[:, c0_:c0_ + nch, :], [cb], cb[:, r0:r0 + 128].rearrange("(c p) t -> p c t", p=128))
            c0_ += nch
        xt = P.nxt("xt")
        P.dma("sp", [xt], xt[:], [xrows], xrows[r0:r0 + 128, :])
        y = P.nxt("yps")
        for hh in range(2):
            for k in range(8):
                P.mm(y, y[:, hh, :], cat[:, k, :], w_out_b[:, k, hh * 512:(hh + 1) * 512], k == 0, k == 7, [cat, w_out_b])
            yield
        xs = P.nxt("xsr")
        g1r = mods["g1"][w]
        P.tt("dve", [xs], xs[:], [y, g1r], y[:].rearrange("p a b -> p (a b)"), g1r[:], ALU.mult)
        yield
        P.tt("dve", [xs], xs[:], [xs, xt], xs[:], xt[:], ALU.add)
        P.dma("pool", [xs1], xs1[r0:r0 + 128, :], [xs], xs[:])
        yield
        junk = P.nxt("junk")
        ssq = P.nxt("ssq")
        P.act([junk, ssq], junk[:], [xs], xs[:], AF.Square, accum=ssq[:, 0:1])
        yield
        rs = P.nxt("rs")
        P.rstd(rs, rs[:, 0:1], ssq, ssq[:, 0:1], 1, 1.0 / D)
        yield
        h32 = P.nxt("h32")
        P.stt("dve", [h32], h32[:], [xs, rs, mods["sc2"][w]], xs[:], rs[:, 0:1], mods["sc2"][w][:], ALU.mult, ALU.mult)
        yield
        P.tt("dve", [h32], h32[:], [h32, mods["sh2"][w]], h32[:], mods["sh2"][w][:], ALU.add)
        yield
        hb = P.nxt("hb")
        P.cp("act", [hb], hb[:], [h32], h32[:])
        P.dma("pool", [h2o], h2o[r0:r0 + 128, :], [hb], hb[:])
        out["h32"] = h32
        yield

    def p2(t, h32):
        r0 = t * 128
        tp = P.nxt("tp32")
        for k in range(8):
            P.tr(tp, tp[:, k, :], h32[:, k * 128:(k + 1) * 128], P.idf[:], [h32, P.idf])
        yield
        hT = P.nxt("hT32")
        P.cp("act", [hT], hT[:], [tp], tp[:])
        yield
        lg = P.nxt("ps")
        for k in range(8):
            P.mm(lg, lg[:, 0:NE], hT[:, k, :], rws[:, k, :], k == 0, k == 7, [hT, rws])
        yield
        sm = P.nxt("sm")
        P.red("dve", [sm], sm[:, 0:1], [lg], lg[:, 0:NE], ALU.max)
        P.ts("dve", [sm], sm[:, 1:2], [sm], sm[:, 0:1], -1.0, None, ALU.mult)
        yield
        ex = P.nxt("ex")
        P.act([ex, sm], ex[:], [lg, sm], lg[:, 0:NE], AF.Exp, bias=sm[:, 1:2], accum=sm[:, 2:3])
        yield
        P.op("dve", lambda e, sm=sm: e.reciprocal(sm[:, 3:4], sm[:, 2:3]), reads=[sm], writes=[sm])
        af = P.nxt("af")
        P.ts("dve", [af], af[:], [ex, sm], ex[:], sm[:, 3:4], None, ALU.mult)
        P.dma("pool", [affo], affo[r0:r0 + 128, :], [af], af[:])
        if t < NL // 128 and affl is not affo:
            P.dma("pool", [affl], affl[r0:r0 + 128, :], [af], af[:])
        yield

    ntile = ntok // 128
    o0 = {}
    for _ in p1(0, o0):
        pass
    hprev = o0["h32"]
    for t in range(ntile):
        on = {}
        ga = p1(t + 1, on) if t + 1 < ntile else iter(())
        gb = p2(t, hprev)
        da = db = False
        while not (da and db):
            if not da:
                da = next(ga, "end") == "end"
            if not db:
                db = next(gb, "end") == "end"
        hprev = on.get("h32")
    P.phase_end()


def _ccols(inp, b):
    cc = np.zeros((128, 16), np.float32)
    cc[:, 0::2] = _cols(inp["c"][b], 8)
    cc[:, 1::2] = _cols(inp["c_ctx"], 8)
    return cc


SMAX = 640
CAP_L = 2048
CAP_C = 32
NBIS = 30


def bisect_gen(P, affs, J, e0, e1, kcap, ones_f, name, psbuf, thr):
    ne = e1 - e0
    lo = P.sb(name + "_lo", [128, ne], F32)
    mid = P.sb(name + "_mid", [128, ne], F32)
    cnt = P.sb(name + "_cnt", [128, ne], F32)
    stp = P.sb(name + "_stp", [128, ne], F32)
    cmp = P.sb(name + "_cmp", [128, J, ne], BF16)
    P.memset("dve", [lo], lo[:], 0.0)
    for it in range(NBIS):
        c = 2.0 ** -(it + 1)
        P.ts("dve", [mid], mid[:], [lo], lo[:], c, None, ALU.add)
        P.tt("dve", [cmp], cmp[:], [affs, mid], affs[:, :, e0:e1], mid[:].unsqueeze(1).to_broadcast([128, J, ne]), ALU.is_ge)
        P.red("dve", [cnt], cnt[:], [cmp], cmp[:].rearrange("p j e -> p e j"), ALU.add)
        P.mm(psbuf, psbuf[:, 0:ne], ones_f[:], cnt[:], True, True, [ones_f, cnt])
        P.ts("dve", [stp], stp[:], [psbuf], psbuf[:, 0:ne], float(kcap) - 0.5, c, ALU.is_ge, ALU.mult)
        P.tt("dve", [lo], lo[:], [lo, stp], lo[:], stp[:], ALU.add)
        yield
    P.cp("dve", [thr], thr[:, e0:e1], [lo], lo[:])
    yield


def phase_moe(P, T, layer):
    has_ctx = (layer == 0)
    ntok = NT if has_ctx else NL
    T_ = ntok // 128
    TL = NL // 128
    NS = SMAX // 128
    NSLOT = SMAX + (CAP_C if has_ctx else 0)
    P.phase_begin("moe%d" % layer)
    h2, aff, aff_all, ccols, modw, modb = T["h2_%d" % layer], T["aff_%d" % layer], T["affall_%d" % layer], T["ccols"], T["modw%d" % layer], T["modb%d" % layer]
    wg, wu, wd = T["wg%d" % layer], T["wu%d" % layer], T["wd%d" % layer]
    xacc = T["xacc%d" % layer]
    if not has_ctx:
        fing, outp = T["fing"], T["out"]
    TRASH0 = ntok
    T = T_

    P.make_ident()
    P.ring("ps", [128, 512], F32, 1, psum=True)
    P.ring("tp", [128, 8, 128], BF16, 1, psum=True)
    P.ring("accb", [128, 512], F32, 2, psum=True)
    P.ring("hb", [128, 512], F32, 4, psum=True)
    P.ring("rstd_tmp", [128, 512], F32, 1)

    P.ring("xt", [128, D], F32, 1)
    zt = P.nxt("xt")
    P.memset("dve", [zt], zt[:], 0.0)
    zt_op = P.op("sp", lambda e: e.dma_start(out=xacc[ntok:ntok + 128, :], in_=zt[:]), reads=[zt], writes=[xacc], dma=True)

    mods = compute_mod(P, ccols.t, modw.t, modb.t, 5120, 1024, ["g2"])
    g2rep = mods["g2"]

    ones_f = P.sb("ones_f", [128, 128], F32)
    P.memset("dve", [ones_f], ones_f[:], 1.0)
    ones_b = P.sb("ones_b", [128, 128], BF16)
    P.memset("dve", [ones_b], ones_b[:], 1.0)
    ltf = P.sb("ltf", [128, 128], F32)
    P.memset("pool", [ltf], ltf[:], 1.0)
    P.op("pool", lambda e: e.affine_select(ltf[:], ltf[:], [[1, 128]], ALU.is_gt, 0.0, base=0, channel_multiplier=-1),
         reads=[ltf], writes=[ltf])
    ltb = P.sb("ltb", [128, 128], BF16)
    P.cp("dve", [ltb], ltb[:], [ltf], ltf[:])
    io_i = P.sb("io_i", [128, SMAX], I32)
    P.op("pool", lambda e: e.iota(io_i[:], [[1, SMAX]], base=0, channel_multiplier=0), writes=[io_i])
    io_f = P.sb("io_f", [128, SMAX], F32)
    P.cp("dve", [io_f], io_f[:], [io_i], io_i[:])
    ip_i = P.sb("ip_i", [128, 1], I32)
    P.op("pool", lambda e: e.iota(ip_i[:], [[0, 1]], base=0, channel_multiplier=1), writes=[ip_i])
    ip_f = P.sb("ip_f", [128, 2], F32)
    P.cp("dve", [ip_f], ip_f[:, 0:1], [ip_i], ip_i[:])
    P.ts("dve", [ip_f], ip_f[:, 1:2], [ip_f], ip_f[:, 0:1], float(TRASH0), None, ALU.add)

    P.ring("wgb", [128, 8, D], BF16, 2)
    P.ring("wub", [128, 8, D], BF16, 2)
    P.ring("wdb", [128, 8, D], BF16, 1)
    P.ring("wstg", [128, 2, D], F32, 2)

    def wd_dma(e, q):
        stg = P.nxt("wstg")
        P.dma("sp", [stg], stg[:], [], wd[e, q * 256:(q + 1) * 256, :].rearrange("(k p) n -> p k n", p=128))
        return stg

    def wd_cast(b, q, stg):
        P.cp("act", [b], b[:, 2 * q:2 * q + 2, :], [stg], stg[:])

    def load_one(nm, src, e):
        b = P.nxt(nm)
        for k in range(8):
            P.dma("pool", [b], b[:, k, :], [], src[e, k * 128:(k + 1) * 128, :])
        return b

    wdb0 = P.nxt("wdb")
    for q_ in range(4):
        wd_cast(wdb0, q_, wd_dma(0, q_))
    w_next = (load_one("wgb", wg, 0), load_one("wub", wu, 0), wdb0)

    af = P.sb("af", [128, T, NE], F32)
    P.dma("sp", [af], af[:], [aff], aff.t.rearrange("(t p) e -> p t e", p=128))
    thr_l = P.sb("thr_l", [128, NE], F32)
    thr_c = P.sb("thr_c", [128, NE], F32)
    main_stack = P.pstack
    sub = ExitStack()
    P.pstack = sub
    affs = P.sb("affs", [128, 128, NE], F32)
    P.dma("sp", [affs], affs[:], [aff_all], aff_all.t.rearrange("(p j) e -> p j e", p=128))
    pbanks = P.rings["accb"][0] + P.rings["hb"][0]
    gens = [bisect_gen(P, affs, 128, 0, 8, CAP_L, ones_f, "bla", pbanks[0], thr_l),
            bisect_gen(P, affs, 128, 8, 16, CAP_L, ones_f, "blb", pbanks[1], thr_l)]
    if has_ctx:
        afc = P.sb("afc", [128, 2, NE], F32)
        P.cp("dve", [afc], afc[:], [af], af[:, TL:T, :])
        gens.append(bisect_gen(P, afc, 2, 0, 16, CAP_C, ones_f, "bc", pbanks[2], thr_c))
    for _ in range(NBIS + 1):
        for g_ in gens:
            next(g_)
    P.pstack = main_stack
    P.ops.append(None)
    sub.close()

    mask = P.sb("mask", [128, T, NE], F32)
    P.tt("dve", [mask], mask[:, 0:TL, :], [af, thr_l], af[:, 0:TL, :], thr_l[:].unsqueeze(1).to_broadcast([128, TL, NE]), ALU.is_ge)
    if has_ctx:
        P.tt("dve", [mask], mask[:, TL:T, :], [af, thr_c], af[:, TL:T, :], thr_c[:].unsqueeze(1).to_broadcast([128, 2, NE]), ALU.is_ge)
    maskb = P.sb("maskb", [128, T, NE], BF16)
    P.cp("dve", [maskb], maskb[:], [mask], mask[:])
    pos = P.sb("pos", [128, T, NE], F32)
    tot = P.sb("tot", [128, T, NE], F32)
    mflat = maskb[:].rearrange("p t e -> p (t e)")
    pflat = pos[:].rearrange("p t e -> p (t e)")
    tflat = tot[:].rearrange("p t e -> p (t e)")
    n_all = T * NE
    c = 0
    while c < n_all:
        w_ = min(512, n_all - c)
        ps = P.nxt("ps")
        P.mm(ps, ps[:, 0:w_], ltb[:], mflat[:, c:c + w_], True, True, [ltb, maskb])
        P.cp("dve", [pos], pflat[:, c:c + w_], [ps], ps[:, 0:w_])
        ps = P.nxt("ps")
        P.mm(ps, ps[:, 0:w_], ones_b[:], mflat[:, c:c + w_], True, True, [ones_b, maskb])
        P.cp("dve", [tot], tflat[:, c:c + w_], [ps], ps[:, 0:w_])
        c += w_
    base = P.sb("base", [128, T, NE], F32)
    P.memset("dve", [base], base[:], 0.0)
    for t in range(1, TL):
        P.tt("dve", [base], base[:, t, :], [base, tot], base[:, t - 1, :], tot[:, t - 1, :], ALU.add)
    if has_ctx:
        P.cp("dve", [base], base[:, TL + 1, :], [tot], tot[:, TL, :])
    P.tt("dve", [pos], pos[:], [pos, base], pos[:], base[:], ALU.add)

    vals = P.sb("vals", [128, T, NE, 6], BF16)
    for t in range(T):
        P.memset("dve", [vals], vals[:, t, :, 0], float(t))
    P.ts("dve", [vals], vals[:, :, :, 1], [af, ip_f], af[:], 0.0, ip_f[:, 0:1], ALU.mult, ALU.add)
    P.memset("dve", [vals], vals[:, :, :, 5], 1.0)
    r1 = tot
    P.cp("dve", [vals], vals[:, :, :, 2], [af], af[:])
    P.tt("dve", [r1], r1[:], [af, vals], af[:], vals[:, :, :, 2], ALU.subtract)
    P.cp("dve", [vals], vals[:, :, :, 3], [r1], r1[:])
    P.tt("dve", [r1], r1[:], [r1, vals], r1[:], vals[:, :, :, 3], ALU.subtract)
    P.cp("dve", [vals], vals[:, :, :, 4], [r1], r1[:])

    P.ring("oh", [128, SMAX], BF16, 3)
    P.ring("siT", [6, NSLOT], F32, 1)
    P.ring("si", [128, 8, 6], F32, 2)
    P.ring("sx", [128, 8, 4], F32, 2)
    P.ring("ixg", [128, 8], I32, 2)
    P.ring("ixs", [128, 8], I32, 2)
    P.ring("xg", [128, D], BF16, 6 if has_ctx else 5)
    P.ring("xgT", [128, 8, NSLOT], BF16, 1)
    P.ring("sil", [128, 512], F32, 2)
    P.ring("hidT", [128, 8, NSLOT], BF16, 1)
    P.ring("ye", [128, D], F32, 2)

    NST = NS + (1 if has_ctx else 0)
    n2 = NSLOT - 512

    def stage1a(e, st):
        acc0 = P.nxt("accb")
        acc1 = P.nxt("accb")
        ohq = st["ohq"] = []

        def flush():
            while ohq:
                t, oh = ohq.pop(0)
                P.mm(acc0, acc0[0:6, :], vals[:, t, e, :], oh[:, 0:512], t == 0, t == TL - 1, [vals, oh])
                P.mm(acc1, acc1[0:6, 0:SMAX - 512], vals[:, t, e, :], oh[:, 512:SMAX], t == 0, t == TL - 1, [vals, oh])
        st["flush"] = flush
        for t in range(TL):
            oh = P.nxt("oh")
            P.ts("dve", [oh], oh[:], [io_f, pos, mask], io_f[:], pos[:, t, e:e + 1], mask[:, t, e:e + 1], ALU.is_equal, ALU.mult)
            ohq.append((t, oh))
            yield "oh"
        flush()
        siT = P.nxt("siT")
        P.cp("act", [siT], siT[:, 0:512], [acc0], acc0[0:6, :])
        P.cp("act", [siT], siT[:, 512:SMAX], [acc1], acc1[0:6, 0:SMAX - 512])
        if has_ctx:
            accc = P.nxt("ps")
            for t in range(TL, T):
                oh = P.nxt("oh")
                P.ts("dve", [oh], oh[:, 0:CAP_C], [io_f, pos, mask], io_f[:, 0:CAP_C], pos[:, t, e:e + 1], mask[:, t, e:e + 1],
                     ALU.is_equal, ALU.mult)
                P.mm(accc, accc[0:6, 0:CAP_C], vals[:, t, e, :], oh[:, 0:CAP_C], t == TL, t == T - 1, [vals, oh])
            P.cp("act", [siT], siT[:, SMAX:SMAX + CAP_C], [accc], accc[0:6, 0:CAP_C])
        sps = P.nxt("ps")
        for s_ in range(NST):
            ns = 128 if s_ < NS else CAP_C
            P.mm(sps, sps[0:ns, s_ * 6:(s_ + 1) * 6], siT[:, s_ * 128:s_ * 128 + ns], P.idf[0:6, 0:6], True, True, [siT, P.idf])
        si = P.nxt("si")
        P.memset("dve", [si], si[:], 0.0)
        P.cp("dve", [si], si[:, 0:NS, :], [sps], sps[:, 0:NS * 6].rearrange("p (s k) -> p s k", k=6))
        if has_ctx:
            P.cp("dve", [si], si[0:CAP_C, NS, :], [sps], sps[0:CAP_C, NS * 6:NS * 6 + 6])
        sx = P.nxt("sx")
        P.stt("dve", [sx], sx[:, :, 0], [si], si[:, :, 0], 128.0, si[:, :, 1], ALU.mult, ALU.add)
        P.tt("dve", [sx], sx[:, :, 1], [si], si[:, :, 2], si[:, :, 3], ALU.add)
        P.tt("dve", [sx], sx[:, :, 1], [sx, si], sx[:, :, 1], si[:, :, 4], ALU.add)
        P.ts("dve", [sx], sx[:, :, 2], [si], si[:, :, 5], -1.0, 1.0, ALU.mult, ALU.add)
        P.stt("dve", [sx], sx[:, :, 3], [sx, ip_f], sx[:, :, 2], ip_f[:, 1:2], sx[:, :, 0], ALU.mult, ALU.add)
        ixg = P.nxt("ixg")
        P.cp("dve", [ixg], ixg[:], [sx], sx[:, :, 0])
        ixs = P.nxt("ixs")
        P.cp("dve", [ixs], ixs[:], [sx], sx[:, :, 3])
        st["sx"], st["ixs"], st["ixg"], st["xgs"] = sx, ixs, ixg, [None] * NST
        yield

    def issue_gather(st, s_):
        xg = P.nxt("xg")
        ixg = st["ixg"]
        P.op("pool", lambda en: en.indirect_dma_start(
            out=xg[:], out_offset=None, in_=h2.t, in_offset=bass.IndirectOffsetOnAxis(ap=ixg[:, s_:s_ + 1], axis=0)),
            reads=[ixg, h2], writes=[xg], dma=True)
        st["xgs"][s_] = xg

    def stage1b(st):
        xgT = P.nxt("xgT")
        for s_ in range(NST):
            ns = 128 if s_ < NS else CAP_C
            xg = st["xgs"][s_]
            tp = P.nxt("tp")
            for k in range(8):
                P.tr(tp, tp[:, k, :], xg[:, k * 128:(k + 1) * 128], P.idb[:], [xg, P.idb])
            P.cp("act", [xgT], xgT[:, :, s_ * 128:s_ * 128 + ns], [tp], tp[:, :, 0:ns])
        st["xgT"] = xgT

    def stage2(e, st, wgb, wub, wdb, gen, st_next, prev_sc):
        xgT, sx, ixs = st["xgT"], st["sx"], st["ixs"]
        hidT = P.nxt("hidT")
        wdp = st.get("wd")
        gi = -1
        for fc in range(8):
            for (c0, cn) in ((0, 512), (512, n2)):
                gi += 1
                if wdp is not None:
                    if gi == 1:
                        wd_cast(wdb, 0, wdp[0])
                        wdp.append(wd_dma(e, 2))
                    elif gi == 3:
                        wd_cast(wdb, 1, wdp[1])
                        wdp.append(wd_dma(e, 3))
                    elif gi == 6:
                        wd_cast(wdb, 2, wdp[2])
                    elif gi == 9:
                        wd_cast(wdb, 3, wdp[3])
                if gen is not None and not st_next.get("ohdone"):
                    for _ in range(3):
                        if next(gen, "end") != "oh":
                            st_next["ohdone"] = True
                            break
                gp = P.nxt("hb")
                up = P.nxt("hb")
                for (dst, wb) in ((gp, wgb), (up, wub)):
                    for k in range(8):
                        P.mm(dst, dst[:, 0:cn], wb[:, k, fc * 128:(fc + 1) * 128], xgT[:, k, c0:c0 + cn], k == 0, k == 7, [wb, xgT])
                if gen is not None and "flush" in st_next:
                    st_next["flush"]()
                sil = P.nxt("sil")
                P.act([sil], sil[:, 0:cn], [gp], gp[:, 0:cn], AF.Silu)
                P.tt("dve", [hidT], hidT[:, fc, c0:c0 + cn], [sil, up], sil[:, 0:cn], up[:, 0:cn], ALU.mult)
        if gen is not None:
            for _ in gen:
                pass
        my_sc = []
        for s_ in range(NST):
            ns = 128 if s_ < NS else CAP_C
            ye = P.nxt("ye")
            g2r = g2rep[0] if s_ < NS else g2rep[1]
            for hh in range(2):
                yp = P.nxt("hb")
                for fc in range(8):
                    P.mm(yp, yp[0:ns, :], hidT[:, fc, s_ * 128:s_ * 128 + ns], wdb[:, fc, hh * 512:(hh + 1) * 512], fc == 0, fc == 7,
                         [hidT, wdb])
                P.stt("dve", [ye], ye[0:ns, hh * 512:(hh + 1) * 512], [yp, sx, g2r], yp[0:ns, :], sx[0:ns, s_, 1:2],
                      g2r[0:ns, hh * 512:(hh + 1) * 512], ALU.mult, ALU.mult)
            o_ = P.op("pool", lambda en, ye=ye, ixs=ixs, s_=s_: en.indirect_dma_start(
                out=xacc.t, out_offset=bass.IndirectOffsetOnAxis(ap=ixs[:, s_:s_ + 1], axis=0), in_=ye[:, :], in_offset=None,
                compute_op=ALU.add), reads=[ixs, ye], writes=[], dma=True, extra=prev_sc)
            my_sc.append(o_)
            if st_next is not None:
                issue_gather(st_next, s_)
        return my_sc

    st = {}
    for _ in stage1a(0, st):
        st["flush"]()
    for s_ in range(NST):
        issue_gather(st, s_)
    stage1b(st)
    prev_sc = [zt_op]
    for e in range(NE):
        wgb, wub, wdb = w_next
        st_next = None
        gen = None
        if e + 1 < NE:
            wg_n, wu_n = load_one("wgb", wg, e + 1), load_one("wub", wu, e + 1)
            st_next = {}
            gen = stage1a(e + 1, st_next)
        prev_sc = stage2(e, st, wgb, wub, wdb, gen, st_next, prev_sc)
        if e + 1 < NE:
            w_next = (wg_n, wu_n, P.nxt("wdb"))
            st_next["wd"] = [wd_dma(e + 1, 0), wd_dma(e + 1, 1)]
            stage1b(st_next)
        st = st_next

    if not has_ctx:
        fg = P.sb("fg", [128, D], F32)
        P.dma("sp", [fg], fg[:], [], fing.t)
        P.ring("junk", [128, D], BF16, 1)
        P.ring("ssq", [128, 1], F32, 2)
        P.ring("rs", [128, 1], F32, 2)
        for t in range(T):
            xt = P.nxt("xt")
            P.op("sp", lambda e, xt=xt, t=t: e.dma_start(out=xt[:], in_=xacc[t * 128:(t + 1) * 128, :]), reads=[xacc], writes=[xt],
                 dma=True, extra=prev_sc)
            junk = P.nxt("junk")
            ssq = P.nxt("ssq")
            P.act([junk, ssq], junk[:], [xt], xt[:], AF.Square, accum=ssq[:, 0:1])
            rs = P.nxt("rs")
            P.rstd(rs, rs[:, 0:1], ssq, ssq[:, 0:1], 1, 1.0 / D)
            P.stt("dve", [xt], xt[:], [xt, rs, fg], xt[:], rs[:, 0:1], fg[:], ALU.mult, ALU.mult)
            P.dma("sp", [outp], outp[t * 128:(t + 1) * 128, :], [xt], xt[:])
    P.phase_end()


def phase_qkv1(P, T):
    P.phase_begin("qkv1")
    xrows, ccols, modw, modb, n1g, wqkv, wqks, rope = T["xacc0"], T["ccols"], T["modw1"], T["modb1"], T["n1g1"], T["wqkv"], T["wqks"], T["rope1"]
    Q1T, K1T, V1, Kb, Vb = T["Q1T"], T["K1T"], T["V1"], T["Kb"], T["Vb"]

    P.make_ident()
    P.ring("ps", [128, 512], F32, 6, psum=True)
    P.ring("tp", [128, 8, 128], BF16, 2, psum=True)
    P.ring("xt", [128, D], F32, 2)
    P.ring("junk", [128, D], BF16, 1)
    P.ring("ssq", [128, 1], F32, 2)
    P.ring("rs", [128, 1], F32, 2)
    P.ring("rstd_tmp", [128, 512], F32, 1)
    P.ring("h32", [128, D], F32, 1)
    P.ring("hb", [128, D], BF16, 2)
    mods = compute_mod(P, ccols.t, modw.t, modb.t, 0, 2048, ["sh1", "sc1"])
    g1 = P.sb("g1", [128, D], F32)
    P.dma("sp", [g1], g1[:], [], n1g.t)
    AB = []
    for w in range(2):
        A = mods["sc1"][w]
        P.stt("dve", [A], A[:], [A, g1], A[:], 1.0, g1[:], ALU.add, ALU.mult)
        AB.append((A, mods["sh1"][w]))
    wq_b = load_bf16(P, "wq_b", wqkv.t.rearrange("(k p) n -> p k n", p=128), [128, 8, 1536])
    ws_b = load_bf16(P, "ws_b", wqks.t.rearrange("(k p) n -> p k n", p=128), [128, 8, 1280])
    P.ring("hT", [128, 8, 512], BF16, 2)
    P.ring("rp", [128, 2, 512], F32, 2)
    P.ring("t1", [128, 512], F32, 2)
    P.ring("t2", [128, 512], F32, 2)
    P.ring("qo", [128, 512], BF16, 3)
    P.ring("vo", [128, 256], BF16, 3)

    for blk in range(NT // 512 + 1):
        tok0 = blk * 512
        n = min(512, NT - tok0)
        if n <= 0:
            break
        is_ctx = tok0 >= NL
        hT = P.nxt("hT")
        for t in range(n // 128):
            tp = tile_T(P, xrows, tok0 + t * 128, AB[1 if is_ctx else 0][0], AB[1 if is_ctx else 0][1])
            P.cp("act", [hT], hT[:, :, t * 128:(t + 1) * 128], [tp], tp[:])
        if not is_ctx:
            rp = P.nxt("rp")
            P.dma("pool", [rp], rp[:], [], rope[:, :, tok0:tok0 + 512])
        for c in (range(10) if not is_ctx else (8, 9)):
            ps = P.nxt("ps")
            for k in range(8):
                P.mm(ps, ps[:, 0:n], wq_b[:, k, c * 128:(c + 1) * 128], hT[:, k, 0:n], k == 0, k == 7, [wq_b, hT])
            qo = P.nxt("qo")
            if is_ctx:
                P.cp("act", [qo], qo[:, 0:n], [ps], ps[:, 0:n])
            else:
                pss = P.nxt("ps")
                for k in range(8):
                    P.mm(pss, pss[:, 0:n], ws_b[:, k, c * 128:(c + 1) * 128], hT[:, k, 0:n], k == 0, k == 7, [ws_b, hT])
                t1 = P.nxt("t1")
                P.tt("dve", [t1], t1[:, 0:n], [ps, rp], ps[:, 0:n], rp[:, 0, 0:n], ALU.mult)
                t2 = P.nxt("t2")
                P.tt("dve", [t2], t2[:, 0:n], [pss, rp], pss[:, 0:n], rp[:, 1, 0:n], ALU.mult)
                P.tt("dve", [qo], qo[:, 0:n], [t1, t2], t1[:, 0:n], t2[:, 0:n], ALU.add)
            if c < 8:
                P.dma("sp", [Q1T], Q1T[c * 128:(c + 1) * 128, tok0:tok0 + n], [qo], qo[:, 0:n])
            else:
                P.dma("sp", [K1T], K1T[(c - 8) * 128:(c - 7) * 128, tok0:tok0 + n], [qo], qo[:, 0:n])
                if tok0 == 0:
                    P.dma("sp", [Kb], Kb[(c - 8) * 128:(c - 7) * 128, 0:128], [qo], qo[:, 0:128])
                if tok0 == NL - 512:
                    P.dma("sp", [Kb], Kb[(c - 8) * 128:(c - 7) * 128, 128:256], [qo], qo[:, 384:512])
        for t in range(n // 128):
            psv = P.nxt("ps")
            for k in range(8):
                P.mm(psv, psv[:, 0:256], hT[:, k, t * 128:(t + 1) * 128], wq_b[:, k, 1280:1536], k == 0, k == 7, [hT, wq_b])
            vo = P.nxt("vo")
            P.cp("act", [vo], vo[:], [psv], psv[:, 0:256])
            P.dma("sp", [V1], V1[tok0 + t * 128:tok0 + (t + 1) * 128, :], [vo], vo[:])
            if tok0 + t * 128 == 0:
                P.dma("sp", [Vb], Vb[0:128, :], [vo], vo[:])
            if tok0 + t * 128 == NL - 128:
                P.dma("sp", [Vb], Vb[128:256, :], [vo], vo[:])
    P.phase_end()


NB1 = NL // 128
NKL = NL + 256


def phase_attn1(P, T):
    P.phase_begin("attn1")
    Q1T, K1T, V1, Kb_all, Vb_all, masks, sinkr, selh, catT1 = (T[k] for k in ("Q1T", "K1T", "V1", "Kb_all", "Vb_all", "masks", "sinkr", "selh", "catT1"))
    scale = 64.0 ** -0.5

    P.ring("s1b", [128, 512], F32, 5, psum=True)
    P.ring("ops", [128, 512], F32, 2, psum=True)
    P.ring("bps", [128, 512], F32, 1, psum=True)
    P.ring("p1", [128, 512], BF16, 10)
    P.ring("rl", [1, 512], F32, 3)
    P.ring("lsum", [1, 512], F32, 4)
    P.ring("rhl", [1, 2, 512], BF16, 3)
    onesb16 = P.sb("onesb16", [1, 128], BF16)
    P.memset("dve", [onesb16], onesb16[:], 1.0)

    P.ring("bsb", [65, 512], F32, 2)
    P.ring("ao", [65, 512], BF16, 3)
    P.ring("qg", [64, NB1 * 512], BF16, 2)
    P.ring("kl", [64, NKL], BF16, 2)
    P.ring("kc", [64, NCX], BF16, 2)
    P.ring("vl", [128, NKL // 128, 65], BF16, 2)
    P.ring("vc", [128, 2, 65], BF16, 2)
    ones = P.sb("ones", [1, 128], F32)
    P.memset("dve", [ones], ones[:], 1.0)
    mk = P.sb("mk", [128, 4, 512], BF16)
    P.dma("sp", [mk], mk[:], [], masks.t)
    sk = P.sb("sk", [1, 2048], F32)
    P.dma("sp", [sk], sk[:], [], sinkr.t)
    P.act([sk], sk[:], [sk], sk[:], AF.Exp)
    skb = P.sb("skb", [1, 2, 2048], BF16)
    skr = P.sb("skr", [1, 2048], F32)
    P.cp("dve", [skb], skb[:, 0, :], [sk], sk[:])
    P.tt("dve", [skr], skr[:], [sk, skb], sk[:], skb[:, 0, :], ALU.subtract)
    P.cp("dve", [skb], skb[:, 1, :], [skr], skr[:])
    e0 = P.sb("e0", [1, 65], BF16)
    P.memset("dve", [e0], e0[:], 0.0)
    P.memset("dve", [e0], e0[:, 0:1], 1.0)

    sel = P.sb("sel", [128, 8], F32)
    P.dma("sp", [sel], sel[:], [], selh.t)
    P.ring("kcand", [64, 4, 256], BF16, 2)
    P.ring("vcand", [128, 4, 2, 64], BF16, 2)
    for b_ in P.rings["vl"][0]:
        P.memset("dve", [b_], b_[:, :, 0:1], 1.0)
    for b_ in P.rings["vc"][0]:
        P.memset("dve", [b_], b_[:, :, 0:1], 1.0)
    for g in range(4):
        qg = P.nxt("qg")
        for j in range(4):
            P.dma("sp", [qg], qg[:].rearrange("d (i j r) -> d i j r", j=4, r=128)[:, :, j, :], [Q1T],
                  Q1T[(g * 4 + j) * 64:(g * 4 + j + 1) * 64, :].rearrange("d (i r) -> d i r", r=128))
        kl = P.nxt("kl")
        P.dma("sp", [kl], kl[:, 128:128 + NL], [K1T], K1T[g * 64:(g + 1) * 64, 0:NL])
        kcand = P.nxt("kcand")
        P.dma("sp", [kcand], kcand[:], [Kb_all], Kb_all.t.rearrange("(r c) k -> c r k", r=4)[g * 64:(g + 1) * 64])
        for (dst0, src0, so) in ((0, 128, 0), (128 + NL, 0, 4)):
            P.ts("dve", [kl], kl[:, dst0:dst0 + 128], [kcand, sel], kcand[:, 0, src0:src0 + 128], sel[0:64, so:so + 1], None, ALU.mult)
            for r in range(1, 4):
                P.stt("dve", [kl], kl[:, dst0:dst0 + 128], [kcand, sel, kl], kcand[:, r, src0:src0 + 128], sel[0:64, so + r:so + r + 1],
                      kl[:, dst0:dst0 + 128], ALU.mult, ALU.add)
        kc = P.nxt("kc")
        P.dma("sp", [kc], kc[:], [K1T], K1T[g * 64:(g + 1) * 64, NL:NT])
        vl = P.nxt("vl")
        P.dma("sp", [vl], vl[:, 1:1 + NB1, 1:65], [V1], V1[0:NL, g * 64:(g + 1) * 64].rearrange("(t p) d -> p t d", p=128))
        vcand = P.nxt("vcand")
        P.dma("sp", [vcand], vcand[:], [Vb_all], Vb_all.t.rearrange("(r f p) c -> p r f c", r=4, f=2)[:, :, :, g * 64:(g + 1) * 64])
        for (dt_, f_, so) in ((0, 1, 0), (NB1 + 1, 0, 4)):
            P.ts("dve", [vl], vl[:, dt_, 1:65], [vcand, sel], vcand[:, 0, f_, :], sel[:, so:so + 1], None, ALU.mult)
            for r in range(1, 4):
                P.stt("dve", [vl], vl[:, dt_, 1:65], [vcand, sel, vl], vcand[:, r, f_, :], sel[:, so + r:so + r + 1], vl[:, dt_, 1:65],
                      ALU.mult, ALU.add)
        vc = P.nxt("vc")
        P.dma("sp", [vc], vc[:, :, 1:65], [V1], V1[NL:NT, g * 64:(g + 1) * 64].rearrange("(t p) d -> p t d", p=128))
        def emit_s(i):
            q_ap = qg[:, i * 512:(i + 1) * 512]
            tiles = []
            for (kb, c0) in ((kc, 0), (kc, 128), (kl, i * 128), (kl, (i + 1) * 128), (kl, (i + 2) * 128)):
                sp_ = P.nxt("s1b")
                P.mm(sp_, sp_[:, :], kb[:, c0:c0 + 128], q_ap, True, True, [kb, qg])
                tiles.append(sp_)
            return tiles

        def emit_exp(i, tiles):
            ps_ = []
            for j, sp_ in enumerate(tiles):
                pt = P.nxt("p1")
                P.act([pt], pt[:], [sp_], sp_[:], AF.Exp, scale=scale)
                if j == 2:
                    P.tt("dve", [pt], pt[:], [pt, mk], pt[:], mk[:, 2, :] if i == 0 else mk[:, 0, :], ALU.mult)
                if j == 4:
                    P.tt("dve", [pt], pt[:], [pt, mk], pt[:], mk[:, 3, :] if i == NB1 - 1 else mk[:, 1, :], ALU.mult)
                ps_.append(pt)
            return ps_

        def emit_pv(i, ps_, g=g, vc=vc, vl=vl):
            o_ps = P.nxt("ops")
            vs = (vc[:, 0, :], vc[:, 1, :], vl[:, i, :], vl[:, i + 1, :], vl[:, i + 2, :])
            vb = (vc, vc, vl, vl, vl)
            for j in range(5):
                P.mm(o_ps, o_ps[0:65, :], vs[j], ps_[j][:], j == 0, j == 4, [vb[j], ps_[j]])
            lsum = P.nxt("lsum")
            P.tt("dve", [lsum], lsum[:], [o_ps, sk], o_ps[0:1, :], sk[:, g * 512:(g + 1) * 512], ALU.add)
            return (i, o_ps, lsum)

        def emit_rl(i, o_ps, lsum):
            lnl = P.nxt("lsum")
            P.act([lnl], lnl[:], [lsum], lsum[:], AF.Ln)
            rl = P.nxt("rl")
            P.act([rl], rl[:], [lnl], lnl[:], AF.Exp, scale=-1.0)
            rh = P.nxt("rhl")
            P.cp("dve", [rh], rh[:, 0, :], [rl], rl[:])
            P.tt("dve", [rl], rl[:], [rl, rh], rl[:], rh[:, 0, :], ALU.subtract)
            P.cp("dve", [rh], rh[:, 1, :], [rl], rl[:])
            return (i, o_ps, rh)

        def emit_fin(i, o_ps, rl, g=g):
            b_ps = P.nxt("bps")
            P.mm(b_ps, b_ps[0:65, :], onesb16[:, 0:65], rl[:, 0, :], True, False, [onesb16, rl])
            P.mm(b_ps, b_ps[0:65, :], onesb16[:, 0:65], rl[:, 1, :], False, True, [onesb16, rl])
            bsb = P.nxt("bsb")
            P.cp("act", [bsb], bsb[:], [b_ps], b_ps[0:65, :])
            ao = P.nxt("ao")
            P.tt("dve", [ao], ao[:], [o_ps, bsb], o_ps[0:65, :], bsb[:], ALU.mult)
            for j in range(4):
                P.dma("sp", [catT1], catT1[(g * 4 + j) * 64:(g * 4 + j + 1) * 64, i * 128:(i + 1) * 128], [ao], ao[1:65, j * 128:(j + 1) * 128])

        prev = None
        pfin = None
        for i in range(NB1 + 1):
            tiles = emit_s(i) if i < NB1 else None
            o_ = emit_pv(*prev) if prev is not None else None
            pexp = emit_exp(i, tiles) if i < NB1 else None
            if o_ is not None:
                nfin = emit_rl(*o_)
                if pfin is not None:
                    emit_fin(*pfin)
                pfin = nfin
            prev = (i, pexp) if i < NB1 else None
        emit_fin(*pfin)
    P.phase_end()


GRP = [[0, 1, 2, 3], [4, 5, 6, 7]]


def build_fused():
    ctx = ExitStack()
    nc, P = new_prog(ctx)
    T = {}

    def di(n, s, dt=F32):
        T[n] = P.dram(n, s, dt, "ExternalInput")

    def dn(n, s, dt=F32):
        T[n] = P.dram(n, s, dt)
        T[n].relaxed = not n.startswith("xacc")

    di("xrows", [NT, D]); di("xhalo", [128, D]); di("hmask", [128, 2]); di("ccols", [128, 16])
    for l in (0, 1):
        di("modw%d" % l, [D, 6144]); di("modb%d" % l, [1, 6144]); di("n1g%d" % l, [128, D]); di("n2g%d" % l, [128, D])
        di("w_out%d" % l, [D, D]); di("rw%d" % l, [D, NE])
        di("wg%d" % l, [NE, D, D]); di("wu%d" % l, [NE, D, D]); di("wd%d" % l, [NE, D, D])
    di("fing", [128, D])
    di("w_in", [D, 1984]); di("convp", [128, 16]); di("qg", [128, 3]); di("w_uq", [256, 768]); di("w_uqs", [256, 768])
    di("w_ukv", [128, 1024]); di("ropeq", [96, 2, NL]); di("ropek", [32, 2, NL])
    di("wqkv", [D, 1536]); di("wqks", [D, 1280]); di("rope1", [128, 2, NL])
    di("masks", [128, 4, 512], BF16); di("sinkr", [1, 2048]); di("selh", [128, 8])
    T["out"] = P.dram("out", [NL, D], F32, "ExternalOutput")
    dn("QT", [8, 96, NT], BF16); dn("KTn", [512, NT], BF16); dn("KTr", [32, NT], BF16); dn("Vt", [NT, 512], BF16)
    dn("convT", [512, NT], BF16)
    for h_ in range(8):
        dn("KTn_all%d" % h_, [4 * 64, NT], BF16)
    dn("KTr_all", [4 * 32, NT], BF16); dn("Vt_all", [4, 4 * 1024, 512], BF16)
    dn("attT", [512, NT], BF16)
    dn("xacc0", [NT + 128, D]); dn("h2_0", [NT, D], BF16); dn("aff_0", [NT, NE]); dn("affl_0", [NL, NE]); dn("affall_0", [4 * NL, NE])
    dn("Q1T", [1024, NL], BF16); dn("K1T", [256, NT], BF16); dn("V1", [NT, 256], BF16)
    dn("Kb", [256, 256], BF16); dn("Vb", [256, 256], BF16); dn("Kb_all", [1024, 256], BF16); dn("Vb_all", [1024, 256], BF16)
    dn("catT1", [1024, NL], BF16)
    dn("xacc1", [NL + 128, D]); dn("h2_1", [NL, D], BF16); dn("aff_1", [NL, NE]); dn("affall_1", [4 * NL, NE])
    T["affl_1"] = T["aff_1"]
    T["xres0"] = T["xrows"]
    T["xres1"] = T["xacc0"]

    phase_A(P, T)
    P.cc_allgather(T["KTr"], T["KTr_all"], GRP)
    for k in range(4):
        P.cc_allgather(T["Vt"], T["Vt_all"], GRP, T["Vt"].t[k * 1024:(k + 1) * 1024, :], T["Vt_all"].t[k])
    for h in range(8):
        P.cc_allgather(T["KTn"], T["KTn_all%d" % h], GRP, T["KTn"].t[h * 64:(h + 1) * 64, :], T["KTn_all%d" % h].t)
    phase_B(P, T)
    phase_post(P, T, 0, NT, [(T["convT"], 4), (T["attT"], 4)])
    P.cc_allgather(T["affl_0"], T["affall_0"], GRP)
    phase_moe(P, T, 0)
    phase_qkv1(P, T)
    P.cc_allgather(T["Kb"], T["Kb_all"], GRP)
    P.cc_allgather(T["Vb"], T["Vb_all"], GRP)
    phase_attn1(P, T)
    phase_post(P, T, 1, NL, [(T["catT1"], 8)])
    P.cc_allgather(T["aff_1"], T["affall_1"], GRP)
    phase_moe(P, T, 1)
    n = P.finalize()
    return nc, ctx, n


def prep_all(inp):
    bf = ml_dtypes.bfloat16
    maps = prep_A(inp)
    w = inp["swa_w_qkv"][0]
    p64 = _swap_perm(64)
    ws = np.empty((D, 1280), np.float32)
    for h in range(20):
        ws[:, h * 64:(h + 1) * 64] = w[:, h * 64 + p64]
    r = np.arange(128)
    tri_prev = (r[:, None] >= r[None, :]).astype(np.float32)
    tri_next = (r[:, None] <= r[None, :]).astype(np.float32)
    sinkr = np.ascontiguousarray(np.repeat(inp["swa_sink"][0], 128)[None, :].astype(np.float32))
    shared = {}
    for l in (0, 1):
        shared["modw%d" % l] = np.ascontiguousarray(inp["mod_w"][l])
        shared["modb%d" % l] = np.ascontiguousarray(inp["mod_b"][l][None, :])
        shared["n1g%d" % l] = _rep(inp["norm1_g"][l])
        shared["n2g%d" % l] = _rep(inp["norm2_g"][l])
        shared["rw%d" % l] = np.ascontiguousarray(inp["router_w"][l])
        shared["wg%d" % l] = np.ascontiguousarray(inp["exp_w_gate"][l])
        shared["wu%d" % l] = np.ascontiguousarray(inp["exp_w_up"][l])
        shared["wd%d" % l] = np.ascontiguousarray(inp["exp_w_down"][l])
    shared["w_out0"] = np.ascontiguousarray(inp["ab_w_out"][0])
    shared["w_out1"] = np.ascontiguousarray(inp["swa_w_out"][0])
    shared["fing"] = _rep(inp["final_g"])
    shared["wqkv"] = np.ascontiguousarray(w)
    shared["wqks"] = ws
    shared["sinkr"] = sinkr
    out = []
    for core in range(8):
        b, q = core // 4, core % 4
        m = dict(maps[core])
        m["modw0"] = m.pop("modw"); m["modb0"] = m.pop("modb"); m["n1g0"] = m.pop("n1g")
        m.update(shared)
        C, S = _rope_tables(np.arange(q * NL, (q + 1) * NL), 64)
        rp = np.empty((128, 2, NL), np.float32)
        rp[:64, 0] = C; rp[64:, 0] = C; rp[:64, 1] = S; rp[64:, 1] = S
        m["rope1"] = rp
        mk = np.zeros((128, 4, 512), np.float32)
        mk[:, 0] = np.tile(tri_prev, (1, 4))
        mk[:, 1] = np.tile(tri_next, (1, 4))
        mk[:, 2] = mk[:, 0] if q > 0 else 0.0
        mk[:, 3] = mk[:, 1] if q < 3 else 0.0
        m["masks"] = mk.astype(bf)
        sel = np.zeros((128, 8), np.float32)
        if q > 0:
            sel[:, q - 1] = 1.0
        if q < 3:
            sel[:, 4 + q + 1] = 1.0
        m["selh"] = sel
        out.append(m)
    return out


def kernel(**inputs):
    inp = {k: np.asarray(v) for k, v in inputs.items()}
    maps = prep_all(inp)
    nc, ctx, n = build_fused()
    res = run_bass_kernel_spmd(nc, maps, core_ids=list(range(8)))
    ctx.close()
    out = np.empty((2, 4 * NL, D), np.float32)
    for c in range(8):
        out[c // 4, (c % 4) * NL:(c % 4 + 1) * NL] = np.asarray(res.results[c]["out"])
    return out
```

```python
import numpy as np
import ml_dtypes
from contextlib import ExitStack
import concourse.bass as bass
import concourse.mybir as mybir
from concourse.bass_utils import run_bass_kernel_spmd

F32 = mybir.dt.float32
BF16 = mybir.dt.bfloat16
I32 = mybir.dt.int32
ALU = mybir.AluOpType
AF = mybir.ActivationFunctionType
AX = mybir.AxisListType

D = 1024
NL = 4096
NCX = 256
NT = NL + NCX
EPS = 1e-6
NE = 16
ENGS = ["pe", "act", "dve", "pool", "sp"]
NDMA_SEM = 56


class Buf:
    __slots__ = ("name", "last_w", "readers", "t", "relaxed")

    def __init__(self, name, t=None):
        self.name = name
        self.last_w = None
        self.readers = []
        self.t = t
        self.relaxed = False

    def __getitem__(self, k):
        return self.t[k]


class Op:
    __slots__ = ("eng", "fn", "deps", "dma", "idx", "need_inc", "sem", "val", "waits", "cc")


class Prog:
    def __init__(self, nc, ctx):
        self.nc = nc
        self.ctx = ctx
        self.ops = []
        self.rings = {}
        self.phase = "p"
        self.pstack = None

    def phase_begin(self, name):
        self.phase = name
        self.pstack = ExitStack()
        self.rings = {}

    def phase_end(self):
        self.ops.append(None)
        self.pstack.close()
        self.pstack = None

    def sb(self, name, shape, dt):
        t = self.pstack.enter_context(self.nc.sbuf_tensor("%s_%s" % (self.phase, name), shape, dt))
        return Buf(name, t)

    def ps(self, name, shape, dt):
        t = self.pstack.enter_context(self.nc.psum_tensor("%s_%s" % (self.phase, name), shape, dt))
        return Buf(name, t)

    def cc_allgather(self, src, dst, groups, src_ap=None, dst_ap=None):
        src_ap = src.t if src_ap is None else src_ap
        dst_ap = dst.t if dst_ap is None else dst_ap
        o = self.op("pool", lambda e: e.collective_compute("AllGather", ALU.bypass, replica_groups=groups,
                                                           ins=[src_ap], outs=[dst_ap]), reads=[src], writes=[dst])
        o.cc = True
        return o

    def ring(self, name, shape, dt, n, psum=False):
        bufs = [(self.ps if psum else self.sb)("%s%d" % (name, i), shape, dt) for i in range(n)]
        self.rings[name] = [bufs, 0]

    def nxt(self, name):
        r = self.rings[name]
        b = r[0][r[1] % len(r[0])]
        r[1] += 1
        return b

    def dram(self, name, shape, dt, kind=None):
        if kind is None:
            t = self.nc.dram_tensor(name, list(shape), dt)
        else:
            t = self.nc.dram_tensor(name, list(shape), dt, kind=kind)
        return Buf(name, t.ap())

    def op(self, eng, fn, reads=(), writes=(), dma=False, extra=()):
        o = Op()
        o.eng = eng
        o.fn = fn
        o.dma = dma
        o.idx = len(self.ops)
        deps = set()
        for b in reads:
            if b.last_w is not None:
                deps.add(b.last_w)
        for b in writes:
            if b.relaxed:
                continue
            if b.last_w is not None:
                deps.add(b.last_w)
            for r in b.readers:
                deps.add(r)
        for x in extra:
            deps.add(x.idx)
        deps.discard(o.idx)
        o.deps = deps
        for b in reads:
            b.readers.append(o.idx)
        for b in writes:
            b.last_w = o.idx
            b.readers = []
        o.need_inc = False
        o.cc = False
        self.ops.append(o)
        return o

    def finalize(self):
        nc = self.nc
        ctx = self.ctx
        ops = self.ops

        def pe_pe(p, o):
            return p.eng == "pe" and o.eng == "pe" and not p.dma and not o.dma

        last = {}
        for o in ops:
            if o is None:
                for e_, lo in last.items():
                    lo.need_inc = True
                continue
            last[o.eng] = o
            for d in o.deps:
                if not pe_pe(ops[d], o):
                    ops[d].need_inc = True
            if o.dma or o.cc:
                o.need_inc = True
        eng_sem = {e: ctx.enter_context(nc.semaphore("s_" + e)) for e in ENGS}
        dma_sems = [ctx.enter_context(nc.semaphore("d%d" % i)) for i in range(NDMA_SEM)]
        cc_sem = ctx.enter_context(nc.semaphore("cc_sem"))
        cnt = {e: 0 for e in ENGS}
        dcnt = [0] * NDMA_SEM
        ccnt = 0
        dma_rr = 0
        seen = {e: {} for e in ENGS}
        pending = {e: None for e in ENGS}
        for o in ops:
            if o is None:
                snap = [(("e", e), eng_sem[e], cnt[e]) for e in ENGS if cnt[e] > 0]
                snap += [(("d", k), dma_sems[k], dcnt[k]) for k in range(NDMA_SEM) if dcnt[k] > 0]
                if ccnt > 0:
                    snap.append((("cc",), cc_sem, ccnt))
                for e in ENGS:
                    pending[e] = snap
                continue
            waits = []
            s = seen[o.eng]
            if pending[o.eng] is not None:
                for key, sem, val in pending[o.eng]:
                    if s.get(key, 0) < val:
                        s[key] = val
                        waits.append((sem, val))
                pending[o.eng] = None
            for d in sorted(o.deps):
                p = ops[d]
                if pe_pe(p, o):
                    continue
                key, sem = p.sem
                if s.get(key, 0) < p.val:
                    s[key] = p.val
                    waits.append((sem, p.val))
            if o.cc:
                ccnt += 1
                o.sem = (("cc",), cc_sem)
                o.val = ccnt
            elif o.dma:
                k = dma_rr
                dma_rr = (dma_rr + 1) % NDMA_SEM
                key = ("d", k)
                if dcnt[k] > 0 and s.get(key, 0) < dcnt[k]:
                    s[key] = dcnt[k]
                    waits.append((dma_sems[k], dcnt[k]))
                dcnt[k] += 16
                o.sem = (key, dma_sems[k])
                o.val = dcnt[k]
            elif o.need_inc:
                cnt[o.eng] += 1
                o.sem = (("e", o.eng), eng_sem[o.eng])
                o.val = cnt[o.eng]
            o.waits = waits
        final_waits = [(dma_sems[k], dcnt[k]) for k in range(NDMA_SEM) if dcnt[k] > 0]
        final_waits += [(eng_sem[e], cnt[e]) for e in ENGS if cnt[e] > 0]
        if ccnt > 0:
            final_waits.append((cc_sem, ccnt))
        per = {e: [o for o in ops if o is not None and o.eng == e] for e in ENGS}
        engmap = {"pe": "tensor", "act": "scalar", "dve": "vector", "pool": "gpsimd", "sp": "sync"}
        with nc.Block() as block:
            for e in ENGS:
                def body(eng, lst=per[e], final=(e == "sp")):
                    for o in lst:
                        for (sem, val) in o.waits:
                            eng.wait_ge(sem, val)
                        ins = o.fn(eng)
                        if o.cc:
                            ins.then_inc(o.sem[1])
                        elif o.need_inc:
                            ins.then_inc(o.sem[1], 16 if o.dma else 1)
                    if final:
                        for (sem, val) in final_waits:
                            eng.wait_ge(sem, val)
                getattr(block, engmap[e])(body)
        return len(per["pe"]) + len(per["act"]) + len(per["dve"]) + len(per["pool"]) + len(per["sp"])

    def mm(self, ps, out, lhsT, rhs, start, stop, rd):
        self.op("pe", lambda e: e.matmul(out, lhsT=lhsT, rhs=rhs, start=start, stop=stop), reads=rd, writes=[ps])

    def tr(self, ps, out, in_, ident, rd):
        self.op("pe", lambda e: e.transpose(out, in_, ident), reads=rd, writes=[ps])

    def act(self, wr, out, rd, in_, func, scale=1.0, bias=None, accum=None):
        kw = {}
        if bias is not None:
            kw["bias"] = bias
        if accum is not None:
            kw["accum_out"] = accum
        self.op("act", lambda e: e.activation(out=out, in_=in_, func=func, scale=scale, **kw), reads=rd, writes=wr)

    def ts(self, eng, wr, out, rd, in0, s1, s2, op0, op1=None, accum=None):
        kw = {}
        if op1 is not None:
            kw["op1"] = op1
        if accum is not None:
            kw["accum_out"] = accum
        self.op(eng, lambda e: e.tensor_scalar(out, in0, s1, s2, op0, **kw), reads=rd, writes=wr)

    def tt(self, eng, wr, out, rd, in0, in1, op):
        self.op(eng, lambda e: e.tensor_tensor(out, in0, in1, op), reads=rd, writes=wr)

    def stt(self, eng, wr, out, rd, in0, scalar, in1, op0, op1):
        self.op(eng, lambda e: e.scalar_tensor_tensor(out, in0, scalar, in1, op0, op1), reads=rd, writes=wr)

    def cp(self, eng, wr, out, rd, in_):
        if eng == "act":
            self.op("act", lambda e: e.copy(out, in_), reads=rd, writes=wr)
        else:
            self.op(eng, lambda e: e.tensor_copy(out, in_), reads=rd, writes=wr)

    def memset(self, eng, wr, out, val):
        self.op(eng, lambda e: e.memset(out, val), writes=wr)

    def dma(self, q, wr, out, rd, in_):
        self.op(q, lambda e: e.dma_start(out=out, in_=in_), reads=rd, writes=wr, dma=True)

    def red(self, eng, wr, out, rd, in_, op, axis=AX.X):
        self.op(eng, lambda e: e.tensor_reduce(out, in_, axis, op), reads=rd, writes=wr)

    def make_ident(self):
        idf = self.sb("idf", [128, 128], F32)
        idb = self.sb("idb", [128, 128], BF16)
        self.memset("pool", [idf], idf[:], 1.0)
        self.op("pool", lambda e: e.affine_select(idf[:], idf[:], [[-1, 128]], ALU.is_equal, 0.0, base=0,
                                                 channel_multiplier=1), reads=[idf], writes=[idf])
        self.cp("dve", [idb], idb[:], [idf], idf[:])
        self.idf, self.idb = idf, idb
        eb = self.sb("epsb", [128, 1], F32)
        self.memset("dve", [eb], eb[:], EPS)
        self.epsb = eb

    def rstd(self, out_buf, out, ssq_buf, ssq, n, scale):
        tmp = self.nxt("rstd_tmp")
        self.act([tmp], tmp[:, 0:n], [ssq_buf, self.epsb], ssq, AF.Ln, scale=scale, bias=self.epsb[:, 0:1])
        self.act([out_buf], out, [tmp], tmp[:, 0:n], AF.Exp, scale=-0.5)


def new_prog(ctx):
    nc = bass.Bass("TRN2", target_bir_lowering=False)
    P = Prog(nc, ctx)
    return nc, P


def load_bf16(P, name, src_ap, shape):
    b = P.sb(name, shape, BF16)
    P.dma("pool", [b], b[:], [], src_ap)
    return b


def tile_T(P, xrows, row0, A, Bm):
    xt = P.nxt("xt")
    P.dma("pool", [xt], xt[:], [xrows], xrows[row0:row0 + 128, :])
    junk = P.nxt("junk")
    ssq = P.nxt("ssq")
    P.act([junk, ssq], junk[:], [xt], xt[:], AF.Square, accum=ssq[:, 0:1])
    rs = P.nxt("rs")
    P.rstd(rs, rs[:, 0:1], ssq, ssq[:, 0:1], 1, 1.0 / D)
    h32 = P.nxt("h32")
    P.stt("dve", [h32], h32[:], [xt, rs, A], xt[:], rs[:, 0:1], A[:], ALU.mult, ALU.mult)
    hb = P.nxt("hb")
    P.tt("dve", [hb], hb[:], [h32, Bm], h32[:], Bm[:], ALU.add)
    tp = P.nxt("tp")
    for k in range(8):
        P.tr(tp, tp[:, k, :], hb[:, k * 128:(k + 1) * 128], P.idb[:], [hb, P.idb])
    return tp


def compute_mod(P, ccols_ap, modw_ap, modb_ap, col0, ncols, names):
    nseg = ncols // 1024
    cc = P.sb("cc", [128, 16], F32)
    P.dma("sp", [cc], cc[:], [], ccols_ap)
    sc = P.sb("sc", [128, 16], F32)
    P.act([sc], sc[:], [cc], cc[:], AF.Silu)
    ones2 = P.sb("ones2", [1, 2], F32)
    P.memset("dve", [ones2], ones2[:], 1.0)
    mb = P.sb("mb", [1, ncols], F32)
    P.dma("sp", [mb], mb[:], [], modb_ap[0:1, col0:col0 + ncols])
    modrows = P.sb("modrows", [2, ncols], F32)
    MWC = 128
    P.ring("mw", [128, 8, MWC], F32, 2)
    for j in range(ncols // MWC):
        mw = P.nxt("mw")
        P.dma("sp", [mw], mw[:], [], modw_ap[:, col0 + j * MWC: col0 + (j + 1) * MWC].rearrange("(k p) n -> p k n", p=128))
        ps = P.nxt("ps")
        for k in range(8):
            P.mm(ps, ps[0:2, 0:MWC], sc[:, 2 * k:2 * k + 2], mw[:, k, :], k == 0, False, [sc, mw])
        P.mm(ps, ps[0:2, 0:MWC], ones2[:], mb[:, j * MWC:(j + 1) * MWC], False, True, [ones2, mb])
        P.cp("dve", [modrows], modrows[:, j * MWC:(j + 1) * MWC], [ps], ps[0:2, 0:MWC])
    sel = P.sb("sel", [2, 2, 128], F32)
    P.memset("dve", [sel], sel[:], 0.0)
    P.memset("dve", [sel], sel[0:1, 0, :], 1.0)
    P.ts("dve", [sel], sel[:, 1, :], [sel], sel[:, 0, :], -1.0, 1.0, ALU.mult, ALU.add)
    out = {}
    for si, nm in enumerate(names):
        reps = []
        for w in range(2):
            rep = P.sb("mod_%s_%d" % (nm, w), [128, 1024], F32)
            for hh in range(2):
                ps = P.nxt("ps")
                P.mm(ps, ps[:, :], sel[:, w, :], modrows[:, si * 1024 + hh * 512: si * 1024 + (hh + 1) * 512], True, True,
                     [sel, modrows])
                P.cp("act", [rep], rep[:, hh * 512:(hh + 1) * 512], [ps], ps[:, :])
            reps.append(rep)
        out[nm] = reps
    return out


def phase_A(P, T):
    P.phase_begin("A")
    xrows, xhalo, hmask, ccols, modw, modb, n1g = T["xrows"], T["xhalo"], T["hmask"], T["ccols"], T["modw0"], T["modb0"], T["n1g0"]
    w_in, convp, qg, w_uq, w_uqs, w_ukv, ropeq, ropek = (T[k] for k in ("w_in", "convp", "qg", "w_uq", "w_uqs", "w_ukv", "ropeq", "ropek"))
    QT, KTn, KTr, Vt, convT = T["QT"], T["KTn"], T["KTr"], T["Vt"], T["convT"]

    P.make_ident()
    P.ring("ps", [128, 512], F32, 6, psum=True)
    P.ring("tp", [128, 8, 128], BF16, 2, psum=True)
    P.ring("xt", [128, D], F32, 2)
    P.ring("junk", [128, D], BF16, 1)
    P.ring("ssq", [128, 1], F32, 2)
    P.ring("rs", [128, 1], F32, 2)
    P.ring("rstd_tmp", [128, 512], F32, 1)
    P.ring("h32", [128, D], F32, 1)
    P.ring("hb", [128, D], BF16, 2)

    mods = compute_mod(P, ccols.t, modw.t, modb.t, 0, 2048, ["sh1", "sc1"])
    g1 = P.sb("g1", [128, D], F32)
    P.dma("sp", [g1], g1[:], [], n1g.t)
    AB = []
    for w in range(2):
        A = mods["sc1"][w]
        P.stt("dve", [A], A[:], [A, g1], A[:], 1.0, g1[:], ALU.add, ALU.mult)
        AB.append((A, mods["sh1"][w]))

    w_in_b = load_bf16(P, "w_in_b", w_in.t.rearrange("(k p) n -> p k n", p=128), [128, 8, 1984])
    w_uq_b = load_bf16(P, "w_uq_b", w_uq.t.rearrange("(k p) n -> p k n", p=128), [128, 2, 768])
    w_uqs_b = load_bf16(P, "w_uqs_b", w_uqs.t.rearrange("(k p) n -> p k n", p=128), [128, 2, 768])
    w_ukv_b = load_bf16(P, "w_ukv_b", w_ukv.t, [128, 1024])
    cvp = P.sb("cvp", [128, 16], F32)
    P.dma("sp", [cvp], cvp[:], [], convp.t)
    qgs = P.sb("qgs", [128, 3], F32)
    P.dma("sp", [qgs], qgs[:], [], qg.t)
    hm = P.sb("hm", [128, 2], F32)
    P.dma("sp", [hm], hm[:], [], hmask.t)
    P.ring("rq", [96, 2, 512], F32, 1)
    P.ring("rk", [32, 2, 512], F32, 1)
    onesb = P.sb("onesb", [128, 128], BF16)
    P.memset("dve", [onesb], onesb[:], 1.0)

    P.ring("hseg", [128, 8, 1022], BF16, 2)
    halo_tmp = P.sb("halo_tmp", [128, 8, 2], BF16)
    tph = tile_T(P, xhalo, 0, AB[0][0], AB[0][1])
    P.cp("act", [halo_tmp], halo_tmp[:], [tph], tph[:, :, 0:2])

    def fill_seg(hseg, G0, G1):
        t_lo = max(0, (G0 - 1) // 128)
        t_hi = min(NL // 128 - 1, (G1 - 2) // 128)
        for t in range(t_lo, t_hi + 1):
            a_ = max(G0, 1 + 128 * t)
            b_ = min(G1, 129 + 128 * t)
            if b_ <= a_:
                continue
            tp = tile_T(P, xrows, t * 128, AB[0][0], AB[0][1])
            P.cp("act", [hseg], hseg[:, :, a_ - G0:b_ - G0], [tp], tp[:, :, a_ - 1 - 128 * t:b_ - 1 - 128 * t])
        if G0 == 0:
            P.cp("dve", [hseg], hseg[:, :, 0:1], [halo_tmp], halo_tmp[:, :, 0:1])
        if G1 == NL + 2:
            P.cp("dve", [hseg], hseg[:, :, NL + 1 - G0:NL + 2 - G0], [halo_tmp], halo_tmp[:, :, 1:2])

    P.ring("gcs", [128, 512], F32, 2)
    P.ring("zw", [128, 512], F32, 2)
    P.ring("ca", [128, 512], F32, 2)
    P.ring("co", [128, 512], BF16, 3)
    P.ring("lat", [128, 512], F32, 3)
    P.ring("sq", [128, 512], BF16, 3)
    P.ring("rstdL", [128, 512], F32, 1)
    P.ring("qn", [128, 2, 512], BF16, 2)
    P.ring("kvn", [128, 512], BF16, 2)
    P.ring("t1", [96, 512], F32, 1)
    P.ring("t2", [96, 512], F32, 1)
    P.ring("qo", [96, 512], BF16, 3)
    P.ring("ko", [128, 512], BF16, 3)
    P.ring("vo", [128, 512], BF16, 3)

    def proj(hT, c0, n, col_lo, col_n):
        ps = P.nxt("ps")
        for k in range(8):
            P.mm(ps, ps[0:col_n, 0:n], w_in_b[:, k, col_lo:col_lo + col_n], hT[:, k, c0:c0 + n], k == 0, k == 7,
                 [w_in_b, hT])
        return ps

    def latent_norm(pss, nchunk, n, gcol0, out_ring):
        lats, sqs = [], []
        for c in range(nchunk):
            lt = P.nxt("lat")
            P.cp("act", [lt], lt[:, 0:n], [pss[c]], pss[c][:, 0:n])
            sq = P.nxt("sq")
            P.act([sq], sq[:, 0:n], [pss[c]], pss[c][:, 0:n], AF.Square)
            lats.append(lt)
            sqs.append(sq)
        ps = P.nxt("ps")
        for c in range(nchunk):
            P.mm(ps, ps[:, 0:n], onesb[:], sqs[c][:, 0:n], c == 0, c == nchunk - 1, [onesb, sqs[c]])
        rl = P.nxt("rstdL")
        P.rstd(rl, rl[:, 0:n], ps, ps[:, 0:n], n, 1.0 / (128 * nchunk))
        ob = P.nxt(out_ring)
        for c in range(nchunk):
            o_ap = ob[:, c, 0:n] if nchunk > 1 else ob[:, 0:n]
            P.stt("dve", [ob], o_ap, [lats[c], qgs, rl], lats[c][:, 0:n], qgs[:, gcol0 + c:gcol0 + c + 1], rl[:, 0:n],
                  ALU.mult, ALU.mult)
        return ob

    def block(hT, c0, n, tok0, is_ctx, first, last):
        no = n - 2
        if not is_ctx:
            rq = P.nxt("rq")
            P.dma("pool", [rq], rq[:, :, 0:no], [], ropeq[:, :, tok0:tok0 + no])
            rk = P.nxt("rk")
            P.dma("pool", [rk], rk[:, :, 0:no], [], ropek[:, :, tok0:tok0 + no])
        for c in range(4):
            ps_gc = proj(hT, c0, n, 512 + c * 128, 128)
            gcs = P.nxt("gcs")
            P.cp("act", [gcs], gcs[:, 0:n], [ps_gc], ps_gc[:, 0:n])
            ps_u = proj(hT, c0, n, 1024 + c * 128, 128)
            zw = P.nxt("zw")
            P.tt("dve", [zw], zw[:, 0:n], [ps_u, gcs], ps_u[:, 0:n], gcs[:, 0:n], ALU.mult)
            if is_ctx:
                P.memset("dve", [zw], zw[:, 0:1], 0.0)
                P.memset("dve", [zw], zw[:, n - 1:n], 0.0)
            else:
                if first:
                    P.ts("dve", [zw], zw[:, 0:1], [zw, hm], zw[:, 0:1], hm[:, 0:1], None, ALU.mult)
                if last:
                    P.ts("dve", [zw], zw[:, n - 1:n], [zw, hm], zw[:, n - 1:n], hm[:, 1:2], None, ALU.mult)
            ca = P.nxt("ca")
            P.ts("dve", [ca], ca[:, 0:no], [zw, cvp], zw[:, 1:n - 1], cvp[:, c * 4 + 1:c * 4 + 2], cvp[:, c * 4 + 3:c * 4 + 4],
                 ALU.mult, ALU.add)
            P.stt("dve", [ca], ca[:, 0:no], [zw, cvp, ca], zw[:, 0:n - 2], cvp[:, c * 4:c * 4 + 1], ca[:, 0:no], ALU.mult, ALU.add)
            P.stt("dve", [ca], ca[:, 0:no], [zw, cvp, ca], zw[:, 2:n], cvp[:, c * 4 + 2:c * 4 + 3], ca[:, 0:no], ALU.mult, ALU.add)
            ps_gb = proj(hT, c0, n, c * 128, 128)
            co = P.nxt("co")
            P.tt("dve", [co], co[:, 0:no], [ps_gb, ca], ps_gb[:, 1:n - 1], ca[:, 0:no], ALU.mult)
            P.dma("sp", [convT], convT[c * 128:(c + 1) * 128, tok0:tok0 + no], [co], co[:, 0:no])
        pq = [proj(hT, c0, n, 1536 + c * 128, 128) for c in range(2)]
        qn = latent_norm(pq, 2, n, 0, "qn")
        for h in range(8):
            psq = P.nxt("ps")
            for k in range(2):
                P.mm(psq, psq[0:96, 0:n], w_uq_b[:, k, h * 96:(h + 1) * 96], qn[:, k, 0:n], k == 0, k == 1, [w_uq_b, qn])
            qo = P.nxt("qo")
            if is_ctx:
                P.cp("act", [qo], qo[:, 0:no], [psq], psq[0:96, 1:n - 1])
            else:
                pss = P.nxt("ps")
                for k in range(2):
                    P.mm(pss, pss[0:96, 0:n], w_uqs_b[:, k, h * 96:(h + 1) * 96], qn[:, k, 0:n], k == 0, k == 1, [w_uqs_b, qn])
                t1 = P.nxt("t1")
                P.tt("dve", [t1], t1[:, 0:no], [psq, rq], psq[0:96, 1:n - 1], rq[:, 0, 0:no], ALU.mult)
                t2 = P.nxt("t2")
                P.tt("dve", [t2], t2[:, 0:no], [pss, rq], pss[0:96, 1:n - 1], rq[:, 1, 0:no], ALU.mult)
                P.tt("dve", [qo], qo[:, 0:no], [t1, t2], t1[:, 0:no], t2[:, 0:no], ALU.add)
            P.dma("sp", [QT], QT[h, :, tok0:tok0 + no], [qo], qo[:, 0:no])
        pk = [proj(hT, c0, n, 1792, 128)]
        kvn = latent_norm(pk, 1, n, 2, "kvn")
        for c in range(4):
            psk = P.nxt("ps")
            P.mm(psk, psk[:, 0:n], w_ukv_b[:, c * 128:(c + 1) * 128], kvn[:, 0:n], True, True, [w_ukv_b, kvn])
            ko = P.nxt("ko")
            P.cp("act", [ko], ko[:, 0:no], [psk], psk[:, 1:n - 1])
            P.dma("sp", [KTn], KTn[c * 128:(c + 1) * 128, tok0:tok0 + no], [ko], ko[:, 0:no])
        t0 = 0
        while t0 < no:
            m = min(128, no - t0)
            psv = P.nxt("ps")
            P.mm(psv, psv[0:m, :], kvn[:, 1 + t0:1 + t0 + m], w_ukv_b[:, 512:1024], True, True, [kvn, w_ukv_b])
            vo = P.nxt("vo")
            P.cp("act", [vo], vo[0:m, :], [psv], psv[0:m, :])
            P.dma("sp", [Vt], Vt[tok0 + t0:tok0 + t0 + m, :], [vo], vo[0:m, :])
            t0 += m
        psr = proj(hT, c0, n, 1920, 32)
        ko = P.nxt("ko")
        if is_ctx:
            P.cp("act", [ko], ko[0:32, 0:no], [psr], psr[0:32, 1:n - 1])
        else:
            psrs = proj(hT, c0, n, 1952, 32)
            t1 = P.nxt("t1")
            P.tt("dve", [t1], t1[0:32, 0:no], [psr, rk], psr[0:32, 1:n - 1], rk[:, 0, 0:no], ALU.mult)
            t2 = P.nxt("t2")
            P.tt("dve", [t2], t2[0:32, 0:no], [psrs, rk], psrs[0:32, 1:n - 1], rk[:, 1, 0:no], ALU.mult)
            P.tt("dve", [ko], ko[0:32, 0:no], [t1, t2], t1[0:32, 0:no], t2[0:32, 0:no], ALU.add)
        P.dma("sp", [KTr], KTr[:, tok0:tok0 + no], [ko], ko[0:32, 0:no])

    nb = (NL + 509) // 510
    for sg in range((nb + 1) // 2):
        G0 = 1020 * sg
        G1 = min(G0 + 1022, NL + 2)
        hseg = P.nxt("hseg")
        fill_seg(hseg, G0, G1)
        for j in (2 * sg, 2 * sg + 1):
            if j >= nb:
                continue
            c0 = 510 * j
            n = min(512, NL + 2 - c0)
            block(hseg, c0 - G0, n, c0, False, j == 0, j == nb - 1)
    hseg = P.nxt("hseg")
    for t in range(2):
        tp = tile_T(P, xrows, NL + t * 128, AB[1][0], AB[1][1])
        P.cp("act", [hseg], hseg[:, :, 1 + t * 128:129 + t * 128], [tp], tp[:])
    P.cp("dve", [hseg], hseg[:, :, 0:1], [hseg], hseg[:, :, 1:2])
    P.cp("dve", [hseg], hseg[:, :, 257:258], [hseg], hseg[:, :, 1:2])
    block(hseg, 0, 258, NL, True, True, True)
    P.phase_end()


def _swap_perm(dim):
    q = dim // 4
    d = np.arange(dim)
    return np.where((d % (2 * q)) < q, d + q, d - q)


def _rope_tables(pos, dim):
    half = dim // 2
    q = dim // 4
    freqs = (10000.0 ** (-np.arange(0, half, 2, dtype=np.float32) / np.float32(half))).astype(np.float32)
    row = (pos // 64).astype(np.float32)
    col = (pos % 64).astype(np.float32)
    ang = np.concatenate([row[:, None] * freqs, col[:, None] * freqs], axis=-1).astype(np.float32)
    cos, sin = np.cos(ang).astype(np.float32), np.sin(ang).astype(np.float32)
    d = np.arange(dim)
    j = (d // (2 * q)) * q + d % q
    sign = np.where((d % (2 * q)) < q, -1.0, 1.0).astype(np.float32)
    C = cos[:, j].T.copy()
    S = (sin[:, j] * sign[None, :]).T.copy()
    return C, S


def _rep(v):
    return np.ascontiguousarray(np.broadcast_to(np.asarray(v, np.float32)[None, :], (128, v.shape[0])))


def _cols(v, k):
    return np.ascontiguousarray(np.asarray(v, np.float32).reshape(k, 128).T)


def prep_A(inp):
    x, c, cx, c_ctx = inp["x"], inp["c"], inp["ctx"], inp["c_ctx"]
    w_in = inp["ab_w_in"][0]
    p32 = _swap_perm(32)
    w_in_ext = np.ascontiguousarray(np.concatenate([w_in, w_in[:, 1920 + p32]], axis=1))
    w_uq = inp["mla_w_uq"][0]
    w_uqs = w_uq.copy()
    for h in range(8):
        w_uqs[:, h * 96 + 64:h * 96 + 96] = w_uq[:, h * 96 + 64 + p32]
    wk = inp["mla_w_ukv"][0].reshape(128, 8, 128)
    w_ukv = np.ascontiguousarray(np.concatenate([wk[:, :, :64].reshape(128, 512), wk[:, :, 64:].reshape(128, 512)], axis=1))
    convp = np.zeros((128, 16), np.float32)
    for ch in range(4):
        for i in range(3):
            convp[:, ch * 4 + i] = inp["conv_w"][0][i, ch * 128:(ch + 1) * 128]
        convp[:, ch * 4 + 3] = inp["conv_b"][0][ch * 128:(ch + 1) * 128]
    qg = np.concatenate([_cols(inp["mla_q_norm_g"][0], 2), _cols(inp["mla_kv_norm_g"][0], 1)], axis=1)
    maps = []
    for core in range(8):
        b, q = core // 4, core % 4
        T0 = q * NL
        xr = np.ascontiguousarray(np.concatenate([x[b, T0:T0 + NL], cx[b]], axis=0))
        xh = np.zeros((128, D), np.float32)
        if q > 0:
            xh[0] = x[b, T0 - 1]
        if q < 3:
            xh[1] = x[b, T0 + NL]
        hm = np.zeros((128, 2), np.float32)
        hm[:, 0] = 1.0 if q > 0 else 0.0
        hm[:, 1] = 1.0 if q < 3 else 0.0
        cc = np.zeros((128, 16), np.float32)
        cc[:, 0::2] = _cols(c[b], 8)
        cc[:, 1::2] = _cols(c_ctx, 8)
        C, S = _rope_tables(np.arange(T0, T0 + NL), 32)
        rq = np.zeros((96, 2, NL), np.float32)
        rq[:64, 0] = 1.0
        rq[64:, 0] = C
        rq[64:, 1] = S
        rk = np.stack([C, S], axis=1)
        maps.append({
            "xrows": xr, "xhalo": xh, "hmask": hm, "ccols": cc,
            "modw": np.ascontiguousarray(inp["mod_w"][0]), "modb": np.ascontiguousarray(inp["mod_b"][0][None, :]),
            "n1g": _rep(inp["norm1_g"][0]), "w_in": w_in_ext, "convp": convp, "qg": np.ascontiguousarray(qg),
            "w_uq": np.ascontiguousarray(w_uq), "w_uqs": np.ascontiguousarray(w_uqs), "w_ukv": w_ukv,
            "ropeq": rq, "ropek": np.ascontiguousarray(rk),
        })
    return maps


NK0 = NCX + 4 * NL
NKT0 = NK0 // 128


def phase_B(P, T):
    P.phase_begin("B")
    QT, KTn, KTr, Vt, KTr_all, Vt_all, attT = (T[k] for k in ("QT", "KTn", "KTr", "Vt", "KTr_all", "Vt_all", "attT"))
    scale = 96.0 ** -0.5

    P.ring("sps", [128, 2, 512], F32, 2, psum=True)
    P.ring("ops", [128, 512], F32, 2, psum=True)
    P.ring("bps", [128, 512], F32, 1, psum=True)
    P.ring("kt", [96, NK0], BF16, 2)
    P.ring("vp", [128, NKT0, 65], BF16, 2)
    P.ring("qt", [96, NT], BF16, 2)
    P.ring("pT", [128, 2, 512], BF16, 4)
    P.ring("rl", [1, 512], F32, 2)
    P.ring("bsb", [65, 512], F32, 2)
    P.ring("ao", [65, 512], BF16, 2)
    ones = P.sb("ones", [1, 128], F32)
    P.memset("dve", [ones], ones[:], 1.0)

    pending = [None]

    def qblock(h, kt_b, vp_b, qt_b, q0, nq, ntile):
        o_ps = P.nxt("ops")
        ng = ntile // 2

        def emit_pv(g, pT):
            for i in range(2):
                t = 2 * g + i
                P.mm(o_ps, o_ps[0:65, 0:nq], vp_b[:, t, :], pT[:, i, 0:nq], t == 0, t == ntile - 1, [vp_b, pT])

        prev = None
        for g in range(ng):
            s_ps = P.nxt("sps")
            for i in range(2):
                t = 2 * g + i
                P.mm(s_ps, s_ps[:, i, 0:nq], kt_b[:, t * 128:(t + 1) * 128], qt_b[:, q0:q0 + nq], True, True, [kt_b, qt_b])
            if prev is not None:
                emit_pv(*prev)
            pT = P.nxt("pT")
            P.act([pT], pT[:, :, 0:nq], [s_ps], s_ps[:, :, 0:nq], AF.Exp, scale=scale)
            prev = (g, pT)
            if g == min(3, ng - 1) and pending[0] is not None:
                pending[0]()
                pending[0] = None
        emit_pv(*prev)

        def fin():
            rl = P.nxt("rl")
            P.op("dve", lambda e: e.reciprocal(rl[:, 0:nq], o_ps[0:1, 0:nq]), reads=[o_ps], writes=[rl])
            b_ps = P.nxt("bps")
            P.mm(b_ps, b_ps[0:65, 0:nq], ones[:, 0:65], rl[:, 0:nq], True, True, [ones, rl])
            bsb = P.nxt("bsb")
            P.cp("act", [bsb], bsb[:, 0:nq], [b_ps], b_ps[0:65, 0:nq])
            ao = P.nxt("ao")
            P.tt("dve", [ao], ao[:, 0:nq], [o_ps, bsb], o_ps[0:65, 0:nq], bsb[:, 0:nq], ALU.mult)
            P.dma("pool", [attT], attT[h * 64:(h + 1) * 64, q0:q0 + nq], [ao], ao[1:65, 0:nq])
        pending[0] = fin

    for b_ in P.rings["vp"][0]:
        P.memset("dve", [b_], b_[:, :, 0:1], 1.0)
    for h in range(8):
        kt_b = P.nxt("kt")
        P.dma("sp", [kt_b], kt_b[0:64, 0:NCX], [KTn], KTn[h * 64:(h + 1) * 64, NL:NT])
        P.dma("sp", [kt_b], kt_b[64:96, 0:NCX], [KTr], KTr[:, NL:NT])
        for r in range(4):
            P.dma("sp", [kt_b], kt_b[0:64, NCX + r * NL:NCX + (r + 1) * NL], [T["KTn_all%d" % h]], T["KTn_all%d" % h][r * 64:(r + 1) * 64, 0:NL])
            P.dma("sp", [kt_b], kt_b[64:96, NCX + r * NL:NCX + (r + 1) * NL], [KTr_all], KTr_all[r * 32:(r + 1) * 32, 0:NL])
        vp_b = P.nxt("vp")
        P.dma("sp", [vp_b], vp_b[:, 0:2, 1:65], [Vt], Vt[NL:NT, h * 64:(h + 1) * 64].rearrange("(t p) d -> p t d", p=128))
        for r in range(4):
            for k in range(4):
                P.dma("sp", [vp_b], vp_b[:, 2 + 32 * r + 8 * k:2 + 32 * r + 8 * k + 8, 1:65], [Vt_all],
                      Vt_all[k, r * 1024:(r + 1) * 1024, h * 64:(h + 1) * 64].rearrange("(t p) d -> p t d", p=128))
        qt_b = P.nxt("qt")
        P.dma("sp", [qt_b], qt_b[:], [QT], QT[h])
        for qb in range(NL // 512):
            qblock(h, kt_b, vp_b, qt_b, qb * 512, 512, NKT0)
        qblock(h, kt_b, vp_b, qt_b, NL, NCX, NCX // 128)
    pending[0]()
    P.phase_end()


def phase_post(P, T, layer, ntok, cats):
    P.phase_begin("post%d" % layer)
    xrows, ccols, modw, modb, n2g, w_out, rw = T["xres%d" % layer], T["ccols"], T["modw%d" % layer], T["modb%d" % layer], T["n2g%d" % layer], T["w_out%d" % layer], T["rw%d" % layer]
    xs1, h2o, affo, affl = T["xacc%d" % layer], T["h2_%d" % layer], T["aff_%d" % layer], T["affl_%d" % layer]

    P.make_ident()
    P.ring("ps", [128, 512], F32, 2, psum=True)
    P.ring("yps", [128, 2, 512], F32, 2, psum=True)
    P.ring("tp32", [128, 8, 128], F32, 1, psum=True)
    P.ring("rstd_tmp", [128, 512], F32, 1)
    mods = compute_mod(P, ccols.t, modw.t, modb.t, 2048, 3072, ["g1", "sh2", "sc2"])
    g2n = P.sb("g2n", [128, D], F32)
    P.dma("sp", [g2n], g2n[:], [], n2g.t)
    for w in range(2):
        A = mods["sc2"][w]
        P.stt("dve", [A], A[:], [A, g2n], A[:], 1.0, g2n[:], ALU.add, ALU.mult)
    w_out_b = load_bf16(P, "w_out_b", w_out.t.rearrange("(k p) n -> p k n", p=128), [128, 8, D])
    rws = P.sb("rws", [128, 8, NE], F32)
    P.dma("sp", [rws], rws[:], [], rw.t.rearrange("(k p) n -> p k n", p=128))

    P.ring("cat", [128, 8, 128], BF16, 2)
    P.ring("xt", [128, D], F32, 2)
    P.ring("xsr", [128, D], F32, 2)
    P.ring("junk", [128, D], BF16, 1)
    P.ring("ssq", [128, 1], F32, 2)
    P.ring("rs", [128, 1], F32, 2)
    P.ring("h32", [128, D], F32, 3)
    P.ring("hb", [128, D], BF16, 2)
    P.ring("hT32", [128, 8, 128], F32, 1)
    P.ring("sm", [128, 4], F32, 3)
    P.ring("ex", [128, NE], F32, 2)
    P.ring("af", [128, NE], F32, 2)

    def p1(t, out):
        w = 0 if t < NL // 128 else 1
        r0 = t * 128
        cat = P.nxt("cat")
        c0_ = 0
        for (cb, nch) in cats:
            P.dma("sp", [cat], cat[:, c0_:c0_ + nch, :], [cb], cb[:, r0:r0 + 128].rearrange("(c p) t -> p c t", p=128))
            c0_ += nch
        xt = P.nxt("xt")
        P.dma("sp", [xt], xt[:], [xrows], xrows[r0:r0 + 128, :])
        y = P.nxt("yps")
        for hh in range(2):
            for k in range(8):
                P.mm(y, y[:, hh, :], cat[:, k, :], w_out_b[:, k, hh * 512:(hh + 1) * 512], k == 0, k == 7, [cat, w_out_b])
            yield
        xs = P.nxt("xsr")
        g1r = mods["g1"][w]
        P.tt("dve", [xs], xs[:], [y, g1r], y[:].rearrange("p a b -> p (a b)"), g1r[:], ALU.mult)
        yield
        P.tt("dve", [xs], xs[:], [xs, xt], xs[:], xt[:], ALU.add)
        P.dma("pool", [xs1], xs1[r0:r0 + 128, :], [xs], xs[:])
        yield
        junk = P.nxt("junk")
        ssq = P.nxt("ssq")
        P.act([junk, ssq], junk[:], [xs], xs[:], AF.Square, accum=ssq[:, 0:1])
        yield
        rs = P.nxt("rs")
        P.rstd(rs, rs[:, 0:1], ssq, ssq[:, 0:1], 1, 1.0 / D)
        yield
        h32 = P.nxt("h32")
        P.stt("dve", [h32], h32[:], [xs, rs, mods["sc2"][w]], xs[:], rs[:, 0:1], mods["sc2"][w][:], ALU.mult, ALU.mult)
        yield
        P.tt("dve", [h32], h32[:], [h32, mods["sh2"][w]], h32[:], mods["sh2"][w][:], ALU.add)
        yield
        hb = P.nxt("hb")
        P.cp("act", [hb], hb[:], [h32], h32[:])
        P.dma("pool", [h2o], h2o[r0:r0 + 128, :], [hb], hb[:])
        out["h32"] = h32
        yield

    def p2(t, h32):
        r0 = t * 128
        tp = P.nxt("tp32")
        for k in range(8):
            P.tr(tp, tp[:, k, :], h32[:, k * 128:(k + 1) * 128], P.idf[:], [h32, P.idf])
        yield
        hT = P.nxt("hT32")
        P.cp("act", [hT], hT[:], [tp], tp[:])
        yield
        lg = P.nxt("ps")
        for k in range(8):
            P.mm(lg, lg[:, 0:NE], hT[:, k, :], rws[:, k, :], k == 0, k == 7, [hT, rws])
        yield
        sm = P.nxt("sm")
        P.red("dve", [sm], sm[:, 0:1], [lg], lg[:, 0:NE], ALU.max)
        P.ts("dve", [sm], sm[:, 1:2], [sm], sm[:, 0:1], -1.0, None, ALU.mult)
        yield
        ex = P.nxt("ex")
        P.act([ex, sm], ex[:], [lg, sm], lg[:, 0:NE], AF.Exp, bias=sm[:, 1:2], accum=sm[:, 2:3])
        yield
        P.op("dve", lambda e, sm=sm: e.reciprocal(sm[:, 3:4], sm[:, 2:3]), reads=[sm], writes=[sm])
        af = P.nxt("af")
        P.ts("dve", [af], af[:], [ex, sm], ex[:], sm[:, 3:4], None, ALU.mult)
        P.dma("pool", [affo], affo[r0:r0 + 128, :], [af], af[:])
        if t < NL // 128 and affl is not affo:
            P.dma("pool", [affl], affl[r0:r0 + 128, :], [af], af[:])
        yield

    ntile = ntok // 128
    o0 = {}
    for _ in p1(0, o0):
        pass
    hprev = o0["h32"]
    for t in range(ntile):
        on = {}
        ga = p1(t + 1, on) if t + 1 < ntile else iter(())
        gb = p2(t, hprev)
        da = db = False
        while not (da and db):
            if not da:
                da = next(ga, "end") == "end"
            if not db:
                db = next(gb, "end") == "end"
        hprev = on.get("h32")
    P.phase_end()


def _ccols(inp, b):
    cc = np.zeros((128, 16), np.float32)
    cc[:, 0::2] = _cols(inp["c"][b], 8)
    cc[:, 1::2] = _cols(inp["c_ctx"], 8)
    return cc


SMAX = 640
CAP_L = 2048
CAP_C = 32
NBIS = 30


def bisect_gen(P, affs, J, e0, e1, kcap, ones_f, name, psbuf, thr):
    ne = e1 - e0
    lo = P.sb(name + "_lo", [128, ne], F32)
    mid = P.sb(name + "_mid", [128, ne], F32)
    cnt = P.sb(name + "_cnt", [128, ne], F32)
    stp = P.sb(name + "_stp", [128, ne], F32)
    cmp = P.sb(name + "_cmp", [128, J, ne], BF16)
    P.memset("dve", [lo], lo[:], 0.0)
    for it in range(NBIS):
        c = 2.0 ** -(it + 1)
        P.ts("dve", [mid], mid[:], [lo], lo[:], c, None, ALU.add)
        P.tt("dve", [cmp], cmp[:], [affs, mid], affs[:, :, e0:e1], mid[:].unsqueeze(1).to_broadcast([128, J, ne]), ALU.is_ge)
        P.red("dve", [cnt], cnt[:], [cmp], cmp[:].rearrange("p j e -> p e j"), ALU.add)
        P.mm(psbuf, psbuf[:, 0:ne], ones_f[:], cnt[:], True, True, [ones_f, cnt])
        P.ts("dve", [stp], stp[:], [psbuf], psbuf[:, 0:ne], float(kcap) - 0.5, c, ALU.is_ge, ALU.mult)
        P.tt("dve", [lo], lo[:], [lo, stp], lo[:], stp[:], ALU.add)
        yield
    P.cp("dve", [thr], thr[:, e0:e1], [lo], lo[:])
    yield


def phase_moe(P, T, layer):
    has_ctx = (layer == 0)
    ntok = NT if has_ctx else NL
    T_ = ntok // 128
    TL = NL // 128
    NS = SMAX // 128
    NSLOT = SMAX + (CAP_C if has_ctx else 0)
    P.phase_begin("moe%d" % layer)
    h2, aff, aff_all, ccols, modw, modb = T["h2_%d" % layer], T["aff_%d" % layer], T["affall_%d" % layer], T["ccols"], T["modw%d" % layer], T["modb%d" % layer]
    wg, wu, wd = T["wg%d" % layer], T["wu%d" % layer], T["wd%d" % layer]
    xacc = T["xacc%d" % layer]
    if not has_ctx:
        fing, outp = T["fing"], T["out"]
    TRASH0 = ntok
    T = T_

    P.make_ident()
    P.ring("ps", [128, 512], F32, 1, psum=True)
    P.ring("tp", [128, 8, 128], BF16, 1, psum=True)
    P.ring("accb", [128, 512], F32, 2, psum=True)
    P.ring("hb", [128, 512], F32, 4, psum=True)
    P.ring("rstd_tmp", [128, 512], F32, 1)

    P.ring("xt", [128, D], F32, 1)
    zt = P.nxt("xt")
    P.memset("dve", [zt], zt[:], 0.0)
    zt_op = P.op("sp", lambda e: e.dma_start(out=xacc[ntok:ntok + 128, :], in_=zt[:]), reads=[zt], writes=[xacc], dma=True)

    mods = compute_mod(P, ccols.t, modw.t, modb.t, 5120, 1024, ["g2"])
    g2rep = mods["g2"]

    ones_f = P.sb("ones_f", [128, 128], F32)
    P.memset("dve", [ones_f], ones_f[:], 1.0)
    ones_b = P.sb("ones_b", [128, 128], BF16)
    P.memset("dve", [ones_b], ones_b[:], 1.0)
    ltf = P.sb("ltf", [128, 128], F32)
    P.memset("pool", [ltf], ltf[:], 1.0)
    P.op("pool", lambda e: e.affine_select(ltf[:], ltf[:], [[1, 128]], ALU.is_gt, 0.0, base=0, channel_multiplier=-1),
         reads=[ltf], writes=[ltf])
    ltb = P.sb("ltb", [128, 128], BF16)
    P.cp("dve", [ltb], ltb[:], [ltf], ltf[:])
    io_i = P.sb("io_i", [128, SMAX], I32)
    P.op("pool", lambda e: e.iota(io_i[:], [[1, SMAX]], base=0, channel_multiplier=0), writes=[io_i])
    io_f = P.sb("io_f", [128, SMAX], F32)
    P.cp("dve", [io_f], io_f[:], [io_i], io_i[:])
    ip_i = P.sb("ip_i", [128, 1], I32)
    P.op("pool", lambda e: e.iota(ip_i[:], [[0, 1]], base=0, channel_multiplier=1), writes=[ip_i])
    ip_f = P.sb("ip_f", [128, 2], F32)
    P.cp("dve", [ip_f], ip_f[:, 0:1], [ip_i], ip_i[:])
    P.ts("dve", [ip_f], ip_f[:, 1:2], [ip_f], ip_f[:, 0:1], float(TRASH0), None, ALU.add)

    P.ring("wgb", [128, 8, D], BF16, 2)
    P.ring("wub", [128, 8, D], BF16, 2)
    P.ring("wdb", [128, 8, D], BF16, 1)
    P.ring("wstg", [128, 2, D], F32, 2)

    def wd_dma(e, q, src=None):
        stg = P.nxt("wstg")
        src = wd if src is None else src
        P.dma("sp", [stg], stg[:], [], src[e, q * 256:(q + 1) * 256, :].rearrange("(k p) n -> p k n", p=128))
        return stg

    def wd_cast(b, q, stg):
        P.cp("act", [b], b[:, 2 * q:2 * q + 2, :], [stg], stg[:])

    def load_one(nm, src, e):
        b = P.nxt(nm)
        for k in range(8):
            P.dma("pool", [b], b[:, k, :], [], src[e, k * 128:(k + 1) * 128, :])
        return b

    wdb0 = P.nxt("wdb")
    for q_ in range(4):
        wd_cast(wdb0, q_, wd_dma(0, q_))
    w_next = (load_one("wgb", wg, 0), load_one("wub", wu, 0), wdb0)

    af = P.sb("af", [128, T, NE], F32)
    P.dma("sp", [af], af[:], [aff], aff.t.rearrange("(t p) e -> p t e", p=128))
    thr_l = P.sb("thr_l", [128, NE], F32)
    thr_c = P.sb("thr_c", [128, NE], F32)
    main_stack = P.pstack
    sub = ExitStack()
    P.pstack = sub
    affs = P.sb("affs", [128, 128, NE], F32)
    P.dma("sp", [affs], affs[:], [aff_all], aff_all.t.rearrange("(p j) e -> p j e", p=128))
    pbanks = P.rings["accb"][0] + P.rings["hb"][0]
    gens = [bisect_gen(P, affs, 128, 0, 8, CAP_L, ones_f, "bla", pbanks[0], thr_l),
            bisect_gen(P, affs, 128, 8, 16, CAP_L, ones_f, "blb", pbanks[1], thr_l)]
    if has_ctx:
        afc = P.sb("afc", [128, 2, NE], F32)
        P.cp("dve", [afc], afc[:], [af], af[:, TL:T, :])
        gens.append(bisect_gen(P, afc, 2, 0, 16, CAP_C, ones_f, "bc", pbanks[2], thr_c))
    for _ in range(NBIS + 1):
        for g_ in gens:
            next(g_)
    P.pstack = main_stack
    P.ops.append(None)
    sub.close()

    mask = P.sb("mask", [128, T, NE], F32)
    P.tt("dve", [mask], mask[:, 0:TL, :], [af, thr_l], af[:, 0:TL, :], thr_l[:].unsqueeze(1).to_broadcast([128, TL, NE]), ALU.is_ge)
    if has_ctx:
        P.tt("dve", [mask], mask[:, TL:T, :], [af, thr_c], af[:, TL:T, :], thr_c[:].unsqueeze(1).to_broadcast([128, 2, NE]), ALU.is_ge)
    maskb = P.sb("maskb", [128, T, NE], BF16)
    P.cp("dve", [maskb], maskb[:], [mask], mask[:])
    pos = P.sb("pos", [128, T, NE], F32)
    tot = P.sb("tot", [128, T, NE], F32)
    mflat = maskb[:].rearrange("p t e -> p (t e)")
    pflat = pos[:].rearrange("p t e -> p (t e)")
    tflat = tot[:].rearrange("p t e -> p (t e)")
    n_all = T * NE
    c = 0
    while c < n_all:
        w_ = min(512, n_all - c)
        ps = P.nxt("ps")
        P.mm(ps, ps[:, 0:w_], ltb[:], mflat[:, c:c + w_], True, True, [ltb, maskb])
        P.cp("dve", [pos], pflat[:, c:c + w_], [ps], ps[:, 0:w_])
        ps = P.nxt("ps")
        P.mm(ps, ps[:, 0:w_], ones_b[:], mflat[:, c:c + w_], True, True, [ones_b, maskb])
        P.cp("dve", [tot], tflat[:, c:c + w_], [ps], ps[:, 0:w_])
        c += w_
    base = P.sb("base", [128, T, NE], F32)
    P.memset("dve", [base], base[:], 0.0)
    for t in range(1, TL):
        P.tt("dve", [base], base[:, t, :], [base, tot], base[:, t - 1, :], tot[:, t - 1, :], ALU.add)
    if has_ctx:
        P.cp("dve", [base], base[:, TL + 1, :], [tot], tot[:, TL, :])
    P.tt("dve", [pos], pos[:], [pos, base], pos[:], base[:], ALU.add)

    vals = P.sb("vals", [128, T, NE, 6], BF16)
    for t in range(T):
        P.memset("dve", [vals], vals[:, t, :, 0], float(t))
    P.ts("dve", [vals], vals[:, :, :, 1], [af, ip_f], af[:], 0.0, ip_f[:, 0:1], ALU.mult, ALU.add)
    P.memset("dve", [vals], vals[:, :, :, 5], 1.0)
    r1 = tot
    P.cp("dve", [vals], vals[:, :, :, 2], [af], af[:])
    P.tt("dve", [r1], r1[:], [af, vals], af[:], vals[:, :, :, 2], ALU.subtract)
    P.cp("dve", [vals], vals[:, :, :, 3], [r1], r1[:])
    P.tt("dve", [r1], r1[:], [r1, vals], r1[:], vals[:, :, :, 3], ALU.subtract)
    P.cp("dve", [vals], vals[:, :, :, 4], [r1], r1[:])

    P.ring("oh", [128, SMAX], BF16, 3)
    P.ring("siT", [6, NSLOT], F32, 1)
    P.ring("si", [128, 8, 6], F32, 2)
    P.ring("sx", [128, 8, 4], F32, 2)
    P.ring("ixg", [128, 8], I32, 2)
    P.ring("ixs", [128, 8], I32, 2)
    P.ring("xg", [128, D], BF16, 6 if has_ctx else 5)
    P.ring("xgT", [128, 8, NSLOT], BF16, 1)
    P.ring("sil", [128, 512], F32, 2)
    P.ring("hidT", [128, 8, NSLOT], BF16, 1)
    P.ring("ye", [128, D], F32, 2)

    NST = NS + (1 if has_ctx else 0)
    n2 = NSLOT - 512

    def stage1a(e, st):
        acc0 = P.nxt("accb")
        acc1 = P.nxt("accb")
        ohq = st["ohq"] = []

        def flush():
            while ohq:
                t, oh = ohq.pop(0)
                P.mm(acc0, acc0[0:6, :], vals[:, t, e, :], oh[:, 0:512], t == 0, t == TL - 1, [vals, oh])
                P.mm(acc1, acc1[0:6, 0:SMAX - 512], vals[:, t, e, :], oh[:, 512:SMAX], t == 0, t == TL - 1, [vals, oh])
        st["flush"] = flush
        for t in range(TL):
            oh = P.nxt("oh")
            P.ts("dve", [oh], oh[:], [io_f, pos, mask], io_f[:], pos[:, t, e:e + 1], mask[:, t, e:e + 1], ALU.is_equal, ALU.mult)
            ohq.append((t, oh))
            yield "oh"
        flush()
        siT = P.nxt("siT")
        P.cp("act", [siT], siT[:, 0:512], [acc0], acc0[0:6, :])
        P.cp("act", [siT], siT[:, 512:SMAX], [acc1], acc1[0:6, 0:SMAX - 512])
        if has_ctx:
            accc = P.nxt("ps")
            for t in range(TL, T):
                oh = P.nxt("oh")
                P.ts("dve", [oh], oh[:, 0:CAP_C], [io_f, pos, mask], io_f[:, 0:CAP_C], pos[:, t, e:e + 1], mask[:, t, e:e + 1],
                     ALU.is_equal, ALU.mult)
                P.mm(accc, accc[0:6, 0:CAP_C], vals[:, t, e, :], oh[:, 0:CAP_C], t == TL, t == T - 1, [vals, oh])
            P.cp("act", [siT], siT[:, SMAX:SMAX + CAP_C], [accc], accc[0:6, 0:CAP_C])
        sps = P.nxt("ps")
        for s_ in range(NST):
            ns = 128 if s_ < NS else CAP_C
            P.mm(sps, sps[0:ns, s_ * 6:(s_ + 1) * 6], siT[:, s_ * 128:s_ * 128 + ns], P.idf[0:6, 0:6], True, True, [siT, P.idf])
        si = P.nxt("si")
        P.memset("dve", [si], si[:], 0.0)
        P.cp("dve", [si], si[:, 0:NS, :], [sps], sps[:, 0:NS * 6].rearrange("p (s k) -> p s k", k=6))
        if has_ctx:
            P.cp("dve", [si], si[0:CAP_C, NS, :], [sps], sps[0:CAP_C, NS * 6:NS * 6 + 6])
        sx = P.nxt("sx")
        P.stt("dve", [sx], sx[:, :, 0], [si], si[:, :, 0], 128.0, si[:, :, 1], ALU.mult, ALU.add)
        P.tt("dve", [sx], sx[:, :, 1], [si], si[:, :, 2], si[:, :, 3], ALU.add)
        P.tt("dve", [sx], sx[:, :, 1], [sx, si], sx[:, :, 1], si[:, :, 4], ALU.add)
        P.ts("dve", [sx], sx[:, :, 2], [si], si[:, :, 5], -1.0, 1.0, ALU.mult, ALU.add)
        P.stt("dve", [sx], sx[:, :, 3], [sx, ip_f], sx[:, :, 2], ip_f[:, 1:2], sx[:, :, 0], ALU.mult, ALU.add)
        ixg = P.nxt("ixg")
        P.cp("dve", [ixg], ixg[:], [sx], sx[:, :, 0])
        ixs = P.nxt("ixs")
        P.cp("dve", [ixs], ixs[:], [sx], sx[:, :, 3])
        st["sx"], st["ixs"], st["ixg"], st["xgs"] = sx, ixs, ixg, [None] * NST
        yield

    def issue_gather(st, s_):
        xg = P.nxt("xg")
        ixg = st["ixg"]
        P.op("pool", lambda en: en.indirect_dma_start(
            out=xg[:], out_offset=None, in_=h2.t, in_offset=bass.IndirectOffsetOnAxis(ap=ixg[:, s_:s_ + 1], axis=0)),
            reads=[ixg, h2], writes=[xg], dma=True)
        st["xgs"][s_] = xg

    def stage1b(st):
        xgT = P.nxt("xgT")
        for s_ in range(NST):
            ns = 128 if s_ < NS else CAP_C
            xg = st["xgs"][s_]
            tp = P.nxt("tp")
            for k in range(8):
                P.tr(tp, tp[:, k, :], xg[:, k * 128:(k + 1) * 128], P.idb[:], [xg, P.idb])
            P.cp("act", [xgT], xgT[:, :, s_ * 128:s_ * 128 + ns], [tp], tp[:, :, 0:ns])
        st["xgT"] = xgT

    def stage2(e, st, wgb, wub, wdb, gen, st_next, prev_sc, wgn=None):
        xgT, sx, ixs = st["xgT"], st["sx"], st["ixs"]
        hidT = P.nxt("hidT")
        wdp = st.get("wd")
        wgs = []
        gi = -1
        for fc in range(8):
            for (c0, cn) in ((0, 512), (512, n2)):
                gi += 1
                if wdp is not None:
                    if gi == 1:
                        wd_cast(wdb, 0, wdp[0])
                        wdp.append(wd_dma(e, 2))
                    elif gi == 2:
                        wd_cast(wdb, 1, wdp[1])
                        wdp.append(wd_dma(e, 3))
                    elif gi == 4:
                        wd_cast(wdb, 2, wdp[2])
                        if wgn is not None:
                            wgs.append(wd_dma(e + 1, 0, wg))
                    elif gi == 5:
                        wd_cast(wdb, 3, wdp[3])
                        if wgn is not None:
                            wgs.append(wd_dma(e + 1, 1, wg))
                if wgn is not None and wdp is not None:
                    if gi == 7:
                        wd_cast(wgn, 0, wgs[0])
                        wgs.append(wd_dma(e + 1, 2, wg))
                    elif gi == 8:
                        wd_cast(wgn, 1, wgs[1])
                        wgs.append(wd_dma(e + 1, 3, wg))
                    elif gi == 10:
                        wd_cast(wgn, 2, wgs[2])
                    elif gi == 11:
                        wd_cast(wgn, 3, wgs[3])
                if gen is not None and not st_next.get("ohdone"):
                    for _ in range(3):
                        if next(gen, "end") != "oh":
                            st_next["ohdone"] = True
                            break
                gp = P.nxt("hb")
                up = P.nxt("hb")
                for (dst, wb) in ((gp, wgb), (up, wub)):
                    for k in range(8):
                        P.mm(dst, dst[:, 0:cn], wb[:, k, fc * 128:(fc + 1) * 128], xgT[:, k, c0:c0 + cn], k == 0, k == 7, [wb, xgT])
                if gen is not None and "flush" in st_next:
                    st_next["flush"]()
                sil = P.nxt("sil")
                P.act([sil], sil[:, 0:cn], [gp], gp[:, 0:cn], AF.Silu)
                P.tt("dve", [hidT], hidT[:, fc, c0:c0 + cn], [sil, up], sil[:, 0:cn], up[:, 0:cn], ALU.mult)
        if gen is not None:
            for _ in gen:
                pass
        my_sc = []
        for s_ in range(NST):
            ns = 128 if s_ < NS else CAP_C
            ye = P.nxt("ye")
            g2r = g2rep[0] if s_ < NS else g2rep[1]
            for hh in range(2):
                yp = P.nxt("hb")
                for fc in range(8):
                    P.mm(yp, yp[0:ns, :], hidT[:, fc, s_ * 128:s_ * 128 + ns], wdb[:, fc, hh * 512:(hh + 1) * 512], fc == 0, fc == 7,
                         [hidT, wdb])
                P.stt("dve", [ye], ye[0:ns, hh * 512:(hh + 1) * 512], [yp, sx, g2r], yp[0:ns, :], sx[0:ns, s_, 1:2],
                      g2r[0:ns, hh * 512:(hh + 1) * 512], ALU.mult, ALU.mult)
            o_ = P.op("pool", lambda en, ye=ye, ixs=ixs, s_=s_: en.indirect_dma_start(
                out=xacc.t, out_offset=bass.IndirectOffsetOnAxis(ap=ixs[:, s_:s_ + 1], axis=0), in_=ye[:, :], in_offset=None,
                compute_op=ALU.add), reads=[ixs, ye], writes=[], dma=True, extra=prev_sc)
            my_sc.append(o_)
            if st_next is not None:
                issue_gather(st_next, s_)
        return my_sc

    st = {}
    for _ in stage1a(0, st):
        st["flush"]()
    for s_ in range(NST):
        issue_gather(st, s_)
    stage1b(st)
    prev_sc = [zt_op]
    for e in range(NE):
        wgb, wub, wdb = w_next
        st_next = None
        gen = None
        if e + 1 < NE:
            if e == 0:
                wg_n, wgn_ = load_one("wgb", wg, e + 1), None
            else:
                wg_n = wgn_ = P.nxt("wgb")
            wu_n = load_one("wub", wu, e + 1)
            st_next = {}
            gen = stage1a(e + 1, st_next)
        else:
            wgn_ = None
        prev_sc = stage2(e, st, wgb, wub, wdb, gen, st_next, prev_sc, wgn_)
        if e + 1 < NE:
            w_next = (wg_n, wu_n, P.nxt("wdb"))
            st_next["wd"] = [wd_dma(e + 1, 0), wd_dma(e + 1, 1)]
            stage1b(st_next)
        st = st_next

    if not has_ctx:
        fg = P.sb("fg", [128, D], F32)
        P.dma("sp", [fg], fg[:], [], fing.t)
        P.ring("junk", [128, D], BF16, 1)
        P.ring("ssq", [128, 1], F32, 2)
        P.ring("rs", [128, 1], F32, 2)
        for t in range(T):
            xt = P.nxt("xt")
            P.op("sp", lambda e, xt=xt, t=t: e.dma_start(out=xt[:], in_=xacc[t * 128:(t + 1) * 128, :]), reads=[xacc], writes=[xt],
                 dma=True, extra=prev_sc)
            junk = P.nxt("junk")
            ssq = P.nxt("ssq")
            P.act([junk, ssq], junk[:], [xt], xt[:], AF.Square, accum=ssq[:, 0:1])
            rs = P.nxt("rs")
            P.rstd(rs, rs[:, 0:1], ssq, ssq[:, 0:1], 1, 1.0 / D)
            P.stt("dve", [xt], xt[:], [xt, rs, fg], xt[:], rs[:, 0:1], fg[:], ALU.mult, ALU.mult)
            P.dma("sp", [outp], outp[t * 128:(t + 1) * 128, :], [xt], xt[:])
    P.phase_end()


def phase_qkv1(P, T):
    P.phase_begin("qkv1")
    xrows, ccols, modw, modb, n1g, wqkv, wqks, rope = T["xacc0"], T["ccols"], T["modw1"], T["modb1"], T["n1g1"], T["wqkv"], T["wqks"], T["rope1"]
    Q1T, K1T, V1, Kb, Vb = T["Q1T"], T["K1T"], T["V1"], T["Kb"], T["Vb"]

    P.make_ident()
    P.ring("ps", [128, 512], F32, 6, psum=True)
    P.ring("tp", [128, 8, 128], BF16, 2, psum=True)
    P.ring("xt", [128, D], F32, 2)
    P.ring("junk", [128, D], BF16, 1)
    P.ring("ssq", [128, 1], F32, 2)
    P.ring("rs", [128, 1], F32, 2)
    P.ring("rstd_tmp", [128, 512], F32, 1)
    P.ring("h32", [128, D], F32, 1)
    P.ring("hb", [128, D], BF16, 2)
    mods = compute_mod(P, ccols.t, modw.t, modb.t, 0, 2048, ["sh1", "sc1"])
    g1 = P.sb("g1", [128, D], F32)
    P.dma("sp", [g1], g1[:], [], n1g.t)
    AB = []
    for w in range(2):
        A = mods["sc1"][w]
        P.stt("dve", [A], A[:], [A, g1], A[:], 1.0, g1[:], ALU.add, ALU.mult)
        AB.append((A, mods["sh1"][w]))
    wq_b = load_bf16(P, "wq_b", wqkv.t.rearrange("(k p) n -> p k n", p=128), [128, 8, 1536])
    ws_b = load_bf16(P, "ws_b", wqks.t.rearrange("(k p) n -> p k n", p=128), [128, 8, 1280])
    P.ring("hT", [128, 8, 512], BF16, 2)
    P.ring("rp", [128, 2, 512], F32, 2)
    P.ring("t1", [128, 512], F32, 2)
    P.ring("t2", [128, 512], F32, 2)
    P.ring("qo", [128, 512], BF16, 3)
    P.ring("vo", [128, 256], BF16, 3)

    for blk in range(NT // 512 + 1):
        tok0 = blk * 512
        n = min(512, NT - tok0)
        if n <= 0:
            break
        is_ctx = tok0 >= NL
        hT = P.nxt("hT")
        for t in range(n // 128):
            tp = tile_T(P, xrows, tok0 + t * 128, AB[1 if is_ctx else 0][0], AB[1 if is_ctx else 0][1])
            P.cp("act", [hT], hT[:, :, t * 128:(t + 1) * 128], [tp], tp[:])
        if not is_ctx:
            rp = P.nxt("rp")
            P.dma("pool", [rp], rp[:], [], rope[:, :, tok0:tok0 + 512])
        for c in (range(10) if not is_ctx else (8, 9)):
            ps = P.nxt("ps")
            for k in range(8):
                P.mm(ps, ps[:, 0:n], wq_b[:, k, c * 128:(c + 1) * 128], hT[:, k, 0:n], k == 0, k == 7, [wq_b, hT])
            qo = P.nxt("qo")
            if is_ctx:
                P.cp("act", [qo], qo[:, 0:n], [ps], ps[:, 0:n])
            else:
                pss = P.nxt("ps")
                for k in range(8):
                    P.mm(pss, pss[:, 0:n], ws_b[:, k, c * 128:(c + 1) * 128], hT[:, k, 0:n], k == 0, k == 7, [ws_b, hT])
                t1 = P.nxt("t1")
                P.tt("dve", [t1], t1[:, 0:n], [ps, rp], ps[:, 0:n], rp[:, 0, 0:n], ALU.mult)
                t2 = P.nxt("t2")
                P.tt("dve", [t2], t2[:, 0:n], [pss, rp], pss[:, 0:n], rp[:, 1, 0:n], ALU.mult)
                P.tt("dve", [qo], qo[:, 0:n], [t1, t2], t1[:, 0:n], t2[:, 0:n], ALU.add)
            if c < 8:
                P.dma("sp", [Q1T], Q1T[c * 128:(c + 1) * 128, tok0:tok0 + n], [qo], qo[:, 0:n])
            else:
                P.dma("sp", [K1T], K1T[(c - 8) * 128:(c - 7) * 128, tok0:tok0 + n], [qo], qo[:, 0:n])
                if tok0 == 0:
                    P.dma("sp", [Kb], Kb[(c - 8) * 128:(c - 7) * 128, 0:128], [qo], qo[:, 0:128])
                if tok0 == NL - 512:
                    P.dma("sp", [Kb], Kb[(c - 8) * 128:(c - 7) * 128, 128:256], [qo], qo[:, 384:512])
        for t in range(n // 128):
            psv = P.nxt("ps")
            for k in range(8):
                P.mm(psv, psv[:, 0:256], hT[:, k, t * 128:(t + 1) * 128], wq_b[:, k, 1280:1536], k == 0, k == 7, [hT, wq_b])
            vo = P.nxt("vo")
            P.cp("act", [vo], vo[:], [psv], psv[:, 0:256])
            P.dma("sp", [V1], V1[tok0 + t * 128:tok0 + (t + 1) * 128, :], [vo], vo[:])
            if tok0 + t * 128 == 0:
                P.dma("sp", [Vb], Vb[0:128, :], [vo], vo[:])
            if tok0 + t * 128 == NL - 128:
                P.dma("sp", [Vb], Vb[128:256, :], [vo], vo[:])
    P.phase_end()


NB1 = NL // 128
NKL = NL + 256


def phase_attn1(P, T):
    P.phase_begin("attn1")
    Q1T, K1T, V1, Kb_all, Vb_all, masks, sinkr, selh, catT1 = (T[k] for k in ("Q1T", "K1T", "V1", "Kb_all", "Vb_all", "masks", "sinkr", "selh", "catT1"))
    scale = 64.0 ** -0.5

    P.ring("s1b", [128, 512], F32, 5, psum=True)
    P.ring("ops", [128, 512], F32, 2, psum=True)
    P.ring("bps", [128, 512], F32, 1, psum=True)
    P.ring("p1", [128, 512], BF16, 10)
    P.ring("rl", [1, 512], F32, 3)
    P.ring("lsum", [1, 512], F32, 4)
    P.ring("rhl", [1, 2, 512], BF16, 3)
    onesb16 = P.sb("onesb16", [1, 128], BF16)
    P.memset("dve", [onesb16], onesb16[:], 1.0)

    P.ring("bsb", [65, 512], F32, 2)
    P.ring("ao", [65, 512], BF16, 3)
    P.ring("qg", [64, NB1 * 512], BF16, 2)
    P.ring("kl", [64, NKL], BF16, 2)
    P.ring("kc", [64, NCX], BF16, 2)
    P.ring("vl", [128, NKL // 128, 65], BF16, 2)
    P.ring("vc", [128, 2, 65], BF16, 2)
    ones = P.sb("ones", [1, 128], F32)
    P.memset("dve", [ones], ones[:], 1.0)
    mk = P.sb("mk", [128, 4, 512], BF16)
    P.dma("sp", [mk], mk[:], [], masks.t)
    sk = P.sb("sk", [1, 2048], F32)
    P.dma("sp", [sk], sk[:], [], sinkr.t)
    P.act([sk], sk[:], [sk], sk[:], AF.Exp)
    skb = P.sb("skb", [1, 2, 2048], BF16)
    skr = P.sb("skr", [1, 2048], F32)
    P.cp("dve", [skb], skb[:, 0, :], [sk], sk[:])
    P.tt("dve", [skr], skr[:], [sk, skb], sk[:], skb[:, 0, :], ALU.subtract)
    P.cp("dve", [skb], skb[:, 1, :], [skr], skr[:])
    e0 = P.sb("e0", [1, 65], BF16)
    P.memset("dve", [e0], e0[:], 0.0)
    P.memset("dve", [e0], e0[:, 0:1], 1.0)

    sel = P.sb("sel", [128, 8], F32)
    P.dma("sp", [sel], sel[:], [], selh.t)
    P.ring("kcand", [64, 4, 256], BF16, 2)
    P.ring("vcand", [128, 4, 2, 64], BF16, 2)
    for b_ in P.rings["vl"][0]:
        P.memset("dve", [b_], b_[:, :, 0:1], 1.0)
    for b_ in P.rings["vc"][0]:
        P.memset("dve", [b_], b_[:, :, 0:1], 1.0)
    for g in range(4):
        qg = P.nxt("qg")
        for j in range(4):
            P.dma("sp", [qg], qg[:].rearrange("d (i j r) -> d i j r", j=4, r=128)[:, :, j, :], [Q1T],
                  Q1T[(g * 4 + j) * 64:(g * 4 + j + 1) * 64, :].rearrange("d (i r) -> d i r", r=128))
        kl = P.nxt("kl")
        P.dma("sp", [kl], kl[:, 128:128 + NL], [K1T], K1T[g * 64:(g + 1) * 64, 0:NL])
        kcand = P.nxt("kcand")
        P.dma("sp", [kcand], kcand[:], [Kb_all], Kb_all.t.rearrange("(r c) k -> c r k", r=4)[g * 64:(g + 1) * 64])
        for (dst0, src0, so) in ((0, 128, 0), (128 + NL, 0, 4)):
            P.ts("dve", [kl], kl[:, dst0:dst0 + 128], [kcand, sel], kcand[:, 0, src0:src0 + 128], sel[0:64, so:so + 1], None, ALU.mult)
            for r in range(1, 4):
                P.stt("dve", [kl], kl[:, dst0:dst0 + 128], [kcand, sel, kl], kcand[:, r, src0:src0 + 128], sel[0:64, so + r:so + r + 1],
                      kl[:, dst0:dst0 + 128], ALU.mult, ALU.add)
        kc = P.nxt("kc")
        P.dma("sp", [kc], kc[:], [K1T], K1T[g * 64:(g + 1) * 64, NL:NT])
        vl = P.nxt("vl")
        P.dma("sp", [vl], vl[:, 1:1 + NB1, 1:65], [V1], V1[0:NL, g * 64:(g + 1) * 64].rearrange("(t p) d -> p t d", p=128))
        vcand = P.nxt("vcand")
        P.dma("sp", [vcand], vcand[:], [Vb_all], Vb_all.t.rearrange("(r f p) c -> p r f c", r=4, f=2)[:, :, :, g * 64:(g + 1) * 64])
        for (dt_, f_, so) in ((0, 1, 0), (NB1 + 1, 0, 4)):
            P.ts("dve", [vl], vl[:, dt_, 1:65], [vcand, sel], vcand[:, 0, f_, :], sel[:, so:so + 1], None, ALU.mult)
            for r in range(1, 4):
                P.stt("dve", [vl], vl[:, dt_, 1:65], [vcand, sel, vl], vcand[:, r, f_, :], sel[:, so + r:so + r + 1], vl[:, dt_, 1:65],
                      ALU.mult, ALU.add)
        vc = P.nxt("vc")
        P.dma("sp", [vc], vc[:, :, 1:65], [V1], V1[NL:NT, g * 64:(g + 1) * 64].rearrange("(t p) d -> p t d", p=128))
        def emit_s(i):
            q_ap = qg[:, i * 512:(i + 1) * 512]
            tiles = []
            for (kb, c0) in ((kc, 0), (kc, 128), (kl, i * 128), (kl, (i + 1) * 128), (kl, (i + 2) * 128)):
                sp_ = P.nxt("s1b")
                P.mm(sp_, sp_[:, :], kb[:, c0:c0 + 128], q_ap, True, True, [kb, qg])
                tiles.append(sp_)
            return tiles

        def emit_exp(i, tiles):
            ps_ = []
            for j, sp_ in enumerate(tiles):
                pt = P.nxt("p1")
                P.act([pt], pt[:], [sp_], sp_[:], AF.Exp, scale=scale)
                if j == 2:
                    P.tt("dve", [pt], pt[:], [pt, mk], pt[:], mk[:, 2, :] if i == 0 else mk[:, 0, :], ALU.mult)
                if j == 4:
                    P.tt("dve", [pt], pt[:], [pt, mk], pt[:], mk[:, 3, :] if i == NB1 - 1 else mk[:, 1, :], ALU.mult)
                ps_.append(pt)
            return ps_

        def emit_pv(i, ps_, g=g, vc=vc, vl=vl):
            o_ps = P.nxt("ops")
            vs = (vc[:, 0, :], vc[:, 1, :], vl[:, i, :], vl[:, i + 1, :], vl[:, i + 2, :])
            vb = (vc, vc, vl, vl, vl)
            for j in range(5):
                P.mm(o_ps, o_ps[0:65, :], vs[j], ps_[j][:], j == 0, j == 4, [vb[j], ps_[j]])
            lsum = P.nxt("lsum")
            P.tt("dve", [lsum], lsum[:], [o_ps, sk], o_ps[0:1, :], sk[:, g * 512:(g + 1) * 512], ALU.add)
            return (i, o_ps, lsum)

        def emit_rl(i, o_ps, lsum):
            lnl = P.nxt("lsum")
            P.act([lnl], lnl[:], [lsum], lsum[:], AF.Ln)
            rl = P.nxt("rl")
            P.act([rl], rl[:], [lnl], lnl[:], AF.Exp, scale=-1.0)
            rh = P.nxt("rhl")
            P.cp("dve", [rh], rh[:, 0, :], [rl], rl[:])
            P.tt("dve", [rl], rl[:], [rl, rh], rl[:], rh[:, 0, :], ALU.subtract)
            P.cp("dve", [rh], rh[:, 1, :], [rl], rl[:])
            return (i, o_ps, rh)

        def emit_fin(i, o_ps, rl, g=g):
            b_ps = P.nxt("bps")
            P.mm(b_ps, b_ps[0:65, :], onesb16[:, 0:65], rl[:, 0, :], True, False, [onesb16, rl])
            P.mm(b_ps, b_ps[0:65, :], onesb16[:, 0:65], rl[:, 1, :], False, True, [onesb16, rl])
            bsb = P.nxt("bsb")
            P.cp("act", [bsb], bsb[:], [b_ps], b_ps[0:65, :])
            ao = P.nxt("ao")
            P.tt("dve", [ao], ao[:], [o_ps, bsb], o_ps[0:65, :], bsb[:], ALU.mult)
            for j in range(4):
                P.dma("sp", [catT1], catT1[(g * 4 + j) * 64:(g * 4 + j + 1) * 64, i * 128:(i + 1) * 128], [ao], ao[1:65, j * 128:(j + 1) * 128])

        prev = None
        pfin = None
        for i in range(NB1 + 1):
            tiles = emit_s(i) if i < NB1 else None
            o_ = emit_pv(*prev) if prev is not None else None
            pexp = emit_exp(i, tiles) if i < NB1 else None
            if o_ is not None:
                nfin = emit_rl(*o_)
                if pfin is not None:
                    emit_fin(*pfin)
                pfin = nfin
            prev = (i, pexp) if i < NB1 else None
        emit_fin(*pfin)
    P.phase_end()


GRP = [[0, 1, 2, 3], [4, 5, 6, 7]]


def build_fused():
    ctx = ExitStack()
    nc, P = new_prog(ctx)
    T = {}

    def di(n, s, dt=F32):
        T[n] = P.dram(n, s, dt, "ExternalInput")

    def dn(n, s, dt=F32):
        T[n] = P.dram(n, s, dt)
        T[n].relaxed = not n.startswith("xacc")

    di("xrows", [NT, D]); di("xhalo", [128, D]); di("hmask", [128, 2]); di("ccols", [128, 16])
    for l in (0, 1):
        di("modw%d" % l, [D, 6144]); di("modb%d" % l, [1, 6144]); di("n1g%d" % l, [128, D]); di("n2g%d" % l, [128, D])
        di("w_out%d" % l, [D, D]); di("rw%d" % l, [D, NE])
        di("wg%d" % l, [NE, D, D]); di("wu%d" % l, [NE, D, D]); di("wd%d" % l, [NE, D, D])
    di("fing", [128, D])
    di("w_in", [D, 1984]); di("convp", [128, 16]); di("qg", [128, 3]); di("w_uq", [256, 768]); di("w_uqs", [256, 768])
    di("w_ukv", [128, 1024]); di("ropeq", [96, 2, NL]); di("ropek", [32, 2, NL])
    di("wqkv", [D, 1536]); di("wqks", [D, 1280]); di("rope1", [128, 2, NL])
    di("masks", [128, 4, 512], BF16); di("sinkr", [1, 2048]); di("selh", [128, 8])
    T["out"] = P.dram("out", [NL, D], F32, "ExternalOutput")
    dn("QT", [8, 96, NT], BF16); dn("KTn", [512, NT], BF16); dn("KTr", [32, NT], BF16); dn("Vt", [NT, 512], BF16)
    dn("convT", [512, NT], BF16)
    for h_ in range(8):
        dn("KTn_all%d" % h_, [4 * 64, NT], BF16)
    dn("KTr_all", [4 * 32, NT], BF16); dn("Vt_all", [4, 4 * 1024, 512], BF16)
    dn("attT", [512, NT], BF16)
    dn("xacc0", [NT + 128, D]); dn("h2_0", [NT, D], BF16); dn("aff_0", [NT, NE]); dn("affl_0", [NL, NE]); dn("affall_0", [4 * NL, NE])
    dn("Q1T", [1024, NL], BF16); dn("K1T", [256, NT], BF16); dn("V1", [NT, 256], BF16)
    dn("Kb", [256, 256], BF16); dn("Vb", [256, 256], BF16); dn("Kb_all", [1024, 256], BF16); dn("Vb_all", [1024, 256], BF16)
    dn("catT1", [1024, NL], BF16)
    dn("xacc1", [NL + 128, D]); dn("h2_1", [NL, D], BF16); dn("aff_1", [NL, NE]); dn("affall_1", [4 * NL, NE])
    T["affl_1"] = T["aff_1"]
    T["xres0"] = T["xrows"]
    T["xres1"] = T["xacc0"]

    phase_A(P, T)
    P.cc_allgather(T["KTr"], T["KTr_all"], GRP)
    for k in range(4):
        P.cc_allgather(T["Vt"], T["Vt_all"], GRP, T["Vt"].t[k * 1024:(k + 1) * 1024, :], T["Vt_all"].t[k])
    for h in range(8):
        P.cc_allgather(T["KTn"], T["KTn_all%d" % h], GRP, T["KTn"].t[h * 64:(h + 1) * 64, :], T["KTn_all%d" % h].t)
    phase_B(P, T)
    phase_post(P, T, 0, NT, [(T["convT"], 4), (T["attT"], 4)])
    P.cc_allgather(T["affl_0"], T["affall_0"], GRP)
    phase_moe(P, T, 0)
    phase_qkv1(P, T)
    P.cc_allgather(T["Kb"], T["Kb_all"], GRP)
    P.cc_allgather(T["Vb"], T["Vb_all"], GRP)
    phase_attn1(P, T)
    phase_post(P, T, 1, NL, [(T["catT1"], 8)])
    P.cc_allgather(T["aff_1"], T["affall_1"], GRP)
    phase_moe(P, T, 1)
    n = P.finalize()
    return nc, ctx, n


def prep_all(inp):
    bf = ml_dtypes.bfloat16
    maps = prep_A(inp)
    w = inp["swa_w_qkv"][0]
    p64 = _swap_perm(64)
    ws = np.empty((D, 1280), np.float32)
    for h in range(20):
        ws[:, h * 64:(h + 1) * 64] = w[:, h * 64 + p64]
    r = np.arange(128)
    tri_prev = (r[:, None] >= r[None, :]).astype(np.float32)
    tri_next = (r[:, None] <= r[None, :]).astype(np.float32)
    sinkr = np.ascontiguousarray(np.repeat(inp["swa_sink"][0], 128)[None, :].astype(np.float32))
    shared = {}
    for l in (0, 1):
        shared["modw%d" % l] = np.ascontiguousarray(inp["mod_w"][l])
        shared["modb%d" % l] = np.ascontiguousarray(inp["mod_b"][l][None, :])
        shared["n1g%d" % l] = _rep(inp["norm1_g"][l])
        shared["n2g%d" % l] = _rep(inp["norm2_g"][l])
        shared["rw%d" % l] = np.ascontiguousarray(inp["router_w"][l])
        shared["wg%d" % l] = np.ascontiguousarray(inp["exp_w_gate"][l])
        shared["wu%d" % l] = np.ascontiguousarray(inp["exp_w_up"][l])
        shared["wd%d" % l] = np.ascontiguousarray(inp["exp_w_down"][l])
    shared["w_out0"] = np.ascontiguousarray(inp["ab_w_out"][0])
    shared["w_out1"] = np.ascontiguousarray(inp["swa_w_out"][0])
    shared["fing"] = _rep(inp["final_g"])
    shared["wqkv"] = np.ascontiguousarray(w)
    shared["wqks"] = ws
    shared["sinkr"] = sinkr
    out = []
    for core in range(8):
        b, q = core // 4, core % 4
        m = dict(maps[core])
        m["modw0"] = m.pop("modw"); m["modb0"] = m.pop("modb"); m["n1g0"] = m.pop("n1g")
        m.update(shared)
        C, S = _rope_tables(np.arange(q * NL, (q + 1) * NL), 64)
        rp = np.empty((128, 2, NL), np.float32)
        rp[:64, 0] = C; rp[64:, 0] = C; rp[:64, 1] = S; rp[64:, 1] = S
        m["rope1"] = rp
        mk = np.zeros((128, 4, 512), np.float32)
        mk[:, 0] = np.tile(tri_prev, (1, 4))
        mk[:, 1] = np.tile(tri_next, (1, 4))
        mk[:, 2] = mk[:, 0] if q > 0 else 0.0
        mk[:, 3] = mk[:, 1] if q < 3 else 0.0
        m["masks"] = mk.astype(bf)
        sel = np.zeros((128, 8), np.float32)
        if q > 0:
            sel[:, q - 1] = 1.0
        if q < 3:
            sel[:, 4 + q + 1] = 1.0
        m["selh"] = sel
        out.append(m)
    return out


def kernel(**inputs):
    inp = {k: np.asarray(v) for k, v in inputs.items()}
    maps = prep_all(inp)
    nc, ctx, n = build_fused()
    res = run_bass_kernel_spmd(nc, maps, core_ids=list(range(8)))
    ctx.close()
    out = np.empty((2, 4 * NL, D), np.float32)
    for c in range(8):
        out[c // 4, (c % 4) * NL:(c % 4 + 1) * NL] = np.asarray(res.results[c]["out"])
    return out
```

```python
import numpy as np
import ml_dtypes
from contextlib import ExitStack
import concourse.bass as bass
import concourse.mybir as mybir
from concourse.bass_utils import run_bass_kernel_spmd

F32 = mybir.dt.float32
BF16 = mybir.dt.bfloat16
I32 = mybir.dt.int32
ALU = mybir.AluOpType
AF = mybir.ActivationFunctionType
AX = mybir.AxisListType

D = 1024
NL = 4096
NCX = 256
NT = NL + NCX
EPS = 1e-6
NE = 16
ENGS = ["pe", "act", "dve", "pool", "sp"]
NDMA_SEM = 56


class Buf:
    __slots__ = ("name", "last_w", "readers", "t", "relaxed")

    def __init__(self, name, t=None):
        self.name = name
        self.last_w = None
        self.readers = []
        self.t = t
        self.relaxed = False

    def __getitem__(self, k):
        return self.t[k]


class Op:
    __slots__ = ("eng", "fn", "deps", "dma", "idx", "need_inc", "sem", "val", "waits", "cc")


class Prog:
    def __init__(self, nc, ctx):
        self.nc = nc
        self.ctx = ctx
        self.ops = []
        self.rings = {}
        self.phase = "p"
        self.pstack = None

    def phase_begin(self, name):
        self.phase = name
        self.pstack = ExitStack()
        self.rings = {}

    def phase_end(self):
        self.ops.append(None)
        self.pstack.close()
        self.pstack = None

    def sb(self, name, shape, dt):
        t = self.pstack.enter_context(self.nc.sbuf_tensor("%s_%s" % (self.phase, name), shape, dt))
        return Buf(name, t)

    def ps(self, name, shape, dt):
        t = self.pstack.enter_context(self.nc.psum_tensor("%s_%s" % (self.phase, name), shape, dt))
        return Buf(name, t)

    def cc_allgather(self, src, dst, groups, src_ap=None, dst_ap=None):
        src_ap = src.t if src_ap is None else src_ap
        dst_ap = dst.t if dst_ap is None else dst_ap
        o = self.op("pool", lambda e: e.collective_compute("AllGather", ALU.bypass, replica_groups=groups,
                                                           ins=[src_ap], outs=[dst_ap]), reads=[src], writes=[dst])
        o.cc = True
        return o

    def ring(self, name, shape, dt, n, psum=False):
        bufs = [(self.ps if psum else self.sb)("%s%d" % (name, i), shape, dt) for i in range(n)]
        self.rings[name] = [bufs, 0]

    def nxt(self, name):
        r = self.rings[name]
        b = r[0][r[1] % len(r[0])]
        r[1] += 1
        return b

    def dram(self, name, shape, dt, kind=None):
        if kind is None:
            t = self.nc.dram_tensor(name, list(shape), dt)
        else:
            t = self.nc.dram_tensor(name, list(shape), dt, kind=kind)
        return Buf(name, t.ap())

    def op(self, eng, fn, reads=(), writes=(), dma=False, extra=()):
        o = Op()
        o.eng = eng
        o.fn = fn
        o.dma = dma
        o.idx = len(self.ops)
        deps = set()
        for b in reads:
            if b.last_w is not None:
                deps.add(b.last_w)
        for b in writes:
            if b.relaxed:
                continue
            if b.last_w is not None:
                deps.add(b.last_w)
            for r in b.readers:
                deps.add(r)
        for x in extra:
            deps.add(x.idx)
        deps.discard(o.idx)
        o.deps = deps
        for b in reads:
            b.readers.append(o.idx)
        for b in writes:
            b.last_w = o.idx
            b.readers = []
        o.need_inc = False
        o.cc = False
        self.ops.append(o)
        return o

    def finalize(self):
        nc = self.nc
        ctx = self.ctx
        ops = self.ops

        def pe_pe(p, o):
            return p.eng == "pe" and o.eng == "pe" and not p.dma and not o.dma

        last = {}
        for o in ops:
            if o is None:
                for e_, lo in last.items():
                    lo.need_inc = True
                continue
            last[o.eng] = o
            for d in o.deps:
                if not pe_pe(ops[d], o):
                    ops[d].need_inc = True
            if o.dma or o.cc:
                o.need_inc = True
        eng_sem = {e: ctx.enter_context(nc.semaphore("s_" + e)) for e in ENGS}
        dma_sems = [ctx.enter_context(nc.semaphore("d%d" % i)) for i in range(NDMA_SEM)]
        cc_sem = ctx.enter_context(nc.semaphore("cc_sem"))
        cnt = {e: 0 for e in ENGS}
        dcnt = [0] * NDMA_SEM
        ccnt = 0
        dma_rr = 0
        seen = {e: {} for e in ENGS}
        pending = {e: None for e in ENGS}
        for o in ops:
            if o is None:
                snap = [(("e", e), eng_sem[e], cnt[e]) for e in ENGS if cnt[e] > 0]
                snap += [(("d", k), dma_sems[k], dcnt[k]) for k in range(NDMA_SEM) if dcnt[k] > 0]
                if ccnt > 0:
                    snap.append((("cc",), cc_sem, ccnt))
                for e in ENGS:
                    pending[e] = snap
                continue
            waits = []
            s = seen[o.eng]
            if pending[o.eng] is not None:
                for key, sem, val in pending[o.eng]:
                    if s.get(key, 0) < val:
                        s[key] = val
                        waits.append((sem, val))
                pending[o.eng] = None
            for d in sorted(o.deps):
                p = ops[d]
                if pe_pe(p, o):
                    continue
                key, sem = p.sem
                if s.get(key, 0) < p.val:
                    s[key] = p.val
                    waits.append((sem, p.val))
            if o.cc:
                ccnt += 1
                o.sem = (("cc",), cc_sem)
                o.val = ccnt
            elif o.dma:
                k = dma_rr
                dma_rr = (dma_rr + 1) % NDMA_SEM
                key = ("d", k)
                if dcnt[k] > 0 and s.get(key, 0) < dcnt[k]:
                    s[key] = dcnt[k]
                    waits.append((dma_sems[k], dcnt[k]))
                dcnt[k] += 16
                o.sem = (key, dma_sems[k])
                o.val = dcnt[k]
            elif o.need_inc:
                cnt[o.eng] += 1
                o.sem = (("e", o.eng), eng_sem[o.eng])
                o.val = cnt[o.eng]
            o.waits = waits
        final_waits = [(dma_sems[k], dcnt[k]) for k in range(NDMA_SEM) if dcnt[k] > 0]
        final_waits += [(eng_sem[e], cnt[e]) for e in ENGS if cnt[e] > 0]
        if ccnt > 0:
            final_waits.append((cc_sem, ccnt))
        per = {e: [o for o in ops if o is not None and o.eng == e] for e in ENGS}
        engmap = {"pe": "tensor", "act": "scalar", "dve": "vector", "pool": "gpsimd", "sp": "sync"}
        with nc.Block() as block:
            for e in ENGS:
                def body(eng, lst=per[e], final=(e == "sp")):
                    for o in lst:
                        for (sem, val) in o.waits:
                            eng.wait_ge(sem, val)
                        ins = o.fn(eng)
                        if o.cc:
                            ins.then_inc(o.sem[1])
                        elif o.need_inc:
                            ins.then_inc(o.sem[1], 16 if o.dma else 1)
                    if final:
                        for (sem, val) in final_waits:
                            eng.wait_ge(sem, val)
                getattr(block, engmap[e])(body)
        return len(per["pe"]) + len(per["act"]) + len(per["dve"]) + len(per["pool"]) + len(per["sp"])

    def mm(self, ps, out, lhsT, rhs, start, stop, rd):
        self.op("pe", lambda e: e.matmul(out, lhsT=lhsT, rhs=rhs, start=start, stop=stop), reads=rd, writes=[ps])

    def tr(self, ps, out, in_, ident, rd):
        self.op("pe", lambda e: e.transpose(out, in_, ident), reads=rd, writes=[ps])

    def act(self, wr, out, rd, in_, func, scale=1.0, bias=None, accum=None):
        kw = {}
        if bias is not None:
            kw["bias"] = bias
        if accum is not None:
            kw["accum_out"] = accum
        self.op("act", lambda e: e.activation(out=out, in_=in_, func=func, scale=scale, **kw), reads=rd, writes=wr)

    def ts(self, eng, wr, out, rd, in0, s1, s2, op0, op1=None, accum=None):
        kw = {}
        if op1 is not None:
            kw["op1"] = op1
        if accum is not None:
            kw["accum_out"] = accum
        self.op(eng, lambda e: e.tensor_scalar(out, in0, s1, s2, op0, **kw), reads=rd, writes=wr)

    def tt(self, eng, wr, out, rd, in0, in1, op):
        self.op(eng, lambda e: e.tensor_tensor(out, in0, in1, op), reads=rd, writes=wr)

    def stt(self, eng, wr, out, rd, in0, scalar, in1, op0, op1):
        self.op(eng, lambda e: e.scalar_tensor_tensor(out, in0, scalar, in1, op0, op1), reads=rd, writes=wr)

    def cp(self, eng, wr, out, rd, in_):
        if eng == "act":
            self.op("act", lambda e: e.copy(out, in_), reads=rd, writes=wr)
        else:
            self.op(eng, lambda e: e.tensor_copy(out, in_), reads=rd, writes=wr)

    def memset(self, eng, wr, out, val):
        self.op(eng, lambda e: e.memset(out, val), writes=wr)

    def dma(self, q, wr, out, rd, in_):
        self.op(q, lambda e: e.dma_start(out=out, in_=in_), reads=rd, writes=wr, dma=True)

    def red(self, eng, wr, out, rd, in_, op, axis=AX.X):
        self.op(eng, lambda e: e.tensor_reduce(out, in_, axis, op), reads=rd, writes=wr)

    def make_ident(self):
        idf = self.sb("idf", [128, 128], F32)
        idb = self.sb("idb", [128, 128], BF16)
        self.memset("pool", [idf], idf[:], 1.0)
        self.op("pool", lambda e: e.affine_select(idf[:], idf[:], [[-1, 128]], ALU.is_equal, 0.0, base=0,
                                                 channel_multiplier=1), reads=[idf], writes=[idf])
        self.cp("dve", [idb], idb[:], [idf], idf[:])
        self.idf, self.idb = idf, idb
        eb = self.sb("epsb", [128, 1], F32)
        self.memset("dve", [eb], eb[:], EPS)
        self.epsb = eb

    def rstd(self, out_buf, out, ssq_buf, ssq, n, scale):
        tmp = self.nxt("rstd_tmp")
        self.act([tmp], tmp[:, 0:n], [ssq_buf, self.epsb], ssq, AF.Ln, scale=scale, bias=self.epsb[:, 0:1])
        self.act([out_buf], out, [tmp], tmp[:, 0:n], AF.Exp, scale=-0.5)


def new_prog(ctx):
    nc = bass.Bass("TRN2", target_bir_lowering=False)
    P = Prog(nc, ctx)
    return nc, P


def load_bf16(P, name, src_ap, shape):
    b = P.sb(name, shape, BF16)
    P.dma("pool", [b], b[:], [], src_ap)
    return b


def tile_T(P, xrows, row0, A, Bm):
    xt = P.nxt("xt")
    P.dma("pool", [xt], xt[:], [xrows], xrows[row0:row0 + 128, :])
    junk = P.nxt("junk")
    ssq = P.nxt("ssq")
    P.act([junk, ssq], junk[:], [xt], xt[:], AF.Square, accum=ssq[:, 0:1])
    rs = P.nxt("rs")
    P.rstd(rs, rs[:, 0:1], ssq, ssq[:, 0:1], 1, 1.0 / D)
    h32 = P.nxt("h32")
    P.stt("dve", [h32], h32[:], [xt, rs, A], xt[:], rs[:, 0:1], A[:], ALU.mult, ALU.mult)
    hb = P.nxt("hb")
    P.tt("dve", [hb], hb[:], [h32, Bm], h32[:], Bm[:], ALU.add)
    tp = P.nxt("tp")
    for k in range(8):
        P.tr(tp, tp[:, k, :], hb[:, k * 128:(k + 1) * 128], P.idb[:], [hb, P.idb])
    return tp


def compute_mod(P, ccols_ap, modw_ap, modb_ap, col0, ncols, names):
    nseg = ncols // 1024
    cc = P.sb("cc", [128, 16], F32)
    P.dma("sp", [cc], cc[:], [], ccols_ap)
    sc = P.sb("sc", [128, 16], F32)
    P.act([sc], sc[:], [cc], cc[:], AF.Silu)
    ones2 = P.sb("ones2", [1, 2], F32)
    P.memset("dve", [ones2], ones2[:], 1.0)
    mb = P.sb("mb", [1, ncols], F32)
    P.dma("sp", [mb], mb[:], [], modb_ap[0:1, col0:col0 + ncols])
    modrows = P.sb("modrows", [2, ncols], F32)
    MWC = 128
    P.ring("mw", [128, 8, MWC], F32, 2)
    for j in range(ncols // MWC):
        mw = P.nxt("mw")
        P.dma("sp", [mw], mw[:], [], modw_ap[:, col0 + j * MWC: col0 + (j + 1) * MWC].rearrange("(k p) n -> p k n", p=128))
        ps = P.nxt("ps")
        for k in range(8):
            P.mm(ps, ps[0:2, 0:MWC], sc[:, 2 * k:2 * k + 2], mw[:, k, :], k == 0, False, [sc, mw])
        P.mm(ps, ps[0:2, 0:MWC], ones2[:], mb[:, j * MWC:(j + 1) * MWC], False, True, [ones2, mb])
        P.cp("dve", [modrows], modrows[:, j * MWC:(j + 1) * MWC], [ps], ps[0:2, 0:MWC])
    sel = P.sb("sel", [2, 2, 128], F32)
    P.memset("dve", [sel], sel[:], 0.0)
    P.memset("dve", [sel], sel[0:1, 0, :], 1.0)
    P.ts("dve", [sel], sel[:, 1, :], [sel], sel[:, 0, :], -1.0, 1.0, ALU.mult, ALU.add)
    out = {}
    for si, nm in enumerate(names):
        reps = []
        for w in range(2):
            rep = P.sb("mod_%s_%d" % (nm, w), [128, 1024], F32)
            for hh in range(2):
                ps = P.nxt("ps")
                P.mm(ps, ps[:, :], sel[:, w, :], modrows[:, si * 1024 + hh * 512: si * 1024 + (hh + 1) * 512], True, True,
                     [sel, modrows])
                P.cp("act", [rep], rep[:, hh * 512:(hh + 1) * 512], [ps], ps[:, :])
            reps.append(rep)
        out[nm] = reps
    return out


def phase_A(P, T):
    P.phase_begin("A")
    xrows, xhalo, hmask, ccols, modw, modb, n1g = T["xrows"], T["xhalo"], T["hmask"], T["ccols"], T["modw0"], T["modb0"], T["n1g0"]
    w_in, convp, qg, w_uq, w_uqs, w_ukv, ropeq, ropek = (T[k] for k in ("w_in", "convp", "qg", "w_uq", "w_uqs", "w_ukv", "ropeq", "ropek"))
    QT, KTn, KTr, Vt, convT = T["QT"], T["KTn"], T["KTr"], T["Vt"], T["convT"]

    P.make_ident()
    P.ring("ps", [128, 512], F32, 6, psum=True)
    P.ring("tp", [128, 8, 128], BF16, 2, psum=True)
    P.ring("xt", [128, D], F32, 2)
    P.ring("junk", [128, D], BF16, 1)
    P.ring("ssq", [128, 1], F32, 2)
    P.ring("rs", [128, 1], F32, 2)
    P.ring("rstd_tmp", [128, 512], F32, 1)
    P.ring("h32", [128, D], F32, 1)
    P.ring("hb", [128, D], BF16, 2)

    mods = compute_mod(P, ccols.t, modw.t, modb.t, 0, 2048, ["sh1", "sc1"])
    g1 = P.sb("g1", [128, D], F32)
    P.dma("sp", [g1], g1[:], [], n1g.t)
    AB = []
    for w in range(2):
        A = mods["sc1"][w]
        P.stt("dve", [A], A[:], [A, g1], A[:], 1.0, g1[:], ALU.add, ALU.mult)
        AB.append((A, mods["sh1"][w]))

    w_in_b = load_bf16(P, "w_in_b", w_in.t.rearrange("(k p) n -> p k n", p=128), [128, 8, 1984])
    w_uq_b = load_bf16(P, "w_uq_b", w_uq.t.rearrange("(k p) n -> p k n", p=128), [128, 2, 768])
    w_uqs_b = load_bf16(P, "w_uqs_b", w_uqs.t.rearrange("(k p) n -> p k n", p=128), [128, 2, 768])
    w_ukv_b = load_bf16(P, "w_ukv_b", w_ukv.t, [128, 1024])
    cvp = P.sb("cvp", [128, 16], F32)
    P.dma("sp", [cvp], cvp[:], [], convp.t)
    qgs = P.sb("qgs", [128, 3], F32)
    P.dma("sp", [qgs], qgs[:], [], qg.t)
    hm = P.sb("hm", [128, 2], F32)
    P.dma("sp", [hm], hm[:], [], hmask.t)
    P.ring("rq", [96, 2, 512], F32, 1)
    P.ring("rk", [32, 2, 512], F32, 1)
    onesb = P.sb("onesb", [128, 128], BF16)
    P.memset("dve", [onesb], onesb[:], 1.0)

    P.ring("hseg", [128, 8, 1022], BF16, 2)
    halo_tmp = P.sb("halo_tmp", [128, 8, 2], BF16)
    tph = tile_T(P, xhalo, 0, AB[0][0], AB[0][1])
    P.cp("act", [halo_tmp], halo_tmp[:], [tph], tph[:, :, 0:2])

    def fill_seg(hseg, G0, G1):
        t_lo = max(0, (G0 - 1) // 128)
        t_hi = min(NL // 128 - 1, (G1 - 2) // 128)
        for t in range(t_lo, t_hi + 1):
            a_ = max(G0, 1 + 128 * t)
            b_ = min(G1, 129 + 128 * t)
            if b_ <= a_:
                continue
            tp = tile_T(P, xrows, t * 128, AB[0][0], AB[0][1])
            P.cp("act", [hseg], hseg[:, :, a_ - G0:b_ - G0], [tp], tp[:, :, a_ - 1 - 128 * t:b_ - 1 - 128 * t])
        if G0 == 0:
            P.cp("dve", [hseg], hseg[:, :, 0:1], [halo_tmp], halo_tmp[:, :, 0:1])
        if G1 == NL + 2:
            P.cp("dve", [hseg], hseg[:, :, NL + 1 - G0:NL + 2 - G0], [halo_tmp], halo_tmp[:, :, 1:2])

    P.ring("gcs", [128, 512], F32, 2)
    P.ring("zw", [128, 512], F32, 2)
    P.ring("ca", [128, 512], F32, 2)
    P.ring("co", [128, 512], BF16, 3)
    P.ring("lat", [128, 512], F32, 3)
    P.ring("sq", [128, 512], BF16, 3)
    P.ring("rstdL", [128, 512], F32, 1)
    P.ring("qn", [128, 2, 512], BF16, 2)
    P.ring("kvn", [128, 512], BF16, 2)
    P.ring("t1", [96, 512], F32, 1)
    P.ring("t2", [96, 512], F32, 1)
    P.ring("qo", [96, 512], BF16, 3)
    P.ring("ko", [128, 512], BF16, 3)
    P.ring("vo", [128, 512], BF16, 3)

    def proj(hT, c0, n, col_lo, col_n):
        ps = P.nxt("ps")
        for k in range(8):
            P.mm(ps, ps[0:col_n, 0:n], w_in_b[:, k, col_lo:col_lo + col_n], hT[:, k, c0:c0 + n], k == 0, k == 7,
                 [w_in_b, hT])
        return ps

    def latent_norm(pss, nchunk, n, gcol0, out_ring):
        lats, sqs = [], []
        for c in range(nchunk):
            lt = P.nxt("lat")
            P.cp("act", [lt], lt[:, 0:n], [pss[c]], pss[c][:, 0:n])
            sq = P.nxt("sq")
            P.act([sq], sq[:, 0:n], [pss[c]], pss[c][:, 0:n], AF.Square)
            lats.append(lt)
            sqs.append(sq)
        ps = P.nxt("ps")
        for c in range(nchunk):
            P.mm(ps, ps[:, 0:n], onesb[:], sqs[c][:, 0:n], c == 0, c == nchunk - 1, [onesb, sqs[c]])
        rl = P.nxt("rstdL")
        P.rstd(rl, rl[:, 0:n], ps, ps[:, 0:n], n, 1.0 / (128 * nchunk))
        ob = P.nxt(out_ring)
        for c in range(nchunk):
            o_ap = ob[:, c, 0:n] if nchunk > 1 else ob[:, 0:n]
            P.stt("dve", [ob], o_ap, [lats[c], qgs, rl], lats[c][:, 0:n], qgs[:, gcol0 + c:gcol0 + c + 1], rl[:, 0:n],
                  ALU.mult, ALU.mult)
        return ob

    def block(hT, c0, n, tok0, is_ctx, first, last):
        no = n - 2
        if not is_ctx:
            rq = P.nxt("rq")
            P.dma("pool", [rq], rq[:, :, 0:no], [], ropeq[:, :, tok0:tok0 + no])
            rk = P.nxt("rk")
            P.dma("pool", [rk], rk[:, :, 0:no], [], ropek[:, :, tok0:tok0 + no])
        for c in range(4):
            ps_gc = proj(hT, c0, n, 512 + c * 128, 128)
            gcs = P.nxt("gcs")
            P.cp("act", [gcs], gcs[:, 0:n], [ps_gc], ps_gc[:, 0:n])
            ps_u = proj(hT, c0, n, 1024 + c * 128, 128)
            zw = P.nxt("zw")
            P.tt("dve", [zw], zw[:, 0:n], [ps_u, gcs], ps_u[:, 0:n], gcs[:, 0:n], ALU.mult)
            if is_ctx:
                P.memset("dve", [zw], zw[:, 0:1], 0.0)
                P.memset("dve", [zw], zw[:, n - 1:n], 0.0)
            else:
                if first:
                    P.ts("dve", [zw], zw[:, 0:1], [zw, hm], zw[:, 0:1], hm[:, 0:1], None, ALU.mult)
                if last:
                    P.ts("dve", [zw], zw[:, n - 1:n], [zw, hm], zw[:, n - 1:n], hm[:, 1:2], None, ALU.mult)
            ca = P.nxt("ca")
            P.ts("dve", [ca], ca[:, 0:no], [zw, cvp], zw[:, 1:n - 1], cvp[:, c * 4 + 1:c * 4 + 2], cvp[:, c * 4 + 3:c * 4 + 4],
                 ALU.mult, ALU.add)
            P.stt("dve", [ca], ca[:, 0:no], [zw, cvp, ca], zw[:, 0:n - 2], cvp[:, c * 4:c * 4 + 1], ca[:, 0:no], ALU.mult, ALU.add)
            P.stt("dve", [ca], ca[:, 0:no], [zw, cvp, ca], zw[:, 2:n], cvp[:, c * 4 + 2:c * 4 + 3], ca[:, 0:no], ALU.mult, ALU.add)
            ps_gb = proj(hT, c0, n, c * 128, 128)
            co = P.nxt("co")
            P.tt("dve", [co], co[:, 0:no], [ps_gb, ca], ps_gb[:, 1:n - 1], ca[:, 0:no], ALU.mult)
            P.dma("sp", [convT], convT[c * 128:(c + 1) * 128, tok0:tok0 + no], [co], co[:, 0:no])
        pq = [proj(hT, c0, n, 1536 + c * 128, 128) for c in range(2)]
        qn = latent_norm(pq, 2, n, 0, "qn")
        for h in range(8):
            psq = P.nxt("ps")
            for k in range(2):
                P.mm(psq, psq[0:96, 0:n], w_uq_b[:, k, h * 96:(h + 1) * 96], qn[:, k, 0:n], k == 0, k == 1, [w_uq_b, qn])
            qo = P.nxt("qo")
            if is_ctx:
                P.cp("act", [qo], qo[:, 0:no], [psq], psq[0:96, 1:n - 1])
            else:
                pss = P.nxt("ps")
                for k in range(2):
                    P.mm(pss, pss[0:96, 0:n], w_uqs_b[:, k, h * 96:(h + 1) * 96], qn[:, k, 0:n], k == 0, k == 1, [w_uqs_b, qn])
                t1 = P.nxt("t1")
                P.tt("dve", [t1], t1[:, 0:no], [psq, rq], psq[0:96, 1:n - 1], rq[:, 0, 0:no], ALU.mult)
                t2 = P.nxt("t2")
                P.tt("dve", [t2], t2[:, 0:no], [pss, rq], pss[0:96, 1:n - 1], rq[:, 1, 0:no], ALU.mult)
                P.tt("dve", [qo], qo[:, 0:no], [t1, t2], t1[:, 0:no], t2[:, 0:no], ALU.add)
            P.dma("sp", [QT], QT[h, :, tok0:tok0 + no], [qo], qo[:, 0:no])
        pk = [proj(hT, c0, n, 1792, 128)]
        kvn = latent_norm(pk, 1, n, 2, "kvn")
        for c in range(4):
            psk = P.nxt("ps")
            P.mm(psk, psk[:, 0:n], w_ukv_b[:, c * 128:(c + 1) * 128], kvn[:, 0:n], True, True, [w_ukv_b, kvn])
            ko = P.nxt("ko")
            P.cp("act", [ko], ko[:, 0:no], [psk], psk[:, 1:n - 1])
            P.dma("sp", [KTn], KTn[c * 128:(c + 1) * 128, tok0:tok0 + no], [ko], ko[:, 0:no])
        t0 = 0
        while t0 < no:
            m = min(128, no - t0)
            psv = P.nxt("ps")
            P.mm(psv, psv[0:m, :], kvn[:, 1 + t0:1 + t0 + m], w_ukv_b[:, 512:1024], True, True, [kvn, w_ukv_b])
            vo = P.nxt("vo")
            P.cp("act", [vo], vo[0:m, :], [psv], psv[0:m, :])
            P.dma("sp", [Vt], Vt[tok0 + t0:tok0 + t0 + m, :], [vo], vo[0:m, :])
            t0 += m
        psr = proj(hT, c0, n, 1920, 32)
        ko = P.nxt("ko")
        if is_ctx:
            P.cp("act", [ko], ko[0:32, 0:no], [psr], psr[0:32, 1:n - 1])
        else:
            psrs = proj(hT, c0, n, 1952, 32)
            t1 = P.nxt("t1")
            P.tt("dve", [t1], t1[0:32, 0:no], [psr, rk], psr[0:32, 1:n - 1], rk[:, 0, 0:no], ALU.mult)
            t2 = P.nxt("t2")
            P.tt("dve", [t2], t2[0:32, 0:no], [psrs, rk], psrs[0:32, 1:n - 1], rk[:, 1, 0:no], ALU.mult)
            P.tt("dve", [ko], ko[0:32, 0:no], [t1, t2], t1[0:32, 0:no], t2[0:32, 0:no], ALU.add)
        P.dma("sp", [KTr], KTr[:, tok0:tok0 + no], [ko], ko[0:32, 0:no])

    nb = (NL + 509) // 510
    for sg in range((nb + 1) // 2):
        G0 = 1020 * sg
        G1 = min(G0 + 1022, NL + 2)
        hseg = P.nxt("hseg")
        fill_seg(hseg, G0, G1)
        for j in (2 * sg, 2 * sg + 1):
            if j >= nb:
                continue
            c0 = 510 * j
            n = min(512, NL + 2 - c0)
            block(hseg, c0 - G0, n, c0, False, j == 0, j == nb - 1)
    hseg = P.nxt("hseg")
    for t in range(2):
        tp = tile_T(P, xrows, NL + t * 128, AB[1][0], AB[1][1])
        P.cp("act", [hseg], hseg[:, :, 1 + t * 128:129 + t * 128], [tp], tp[:])
    P.cp("dve", [hseg], hseg[:, :, 0:1], [hseg], hseg[:, :, 1:2])
    P.cp("dve", [hseg], hseg[:, :, 257:258], [hseg], hseg[:, :, 1:2])
    block(hseg, 0, 258, NL, True, True, True)
    P.phase_end()


def _swap_perm(dim):
    q = dim // 4
    d = np.arange(dim)
    return np.where((d % (2 * q)) < q, d + q, d - q)


def _rope_tables(pos, dim):
    half = dim // 2
    q = dim // 4
    freqs = (10000.0 ** (-np.arange(0, half, 2, dtype=np.float32) / np.float32(half))).astype(np.float32)
    row = (pos // 64).astype(np.float32)
    col = (pos % 64).astype(np.float32)
    ang = np.concatenate([row[:, None] * freqs, col[:, None] * freqs], axis=-1).astype(np.float32)
    cos, sin = np.cos(ang).astype(np.float32), np.sin(ang).astype(np.float32)
    d = np.arange(dim)
    j = (d // (2 * q)) * q + d % q
    sign = np.where((d % (2 * q)) < q, -1.0, 1.0).astype(np.float32)
    C = cos[:, j].T.copy()
    S = (sin[:, j] * sign[None, :]).T.copy()
    return C, S


def _rep(v):
    return np.ascontiguousarray(np.broadcast_to(np.asarray(v, np.float32)[None, :], (128, v.shape[0])))


def _cols(v, k):
    return np.ascontiguousarray(np.asarray(v, np.float32).reshape(k, 128).T)


def prep_A(inp):
    x, c, cx, c_ctx = inp["x"], inp["c"], inp["ctx"], inp["c_ctx"]
    w_in = inp["ab_w_in"][0]
    p32 = _swap_perm(32)
    w_in_ext = np.ascontiguousarray(np.concatenate([w_in, w_in[:, 1920 + p32]], axis=1))
    w_uq = inp["mla_w_uq"][0]
    w_uqs = w_uq.copy()
    for h in range(8):
        w_uqs[:, h * 96 + 64:h * 96 + 96] = w_uq[:, h * 96 + 64 + p32]
    wk = inp["mla_w_ukv"][0].reshape(128, 8, 128)
    w_ukv = np.ascontiguousarray(np.concatenate([wk[:, :, :64].reshape(128, 512), wk[:, :, 64:].reshape(128, 512)], axis=1))
    convp = np.zeros((128, 16), np.float32)
    for ch in range(4):
        for i in range(3):
            convp[:, ch * 4 + i] = inp["conv_w"][0][i, ch * 128:(ch + 1) * 128]
        convp[:, ch * 4 + 3] = inp["conv_b"][0][ch * 128:(ch + 1) * 128]
    qg = np.concatenate([_cols(inp["mla_q_norm_g"][0], 2), _cols(inp["mla_kv_norm_g"][0], 1)], axis=1)
    maps = []
    for core in range(8):
        b, q = core // 4, core % 4
        T0 = q * NL
        xr = np.ascontiguousarray(np.concatenate([x[b, T0:T0 + NL], cx[b]], axis=0))
        xh = np.zeros((128, D), np.float32)
        if q > 0:
            xh[0] = x[b, T0 - 1]
        if q < 3:
            xh[1] = x[b, T0 + NL]
        hm = np.zeros((128, 2), np.float32)
        hm[:, 0] = 1.0 if q > 0 else 0.0
        hm[:, 1] = 1.0 if q < 3 else 0.0
        cc = np.zeros((128, 16), np.float32)
        cc[:, 0::2] = _cols(c[b], 8)
        cc[:, 1::2] = _cols(c_ctx, 8)
        C, S = _rope_tables(np.arange(T0, T0 + NL), 32)
        rq = np.zeros((96, 2, NL), np.float32)
        rq[:64, 0] = 1.0
        rq[64:, 0] = C
        rq[64:, 1] = S
        rk = np.stack([C, S], axis=1)
        maps.append({
            "xrows": xr, "xhalo": xh, "hmask": hm, "ccols": cc,
            "modw": np.ascontiguousarray(inp["mod_w"][0]), "modb": np.ascontiguousarray(inp["mod_b"][0][None, :]),
            "n1g": _rep(inp["norm1_g"][0]), "w_in": w_in_ext, "convp": convp, "qg": np.ascontiguousarray(qg),
            "w_uq": np.ascontiguousarray(w_uq), "w_uqs": np.ascontiguousarray(w_uqs), "w_ukv": w_ukv,
            "ropeq": rq, "ropek": np.ascontiguousarray(rk),
        })
    return maps


NK0 = NCX + 4 * NL
NKT0 = NK0 // 128


def phase_B(P, T):
    P.phase_begin("B")
    QT, KTn, KTr, Vt, KTr_all, Vt_all, attT = (T[k] for k in ("QT", "KTn", "KTr", "Vt", "KTr_all", "Vt_all", "attT"))
    scale = 96.0 ** -0.5

    P.ring("sps", [128, 2, 512], F32, 2, psum=True)
    P.ring("ops", [128, 512], F32, 2, psum=True)
    P.ring("bps", [128, 512], F32, 1, psum=True)
    P.ring("kt", [96, NK0], BF16, 2)
    P.ring("vp", [128, NKT0, 65], BF16, 2)
    P.ring("qt", [96, NT], BF16, 2)
    P.ring("pT", [128, 2, 512], BF16, 4)
    P.ring("rl", [1, 512], F32, 2)
    P.ring("bsb", [65, 512], F32, 2)
    P.ring("ao", [65, 512], BF16, 2)
    ones = P.sb("ones", [1, 128], F32)
    P.memset("dve", [ones], ones[:], 1.0)

    pending = [None]

    def qblock(h, kt_b, vp_b, qt_b, q0, nq, ntile):
        o_ps = P.nxt("ops")
        ng = ntile // 2

        def emit_pv(g, pT):
            for i in range(2):
                t = 2 * g + i
                P.mm(o_ps, o_ps[0:65, 0:nq], vp_b[:, t, :], pT[:, i, 0:nq], t == 0, t == ntile - 1, [vp_b, pT])

        prev = None
        for g in range(ng):
            s_ps = P.nxt("sps")
            for i in range(2):
                t = 2 * g + i
                P.mm(s_ps, s_ps[:, i, 0:nq], kt_b[:, t * 128:(t + 1) * 128], qt_b[:, q0:q0 + nq], True, True, [kt_b, qt_b])
            if prev is not None:
                emit_pv(*prev)
            pT = P.nxt("pT")
            P.act([pT], pT[:, :, 0:nq], [s_ps], s_ps[:, :, 0:nq], AF.Exp, scale=scale)
            prev = (g, pT)
            if g == min(3, ng - 1) and pending[0] is not None:
                pending[0]()
                pending[0] = None
        emit_pv(*prev)

        def fin():
            rl = P.nxt("rl")
            P.op("dve", lambda e: e.reciprocal(rl[:, 0:nq], o_ps[0:1, 0:nq]), reads=[o_ps], writes=[rl])
            b_ps = P.nxt("bps")
            P.mm(b_ps, b_ps[0:65, 0:nq], ones[:, 0:65], rl[:, 0:nq], True, True, [ones, rl])
            bsb = P.nxt("bsb")
            P.cp("act", [bsb], bsb[:, 0:nq], [b_ps], b_ps[0:65, 0:nq])
            ao = P.nxt("ao")
            P.tt("dve", [ao], ao[:, 0:nq], [o_ps, bsb], o_ps[0:65, 0:nq], bsb[:, 0:nq], ALU.mult)
            P.dma("pool", [attT], attT[h * 64:(h + 1) * 64, q0:q0 + nq], [ao], ao[1:65, 0:nq])
        pending[0] = fin

    for b_ in P.rings["vp"][0]:
        P.memset("dve", [b_], b_[:, :, 0:1], 1.0)
    for h in range(8):
        kt_b = P.nxt("kt")
        P.dma("sp", [kt_b], kt_b[0:64, 0:NCX], [KTn], KTn[h * 64:(h + 1) * 64, NL:NT])
        P.dma("sp", [kt_b], kt_b[64:96, 0:NCX], [KTr], KTr[:, NL:NT])
        for r in range(4):
            P.dma("sp", [kt_b], kt_b[0:64, NCX + r * NL:NCX + (r + 1) * NL], [T["KTn_all%d" % h]], T["KTn_all%d" % h][r * 64:(r + 1) * 64, 0:NL])
            P.dma("sp", [kt_b], kt_b[64:96, NCX + r * NL:NCX + (r + 1) * NL], [KTr_all], KTr_all[r * 32:(r + 1) * 32, 0:NL])
        vp_b = P.nxt("vp")
        P.dma("sp", [vp_b], vp_b[:, 0:2, 1:65], [Vt], Vt[NL:NT, h * 64:(h + 1) * 64].rearrange("(t p) d -> p t d", p=128))
        for r in range(4):
            for k in range(4):
                P.dma("sp", [vp_b], vp_b[:, 2 + 32 * r + 8 * k:2 + 32 * r + 8 * k + 8, 1:65], [Vt_all],
                      Vt_all[k, r * 1024:(r + 1) * 1024, h * 64:(h + 1) * 64].rearrange("(t p) d -> p t d", p=128))
        qt_b = P.nxt("qt")
        P.dma("sp", [qt_b], qt_b[:], [QT], QT[h])
        for qb in range(NL // 512):
            qblock(h, kt_b, vp_b, qt_b, qb * 512, 512, NKT0)
        qblock(h, kt_b, vp_b, qt_b, NL, NCX, NCX // 128)
    pending[0]()
    P.phase_end()


def phase_post(P, T, layer, ntok, cats):
    P.phase_begin("post%d" % layer)
    xrows, ccols, modw, modb, n2g, w_out, rw = T["xres%d" % layer], T["ccols"], T["modw%d" % layer], T["modb%d" % layer], T["n2g%d" % layer], T["w_out%d" % layer], T["rw%d" % layer]
    xs1, h2o, affo, affl = T["xacc%d" % layer], T["h2_%d" % layer], T["aff_%d" % layer], T["affl_%d" % layer]

    P.make_ident()
    P.ring("ps", [128, 512], F32, 2, psum=True)
    P.ring("yps", [128, 2, 512], F32, 2, psum=True)
    P.ring("tp32", [128, 8, 128], F32, 1, psum=True)
    P.ring("rstd_tmp", [128, 512], F32, 1)
    mods = compute_mod(P, ccols.t, modw.t, modb.t, 2048, 3072, ["g1", "sh2", "sc2"])
    g2n = P.sb("g2n", [128, D], F32)
    P.dma("sp", [g2n], g2n[:], [], n2g.t)
    for w in range(2):
        A = mods["sc2"][w]
        P.stt("dve", [A], A[:], [A, g2n], A[:], 1.0, g2n[:], ALU.add, ALU.mult)
    w_out_b = load_bf16(P, "w_out_b", w_out.t.rearrange("(k p) n -> p k n", p=128), [128, 8, D])
    rws = P.sb("rws", [128, 8, NE], F32)
    P.dma("sp", [rws], rws[:], [], rw.t.rearrange("(k p) n -> p k n", p=128))

    P.ring("cat", [128, 8, 128], BF16, 2)
    P.ring("xt", [128, D], F32, 2)
    P.ring("xsr", [128, D], F32, 2)
    P.ring("junk", [128, D], BF16, 1)
    P.ring("ssq", [128, 1], F32, 2)
    P.ring("rs", [128, 1], F32, 2)
    P.ring("h32", [128, D], F32, 3)
    P.ring("hb", [128, D], BF16, 2)
    P.ring("hT32", [128, 8, 128], F32, 1)
    P.ring("sm", [128, 4], F32, 3)
    P.ring("ex", [128, NE], F32, 2)
    P.ring("af", [128, NE], F32, 2)

    def p1(t, out):
        w = 0 if t < NL // 128 else 1
        r0 = t * 128
        cat = P.nxt("cat")
        c0_ = 0
        for (cb, nch) in cats:
            P.dma("sp", [cat], cat[:, c0_:c0_ + nch, :], [cb], cb[:, r0:r0 + 128].rearrange("(c p) t -> p c t", p=128))
            c0_ += nch
        xt = P.nxt("xt")
        P.dma("sp", [xt], xt[:], [xrows], xrows[r0:r0 + 128, :])
        y = P.nxt("yps")
        for hh in range(2):
            for k in range(8):
                P.mm(y, y[:, hh, :], cat[:, k, :], w_out_b[:, k, hh * 512:(hh + 1) * 512], k == 0, k == 7, [cat, w_out_b])
            yield
        xs = P.nxt("xsr")
        g1r = mods["g1"][w]
        P.tt("dve", [xs], xs[:], [y, g1r], y[:].rearrange("p a b -> p (a b)"), g1r[:], ALU.mult)
        yield
        P.tt("dve", [xs], xs[:], [xs, xt], xs[:], xt[:], ALU.add)
        P.dma("pool", [xs1], xs1[r0:r0 + 128, :], [xs], xs[:])
        yield
        junk = P.nxt("junk")
        ssq = P.nxt("ssq")
        P.act([junk, ssq], junk[:], [xs], xs[:], AF.Square, accum=ssq[:, 0:1])
        yield
        rs = P.nxt("rs")
        P.rstd(rs, rs[:, 0:1], ssq, ssq[:, 0:1], 1, 1.0 / D)
        yield
        h32 = P.nxt("h32")
        P.stt("dve", [h32], h32[:], [xs, rs, mods["sc2"][w]], xs[:], rs[:, 0:1], mods["sc2"][w][:], ALU.mult, ALU.mult)
        yield
        P.tt("dve", [h32], h32[:], [h32, mods["sh2"][w]], h32[:], mods["sh2"][w][:], ALU.add)
        yield
        hb = P.nxt("hb")
        P.cp("act", [hb], hb[:], [h32], h32[:])
        P.dma("pool", [h2o], h2o[r0:r0 + 128, :], [hb], hb[:])
        out["h32"] = h32
        yield

    def p2(t, h32):
        r0 = t * 128
        tp = P.nxt("tp32")
        for k in range(8):
            P.tr(tp, tp[:, k, :], h32[:, k * 128:(k + 1) * 128], P.idf[:], [h32, P.idf])
        yield
        hT = P.nxt("hT32")
        P.cp("act", [hT], hT[:], [tp], tp[:])
        yield
        lg = P.nxt("ps")
        for k in range(8):
            P.mm(lg, lg[:, 0:NE], hT[:, k, :], rws[:, k, :], k == 0, k == 7, [hT, rws])
        yield
        sm = P.nxt("sm")
        P.red("dve", [sm], sm[:, 0:1], [lg], lg[:, 0:NE], ALU.max)
        P.ts("dve", [sm], sm[:, 1:2], [sm], sm[:, 0:1], -1.0, None, ALU.mult)
        yield
        ex = P.nxt("ex")
        P.act([ex, sm], ex[:], [lg, sm], lg[:, 0:NE], AF.Exp, bias=sm[:, 1:2], accum=sm[:, 2:3])
        yield
        P.op("dve", lambda e, sm=sm: e.reciprocal(sm[:, 3:4], sm[:, 2:3]), reads=[sm], writes=[sm])
        af = P.nxt("af")
        P.ts("dve", [af], af[:], [ex, sm], ex[:], sm[:, 3:4], None, ALU.mult)
        P.dma("pool", [affo], affo[r0:r0 + 128, :], [af], af[:])
        if t < NL // 128 and affl is not affo:
            P.dma("pool", [affl], affl[r0:r0 + 128, :], [af], af[:])
        yield

    ntile = ntok // 128
    o0 = {}
    for _ in p1(0, o0):
        pass
    hprev = o0["h32"]
    for t in range(ntile):
        on = {}
        ga = p1(t + 1, on) if t + 1 < ntile else iter(())
        gb = p2(t, hprev)
        da = db = False
        while not (da and db):
            if not da:
                da = next(ga, "end") == "end"
            if not db:
                db = next(gb, "end") == "end"
        hprev = on.get("h32")
    P.phase_end()


def _ccols(inp, b):
    cc = np.zeros((128, 16), np.float32)
    cc[:, 0::2] = _cols(inp["c"][b], 8)
    cc[:, 1::2] = _cols(inp["c_ctx"], 8)
    return cc


SMAX = 640
CAP_L = 2048
CAP_C = 32
NBIS = 30


def bisect_gen(P, affs, J, e0, e1, kcap, ones_f, name, psbuf, thr):
    ne = e1 - e0
    lo = P.sb(name + "_lo", [128, ne], F32)
    mid = P.sb(name + "_mid", [128, ne], F32)
    cnt = P.sb(name + "_cnt", [128, ne], F32)
    stp = P.sb(name + "_stp", [128, ne], F32)
    cmp = P.sb(name + "_cmp", [128, J, ne], BF16)
    P.memset("dve", [lo], lo[:], 0.0)
    for it in range(NBIS):
        c = 2.0 ** -(it + 1)
        P.ts("dve", [mid], mid[:], [lo], lo[:], c, None, ALU.add)
        P.tt("dve", [cmp], cmp[:], [affs, mid], affs[:, :, e0:e1], mid[:].unsqueeze(1).to_broadcast([128, J, ne]), ALU.is_ge)
        P.red("dve", [cnt], cnt[:], [cmp], cmp[:].rearrange("p j e -> p e j"), ALU.add)
        P.mm(psbuf, psbuf[:, 0:ne], ones_f[:], cnt[:], True, True, [ones_f, cnt])
        P.ts("dve", [stp], stp[:], [psbuf], psbuf[:, 0:ne], float(kcap) - 0.5, c, ALU.is_ge, ALU.mult)
        P.tt("dve", [lo], lo[:], [lo, stp], lo[:], stp[:], ALU.add)
        yield
    P.cp("dve", [thr], thr[:, e0:e1], [lo], lo[:])
    yield


def phase_moe(P, T, layer):
    has_ctx = (layer == 0)
    ntok = NT if has_ctx else NL
    T_ = ntok // 128
    TL = NL // 128
    NS = SMAX // 128
    NSLOT = SMAX + (CAP_C if has_ctx else 0)
    P.phase_begin("moe%d" % layer)
    h2, aff, aff_all, ccols, modw, modb = T["h2_%d" % layer], T["aff_%d" % layer], T["affall_%d" % layer], T["ccols"], T["modw%d" % layer], T["modb%d" % layer]
    wg, wu, wd = T["wg%d" % layer], T["wu%d" % layer], T["wd%d" % layer]
    xacc = T["xacc%d" % layer]
    if not has_ctx:
        fing, outp = T["fing"], T["out"]
    TRASH0 = ntok
    T = T_

    P.make_ident()
    P.ring("ps", [128, 512], F32, 1, psum=True)
    P.ring("tp", [128, 8, 128], BF16, 1, psum=True)
    P.ring("accb", [128, 512], F32, 2, psum=True)
    P.ring("hb", [128, 512], F32, 4, psum=True)
    P.ring("rstd_tmp", [128, 512], F32, 1)

    P.ring("xt", [128, D], F32, 1)
    zt = P.nxt("xt")
    P.memset("dve", [zt], zt[:], 0.0)
    zt_op = P.op("sp", lambda e: e.dma_start(out=xacc[ntok:ntok + 128, :], in_=zt[:]), reads=[zt], writes=[xacc], dma=True)

    mods = compute_mod(P, ccols.t, modw.t, modb.t, 5120, 1024, ["g2"])
    g2rep = mods["g2"]

    ones_f = P.sb("ones_f", [128, 128], F32)
    P.memset("dve", [ones_f], ones_f[:], 1.0)
    ones_b = P.sb("ones_b", [128, 128], BF16)
    P.memset("dve", [ones_b], ones_b[:], 1.0)
    ltf = P.sb("ltf", [128, 128], F32)
    P.memset("pool", [ltf], ltf[:], 1.0)
    P.op("pool", lambda e: e.affine_select(ltf[:], ltf[:], [[1, 128]], ALU.is_gt, 0.0, base=0, channel_multiplier=-1),
         reads=[ltf], writes=[ltf])
    ltb = P.sb("ltb", [128, 128], BF16)
    P.cp("dve", [ltb], ltb[:], [ltf], ltf[:])
    io_i = P.sb("io_i", [128, SMAX], I32)
    P.op("pool", lambda e: e.iota(io_i[:], [[1, SMAX]], base=0, channel_multiplier=0), writes=[io_i])
    io_f = P.sb("io_f", [128, SMAX], F32)
    P.cp("dve", [io_f], io_f[:], [io_i], io_i[:])
    ip_i = P.sb("ip_i", [128, 1], I32)
    P.op("pool", lambda e: e.iota(ip_i[:], [[0, 1]], base=0, channel_multiplier=1), writes=[ip_i])
    ip_f = P.sb("ip_f", [128, 2], F32)
    P.cp("dve", [ip_f], ip_f[:, 0:1], [ip_i], ip_i[:])
    P.ts("dve", [ip_f], ip_f[:, 1:2], [ip_f], ip_f[:, 0:1], float(TRASH0), None, ALU.add)

    P.ring("wgb", [128, 8, D], BF16, 2)
    P.ring("wub", [128, 8, D], BF16, 2)
    P.ring("wdb", [128, 8, D], BF16, 1)
    P.ring("wstg", [128, 2, D], F32, 2)

    def wd_dma(e, q, src=None):
        stg = P.nxt("wstg")
        src = wd if src is None else src
        P.dma("sp", [stg], stg[:], [], src[e, q * 256:(q + 1) * 256, :].rearrange("(k p) n -> p k n", p=128))
        return stg

    def wd_cast(b, q, stg):
        P.cp("act", [b], b[:, 2 * q:2 * q + 2, :], [stg], stg[:])

    def load_one(nm, src, e):
        b = P.nxt(nm)
        for k in range(8):
            P.dma("pool", [b], b[:, k, :], [], src[e, k * 128:(k + 1) * 128, :])
        return b

    wdb0 = P.nxt("wdb")
    for q_ in range(4):
        wd_cast(wdb0, q_, wd_dma(0, q_))
    w_next = (load_one("wgb", wg, 0), load_one("wub", wu, 0), wdb0)

    af = P.sb("af", [128, T, NE], F32)
    P.dma("sp", [af], af[:], [aff], aff.t.rearrange("(t p) e -> p t e", p=128))
    thr_l = P.sb("thr_l", [128, NE], F32)
    thr_c = P.sb("thr_c", [128, NE], F32)
    main_stack = P.pstack
    sub = ExitStack()
    P.pstack = sub
    affs = P.sb("affs", [128, 128, NE], F32)
    P.dma("sp", [affs], affs[:], [aff_all], aff_all.t.rearrange("(p j) e -> p j e", p=128))
    pbanks = P.rings["accb"][0] + P.rings["hb"][0]
    gens = [bisect_gen(P, affs, 128, 0, 8, CAP_L, ones_f, "bla", pbanks[0], thr_l),
            bisect_gen(P, affs, 128, 8, 16, CAP_L, ones_f, "blb", pbanks[1], thr_l)]
    if has_ctx:
        afc = P.sb("afc", [128, 2, NE], F32)
        P.cp("dve", [afc], afc[:], [af], af[:, TL:T, :])
        gens.append(bisect_gen(P, afc, 2, 0, 16, CAP_C, ones_f, "bc", pbanks[2], thr_c))
    for _ in range(NBIS + 1):
        for g_ in gens:
            next(g_)
    P.pstack = main_stack
    P.ops.append(None)
    sub.close()

    mask = P.sb("mask", [128, T, NE], F32)
    P.tt("dve", [mask], mask[:, 0:TL, :], [af, thr_l], af[:, 0:TL, :], thr_l[:].unsqueeze(1).to_broadcast([128, TL, NE]), ALU.is_ge)
    if has_ctx:
        P.tt("dve", [mask], mask[:, TL:T, :], [af, thr_c], af[:, TL:T, :], thr_c[:].unsqueeze(1).to_broadcast([128, 2, NE]), ALU.is_ge)
    maskb = P.sb("maskb", [128, T, NE], BF16)
    P.cp("dve", [maskb], maskb[:], [mask], mask[:])
    pos = P.sb("pos", [128, T, NE], F32)
    tot = P.sb("tot", [128, T, NE], F32)
    mflat = maskb[:].rearrange("p t e -> p (t e)")
    pflat = pos[:].rearrange("p t e -> p (t e)")
    tflat = tot[:].rearrange("p t e -> p (t e)")
    n_all = T * NE
    c = 0
    while c < n_all:
        w_ = min(512, n_all - c)
        ps = P.nxt("ps")
        P.mm(ps, ps[:, 0:w_], ltb[:], mflat[:, c:c + w_], True, True, [ltb, maskb])
        P.cp("dve", [pos], pflat[:, c:c + w_], [ps], ps[:, 0:w_])
        ps = P.nxt("ps")
        P.mm(ps, ps[:, 0:w_], ones_b[:], mflat[:, c:c + w_], True, True, [ones_b, maskb])
        P.cp("dve", [tot], tflat[:, c:c + w_], [ps], ps[:, 0:w_])
        c += w_
    base = P.sb("base", [128, T, NE], F32)
    P.memset("dve", [base], base[:], 0.0)
    for t in range(1, TL):
        P.tt("dve", [base], base[:, t, :], [base, tot], base[:, t - 1, :], tot[:, t - 1, :], ALU.add)
    if has_ctx:
        P.cp("dve", [base], base[:, TL + 1, :], [tot], tot[:, TL, :])
    P.tt("dve", [pos], pos[:], [pos, base], pos[:], base[:], ALU.add)

    vals = P.sb("vals", [128, T, NE, 6], BF16)
    for t in range(T):
        P.memset("dve", [vals], vals[:, t, :, 0], float(t))
    P.ts("dve", [vals], vals[:, :, :, 1], [af, ip_f], af[:], 0.0, ip_f[:, 0:1], ALU.mult, ALU.add)
    P.memset("dve", [vals], vals[:, :, :, 5], 1.0)
    r1 = tot
    P.cp("dve", [vals], vals[:, :, :, 2], [af], af[:])
    P.tt("dve", [r1], r1[:], [af, vals], af[:], vals[:, :, :, 2], ALU.subtract)
    P.cp("dve", [vals], vals[:, :, :, 3], [r1], r1[:])
    P.tt("dve", [r1], r1[:], [r1, vals], r1[:], vals[:, :, :, 3], ALU.subtract)
    P.cp("dve", [vals], vals[:, :, :, 4], [r1], r1[:])

    P.ring("oh", [128, SMAX], BF16, 3)
    P.ring("siT", [6, NSLOT], F32, 1)
    P.ring("si", [128, 8, 6], F32, 2)
    P.ring("sx", [128, 8, 4], F32, 2)
    P.ring("ixg", [128, 8], I32, 2)
    P.ring("ixs", [128, 8], I32, 2)
    P.ring("xg", [128, D], BF16, 6 if has_ctx else 5)
    P.ring("xgT", [128, 8, NSLOT], BF16, 1)
    P.ring("sil", [128, 512], F32, 2)
    P.ring("hidT", [128, 8, NSLOT], BF16, 1)
    P.ring("ye", [128, D], F32, 2)

    NST = NS + (1 if has_ctx else 0)
    n2 = NSLOT - 512

    def stage1a(e, st):
        acc0 = P.nxt("accb")
        acc1 = P.nxt("accb")
        ohq = st["ohq"] = []

        def flush():
            while ohq:
                t, oh = ohq.pop(0)
                P.mm(acc0, acc0[0:6, :], vals[:, t, e, :], oh[:, 0:512], t == 0, t == TL - 1, [vals, oh])
                P.mm(acc1, acc1[0:6, 0:SMAX - 512], vals[:, t, e, :], oh[:, 512:SMAX], t == 0, t == TL - 1, [vals, oh])
        st["flush"] = flush
        for t in range(TL):
            oh = P.nxt("oh")
            P.ts("dve", [oh], oh[:], [io_f, pos, mask], io_f[:], pos[:, t, e:e + 1], mask[:, t, e:e + 1], ALU.is_equal, ALU.mult)
            ohq.append((t, oh))
            yield "oh"
        flush()
        siT = P.nxt("siT")
        P.cp("act", [siT], siT[:, 0:512], [acc0], acc0[0:6, :])
        P.cp("act", [siT], siT[:, 512:SMAX], [acc1], acc1[0:6, 0:SMAX - 512])
        if has_ctx:
            accc = P.nxt("ps")
            for t in range(TL, T):
                oh = P.nxt("oh")
                P.ts("dve", [oh], oh[:, 0:CAP_C], [io_f, pos, mask], io_f[:, 0:CAP_C], pos[:, t, e:e + 1], mask[:, t, e:e + 1],
                     ALU.is_equal, ALU.mult)
                P.mm(accc, accc[0:6, 0:CAP_C], vals[:, t, e, :], oh[:, 0:CAP_C], t == TL, t == T - 1, [vals, oh])
            P.cp("act", [siT], siT[:, SMAX:SMAX + CAP_C], [accc], accc[0:6, 0:CAP_C])
        sps = P.nxt("ps")
        for s_ in range(NST):
            ns = 128 if s_ < NS else CAP_C
            P.mm(sps, sps[0:ns, s_ * 6:(s_ + 1) * 6], siT[:, s_ * 128:s_ * 128 + ns], P.idf[0:6, 0:6], True, True, [siT, P.idf])
        si = P.nxt("si")
        P.memset("dve", [si], si[:], 0.0)
        P.cp("dve", [si], si[:, 0:NS, :], [sps], sps[:, 0:NS * 6].rearrange("p (s k) -> p s k", k=6))
        if has_ctx:
            P.cp("dve", [si], si[0:CAP_C, NS, :], [sps], sps[0:CAP_C, NS * 6:NS * 6 + 6])
        sx = P.nxt("sx")
        P.stt("dve", [sx], sx[:, :, 0], [si], si[:, :, 0], 128.0, si[:, :, 1], ALU.mult, ALU.add)
        P.tt("dve", [sx], sx[:, :, 1], [si], si[:, :, 2], si[:, :, 3], ALU.add)
        P.tt("dve", [sx], sx[:, :, 1], [sx, si], sx[:, :, 1], si[:, :, 4], ALU.add)
        P.ts("dve", [sx], sx[:, :, 2], [si], si[:, :, 5], -1.0, 1.0, ALU.mult, ALU.add)
        P.stt("dve", [sx], sx[:, :, 3], [sx, ip_f], sx[:, :, 2], ip_f[:, 1:2], sx[:, :, 0], ALU.mult, ALU.add)
        ixg = P.nxt("ixg")
        P.cp("dve", [ixg], ixg[:], [sx], sx[:, :, 0])
        ixs = P.nxt("ixs")
        P.cp("dve", [ixs], ixs[:], [sx], sx[:, :, 3])
        st["sx"], st["ixs"], st["ixg"], st["xgs"] = sx, ixs, ixg, [None] * NST
        yield

    def issue_gather(st, s_):
        xg = P.nxt("xg")
        ixg = st["ixg"]
        P.op("pool", lambda en: en.indirect_dma_start(
            out=xg[:], out_offset=None, in_=h2.t, in_offset=bass.IndirectOffsetOnAxis(ap=ixg[:, s_:s_ + 1], axis=0)),
            reads=[ixg, h2], writes=[xg], dma=True)
        st["xgs"][s_] = xg

    def stage1b(st):
        xgT = P.nxt("xgT")
        for s_ in range(NST):
            ns = 128 if s_ < NS else CAP_C
            xg = st["xgs"][s_]
            tp = P.nxt("tp")
            for k in range(8):
                P.tr(tp, tp[:, k, :], xg[:, k * 128:(k + 1) * 128], P.idb[:], [xg, P.idb])
            P.cp("act", [xgT], xgT[:, :, s_ * 128:s_ * 128 + ns], [tp], tp[:, :, 0:ns])
        st["xgT"] = xgT

    def stage2(e, st, wgb, wub, wdb, gen, st_next, prev_sc, wgn=None, wun=None):
        xgT, sx, ixs = st["xgT"], st["sx"], st["ixs"]
        hidT = P.nxt("hidT")
        wdp = st.get("wd")
        wgs = []
        wus = []
        gi = -1
        for fc in range(8):
            for (c0, cn) in ((0, 512), (512, n2)):
                gi += 1
                if wdp is not None:
                    if gi == 1:
                        wd_cast(wdb, 0, wdp[0])
                        wdp.append(wd_dma(e, 2))
                    elif gi == 2:
                        wd_cast(wdb, 1, wdp[1])
                        wdp.append(wd_dma(e, 3))
                    elif gi == 4:
                        wd_cast(wdb, 2, wdp[2])
                        if wgn is not None:
                            wgs.append(wd_dma(e + 1, 0, wg))
                    elif gi == 5:
                        wd_cast(wdb, 3, wdp[3])
                        if wgn is not None:
                            wgs.append(wd_dma(e + 1, 1, wg))
                if wgn is not None and wdp is not None:
                    if gi == 7:
                        wd_cast(wgn, 0, wgs[0])
                        wgs.append(wd_dma(e + 1, 2, wg))
                    elif gi == 8:
                        wd_cast(wgn, 1, wgs[1])
                        wgs.append(wd_dma(e + 1, 3, wg))
                    elif gi == 10:
                        wd_cast(wgn, 2, wgs[2])
                        wus.append(wd_dma(e + 1, 0, wu))
                    elif gi == 11:
                        wd_cast(wgn, 3, wgs[3])
                        wus.append(wd_dma(e + 1, 1, wu))
                    elif gi == 13:
                        wd_cast(wun, 0, wus[0])
                        wus.append(wd_dma(e + 1, 2, wu))
                    elif gi == 14:
                        wd_cast(wun, 1, wus[1])
                        wus.append(wd_dma(e + 1, 3, wu))
                if gen is not None and not st_next.get("ohdone"):
                    for _ in range(3):
                        if next(gen, "end") != "oh":
                            st_next["ohdone"] = True
                            break
                gp = P.nxt("hb")
                up = P.nxt("hb")
                for (dst, wb) in ((gp, wgb), (up, wub)):
                    for k in range(8):
                        P.mm(dst, dst[:, 0:cn], wb[:, k, fc * 128:(fc + 1) * 128], xgT[:, k, c0:c0 + cn], k == 0, k == 7, [wb, xgT])
                if gen is not None and "flush" in st_next:
                    st_next["flush"]()
                sil = P.nxt("sil")
                P.act([sil], sil[:, 0:cn], [gp], gp[:, 0:cn], AF.Silu)
                P.tt("dve", [hidT], hidT[:, fc, c0:c0 + cn], [sil, up], sil[:, 0:cn], up[:, 0:cn], ALU.mult)
        if gen is not None:
            for _ in gen:
                pass
        my_sc = []
        for s_ in range(NST):
            if len(wus) == 4 and s_ in (1, 2):
                wd_cast(wun, s_ + 1, wus[s_ + 1])
            ns = 128 if s_ < NS else CAP_C
            ye = P.nxt("ye")
            g2r = g2rep[0] if s_ < NS else g2rep[1]
            for hh in range(2):
                yp = P.nxt("hb")
                for fc in range(8):
                    P.mm(yp, yp[0:ns, :], hidT[:, fc, s_ * 128:s_ * 128 + ns], wdb[:, fc, hh * 512:(hh + 1) * 512], fc == 0, fc == 7,
                         [hidT, wdb])
                P.stt("dve", [ye], ye[0:ns, hh * 512:(hh + 1) * 512], [yp, sx, g2r], yp[0:ns, :], sx[0:ns, s_, 1:2],
                      g2r[0:ns, hh * 512:(hh + 1) * 512], ALU.mult, ALU.mult)
            o_ = P.op("pool", lambda en, ye=ye, ixs=ixs, s_=s_: en.indirect_dma_start(
                out=xacc.t, out_offset=bass.IndirectOffsetOnAxis(ap=ixs[:, s_:s_ + 1], axis=0), in_=ye[:, :], in_offset=None,
                compute_op=ALU.add), reads=[ixs, ye], writes=[], dma=True, extra=prev_sc)
            my_sc.append(o_)
            if st_next is not None:
                issue_gather(st_next, s_)
        return my_sc

    st = {}
    for _ in stage1a(0, st):
        st["flush"]()
    for s_ in range(NST):
        issue_gather(st, s_)
    stage1b(st)
    prev_sc = [zt_op]
    for e in range(NE):
        wgb, wub, wdb = w_next
        st_next = None
        gen = None
        if e + 1 < NE:
            if e == 0:
                wg_n, wgn_ = load_one("wgb", wg, e + 1), None
                wu_n, wun_ = load_one("wub", wu, e + 1), None
            else:
                wg_n = wgn_ = P.nxt("wgb")
                wu_n = wun_ = P.nxt("wub")
            st_next = {}
            gen = stage1a(e + 1, st_next)
        else:
            wgn_ = wun_ = None
        prev_sc = stage2(e, st, wgb, wub, wdb, gen, st_next, prev_sc, wgn_, wun_)
        if e + 1 < NE:
            w_next = (wg_n, wu_n, P.nxt("wdb"))
            st_next["wd"] = [wd_dma(e + 1, 0), wd_dma(e + 1, 1)]
            stage1b(st_next)
        st = st_next

    if not has_ctx:
        fg = P.sb("fg", [128, D], F32)
        P.dma("sp", [fg], fg[:], [], fing.t)
        P.ring("junk", [128, D], BF16, 1)
        P.ring("ssq", [128, 1], F32, 2)
        P.ring("rs", [128, 1], F32, 2)
        for t in range(T):
            xt = P.nxt("xt")
            P.op("sp", lambda e, xt=xt, t=t: e.dma_start(out=xt[:], in_=xacc[t * 128:(t + 1) * 128, :]), reads=[xacc], writes=[xt],
                 dma=True, extra=prev_sc)
            junk = P.nxt("junk")
            ssq = P.nxt("ssq")
            P.act([junk, ssq], junk[:], [xt], xt[:], AF.Square, accum=ssq[:, 0:1])
            rs = P.nxt("rs")
            P.rstd(rs, rs[:, 0:1], ssq, ssq[:, 0:1], 1, 1.0 / D)
            P.stt("dve", [xt], xt[:], [xt, rs, fg], xt[:], rs[:, 0:1], fg[:], ALU.mult, ALU.mult)
            P.dma("sp", [outp], outp[t * 128:(t + 1) * 128, :], [xt], xt[:])
    P.phase_end()


def phase_qkv1(P, T):
    P.phase_begin("qkv1")
    xrows, ccols, modw, modb, n1g, wqkv, wqks, rope = T["xacc0"], T["ccols"], T["modw1"], T["modb1"], T["n1g1"], T["wqkv"], T["wqks"], T["rope1"]
    Q1T, K1T, V1, Kb, Vb = T["Q1T"], T["K1T"], T["V1"], T["Kb"], T["Vb"]

    P.make_ident()
    P.ring("ps", [128, 512], F32, 6, psum=True)
    P.ring("tp", [128, 8, 128], BF16, 2, psum=True)
    P.ring("xt", [128, D], F32, 2)
    P.ring("junk", [128, D], BF16, 1)
    P.ring("ssq", [128, 1], F32, 2)
    P.ring("rs", [128, 1], F32, 2)
    P.ring("rstd_tmp", [128, 512], F32, 1)
    P.ring("h32", [128, D], F32, 1)
    P.ring("hb", [128, D], BF16, 2)
    mods = compute_mod(P, ccols.t, modw.t, modb.t, 0, 2048, ["sh1", "sc1"])
    g1 = P.sb("g1", [128, D], F32)
    P.dma("sp", [g1], g1[:], [], n1g.t)
    AB = []
    for w in range(2):
        A = mods["sc1"][w]
        P.stt("dve", [A], A[:], [A, g1], A[:], 1.0, g1[:], ALU.add, ALU.mult)
        AB.append((A, mods["sh1"][w]))
    wq_b = load_bf16(P, "wq_b", wqkv.t.rearrange("(k p) n -> p k n", p=128), [128, 8, 1536])
    ws_b = load_bf16(P, "ws_b", wqks.t.rearrange("(k p) n -> p k n", p=128), [128, 8, 1280])
    P.ring("hT", [128, 8, 512], BF16, 2)
    P.ring("rp", [128, 2, 512], F32, 2)
    P.ring("t1", [128, 512], F32, 2)
    P.ring("t2", [128, 512], F32, 2)
    P.ring("qo", [128, 512], BF16, 3)
    P.ring("vo", [128, 256], BF16, 3)

    for blk in range(NT // 512 + 1):
        tok0 = blk * 512
        n = min(512, NT - tok0)
        if n <= 0:
            break
        is_ctx = tok0 >= NL
        hT = P.nxt("hT")
        for t in range(n // 128):
            tp = tile_T(P, xrows, tok0 + t * 128, AB[1 if is_ctx else 0][0], AB[1 if is_ctx else 0][1])
            P.cp("act", [hT], hT[:, :, t * 128:(t + 1) * 128], [tp], tp[:])
        if not is_ctx:
            rp = P.nxt("rp")
            P.dma("pool", [rp], rp[:], [], rope[:, :, tok0:tok0 + 512])
        for c in (range(10) if not is_ctx else (8, 9)):
            ps = P.nxt("ps")
            for k in range(8):
                P.mm(ps, ps[:, 0:n], wq_b[:, k, c * 128:(c + 1) * 128], hT[:, k, 0:n], k == 0, k == 7, [wq_b, hT])
            qo = P.nxt("qo")
            if is_ctx:
                P.cp("act", [qo], qo[:, 0:n], [ps], ps[:, 0:n])
            else:
                pss = P.nxt("ps")
                for k in range(8):
                    P.mm(pss, pss[:, 0:n], ws_b[:, k, c * 128:(c + 1) * 128], hT[:, k, 0:n], k == 0, k == 7, [ws_b, hT])
                t1 = P.nxt("t1")
                P.tt("dve", [t1], t1[:, 0:n], [ps, rp], ps[:, 0:n], rp[:, 0, 0:n], ALU.mult)
                t2 = P.nxt("t2")
                P.tt("dve", [t2], t2[:, 0:n], [pss, rp], pss[:, 0:n], rp[:, 1, 0:n], ALU.mult)
                P.tt("dve", [qo], qo[:, 0:n], [t1, t2], t1[:, 0:n], t2[:, 0:n], ALU.add)
            if c < 8:
                P.dma("sp", [Q1T], Q1T[c * 128:(c + 1) * 128, tok0:tok0 + n], [qo], qo[:, 0:n])
            else:
                P.dma("sp", [K1T], K1T[(c - 8) * 128:(c - 7) * 128, tok0:tok0 + n], [qo], qo[:, 0:n])
                if tok0 == 0:
                    P.dma("sp", [Kb], Kb[(c - 8) * 128:(c - 7) * 128, 0:128], [qo], qo[:, 0:128])
                if tok0 == NL - 512:
                    P.dma("sp", [Kb], Kb[(c - 8) * 128:(c - 7) * 128, 128:256], [qo], qo[:, 384:512])
        for t in range(n // 128):
            psv = P.nxt("ps")
            for k in range(8):
                P.mm(psv, psv[:, 0:256], hT[:, k, t * 128:(t + 1) * 128], wq_b[:, k, 1280:1536], k == 0, k == 7, [hT, wq_b])
            vo = P.nxt("vo")
            P.cp("act", [vo], vo[:], [psv], psv[:, 0:256])
            P.dma("sp", [V1], V1[tok0 + t * 128:tok0 + (t + 1) * 128, :], [vo], vo[:])
            if tok0 + t * 128 == 0:
                P.dma("sp", [Vb], Vb[0:128, :], [vo], vo[:])
            if tok0 + t * 128 == NL - 128:
                P.dma("sp", [Vb], Vb[128:256, :], [vo], vo[:])
    P.phase_end()


NB1 = NL // 128
NKL = NL + 256


def phase_attn1(P, T):
    P.phase_begin("attn1")
    Q1T, K1T, V1, Kb_all, Vb_all, masks, sinkr, selh, catT1 = (T[k] for k in ("Q1T", "K1T", "V1", "Kb_all", "Vb_all", "masks", "sinkr", "selh", "catT1"))
    scale = 64.0 ** -0.5

    P.ring("s1b", [128, 512], F32, 5, psum=True)
    P.ring("ops", [128, 512], F32, 2, psum=True)
    P.ring("bps", [128, 512], F32, 1, psum=True)
    P.ring("p1", [128, 512], BF16, 10)
    P.ring("rl", [1, 512], F32, 3)
    P.ring("lsum", [1, 512], F32, 4)
    P.ring("rhl", [1, 2, 512], BF16, 3)
    onesb16 = P.sb("onesb16", [1, 128], BF16)
    P.memset("dve", [onesb16], onesb16[:], 1.0)

    P.ring("bsb", [65, 512], F32, 2)
    P.ring("ao", [65, 512], BF16, 3)
    P.ring("qg", [64, NB1 * 512], BF16, 2)
    P.ring("kl", [64, NKL], BF16, 2)
    P.ring("kc", [64, NCX], BF16, 2)
    P.ring("vl", [128, NKL // 128, 65], BF16, 2)
    P.ring("vc", [128, 2, 65], BF16, 2)
    ones = P.sb("ones", [1, 128], F32)
    P.memset("dve", [ones], ones[:], 1.0)
    mk = P.sb("mk", [128, 4, 512], BF16)
    P.dma("sp", [mk], mk[:], [], masks.t)
    sk = P.sb("sk", [1, 2048], F32)
    P.dma("sp", [sk], sk[:], [], sinkr.t)
    P.act([sk], sk[:], [sk], sk[:], AF.Exp)
    skb = P.sb("skb", [1, 2, 2048], BF16)
    skr = P.sb("skr", [1, 2048], F32)
    P.cp("dve", [skb], skb[:, 0, :], [sk], sk[:])
    P.tt("dve", [skr], skr[:], [sk, skb], sk[:], skb[:, 0, :], ALU.subtract)
    P.cp("dve", [skb], skb[:, 1, :], [skr], skr[:])
    e0 = P.sb("e0", [1, 65], BF16)
    P.memset("dve", [e0], e0[:], 0.0)
    P.memset("dve", [e0], e0[:, 0:1], 1.0)

    sel = P.sb("sel", [128, 8], F32)
    P.dma("sp", [sel], sel[:], [], selh.t)
    P.ring("kcand", [64, 4, 256], BF16, 2)
    P.ring("vcand", [128, 4, 2, 64], BF16, 2)
    for b_ in P.rings["vl"][0]:
        P.memset("dve", [b_], b_[:, :, 0:1], 1.0)
    for b_ in P.rings["vc"][0]:
        P.memset("dve", [b_], b_[:, :, 0:1], 1.0)
    for g in range(4):
        qg = P.nxt("qg")
        for j in range(4):
            P.dma("sp", [qg], qg[:].rearrange("d (i j r) -> d i j r", j=4, r=128)[:, :, j, :], [Q1T],
                  Q1T[(g * 4 + j) * 64:(g * 4 + j + 1) * 64, :].rearrange("d (i r) -> d i r", r=128))
        kl = P.nxt("kl")
        P.dma("sp", [kl], kl[:, 128:128 + NL], [K1T], K1T[g * 64:(g + 1) * 64, 0:NL])
        kcand = P.nxt("kcand")
        P.dma("sp", [kcand], kcand[:], [Kb_all], Kb_all.t.rearrange("(r c) k -> c r k", r=4)[g * 64:(g + 1) * 64])
        for (dst0, src0, so) in ((0, 128, 0), (128 + NL, 0, 4)):
            P.ts("dve", [kl], kl[:, dst0:dst0 + 128], [kcand, sel], kcand[:, 0, src0:src0 + 128], sel[0:64, so:so + 1], None, ALU.mult)
            for r in range(1, 4):
                P.stt("dve", [kl], kl[:, dst0:dst0 + 128], [kcand, sel, kl], kcand[:, r, src0:src0 + 128], sel[0:64, so + r:so + r + 1],
                      kl[:, dst0:dst0 + 128], ALU.mult, ALU.add)
        kc = P.nxt("kc")
        P.dma("sp", [kc], kc[:], [K1T], K1T[g * 64:(g + 1) * 64, NL:NT])
        vl = P.nxt("vl")
        P.dma("sp", [vl], vl[:, 1:1 + NB1, 1:65], [V1], V1[0:NL, g * 64:(g + 1) * 64].rearrange("(t p) d -> p t d", p=128))
        vcand = P.nxt("vcand")
        P.dma("sp", [vcand], vcand[:], [Vb_all], Vb_all.t.rearrange("(r f p) c -> p r f c", r=4, f=2)[:, :, :, g * 64:(g + 1) * 64])
        for (dt_, f_, so) in ((0, 1, 0), (NB1 + 1, 0, 4)):
            P.ts("dve", [vl], vl[:, dt_, 1:65], [vcand, sel], vcand[:, 0, f_, :], sel[:, so:so + 1], None, ALU.mult)
            for r in range(1, 4):
                P.stt("dve", [vl], vl[:, dt_, 1:65], [vcand, sel, vl], vcand[:, r, f_, :], sel[:, so + r:so + r + 1], vl[:, dt_, 1:65],
                      ALU.mult, ALU.add)
        vc = P.nxt("vc")
        P.dma("sp", [vc], vc[:, :, 1:65], [V1], V1[NL:NT, g * 64:(g + 1) * 64].rearrange("(t p) d -> p t d", p=128))
        def emit_s(i):
            q_ap = qg[:, i * 512:(i + 1) * 512]
            tiles = []
            for (kb, c0) in ((kc, 0), (kc, 128), (kl, i * 128), (kl, (i + 1) * 128), (kl, (i + 2) * 128)):
                sp_ = P.nxt("s1b")
                P.mm(sp_, sp_[:, :], kb[:, c0:c0 + 128], q_ap, True, True, [kb, qg])
                tiles.append(sp_)
            return tiles

        def emit_exp(i, tiles):
            ps_ = []
            for j, sp_ in enumerate(tiles):
                pt = P.nxt("p1")
                P.act([pt], pt[:], [sp_], sp_[:], AF.Exp, scale=scale)
                if j == 2:
                    P.tt("dve", [pt], pt[:], [pt, mk], pt[:], mk[:, 2, :] if i == 0 else mk[:, 0, :], ALU.mult)
                if j == 4:
                    P.tt("dve", [pt], pt[:], [pt, mk], pt[:], mk[:, 3, :] if i == NB1 - 1 else mk[:, 1, :], ALU.mult)
                ps_.append(pt)
            return ps_

        def emit_pv(i, ps_, g=g, vc=vc, vl=vl):
            o_ps = P.nxt("ops")
            vs = (vc[:, 0, :], vc[:, 1, :], vl[:, i, :], vl[:, i + 1, :], vl[:, i + 2, :])
            vb = (vc, vc, vl, vl, vl)
            for j in range(5):
                P.mm(o_ps, o_ps[0:65, :], vs[j], ps_[j][:], j == 0, j == 4, [vb[j], ps_[j]])
            lsum = P.nxt("lsum")
            P.tt("dve", [lsum], lsum[:], [o_ps, sk], o_ps[0:1, :], sk[:, g * 512:(g + 1) * 512], ALU.add)
            return (i, o_ps, lsum)

        def emit_rl(i, o_ps, lsum):
            lnl = P.nxt("lsum")
            P.act([lnl], lnl[:], [lsum], lsum[:], AF.Ln)
            rl = P.nxt("rl")
            P.act([rl], rl[:], [lnl], lnl[:], AF.Exp, scale=-1.0)
            rh = P.nxt("rhl")
            P.cp("dve", [rh], rh[:, 0, :], [rl], rl[:])
            P.tt("dve", [rl], rl[:], [rl, rh], rl[:], rh[:, 0, :], ALU.subtract)
            P.cp("dve", [rh], rh[:, 1, :], [rl], rl[:])
            return (i, o_ps, rh)

        def emit_fin(i, o_ps, rl, g=g):
            b_ps = P.nxt("bps")
            P.mm(b_ps, b_ps[0:65, :], onesb16[:, 0:65], rl[:, 0, :], True, False, [onesb16, rl])
            P.mm(b_ps, b_ps[0:65, :], onesb16[:, 0:65], rl[:, 1, :], False, True, [onesb16, rl])
            bsb = P.nxt("bsb")
            P.cp("act", [bsb], bsb[:], [b_ps], b_ps[0:65, :])
            ao = P.nxt("ao")
            P.tt("dve", [ao], ao[:], [o_ps, bsb], o_ps[0:65, :], bsb[:], ALU.mult)
            for j in range(4):
                P.dma("sp", [catT1], catT1[(g * 4 + j) * 64:(g * 4 + j + 1) * 64, i * 128:(i + 1) * 128], [ao], ao[1:65, j * 128:(j + 1) * 128])

        prev = None
        pfin = None
        for i in range(NB1 + 1):
            tiles = emit_s(i) if i < NB1 else None
            o_ = emit_pv(*prev) if prev is not None else None
            pexp = emit_exp(i, tiles) if i < NB1 else None
            if o_ is not None:
                nfin = emit_rl(*o_)
                if pfin is not None:
                    emit_fin(*pfin)
                pfin = nfin
            prev = (i, pexp) if i < NB1 else None
        emit_fin(*pfin)
    P.phase_end()


GRP = [[0, 1, 2, 3], [4, 5, 6, 7]]


def build_fused():
    ctx = ExitStack()
    nc, P = new_prog(ctx)
    T = {}

    def di(n, s, dt=F32):
        T[n] = P.dram(n, s, dt, "ExternalInput")

    def dn(n, s, dt=F32):
        T[n] = P.dram(n, s, dt)
        T[n].relaxed = not n.startswith("xacc")

    di("xrows", [NT, D]); di("xhalo", [128, D]); di("hmask", [128, 2]); di("ccols", [128, 16])
    for l in (0, 1):
        di("modw%d" % l, [D, 6144]); di("modb%d" % l, [1, 6144]); di("n1g%d" % l, [128, D]); di("n2g%d" % l, [128, D])
        di("w_out%d" % l, [D, D]); di("rw%d" % l, [D, NE])
        di("wg%d" % l, [NE, D, D]); di("wu%d" % l, [NE, D, D]); di("wd%d" % l, [NE, D, D])
    di("fing", [128, D])
    di("w_in", [D, 1984]); di("convp", [128, 16]); di("qg", [128, 3]); di("w_uq", [256, 768]); di("w_uqs", [256, 768])
    di("w_ukv", [128, 1024]); di("ropeq", [96, 2, NL]); di("ropek", [32, 2, NL])
    di("wqkv", [D, 1536]); di("wqks", [D, 1280]); di("rope1", [128, 2, NL])
    di("masks", [128, 4, 512], BF16); di("sinkr", [1, 2048]); di("selh", [128, 8])
    T["out"] = P.dram("out", [NL, D], F32, "ExternalOutput")
    dn("QT", [8, 96, NT], BF16); dn("KTn", [512, NT], BF16); dn("KTr", [32, NT], BF16); dn("Vt", [NT, 512], BF16)
    dn("convT", [512, NT], BF16)
    for h_ in range(8):
        dn("KTn_all%d" % h_, [4 * 64, NT], BF16)
    dn("KTr_all", [4 * 32, NT], BF16); dn("Vt_all", [4, 4 * 1024, 512], BF16)
    dn("attT", [512, NT], BF16)
    dn("xacc0", [NT + 128, D]); dn("h2_0", [NT, D], BF16); dn("aff_0", [NT, NE]); dn("affl_0", [NL, NE]); dn("affall_0", [4 * NL, NE])
    dn("Q1T", [1024, NL], BF16); dn("K1T", [256, NT], BF16); dn("V1", [NT, 256], BF16)
    dn("Kb", [256, 256], BF16); dn("Vb", [256, 256], BF16); dn("Kb_all", [1024, 256], BF16); dn("Vb_all", [1024, 256], BF16)
    dn("catT1", [1024, NL], BF16)
    dn("xacc1", [NL + 128, D]); dn("h2_1", [NL, D], BF16); dn("aff_1", [NL, NE]); dn("affall_1", [4 * NL, NE])
    T["affl_1"] = T["aff_1"]
    T["xres0"] = T["xrows"]
    T["xres1"] = T["xacc0"]

    phase_A(P, T)
    P.cc_allgather(T["KTr"], T["KTr_all"], GRP)
    for k in range(4):
        P.cc_allgather(T["Vt"], T["Vt_all"], GRP, T["Vt"].t[k * 1024:(k + 1) * 1024, :], T["Vt_all"].t[k])
    for h in range(8):
        P.cc_allgather(T["KTn"], T["KTn_all%d" % h], GRP, T["KTn"].t[h * 64:(h + 1) * 64, :], T["KTn_all%d" % h].t)
    phase_B(P, T)
    phase_post(P, T, 0, NT, [(T["convT"], 4), (T["attT"], 4)])
    P.cc_allgather(T["affl_0"], T["affall_0"], GRP)
    phase_moe(P, T, 0)
    phase_qkv1(P, T)
    P.cc_allgather(T["Kb"], T["Kb_all"], GRP)
    P.cc_allgather(T["Vb"], T["Vb_all"], GRP)
    phase_attn1(P, T)
    phase_post(P, T, 1, NL, [(T["catT1"], 8)])
    P.cc_allgather(T["aff_1"], T["affall_1"], GRP)
    phase_moe(P, T, 1)
    n = P.finalize()
    return nc, ctx, n


def prep_all(inp):
    bf = ml_dtypes.bfloat16
    maps = prep_A(inp)
    w = inp["swa_w_qkv"][0]
    p64 = _swap_perm(64)
    ws = np.empty((D, 1280), np.float32)
    for h in range(20):
        ws[:, h * 64:(h + 1) * 64] = w[:, h * 64 + p64]
    r = np.arange(128)
    tri_prev = (r[:, None] >= r[None, :]).astype(np.float32)
    tri_next = (r[:, None] <= r[None, :]).astype(np.float32)
    sinkr = np.ascontiguousarray(np.repeat(inp["swa_sink"][0], 128)[None, :].astype(np.float32))
    shared = {}
    for l in (0, 1):
        shared["modw%d" % l] = np.ascontiguousarray(inp["mod_w"][l])
        shared["modb%d" % l] = np.ascontiguousarray(inp["mod_b"][l][None, :])
        shared["n1g%d" % l] = _rep(inp["norm1_g"][l])
        shared["n2g%d" % l] = _rep(inp["norm2_g"][l])
        shared["rw%d" % l] = np.ascontiguousarray(inp["router_w"][l])
        shared["wg%d" % l] = np.ascontiguousarray(inp["exp_w_gate"][l])
        shared["wu%d" % l] = np.ascontiguousarray(inp["exp_w_up"][l])
        shared["wd%d" % l] = np.ascontiguousarray(inp["exp_w_down"][l])
    shared["w_out0"] = np.ascontiguousarray(inp["ab_w_out"][0])
    shared["w_out1"] = np.ascontiguousarray(inp["swa_w_out"][0])
    shared["fing"] = _rep(inp["final_g"])
    shared["wqkv"] = np.ascontiguousarray(w)
    shared["wqks"] = ws
    shared["sinkr"] = sinkr
    out = []
    for core in range(8):
        b, q = core // 4, core % 4
        m = dict(maps[core])
        m["modw0"] = m.pop("modw"); m["modb0"] = m.pop("modb"); m["n1g0"] = m.pop("n1g")
        m.update(shared)
        C, S = _rope_tables(np.arange(q * NL, (q + 1) * NL), 64)
        rp = np.empty((128, 2, NL), np.float32)
        rp[:64, 0] = C; rp[64:, 0] = C; rp[:64, 1] = S; rp[64:, 1] = S
        m["rope1"] = rp
        mk = np.zeros((128, 4, 512), np.float32)
        mk[:, 0] = np.tile(tri_prev, (1, 4))
        mk[:, 1] = np.tile(tri_next, (1, 4))
        mk[:, 2] = mk[:, 0] if q > 0 else 0.0
        mk[:, 3] = mk[:, 1] if q < 3 else 0.0
        m["masks"] = mk.astype(bf)
        sel = np.zeros((128, 8), np.float32)
        if q > 0:
            sel[:, q - 1] = 1.0
        if q < 3:
            sel[:, 4 + q + 1] = 1.0
        m["selh"] = sel
        out.append(m)
    return out


def kernel(**inputs):
    inp = {k: np.asarray(v) for k, v in inputs.items()}
    maps = prep_all(inp)
    nc, ctx, n = build_fused()
    res = run_bass_kernel_spmd(nc, maps, core_ids=list(range(8)))
    ctx.close()
    out = np.empty((2, 4 * NL, D), np.float32)
    for c in range(8):
        out[c // 4, (c % 4) * NL:(c % 4 + 1) * NL] = np.asarray(res.results[c]["out"])
    return out
```
